# Optimizing a Trainium2 kernel written in Bass

```python
import jax
import jax.numpy as jnp
from jax import lax
import numpy as np

D_MODEL = 1024
BATCH = 8
SEQ = 2048
DEPTH = 2

CTX_LEN = 256
GRID_W = 64
HEAD_DIM = 64
ATTN_SCALE = HEAD_DIM ** -0.5
A_HEADS = 4
D_A = A_HEADS * HEAD_DIM
LORA_W = 64
LORA_A = 64
LORA_G = 160
A_IN = 3 * D_A + 2 * LORA_W + 2 * LORA_A + LORA_G
GN_EPS = 64e-5
B_QHEADS = 6
B_KVHEADS = 2
B_GROUP = B_QHEADS // B_KVHEADS
D_B = B_QHEADS * HEAD_DIM
D_BKV = B_KVHEADS * HEAD_DIM
B_IN = D_B + 2 * D_BKV
WINDOW = 128
BAND_BLK = 128
C_HEADS = 6
D_C = C_HEADS * HEAD_DIM
C_IN = 3 * D_C
NA_KH = 8
NA_KW = 16
D_MIX = D_A + D_B + D_C
N_IN = A_IN + B_IN + C_IN
ROPE_BASE = 10000.0
ROPE_NF = HEAD_DIM // 4
N_GROUPS = 4
EXP_PER_GROUP = 8
N_EXPERTS = N_GROUPS * EXP_PER_GROUP
TOP_K = 2
D_EXPERT = 512
MOE_BLK = 128
N_MOD = 6
LN_EPS = 1e-6
F32 = jnp.float32

kernel_name = 'hybrid_rwkv7_swa_natten_hmoe_dit'


def _layer_norm(x, gain=None, bias=None):
    xf = x.astype(F32)
    mu = xf.mean(-1, keepdims=True)
    var = jnp.square(xf - mu).mean(-1, keepdims=True)
    y = (xf - mu) * lax.rsqrt(var + LN_EPS)
    if gain is not None:
        y = y * gain.astype(F32) + bias.astype(F32)
    return y.astype(x.dtype)


def _modulate(h, shift, scale):
    return h * (1 + scale) + shift


def _split(z, sizes):
    return jnp.split(z, np.cumsum(sizes)[:-1].tolist(), axis=-1)


def _centred_shift(z, w):
    zp = jnp.pad(z, ((0, 0), (1, 1), (0, 0)))
    return w[0] * zp[:, :-2] + w[1] * zp[:, 1:-1] + w[2] * zp[:, 2:]


def _axial_rope(x):
    T = x.shape[1]
    t = jnp.arange(T)
    pos = jnp.stack([t // GRID_W, t % GRID_W], -1).astype(F32)
    inv = ROPE_BASE ** (-jnp.arange(ROPE_NF, dtype=F32) / ROPE_NF)
    ang = pos[:, :, None] * inv
    cos = jnp.cos(ang)[None, :, None]
    sin = jnp.sin(ang)[None, :, None]
    xr = x.astype(F32).reshape(*x.shape[:-1], 2, 2, ROPE_NF)
    x0, x1 = xr[..., 0, :], xr[..., 1, :]
    out = jnp.stack([x0 * cos - x1 * sin, x1 * cos + x0 * sin], -2)
    return out.reshape(x.shape).astype(x.dtype)


def _rwkv7_streams(z, lp):
    Bn, T, _ = z.shape
    r, k, v, wd, ad, gd = _split(z, [D_A, D_A, D_A, 2 * LORA_W, 2 * LORA_A, LORA_G])
    wd = jnp.tanh(wd.reshape(Bn, T, 2, LORA_W))
    ad = ad.reshape(Bn, T, 2, LORA_A)
    w_log = -jax.nn.softplus(-(lp['a_w0'] + jnp.einsum('btdr,drc->btdc', wd, lp['a_w_up']))) - 0.5
    decay = jnp.exp(-jnp.exp(w_log.astype(F32)))
    a = jax.nn.sigmoid(lp['a_a0'] + jnp.einsum('btdr,drc->btdc', ad, lp['a_a_up']))
    g = jax.nn.sigmoid(gd) @ lp['a_g_up']
    heads = lambda t: t.reshape(*t.shape[:-1], A_HEADS, HEAD_DIM).astype(F32)
    kk = heads(k * lp['a_k_k'])
    kk = kk * lax.rsqrt(jnp.sum(kk * kk, -1, keepdims=True) + 1e-12)
    k_dir = k[:, :, None, :] * (1 + (a - 1) * lp['a_k_a'])
    return {'r': heads(r), 'k': heads(k_dir), 'v': heads(v), 'kk': kk,
            'w': heads(decay), 'a': heads(a), 'g': g}


def _dir_sequence(qc, ql):
    fwd = jnp.concatenate([qc[:, :, 0], ql[:, :, 0]], 1)
    bwd = jnp.concatenate([qc[:, ::-1, 1], ql[:, ::-1, 1]], 1)
    return jnp.stack([fwd, bwd], 0).transpose(2, 0, 1, 3, 4)


def _rwkv7_step(state, inp):
    r, w, k, v, kk, a = inp
    sk = jnp.einsum('dbhvk,dbhk->dbhv', state, kk)
    state = state * w[..., None, :] - sk[..., :, None] * (kk * a)[..., None, :] + v[..., :, None] * k[..., None, :]
    return state, jnp.einsum('dbhvk,dbhk->dbhv', state, r)


def _rwkv7_output(y, s, lp, dtype):
    Bn, T = y.shape[:2]
    mu = y.mean(-1, keepdims=True)
    var = jnp.square(y - mu).mean(-1, keepdims=True)
    yn = ((y - mu) * lax.rsqrt(var + GN_EPS)).reshape(Bn, T, D_A) * lp['a_lnx_g'] + lp['a_lnx_b']
    r_k = lp['a_r_k'].reshape(A_HEADS, HEAD_DIM).astype(F32)
    bonus = jnp.einsum('bthn,btdhn,hn->bth', s['r'], s['k'], r_k)[..., None] * s['v']
    return ((yn + bonus.reshape(Bn, T, D_A)) * s['g']).astype(dtype)


def _rwkv7_mixer(za, za_c, lp, with_ctx):
    Bn = za.shape[0]
    L = za_c.shape[1]
    sl = _rwkv7_streams(_centred_shift(za, lp['a_shift']), lp)
    sc = _rwkv7_streams(_centred_shift(za_c, lp['a_shift']), lp)
    both = lambda t: jnp.stack([t, t], 2)
    shared = lambda n: _dir_sequence(both(sc[n]), both(sl[n]))
    per_dir = lambda n: _dir_sequence(sc[n], sl[n])
    xs = (shared('r'), per_dir('w'), per_dir('k'), shared('v'), shared('kk'), per_dir('a'))
    s0 = jnp.zeros((2, Bn, A_HEADS, HEAD_DIM, HEAD_DIM), F32)
    _, ys = lax.scan(_rwkv7_step, s0, xs)
    ys = ys.transpose(2, 0, 1, 3, 4)
    y_lat = ys[:, L:, 0] + ys[:, L:, 1][:, ::-1]
    out = _rwkv7_output(y_lat, sl, lp, za.dtype)
    if not with_ctx:
        return out, None
    y_ctx = ys[:, :L, 0] + ys[:, :L, 1][:, ::-1]
    return out, _rwkv7_output(y_ctx, sc, lp, za.dtype)


def _window_attention(q, k, v, kc, vc, sink):
    Bn, S = q.shape[:2]
    L = kc.shape[1]
    nb = S // BAND_BLK
    qb = q.reshape(Bn, nb, BAND_BLK, B_KVHEADS, B_GROUP, HEAD_DIM)

    def band(t):
        tp = jnp.pad(t, ((0, 0), (WINDOW, WINDOW), (0, 0), (0, 0))).reshape(Bn, nb + 2, BAND_BLK, B_KVHEADS, HEAD_DIM)
        return jnp.concatenate([tp[:, :-2], tp[:, 1:-1], tp[:, 2:]], axis=2)

    kb, vb = band(k), band(v)
    s_loc = jnp.einsum('bnqhgd,bnkhd->bnhgqk', qb, kb).astype(F32) * ATTN_SCALE
    qi = jnp.arange(BAND_BLK)
    kj = jnp.arange(3 * BAND_BLK)
    rel = kj[None, :] - BAND_BLK - qi[:, None]
    kpos = jnp.arange(nb)[:, None] * BAND_BLK - BAND_BLK + kj[None, :]
    valid = (jnp.abs(rel) <= WINDOW)[None] & ((kpos >= 0) & (kpos < S))[:, None, :]
    s_loc = jnp.where(valid[None, :, None, None], s_loc, -jnp.inf)
    s_ctx = jnp.einsum('bnqhgd,bmhd->bnhgqm', qb, kc).astype(F32) * ATTN_SCALE
    sink_col = jnp.broadcast_to(sink.astype(F32)[None, None, :, :, None, None], s_loc.shape[:-1] + (1,))
    p = jax.nn.softmax(jnp.concatenate([s_loc, s_ctx, sink_col], -1), -1).astype(v.dtype)
    nk = 3 * BAND_BLK
    o = (jnp.einsum('bnhgqk,bnkhd->bnqhgd', p[..., :nk], vb)
         + jnp.einsum('bnhgqm,bmhd->bnqhgd', p[..., nk:nk + L], vc))
    return o.reshape(Bn, S, D_B)


def _context_attention(q, k, v, sink):
    Bn, L, KV, G, dh = q.shape
    s = jnp.einsum('blhgd,bmhd->bhglm', q, k).astype(F32) * ATTN_SCALE
    if sink is not None:
        s = jnp.concatenate([s, jnp.broadcast_to(sink.astype(F32)[None, :, :, None, None], s.shape[:-1] + (1,))], -1)
    p = jax.nn.softmax(s, -1)[..., :L].astype(v.dtype)
    return jnp.einsum('bhglm,bmhd->blhgd', p, v).reshape(Bn, L, KV * G * dh)


def _neighbourhood_attention(q, k, v, kc, vc, rpb):
    Bn, S, H, dh = q.shape
    rows = S // GRID_W
    kh = min(NA_KH, rows)
    qg = q.reshape(Bn, rows, GRID_W, H, dh)
    r = jnp.arange(rows)
    row_idx = jnp.clip(r - kh // 2, 0, rows - kh)[:, None] + jnp.arange(kh)[None, :]
    kband = k.reshape(Bn, rows, GRID_W, H, dh)[:, row_idx]
    vband = v.reshape(Bn, rows, GRID_W, H, dh)[:, row_idx]
    col = jnp.arange(GRID_W)
    col_start = jnp.clip(col - NA_KW // 2, 0, GRID_W - NA_KW)
    col_valid = (col[None, :] >= col_start[:, None]) & (col[None, :] < col_start[:, None] + NA_KW)
    row_off = row_idx - r[:, None] + (NA_KH - 1)
    col_off = jnp.clip(col[None, :] - col[:, None], -(NA_KW - 1), NA_KW - 1) + (NA_KW - 1)
    bias = rpb.astype(F32)[:, row_off[:, None, :, None], col_off[None, :, None, :]]
    bias = jnp.where(col_valid[None, None, :, None, :], bias, -jnp.inf).transpose(1, 0, 2, 3, 4)
    s_loc = jnp.einsum('brqhd,brakhd->brhqak', qg, kband).astype(F32) * ATTN_SCALE + bias[None]
    s_ctx = jnp.einsum('brqhd,bmhd->brhqm', qg, kc).astype(F32) * ATTN_SCALE
    n_loc = kh * GRID_W
    p = jax.nn.softmax(jnp.concatenate([s_loc.reshape(Bn, rows, H, GRID_W, n_loc), s_ctx], -1), -1).astype(v.dtype)
    o = (jnp.einsum('brhqak,brakhd->brqhd', p[..., :n_loc].reshape(Bn, rows, H, GRID_W, kh, GRID_W), vband)
         + jnp.einsum('brhqm,bmhd->brqhd', p[..., n_loc:], vc))
    return o.reshape(Bn, S, H * dh)


def _token_mixer(h, hc, lp, with_ctx):
    Bn, S, _ = h.shape
    L = hc.shape[1]
    za, zb, zn = _split(h @ lp['w_in'], [A_IN, B_IN, C_IN])
    za_c, zb_c, zn_c = _split(hc @ lp['w_in'], [A_IN, B_IN, C_IN])
    oa, oa_c = _rwkv7_mixer(za, za_c, lp, with_ctx)
    qb, kb, vb = _split(zb, [D_B, D_BKV, D_BKV])
    qb = _axial_rope(qb.reshape(Bn, S, B_QHEADS, HEAD_DIM)).reshape(Bn, S, B_KVHEADS, B_GROUP, HEAD_DIM)
    kb = _axial_rope(kb.reshape(Bn, S, B_KVHEADS, HEAD_DIM))
    vb = vb.reshape(Bn, S, B_KVHEADS, HEAD_DIM)
    qb_c, kb_c, vb_c = _split(zb_c, [D_B, D_BKV, D_BKV])
    kb_c = kb_c.reshape(Bn, L, B_KVHEADS, HEAD_DIM)
    vb_c = vb_c.reshape(Bn, L, B_KVHEADS, HEAD_DIM)
    sink = lp['b_sink'].reshape(B_KVHEADS, B_GROUP)
    ob = _window_attention(qb, kb, vb, kb_c, vb_c, sink)
    qn, kn, vn = [t.reshape(Bn, S, C_HEADS, HEAD_DIM) for t in _split(zn, [D_C, D_C, D_C])]
    qn_c, kn_c, vn_c = [t.reshape(Bn, L, C_HEADS, HEAD_DIM) for t in _split(zn_c, [D_C, D_C, D_C])]
    on = _neighbourhood_attention(qn, kn, vn, kn_c, vn_c, lp['c_rpb'])
    out = jnp.concatenate([oa, ob, on], -1) @ lp['w_out']
    if not with_ctx:
        return out, None
    ob_c = _context_attention(qb_c.reshape(Bn, L, B_KVHEADS, B_GROUP, HEAD_DIM), kb_c, vb_c, sink)
    on_c = _context_attention(qn_c[:, :, :, None], kn_c, vn_c, None)
    out_c = jnp.concatenate([oa_c, ob_c, on_c], -1) @ lp['w_out']
    return out, out_c


def _hier_moe(h, lp):
    T, D = h.shape
    gl = (h @ lp['w_rg'] + lp['b_rg']).astype(F32)
    gsel = jnp.argmax(gl, -1)
    p_grp = jnp.take_along_axis(jax.nn.softmax(gl, -1), gsel[:, None], 1)
    el = (h @ lp['w_re'] + lp['b_re']).astype(F32).reshape(T, N_GROUPS, EXP_PER_GROUP)
    el = jnp.take_along_axis(el, gsel[:, None, None], 1)[:, 0]
    top_v, top_i = lax.top_k(el, TOP_K)
    wts = jax.nn.softmax(top_v, -1) * p_grp
    eid = gsel[:, None] * EXP_PER_GROUP + top_i
    A = T * TOP_K
    flat_e = eid.reshape(-1)
    order = jnp.argsort(flat_e)
    se = flat_e[order]
    st = jnp.repeat(jnp.arange(T), TOP_K)[order]
    sw = wts.reshape(-1)[order]
    counts = jnp.bincount(flat_e, length=N_EXPERTS)
    starts = jnp.cumsum(counts) - counts
    pcounts = (counts + MOE_BLK - 1) // MOE_BLK * MOE_BLK
    pends = jnp.cumsum(pcounts)
    pstarts = pends - pcounts
    dest = pstarts[se] + jnp.arange(A) - starts[se]
    n_blk = (A + MOE_BLK - 1) // MOE_BLK + N_EXPERTS
    rows = jnp.full((n_blk * MOE_BLK,), T, jnp.int32).at[dest].set(st)
    hp = jnp.concatenate([h, jnp.zeros((1, D), h.dtype)], 0)
    xb = hp[rows].reshape(n_blk, MOE_BLK, D)
    blk_e = jnp.minimum(jnp.searchsorted(pends, jnp.arange(n_blk) * MOE_BLK, side='right'), N_EXPERTS - 1)
    w_gate, w_up, w_down = lp['w_gate'], lp['w_up'], lp['w_down']

    def expert_block(args):
        xk, e = args
        return (jax.nn.silu(xk @ w_gate[e]) * (xk @ w_up[e])) @ w_down[e]

    yb = lax.map(expert_block, (xb, blk_e)).reshape(n_blk * MOE_BLK, D)
    return jnp.zeros((T, D), h.dtype).at[st].add(yb[dest] * sw[:, None].astype(yb.dtype))


def setup_inputs(seed: int = 0) -> dict:
    key = jax.random.key(seed)
    ks = iter(jax.random.split(key, 40))
    nrm = lambda shape, scale: jax.random.normal(next(ks), shape, F32) * scale
    beta = (8.0 * DEPTH) ** -0.25
    Ld = DEPTH
    frac = jnp.linspace(0.0, 1.0, D_A)
    return {
        'x': nrm((BATCH, SEQ, D_MODEL), 1.0),
        'c': nrm((BATCH, D_MODEL), 1.0),
        'ctx': nrm((BATCH, CTX_LEN, D_MODEL), 1.0),
        'c_ctx': nrm((D_MODEL,), 1.0),
        'w_mod': nrm((Ld, D_MODEL, N_MOD * D_MODEL), 0.5 * D_MODEL ** -0.5),
        'b_mod': nrm((Ld, N_MOD * D_MODEL), 0.02),
        'w_in': nrm((Ld, D_MODEL, N_IN), D_MODEL ** -0.5),
        'w_out': nrm((Ld, D_MIX, D_MODEL), beta * D_MIX ** -0.5),
        'a_shift': jnp.array([0.0, 1.0, 0.0], F32)[None, :, None] + nrm((Ld, 3, A_IN), 0.1),
        'a_w0': (-6.0 + 5.0 * frac ** 1.5) + nrm((Ld, 2, D_A), 0.1),
        'a_w_up': nrm((Ld, 2, LORA_W, D_A), 0.1 * LORA_W ** -0.5),
        'a_a0': nrm((Ld, 2, D_A), 0.1),
        'a_a_up': nrm((Ld, 2, LORA_A, D_A), LORA_A ** -0.5),
        'a_g_up': nrm((Ld, LORA_G, D_A), LORA_G ** -0.5),
        'a_k_k': 0.85 + nrm((Ld, D_A), 0.02),
        'a_k_a': 1.0 + nrm((Ld, D_A), 0.02),
        'a_r_k': nrm((Ld, D_A), 0.1),
        'a_lnx_g': 1.0 + nrm((Ld, D_A), 0.05),
        'a_lnx_b': nrm((Ld, D_A), 0.02),
        'b_sink': nrm((Ld, B_QHEADS), 0.5),
        'c_rpb': nrm((Ld, C_HEADS, 2 * NA_KH - 1, 2 * NA_KW - 1), 0.1),
        'ln1_g': 1.0 + nrm((Ld, D_MODEL), 0.05),
        'ln1_b': nrm((Ld, D_MODEL), 0.02),
        'ln2_g': 1.0 + nrm((Ld, D_MODEL), 0.05),
        'ln2_b': nrm((Ld, D_MODEL), 0.02),
        'w_rg': nrm((Ld, D_MODEL, N_GROUPS), D_MODEL ** -0.5),
        'b_rg': nrm((Ld, N_GROUPS), 0.01),
        'w_re': nrm((Ld, D_MODEL, N_EXPERTS), D_MODEL ** -0.5),
        'b_re': nrm((Ld, N_EXPERTS), 0.01),
        'w_gate': nrm((Ld, N_EXPERTS, D_MODEL, D_EXPERT), D_MODEL ** -0.5),
        'w_up': nrm((Ld, N_EXPERTS, D_MODEL, D_EXPERT), D_MODEL ** -0.5),
        'w_down': nrm((Ld, N_EXPERTS, D_EXPERT, D_MODEL), beta * D_EXPERT ** -0.5),
    }


def reference(x, c, ctx, c_ctx, w_mod, b_mod, w_in, w_out, a_shift, a_w0, a_w_up, a_a0, a_a_up, a_g_up,
              a_k_k, a_k_a, a_r_k, a_lnx_g, a_lnx_b, b_sink, c_rpb, ln1_g, ln1_b, ln2_g, ln2_b,
              w_rg, b_rg, w_re, b_re, w_gate, w_up, w_down):
    alpha = (2.0 * DEPTH) ** 0.25
    Bn, S, D = x.shape
    L = ctx.shape[1]
    xc = ctx
    c_act = jax.nn.silu(c)
    cc_act = jax.nn.silu(c_ctx)
    for l in range(DEPTH):
        lp = {'w_in': w_in[l], 'w_out': w_out[l], 'a_shift': a_shift[l], 'a_w0': a_w0[l], 'a_w_up': a_w_up[l],
              'a_a0': a_a0[l], 'a_a_up': a_a_up[l], 'a_g_up': a_g_up[l], 'a_k_k': a_k_k[l], 'a_k_a': a_k_a[l],
              'a_r_k': a_r_k[l], 'a_lnx_g': a_lnx_g[l], 'a_lnx_b': a_lnx_b[l], 'b_sink': b_sink[l],
              'c_rpb': c_rpb[l], 'w_rg': w_rg[l], 'b_rg': b_rg[l], 'w_re': w_re[l], 'b_re': b_re[l],
              'w_gate': w_gate[l], 'w_up': w_up[l], 'w_down': w_down[l]}
        with_ctx = l < DEPTH - 1
        mod = (c_act @ w_mod[l] + b_mod[l]).reshape(Bn, N_MOD, 1, D)
        mod_c = (cc_act @ w_mod[l] + b_mod[l]).reshape(N_MOD, D)
        h = _modulate(_layer_norm(x), mod[:, 0], mod[:, 1])
        hc = _modulate(_layer_norm(xc), mod_c[0], mod_c[1])
        o, o_c = _token_mixer(h, hc, lp, with_ctx)
        x = _layer_norm(alpha * x + mod[:, 2] * o, ln1_g[l], ln1_b[l])
        h = _modulate(_layer_norm(x), mod[:, 3], mod[:, 4])
        if with_ctx:
            xc = _layer_norm(alpha * xc + mod_c[2] * o_c, ln1_g[l], ln1_b[l])
            hc = _modulate(_layer_norm(xc), mod_c[3], mod_c[4])
            f = _hier_moe(jnp.concatenate([h.reshape(-1, D), hc.reshape(-1, D)], 0), lp)
            f_lat = f[:Bn * S].reshape(Bn, S, D)
            xc = _layer_norm(alpha * xc + mod_c[5] * f[Bn * S:].reshape(Bn, L, D), ln2_g[l], ln2_b[l])
        else:
            f_lat = _hier_moe(h.reshape(-1, D), lp).reshape(Bn, S, D)
        x = _layer_norm(alpha * x + mod[:, 5] * f_lat, ln2_g[l], ln2_b[l])
    return x
```

```python
from contextlib import ExitStack
import numpy as np
import concourse.bass as bass
import concourse.mybir as mybir
from concourse.bass_utils import run_bass_kernel_spmd

F32 = mybir.dt.float32
BF16 = mybir.dt.bfloat16
ALU = mybir.AluOpType
AF = mybir.ActivationFunctionType
AX = mybir.AxisListType

DM = 1024; S = 2048; L = 256; T = S + L; NT = T // 128; DEPTH = 2
HD = 64; A_IN = 1184; N_IN = 2976; NE = 32; DE = 512
ALPHA = (2.0 * DEPTH) ** 0.25
SCALE = HD ** -0.5
C0 = float(np.exp(-0.5))
LN_EPS = 1e-6; GN_EPS = 64e-5
OFF_R, OFF_K, OFF_V, OFF_WD, OFF_AD, OFF_GD = 0, 256, 512, 768, 896, 1024
OFF_QB, OFF_KB, OFF_VB = 1184, 1568, 1696
OFF_QN, OFF_KN, OFF_VN = 1824, 2208, 2592
EPOCH = 30000
DEBUG = False
RW_STOP = 0
NCORES = 8
NE_DECL = 32
STOP = None


class Buf:
    __slots__ = ("w", "r")

    def __init__(self):
        self.w = {}
        self.r = {}


class Eng:
    def __init__(self, fw, name, eng, dma_only=False):
        self.fw = fw; self.name = name; self.eng = eng
        self.sem = None; self.cnt = 0; self.epoch = 0; self.seen = {}
        self.dma_only = dma_only
        if not dma_only:
            self.new_sem()

    def new_sem(self):
        self.sem = self.fw.nc.alloc_semaphore(f"p_{self.name}_{self.epoch}")
        self.cnt = 0
        self.epoch += 1
        self.fw.all_sems.append(self)


class FW:
    def __init__(self, nc, n_dma_sems=24):
        self.nc = nc
        self.all_sems = []
        self.pe = Eng(self, "pe", nc.tensor)
        self.act = Eng(self, "act", nc.scalar)
        self.dve = Eng(self, "dve", nc.vector)
        self.pool = Eng(self, "pool", nc.gpsimd)
        self.sp = Eng(self, "sp", nc.sync, dma_only=True)
        self.dma_sems = [[nc.alloc_semaphore(f"d{i}"), 0] for i in range(n_dma_sems)]
        self.dma_rr = 0
        self.sw_sems = [[nc.alloc_semaphore(f"w{i}"), 0] for i in range(8)]
        self.sw_rr = 0
        self.n_inst = 0

    def _wait(self, E, sem, val):
        k = sem.name
        if E.seen.get(k, 0) >= val:
            return
        E.eng.wait_ge(sem, val)
        E.seen[k] = val

    def _deps(self, E, reads, writes, skip_self=True):
        need = {}
        for b in reads:
            for k, sv in b.w.items():
                if k not in need or need[k][1] < sv[1]:
                    need[k] = sv
        for b in writes:
            for d in (b.w, b.r):
                for k, sv in d.items():
                    if k not in need or need[k][1] < sv[1]:
                        need[k] = sv
        me = E.sem.name if (skip_self and E.sem is not None and E.name == 'pe') else None
        for k, (s, v) in need.items():
            if k == me:
                continue
            self._wait(E, s, v)

    def _mark(self, sem, val, reads, writes):
        k = sem.name
        for b in reads:
            o = b.r.get(k)
            if o is None or o[1] < val:
                b.r[k] = (sem, val)
        for b in writes:
            o = b.w.get(k)
            if o is None or o[1] < val:
                b.w[k] = (sem, val)

    def op(self, E, reads, writes, fn):
        if E.cnt >= EPOCH:
            E.new_sem()
        self._deps(E, reads, writes)
        ins = fn()
        E.cnt += 1
        ins.then_inc(E.sem, 1)
        self._mark(E.sem, E.cnt, reads, writes)
        self.n_inst += 1
        return ins

    def dma(self, E, out, in_, reads, writes, **kw):
        if E.name == "pool":
            slot = self.sw_sems[self.sw_rr]
            self.sw_rr = (self.sw_rr + 1) % len(self.sw_sems)
        else:
            slot = self.dma_sems[self.dma_rr]
            self.dma_rr = (self.dma_rr + 1) % len(self.dma_sems)
        sem, cnt = slot
        if cnt > 0:
            self._wait(E, sem, cnt)
        self._deps(E, reads, writes, skip_self=False)
        ins = E.eng.dma_start(out=out, in_=in_, **kw)
        slot[1] = cnt + 16
        ins.then_inc(sem, 16)
        self._mark(sem, slot[1], reads, writes)
        self.n_inst += 1
        return ins

    def barrier(self):
        cur = []
        for X in (self.pe, self.act, self.dve, self.pool):
            if X.cnt > 0:
                cur.append((X.sem, X.cnt))
        for sem, cnt in self.dma_sems + self.sw_sems:
            if cnt > 0:
                cur.append((sem, cnt))
        for E in (self.pe, self.act, self.dve, self.pool, self.sp):
            for sem, val in cur:
                if E.sem is not None and sem.name == E.sem.name:
                    continue
                self._wait(E, sem, val)


def build_program():
    nc = bass.Bass("TRN2", target_bir_lowering=False)
    f = FW(nc)
    PE, ACT, DVE, POOL, SP = f.pe, f.act, f.dve, f.pool, f.sp

    def din(name, shape):
        return nc.dram_tensor(name, list(shape), F32, kind="ExternalInput").ap()

    def scr(name, shape):
        return nc.dram_tensor(name, list(shape), F32, kind=("ExternalOutput" if DEBUG else "Internal")).ap()

    x_in = din("x", [S, DM]); ctx_in = din("ctx", [L, DM]); ccol = din("ccol", [128, 8, 2])
    w_mod = din("w_mod", [DEPTH, DM, 6 * DM]); b_mod = din("b_mod", [DEPTH, 6 * DM])
    w_in = din("w_in", [DEPTH, DM, N_IN]); w_out = din("w_out", [DEPTH, DM, DM])
    a_shift = din("a_shift", [DEPTH, 3, A_IN])
    a_w_up = din("a_w_up", [DEPTH, 128, 256]); a_a_up = din("a_a_up", [DEPTH, 128, 256])
    a_g_up = din("a_g_up", [DEPTH, 160, 256])
    colpar = din("colpar", [DEPTH, 128, 48])
    a_r_k = din("a_r_k", [DEPTH, 256]); a_lnx_g = din("a_lnx_g", [DEPTH, 256]); a_lnx_b = din("a_lnx_b", [DEPTH, 256])
    b_sink = din("b_sink", [DEPTH, 6])
    rpbtab = din("rpbtab", [DEPTH, 6, 64, 15, 64])
    ln1_g = din("ln1_g", [DEPTH, DM]); ln1_b = din("ln1_b", [DEPTH, DM])
    ln2_g = din("ln2_g", [DEPTH, DM]); ln2_b = din("ln2_b", [DEPTH, DM])
    w_r = din("w_r", [DEPTH, DM, 36]); b_r = din("b_r", [DEPTH, 36])
    w_gate = din("w_gate", [DEPTH, NE_DECL, DM, DE]); w_up = din("w_up", [DEPTH, NE_DECL, DM, DE])
    w_down = din("w_down", [DEPTH, NE_DECL, DE, DM])
    cmasks = din("cmasks", [128, 4, 128]); cident = din("cident", [128, 128])
    cblock = din("cblock", [128, 128]); chsel = din("chsel", [128, 2])
    ccos = din("ccos", [64, S]); csin = din("csin", [64, S]); cropeP = din("cropeP", [64, 64])
    ccolvalid = din("ccolvalid", [128, 64]); cscanmask = din("cscanmask", [128, 512])
    y_out = nc.dram_tensor("y", [S, DM], F32, kind="ExternalOutput").ap()

    xs = scr("xs", [T, DM]); modrow = scr("modrow", [2, 6 * DM])
    zT = scr("zT", [N_IN, T]); vtok = scr("vtok", [T, 768]); omix = scr("omix", [T, DM])
    dbgh = scr("dbgh", [T, DM]) if DEBUG else None
    dbgs = scr("dbgs", [T, 4]) if DEBUG else None

    state = {"stop": False}

    def xsrc(l, i):
        if l == 0:
            return ctx_in[i * 128:(i + 1) * 128, :] if i < 2 else x_in[(i - 2) * 128:(i - 1) * 128, :]
        return xs[i * 128:(i + 1) * 128, :]

    top = ExitStack()
    uid = [0]

    def sbt(es, name, shape, dt=F32):
        uid[0] += 1
        return es.enter_context(nc.sbuf_tensor(f"{name}_{uid[0]}", list(shape), dt))

    def pst(es, name, shape, dt=F32):
        uid[0] += 1
        return es.enter_context(nc.psum_tensor(f"{name}_{uid[0]}", list(shape), dt))
    ident = sbt(top, "ident", [128, 128]); b_ident = Buf()
    identb = sbt(top, "identb", [128, 128], BF16); b_identb = Buf()
    f.dma(SP, ident[:], cident[:, :], [], [b_ident])
    f.op(DVE, [b_ident], [b_identb], lambda: nc.vector.tensor_copy(identb[:], ident[:]))

    def V(r, w, fn): return f.op(DVE, r, w, fn)
    def A(r, w, fn): return f.op(ACT, r, w, fn)
    def G(r, w, fn): return f.op(POOL, r, w, fn)
    def P(r, w, fn): return f.op(PE, r, w, fn)
    def D(out, in_, r, w, eng=None, **kw): return f.dma(eng or SP, out, in_, r, w, **kw)

    class Rot:
        def __init__(self, es, name, n, shape, dt=F32, psum=False):
            mk = pst if psum else sbt
            self.t = [mk(es, f"{name}{i}", shape, dt) for i in range(n)]
            self.b = [Buf() for _ in range(n)]
            self.i = 0

        def get(self):
            k = self.i; self.i = (self.i + 1) % len(self.t)
            return self.t[k], self.b[k]

    class Slots:
        def __init__(self, aps):
            self.t = list(aps); self.b = [Buf() for _ in self.t]; self.i = 0

        def get(self):
            k = self.i; self.i = (self.i + 1) % len(self.t)
            return self.t[k], self.b[k]

    def carve(es, name, nbanks, width):
        per = 512 // width
        views = [[None] * nbanks for _ in range(per)]
        bufs = [Buf() for _ in range(nbanks)]
        for i in range(nbanks):
            bk = pst(es, f"{name}{i}", [128, 512])
            for q in range(per):
                views[q][i] = bk[:, q * width:(q + 1) * width]
        sl = Slots([views[q][i] for q in range(per) for i in range(nbanks)])
        sl.b = [bufs[i] for q in range(per) for i in range(nbanks)]
        return sl

    def layer_norm_stats(es_tiles, src_ap, b_src, tagbufs):
        st, b_st, mv, b_mv, rs, b_rs = tagbufs
        V([b_src], [b_st], lambda: nc.vector.bn_stats(st[:, 0, :], src_ap[:, 0:512]))
        V([b_src], [b_st], lambda: nc.vector.bn_stats(st[:, 1, :], src_ap[:, 512:1024]))
        V([b_st], [b_mv], lambda: nc.vector.bn_aggr(mv[:], st[:].rearrange("p a b -> p (a b)")))
        A([b_mv], [b_rs], lambda: nc.scalar.activation(rs[:], mv[:, 1:2], AF.Sqrt, bias=eps_t[:, 0:1], scale=1.0))
        V([b_rs], [b_rs], lambda: nc.vector.reciprocal(rs[:], rs[:]))

    eps_t = sbt(top, "eps_t", [128, 4]); b_eps = Buf()
    G([], [b_eps], lambda: nc.gpsimd.memset(eps_t[:, 0:1], LN_EPS))
    G([], [b_eps], lambda: nc.gpsimd.memset(eps_t[:, 1:2], GN_EPS))
    G([], [b_eps], lambda: nc.gpsimd.memset(eps_t[:, 2:3], 1e-12))
    G([], [b_eps], lambda: nc.gpsimd.memset(eps_t[:, 3:4], 1.0))
    f.barrier()

    def phase_mod(l):
        with ExitStack() as es:
            cr = sbt(es, "cr", [128, 8, 2]); b_cr = Buf()
            cs = sbt(es, "cs", [128, 8, 2]); b_cs = Buf()
            D(cr[:], ccol[:, :, :], [], [b_cr])
            A([b_cr], [b_cs], lambda: nc.scalar.activation(cs[:], cr[:], AF.Silu))
            bm2 = sbt(es, "bm2", [2, 6 * DM]); b_bm2 = Buf()
            D(bm2[:], b_mod[l, :].partition_broadcast(2), [], [b_bm2])
            msb = sbt(es, "msb", [2, 6 * DM]); b_msb = Buf()
            wm = Rot(es, "wm", 3, [128, 3072])
            acc = [pst(es, f"macc{j}", [128, 512]) for j in range(6)]
            b_acc = [Buf() for _ in range(6)]
            for hf in range(2):
                for k in range(8):
                    wt, bw = wm.get()
                    D(wt[:], w_mod[l, k * 128:(k + 1) * 128, hf * 3072:(hf + 1) * 3072], [], [bw])
                    for j in range(6):
                        P([bw, b_cs], [b_acc[j]], lambda j=j, wt=wt, k=k: nc.tensor.matmul(
                            acc[j][0:2, :], cs[:, k, :], wt[:, j * 512:(j + 1) * 512], start=(k == 0), stop=(k == 7)))
                for j in range(6):
                    c0 = hf * 3072 + j * 512
                    V([b_acc[j], b_bm2], [b_msb], lambda j=j, c0=c0: nc.vector.tensor_tensor(
                        msb[:, c0:c0 + 512], acc[j][0:2, :], bm2[:, c0:c0 + 512], ALU.add))
            D(modrow[:, :], msb[:], [b_msb], [])
            f.barrier()

    def phase_ln_proj(l):
        with ExitStack() as es:
            hT = sbt(es, "hT", [128, 8, T], BF16); b_hT = [Buf() for _ in range(NT)]
            with ExitStack() as es2:
                mods = sbt(es2, "mods", [128, 2, 2 * DM]); b_mods = Buf()
                D(mods[:, 0, :], modrow[1, 0:2 * DM].partition_broadcast(128), [], [b_mods])
                D(mods[:, 1, :], modrow[0, 0:2 * DM].partition_broadcast(128), [], [b_mods])
                G([b_mods], [b_mods], lambda: nc.gpsimd.tensor_scalar(mods[:, :, DM:2 * DM], mods[:, :, DM:2 * DM], 1.0, None, ALU.add))
                xt = Rot(es2, "xt", 2, [128, DM]); xn = Rot(es2, "xn", 2, [128, DM]); hb = Rot(es2, "hb", 2, [128, DM], BF16)
                st = Rot(es2, "st", 2, [128, 2, 6]); mv = Rot(es2, "mv", 2, [128, 2]); rs = Rot(es2, "rs", 2, [128, 1])
                pT = Rot(es2, "pT", 2, [128, 8, 128], BF16, psum=True)
                for i in range(NT):
                    x_t, bx = xt.get(); n_t, bn = xn.get(); h_t, bh = hb.get()
                    s_t, bs = st.get(); m_t, bmv = mv.get(); r_t, br = rs.get(); p_t, bp = pT.get()
                    D(x_t[:], xsrc(l, i), [], [bx])
                    layer_norm_stats(None, x_t, bx, (s_t, bs, m_t, bmv, r_t, br))
                    V([bx, bmv, br], [bn], lambda x_t=x_t, n_t=n_t, m_t=m_t, r_t=r_t: nc.vector.tensor_scalar(
                        n_t[:], x_t[:], m_t[:, 0:1], r_t[:, 0:1], ALU.subtract, ALU.mult))
                    w = 0 if i < 2 else 1
                    G([bn, b_mods], [bn], lambda n_t=n_t, w=w: nc.gpsimd.tensor_tensor(n_t[:], n_t[:], mods[:, w, DM:2 * DM], ALU.mult))
                    G([bn, b_mods], [bn], lambda n_t=n_t, w=w: nc.gpsimd.tensor_tensor(n_t[:], n_t[:], mods[:, w, 0:DM], ALU.add))
                    A([bn], [bh], lambda n_t=n_t, h_t=h_t: nc.scalar.copy(h_t[:], n_t[:]))
                    if DEBUG:
                        D(dbgh[i * 128:(i + 1) * 128, :], n_t[:], [bn], [])
                        D(dbgs[i * 128:(i + 1) * 128, 0:2], m_t[:], [bmv], [], allow_slow_non_contiguous=True)
                        D(dbgs[i * 128:(i + 1) * 128, 2:3], r_t[:], [br], [], allow_slow_non_contiguous=True)
                    for k in range(8):
                        P([bh, b_identb], [bp], lambda k=k, h_t=h_t, p_t=p_t: nc.tensor.transpose(p_t[:, k, :], h_t[:, k * 128:(k + 1) * 128], identb[:]))
                    A([bp], [b_hT[i]], lambda i=i, p_t=p_t: nc.scalar.copy(hT[:, :, i * 128:(i + 1) * 128], p_t[:]))
                f.barrier()
            wb = sbt(es, "wb", [128, 8, N_IN], BF16); b_wb = [Buf() for _ in range(8)]
            for k in range(8):
                D(wb[:, k, :], w_in[l, k * 128:(k + 1) * 128, :], [], [b_wb[k]], eng=POOL)
            pp = Rot(es, "pp", 4, [128, 512], psum=True)
            stg = Rot(es, "stg", 4, [128, 512])
            cnt = 0
            for fc in range(24):
                fsz = 128 if fc < 23 else 32
                for tb in range(5):
                    t0 = tb * 512; tsz = min(512, T - t0)
                    p_t, bp = pp.get(); s_t, bs = stg.get()
                    rb = [b_hT[i] for i in range(t0 // 128, (t0 + tsz) // 128)]
                    for k in range(8):
                        P(rb + [b_wb[k]], [bp], lambda k=k, p_t=p_t, fc=fc, fsz=fsz, t0=t0, tsz=tsz: nc.tensor.matmul(
                            p_t[0:fsz, 0:tsz], wb[:, k, fc * 128:fc * 128 + fsz], hT[:, k, t0:t0 + tsz], start=(k == 0), stop=(k == 7)))
                    if cnt % 2 == 0:
                        A([bp], [bs], lambda p_t=p_t, s_t=s_t, fsz=fsz, tsz=tsz: nc.scalar.copy(s_t[0:fsz, 0:tsz], p_t[0:fsz, 0:tsz]))
                    else:
                        V([bp], [bs], lambda p_t=p_t, s_t=s_t, fsz=fsz, tsz=tsz: nc.vector.tensor_copy(s_t[0:fsz, 0:tsz], p_t[0:fsz, 0:tsz]))
                    cnt += 1
                    D(zT[fc * 128:fc * 128 + fsz, t0:t0 + tsz], s_t[0:fsz, 0:tsz], [bs], [])
            vst = Rot(es, "vst", 2, [128, 768])
            for i in range(NT):
                p1, bp1 = pp.get(); p2, bp2 = pp.get(); s_t, bs = vst.get()
                for (pt_, bpt_, c0_, c1_, w0_, wn_) in ((p1, bp1, 0, 256, OFF_V, 256), (p1, bp1, 256, 384, OFF_VB, 128), (p2, bp2, 0, 384, OFF_VN, 384)):
                    for k in range(8):
                        P([b_hT[i], b_wb[k]], [bpt_], lambda k=k: nc.tensor.matmul(
                            pt_[:, c0_:c1_], hT[:, k, i * 128:(i + 1) * 128], wb[:, k, w0_:w0_ + wn_], start=(k == 0), stop=(k == 7)))
                A([bp1], [bs], lambda p1=p1, s_t=s_t: nc.scalar.copy(s_t[:, 0:384], p1[:, 0:384]))
                V([bp2], [bs], lambda p2=p2, s_t=s_t: nc.vector.tensor_copy(s_t[:, 384:768], p2[:, 0:384]))
                D(vtok[i * 128:(i + 1) * 128, :], s_t[:], [bs], [])
            f.barrier()

    def conv_rows(dst, b_dst, src, b_src, wcol, b_w, c0, n):
        V([b_src, b_w], [b_dst], lambda: nc.vector.tensor_scalar(dst[:, 0:n], src[:, 0:n], wcol[:, 1:2], None, ALU.mult))
        segs = []
        for (a, b) in ((0, L), (L, T)):
            lo = max(a, c0); hi = min(b, c0 + n)
            if lo < hi:
                segs.append((lo - c0, hi - c0, lo == a, hi == b))
        return segs

    def phase_rwkv(l):
        with ExitStack() as es:
            cp = sbt(es, "cp", [128, 48]); b_cp = Buf()
            D(cp[:], colpar[l, :, :], [], [b_cp])
            wup = sbt(es, "wup", [128, 256]); aup = sbt(es, "aup", [128, 256]); b_up = Buf()
            D(wup[:], a_w_up[l, :, :], [], [b_up]); D(aup[:], a_a_up[l, :, :], [], [b_up])
            gup0 = sbt(es, "gup0", [128, 256]); gup1 = sbt(es, "gup1", [32, 256]); b_gup = Buf()
            D(gup0[:], a_g_up[l, 0:128, :], [], [b_gup]); D(gup1[:], a_g_up[l, 128:160, :], [], [b_gup])
            msk = sbt(es, "msk", [128, 4, 128]); b_msk = Buf()
            D(msk[:], cmasks[:, :, :], [], [b_msk])
            blk = sbt(es, "blk", [128, 128]); hsel = sbt(es, "hsel", [128, 2]); b_cst = Buf()
            D(blk[:], cblock[:, :], [], [b_cst]); D(hsel[:], chsel[:, :], [], [b_cst])
            scm = sbt(es, "scm", [128, 512]); D(scm[:], cscanmask[:, :], [], [b_cst])
            vsh = sbt(es, "vsh", [128, 3, 256]); b_vsh = Buf()
            D(vsh[:], a_shift[l, :, OFF_V:OFF_V + 256].partition_broadcast(128), [], [b_vsh])
            yacc = sbt(es, "yacc", [128, NT, 256]); b_yacc = [Buf() for _ in range(NT)]
            bacc = sbt(es, "bacc", [128, NT, 4]); b_bacc = [Buf() for _ in range(NT)]
            Vt = sbt(es, "Vt", [128, NT, 256]); b_Vt = [Buf() for _ in range(NT)]
            Vtb = sbt(es, "Vtb", [128, NT, 256], BF16)
            G([], b_yacc, lambda: nc.gpsimd.memset(yacc[:], 0.0))
            G([], b_bacc, lambda: nc.gpsimd.memset(bacc[:], 0.0))
            Mst = [[sbt(es, f"M{d}{j}", [128, 64]) for j in range(2)] for d in range(2)]
            b_M = [[Buf() for j in range(2)] for d in range(2)]
            for d in range(2):
                for j in range(2):
                    G([], [b_M[d][j]], lambda d=d, j=j: nc.gpsimd.memset(Mst[d][j][:], 0.0))
            with ExitStack() as es2:
                vr = Rot(es2, "vr", 6, [128, 256])
                for i in range(NT):
                    tp, bp_ = vr.get(); tc, bc_ = vr.get(); tn, bn_ = vr.get()
                    first = i in (0, 2); last = i in (1, NT - 1)
                    r0 = i * 128
                    D(tc[:], vtok[r0:r0 + 128, 0:256], [], [bc_])
                    if first:
                        G([], [bp_], lambda tp=tp: nc.gpsimd.memset(tp[:], 0.0))
                        D(tp[1:128, :], vtok[r0:r0 + 127, 0:256], [], [bp_])
                    else:
                        D(tp[:], vtok[r0 - 1:r0 + 127, 0:256], [], [bp_])
                    if last:
                        G([], [bn_], lambda tn=tn: nc.gpsimd.memset(tn[:], 0.0))
                        D(tn[0:127, :], vtok[r0 + 1:r0 + 128, 0:256], [], [bn_])
                    else:
                        D(tn[:], vtok[r0 + 1:r0 + 129, 0:256], [], [bn_])
                    V([bc_, b_vsh], [b_Vt[i]], lambda i=i, tc=tc: nc.vector.tensor_tensor(Vt[:, i, :], tc[:], vsh[:, 1, :], ALU.mult))
                    G([bp_, b_vsh], [bp_], lambda tp=tp: nc.gpsimd.tensor_tensor(tp[:], tp[:], vsh[:, 0, :], ALU.mult))
                    G([bn_, b_vsh], [bn_], lambda tn=tn: nc.gpsimd.tensor_tensor(tn[:], tn[:], vsh[:, 2, :], ALU.mult))
                    V([bp_, b_Vt[i]], [b_Vt[i]], lambda i=i, tp=tp: nc.vector.tensor_tensor(Vt[:, i, :], Vt[:, i, :], tp[:], ALU.add))
                    V([bn_, b_Vt[i]], [b_Vt[i]], lambda i=i, tn=tn: nc.vector.tensor_tensor(Vt[:, i, :], Vt[:, i, :], tn[:], ALU.add))
                    A([b_Vt[i]], [b_Vt[i]], lambda i=i: nc.scalar.copy(Vtb[:, i, :], Vt[:, i, :]))
                f.barrier()
            if RW_STOP == 1:
                return

            blocks = [(0, 256)] + [(256 + 256 * q, 256) for q in range(8)]
            order = {0: list(range(9)), 1: [0] + list(range(8, 0, -1))}
            with ExitStack() as es2:
                BW = 256
                raw = Rot(es2, "raw", 3, [128, BW + 2])
                rT = [[sbt(es2, f"rT{d}{j}", [128, BW]) for j in range(2)] for d in range(2)]
                kT = [[sbt(es2, f"kT{d}{j}", [128, BW]) for j in range(2)] for d in range(2)]
                kkT = [[sbt(es2, f"kkT{d}{j}", [128, BW]) for j in range(2)] for d in range(2)]
                twd = [sbt(es2, f"twd{d}", [128, BW]) for d in range(2)]
                tad = [sbt(es2, f"tad{d}", [128, BW]) for d in range(2)]
                names = ["t0", "t1", "t2", "bb", "kd", "p1", "p2", "e1", "e2"]
                rows = {nm: [[sbt(es2, f"{nm}{d}{j}", [128, BW]) for j in range(2)] for d in range(2)] for nm in names}
                q12 = [[sbt(es2, f"q12{d}{j}", [128, 2, 2, 128]) for j in range(2)] for d in range(2)]
                tot = [[sbt(es2, f"tot{d}{j}", [128, 4]) for j in range(2)] for d in range(2)]
                gC = [[sbt(es2, f"gC{d}{j}", [128, 4]) for j in range(2)] for d in range(2)]
                b_row = [[Buf() for j in range(2)] for d in range(2)]
                b_sh = [Buf() for d in range(2)]
                tmp = Rot(es2, "tmp", 3, [128, BW])
                bnames = ["p1b", "p2b", "e1b", "e2b"]
                brows = {nm: [[sbt(es2, f"{nm}{d}{j}", [128, BW], BF16) for j in range(2)] for d in range(2)] for nm in bnames}
                q12b = [[sbt(es2, f"q12b{d}{j}", [128, 2, 2, 128], BF16) for j in range(2)] for d in range(2)]
                Mb = [[sbt(es2, f"Mb{d}{j}", [128, 64], BF16) for j in range(2)] for d in range(2)]
                for d in range(2):
                    for j in range(2):
                        G([], [b_M[d][j]], lambda d=d, j=j: nc.gpsimd.memset(Mb[d][j][:], 0.0))
                bankX = pst(es2, "bankX", [128, 512]); b_bankX = Buf()
                pM = [[bankX[:, (2 * d + j) * 64:(2 * d + j) * 64 + 64] for j in range(2)] for d in range(2)]
                b_pM = [[b_bankX for j in range(2)] for d in range(2)]
                ps5 = Slots([bankX[:, 256:512]]); ps5.b = [b_bankX]
                pN = carve(es2, "pN", 2, 128)
                pW = carve(es2, "pW", 3, 256)
                pA = pW
                pQ = carve(es2, "pQ", 1, 64)
                pEbank = pst(es2, "pEb", [128, 1024], BF16); b_pE = Buf()
                pE = Slots([pEbank[:, q * 256:(q + 1) * 256] for q in range(4)]); pE.b = [b_pE] * 4
                NCH = 16
                cA1 = [sbt(es2, f"cA1_{k}", [128, 256], BF16) for k in range(NCH)]
                cA2 = [sbt(es2, f"cA2_{k}", [128, 256], BF16) for k in range(NCH)]
                cbA = [(Buf(), Buf()) for k in range(NCH)]
                cPR = [Rot(es2, f"cPR{k}_", 3, [128, 256], BF16) for k in range(NCH)]
                cPT = [Rot(es2, f"cPT{k}_", 3, [128, 128], BF16) for k in range(NCH)]
                cSq = [Rot(es2, f"cQ{k}_", 2, [128, 64], BF16) for k in range(NCH)]
                cE = [sbt(es2, f"cE_{k}", [128, 256], BF16) for k in range(NCH // 2)]
                cbE = [Buf() for k in range(NCH // 2)]

                def load_conv(dst, b_dst, frow, nrows, chunk, tap_chunk, c0, n):
                    rt, br = raw.get()
                    seq_lo, seq_hi = (0, L) if c0 < L else (L, T)
                    lo = max(seq_lo, c0 - 1); hi = min(seq_hi, c0 + n + 1)
                    if lo > c0 - 1 or hi < c0 + n + 1:
                        G([], [br], lambda rt=rt: nc.gpsimd.memset(rt[:], 0.0))
                    D(rt[0:nrows, lo - (c0 - 1):hi - (c0 - 1)], zT[frow:frow + nrows, lo:hi], [], [br])
                    w = lambda tap: cp[0:nrows, tap_chunk * 3 + tap:tap_chunk * 3 + tap + 1]
                    V([br, b_cp], [b_dst], lambda: nc.vector.tensor_scalar(dst[0:nrows, 0:n], rt[0:nrows, 1:n + 1], w(1), None, ALU.mult))
                    V([br, b_cp, b_dst], [b_dst], lambda: nc.vector.scalar_tensor_tensor(dst[0:nrows, 0:n], rt[0:nrows, 0:n], w(0), dst[0:nrows, 0:n], ALU.mult, ALU.add))
                    V([br, b_cp, b_dst], [b_dst], lambda: nc.vector.scalar_tensor_tensor(dst[0:nrows, 0:n], rt[0:nrows, 2:n + 2], w(2), dst[0:nrows, 0:n], ALU.mult, ALU.add))

                def prep_block(d, bi):
                    c0, n = blocks[bi]
                    nch = n // 128
                    load_conv(twd[d], b_sh[d], OFF_WD, 128, None, 6, c0, n)
                    A([b_sh[d]], [b_sh[d]], lambda: nc.scalar.activation(twd[d][:, 0:n], twd[d][:, 0:n], AF.Tanh))
                    load_conv(tad[d], b_sh[d], OFF_AD, 128, None, 7, c0, n)
                    hp = slice(64 * d, 64 * d + 64)
                    for j in range(2):
                        R = {nm: rows[nm][d][j] for nm in names}
                        br_ = b_row[d][j]
                        load_conv(rT[d][j], br_, OFF_R + 128 * j, 128, None, j, c0, n)
                        load_conv(kT[d][j], br_, OFF_K + 128 * j, 128, None, 2 + j, c0, n)
                        V([br_, b_cp], [br_], lambda: nc.vector.tensor_scalar(kkT[d][j][:, 0:n], kT[d][j][:, 0:n], cp[:, 38 + j:39 + j], None, ALU.mult))
                        tq, btq = tmp.get()
                        G([br_], [btq], lambda: nc.gpsimd.tensor_tensor(tq[:, 0:n], kkT[d][j][:, 0:n], kkT[d][j][:, 0:n], ALU.mult))
                        p5, bp5 = ps5.get()
                        P([btq, b_cst], [bp5], lambda: nc.tensor.matmul(p5[:, 0:n], blk[:], tq[:, 0:n], start=True, stop=True))
                        A([bp5], [btq], lambda: nc.scalar.activation(tq[:, 0:n], p5[:, 0:n], AF.Sqrt, bias=eps_t[:, 2:3], scale=1.0))
                        V([btq], [btq], lambda: nc.vector.reciprocal(tq[:, 0:n], tq[:, 0:n]))
                        V([btq, br_], [br_], lambda: nc.vector.tensor_tensor(kkT[d][j][:, 0:n], kkT[d][j][:, 0:n], tq[:, 0:n], ALU.mult))
                        p5, bp5 = ps5.get()
                        P([b_sh[d], b_up], [bp5], lambda: nc.tensor.matmul(p5[:, 0:n], wup[hp, j * 128:(j + 1) * 128], twd[d][hp, 0:n], start=True, stop=True))
                        A([bp5, b_cp], [br_], lambda: nc.scalar.activation(R["t0"][:, 0:n], p5[:, 0:n], AF.Sigmoid, bias=cp[:, 30 + d * 2 + j:31 + d * 2 + j], scale=1.0))
                        p5, bp5 = ps5.get()
                        P([b_sh[d], b_up], [bp5], lambda: nc.tensor.matmul(p5[:, 0:n], aup[hp, j * 128:(j + 1) * 128], tad[d][hp, 0:n], start=True, stop=True))
                        A([bp5, b_cp], [br_], lambda: nc.scalar.activation(R["bb"][:, 0:n], p5[:, 0:n], AF.Sigmoid, bias=cp[:, 34 + d * 2 + j:35 + d * 2 + j], scale=1.0))
                        V([br_, b_cp], [br_], lambda: nc.vector.tensor_scalar(R["kd"][:, 0:n], R["bb"][:, 0:n], -1.0, cp[:, 40 + j:41 + j], ALU.add, ALU.mult))
                        V([br_], [br_], lambda: nc.vector.scalar_tensor_tensor(R["kd"][:, 0:n], R["kd"][:, 0:n], 1.0, kT[d][j][:, 0:n], ALU.add, ALU.mult))
                        V([br_], [br_], lambda: nc.vector.tensor_tensor(R["bb"][:, 0:n], R["bb"][:, 0:n], kkT[d][j][:, 0:n], ALU.mult))
                        tq, btq = tmp.get()
                        V([br_, b_cp], [btq], lambda: nc.vector.scalar_tensor_tensor(tq[:, 0:n], rT[d][j][:, 0:n], cp[:, 42 + j:43 + j], R["kd"][:, 0:n], ALU.mult, ALU.mult))
                        for c in range(nch):
                            ti = c0 // 128 + c
                            pq, bpq = pQ.get()
                            P([btq, b_cst], [bpq], lambda c=c, pq=pq: nc.tensor.matmul(pq[:, 0:2], tq[:, c * 128:(c + 1) * 128], hsel[:], start=True, stop=True))
                            V([bpq, b_bacc[ti]], [b_bacc[ti]], lambda ti=ti, pq=pq: nc.vector.tensor_tensor(bacc[:, ti, 2 * j:2 * j + 2], bacc[:, ti, 2 * j:2 * j + 2], pq[:, 0:2], ALU.add))
                        V([br_, b_cst], [br_], lambda: nc.vector.tensor_tensor_scan(R["t1"][:, 0:n], scm[:, 0:n], R["t0"][:, 0:n], 0.0, ALU.mult, ALU.add))
                        V([br_], [br_], lambda: nc.vector.tensor_tensor(R["t0"][:, 0:n], R["t1"][:, 0:n], R["t0"][:, 0:n], ALU.subtract))
                        V([br_], [br_], lambda: nc.vector.tensor_copy(tot[d][j][:, 0:nch], R["t1"][:, 127:n:128]))
                        A([br_], [br_], lambda: nc.scalar.activation(gC[d][j][:, 0:nch], tot[d][j][:, 0:nch], AF.Exp, scale=-C0))
                        totb = tot[d][j][:, 0:nch].unsqueeze(2).broadcast_to([128, nch, 128])
                        v3 = lambda ap: ap[:, 0:n].rearrange("p (c t) -> p c t", t=128)
                        q1v = q12[d][j][:, 0:nch, 0, :]; q2v = q12[d][j][:, 0:nch, 1, :]
                        if d == 0:
                            V([br_], [br_], lambda: nc.vector.tensor_tensor(v3(R["t2"]), totb, v3(R["t1"]), ALU.subtract))
                            A([br_], [br_], lambda: nc.scalar.activation(q1v, v3(R["t0"]), AF.Exp, scale=-C0))
                            A([br_], [br_], lambda: nc.scalar.activation(q2v, v3(R["t1"]), AF.Exp, scale=-C0))
                            A([br_], [br_], lambda: nc.scalar.activation(R["p1"][:, 0:n], R["t1"][:, 0:n], AF.Exp, scale=C0))
                            A([br_], [br_], lambda: nc.scalar.activation(R["e1"][:, 0:n], R["t2"][:, 0:n], AF.Exp, scale=-C0))
                        else:
                            V([br_], [br_], lambda: nc.vector.tensor_tensor(v3(R["t2"]), totb, v3(R["t1"]), ALU.subtract))
                            V([br_], [br_], lambda: nc.vector.tensor_tensor(v3(R["t1"]), totb, v3(R["t0"]), ALU.subtract))
                            A([br_], [br_], lambda: nc.scalar.activation(q1v, v3(R["t2"]), AF.Exp, scale=-C0))
                            A([br_], [br_], lambda: nc.scalar.activation(q2v, v3(R["t1"]), AF.Exp, scale=-C0))
                            A([br_], [br_], lambda: nc.scalar.activation(R["p1"][:, 0:n], R["t1"][:, 0:n], AF.Exp, scale=C0))
                            A([br_], [br_], lambda: nc.scalar.activation(R["e1"][:, 0:n], R["t0"][:, 0:n], AF.Exp, scale=-C0))
                        BR = {nm: brows[nm][d][j] for nm in bnames}
                        q1b = q12b[d][j][:, 0:nch, 0, :]; q2b = q12b[d][j][:, 0:nch, 1, :]
                        V([br_], [br_], lambda: nc.vector.tensor_tensor(q1b, q1v, v3(kkT[d][j]), ALU.mult))
                        G([br_], [br_], lambda: nc.gpsimd.tensor_tensor(q2b, q2v, v3(rT[d][j]), ALU.mult))
                        V([br_], [br_], lambda: nc.vector.tensor_tensor(BR["p2b"][:, 0:n], R["p1"][:, 0:n], R["kd"][:, 0:n], ALU.mult))
                        G([br_], [br_], lambda: nc.gpsimd.tensor_tensor(BR["p1b"][:, 0:n], R["p1"][:, 0:n], R["bb"][:, 0:n], ALU.mult))
                        V([br_], [br_], lambda: nc.vector.tensor_tensor(BR["e2b"][:, 0:n], R["e1"][:, 0:n], R["bb"][:, 0:n], ALU.mult))
                        G([br_], [br_], lambda: nc.gpsimd.tensor_tensor(BR["e1b"][:, 0:n], R["e1"][:, 0:n], R["kd"][:, 0:n], ALU.mult))

                def neumann_chain(k, d, j, hh, bi, c):
                    cs_ = slice(c * 128, (c + 1) * 128)
                    br_ = b_row[d][j]
                    BR = {nm: brows[nm][d][j] for nm in bnames}
                    mT = msk[:, 0:2, :] if d == 0 else msk[:, 2:4, :]
                    mN = msk[:, 2, :] if d == 0 else msk[:, 0, :]
                    hp = slice(64 * hh, 64 * hh + 64)
                    ke = k // 2
                    if hh == 0:
                        pe_, bpe = pE.get()
                        P([br_, b_identb], [bpe], lambda: nc.tensor.transpose(pe_[:, 0:128], BR["e1b"][:, cs_], identb[:]))
                        P([br_, b_identb], [bpe], lambda: nc.tensor.transpose(pe_[:, 128:256], BR["e2b"][:, cs_], identb[:]))
                        A([bpe], [cbE[ke]], lambda: nc.scalar.copy(cE[ke][:], pe_[:]))
                    yield
                    q12c = q12b[d][j][hp, c, :, :].rearrange("p a t -> p (a t)")
                    q1c = q12b[d][j][hp, c, 0, :]
                    A1m, A2m = cA1[k], cA2[k]; bA1, bA2 = cbA[k]
                    a1, ba1 = pA.get()
                    P([br_], [ba1], lambda: nc.tensor.matmul(a1[:], BR["p1b"][hp, cs_], q12c, start=True, stop=True))
                    V([ba1, b_msk], [bA1], lambda: nc.vector.tensor_tensor(A1m[:].rearrange("p (a t) -> p a t", a=2), a1[:].rearrange("p (a t) -> p a t", a=2), mT, ALU.mult))
                    yield
                    a2, ba2 = pA.get()
                    P([br_], [ba2], lambda: nc.tensor.matmul(a2[:], BR["p2b"][hp, cs_], q12c, start=True, stop=True))
                    V([ba2, b_msk], [bA2], lambda: nc.vector.tensor_tensor(A2m[:].rearrange("p (a t) -> p a t", a=2), a2[:].rearrange("p (a t) -> p a t", a=2), mT, ALU.mult))
                    yield
                    an, ban = pN.get()
                    P([br_], [ban], lambda: nc.tensor.matmul(an[:], q1c, BR["p1b"][hp, cs_], start=True, stop=True))
                    PR, bPR = cPR[k].get(); PT, bPT = cPT[k].get()
                    V([ban, b_msk], [bPT], lambda: nc.vector.scalar_tensor_tensor(PT[:], an[:], -1.0, mN, ALU.mult, ALU.mult))
                    G([bA1], [bPR], lambda: nc.gpsimd.tensor_scalar(PR[:, 0:128], A1m[:, 0:128], -1.0, None, ALU.mult))
                    G([b_identb], [bPR], lambda: nc.gpsimd.tensor_copy(PR[:, 128:256], identb[:]))
                    yield
                    for lev in range(7):
                        lastl = lev == 6
                        if not lastl:
                            w1, bw1 = pW.get()
                            P([bPT, bPR], [bw1], lambda: nc.tensor.matmul(w1[:], PT[:], PR[:], start=True, stop=True))
                            PRn, bPRn = cPR[k].get()
                            A([bw1], [bPRn], lambda: nc.scalar.copy(PRn[:, 0:128], w1[:, 0:128]))
                            V([bw1, bPR], [bPRn], lambda: nc.vector.tensor_tensor(PRn[:, 128:256], w1[:, 128:256], PR[:, 128:256], ALU.add))
                            if lev < 5:
                                w2, bw2 = pN.get()
                                P([bPT, bPR], [bw2], lambda: nc.tensor.matmul(w2[:], PR[:, 0:128], PT[:], start=True, stop=True))
                                PTn, bPTn = cPT[k].get()
                                if (k + lev) % 2 == 0:
                                    A([bw2], [bPTn], lambda: nc.scalar.copy(PTn[:], w2[:]))
                                else:
                                    V([bw2], [bPTn], lambda: nc.vector.tensor_copy(PTn[:], w2[:]))
                            elif lev == 5:
                                w2, bw2 = pN.get()
                                P([bPT, bPR], [bw2], lambda: nc.tensor.matmul(w2[:], PR[:, 0:128], PT[:], start=True, stop=True))
                                PTn, bPTn = cPT[k].get()
                                A([bw2], [bPTn], lambda: nc.scalar.copy(PTn[:], w2[:]))
                            PR, bPR = PRn, bPRn
                            PT, bPT = PTn, bPTn
                        else:
                            w2, bw2 = pN.get()
                            P([bPT, bPR], [bw2], lambda: nc.tensor.matmul(w2[:], PT[:], PR[:, 128:256], start=True, stop=True))
                            PRn, bPRn = cPR[k].get()
                            V([bw2, bPR], [bPRn], lambda: nc.vector.tensor_tensor(PRn[:, 128:256], w2[:], PR[:, 128:256], ALU.add))
                            PR, bPR = PRn, bPRn
                        yield
                    chainR[k] = (PR[:, 128:256], bPR)

                def seq_chain(k, d, j, hh, bi, c):
                    c0, n = blocks[bi]
                    ti = c0 // 128 + c
                    br_ = b_row[d][j]
                    hp = slice(64 * hh, 64 * hh + 64)
                    hcol = slice((2 * j + hh) * 64, (2 * j + hh) * 64 + 64)
                    q1c = q12b[d][j][hp, c, 0, :]; q2c = q12b[d][j][hp, c, 1, :]
                    A1m, A2m = cA1[k], cA2[k]; bA1, bA2 = cbA[k]
                    Rm, bR = chainR[k]
                    E_, bE = cE[k // 2], cbE[k // 2]
                    Mh = Mb[d][j][hp, :]
                    vt = Vtb[:, ti, hcol]
                    pp_, bpp = pQ.get()
                    P([br_, b_M[d][j]], [bpp], lambda: nc.tensor.matmul(pp_[:], q1c, Mh, start=True, stop=False))
                    P([bA2, b_Vt[ti]], [bpp], lambda: nc.tensor.matmul(pp_[:], A2m[:, 0:128], vt, start=False, stop=True))
                    Psb, bPsb = cSq[k].get()
                    A([bpp], [bPsb], lambda: nc.scalar.copy(Psb[:], pp_[:]))
                    yield
                    up, bup = pQ.get()
                    P([bR, bPsb], [bup], lambda: nc.tensor.matmul(up[:], Rm, Psb[:], start=True, stop=True))
                    nU, bnU = cSq[k].get()
                    A([bup], [bnU], lambda: nc.scalar.mul(nU[:], up[:], -1.0))
                    yield
                    yp, byp = pQ.get()
                    P([br_, b_M[d][j]], [byp], lambda: nc.tensor.matmul(yp[:], q2c, Mh, start=True, stop=False))
                    P([bA2, b_Vt[ti]], [byp], lambda: nc.tensor.matmul(yp[:], A2m[:, 128:256], vt, start=False, stop=False))
                    P([bA1, bnU], [byp], lambda: nc.tensor.matmul(yp[:], A1m[:, 128:256], nU[:], start=False, stop=True))
                    V([byp, b_yacc[ti]], [b_yacc[ti]], lambda: nc.vector.tensor_tensor(yacc[:, ti, hcol], yacc[:, ti, hcol], yp[:], ALU.add))
                    yield
                    pm = pM[d][j]
                    P([bE, b_Vt[ti]], [b_pM[d][j]], lambda: nc.tensor.matmul(pm[hp, :], E_[:, 64 * hh:64 * hh + 64], vt, start=True, stop=False))
                    P([bE, bnU], [b_pM[d][j]], lambda: nc.tensor.matmul(pm[hp, :], E_[:, 128 + 64 * hh:128 + 64 * hh + 64], nU[:], start=False, stop=True))
                    V([b_pM[d][j], b_M[d][j], br_], [b_M[d][j]], lambda: nc.vector.scalar_tensor_tensor(
                        Mst[d][j][hp, :], Mst[d][j][hp, :], gC[d][j][hp, c:c + 1], pm[hp, :], ALU.mult, ALU.add))
                    G([b_M[d][j]], [b_M[d][j]], lambda: nc.gpsimd.tensor_copy(Mb[d][j][hp, :], Mst[d][j][hp, :]))
                    yield

                def run_rr(gens):
                    gens = list(gens)
                    while gens:
                        nxt = []
                        for g in gens:
                            try:
                                next(g); nxt.append(g)
                            except StopIteration:
                                pass
                        gens = nxt

                chainR = {}
                for step in range(9):
                    if RW_STOP in (2, 3) and step > 0:
                        break
                    for d in range(2):
                        prep_block(d, order[d][step])
                    if RW_STOP == 2:
                        break
                    chains = []
                    for cc in range(2):
                        for d in range(2):
                            bi = order[d][step]
                            c = cc if d == 0 else 1 - cc
                            for j in range(2):
                                for hh in range(2):
                                    chains.append((len(chains), d, j, hh, bi, c))
                    run_rr([neumann_chain(*ch) for ch in chains])
                    for cc in range(2):
                        run_rr([seq_chain(*ch) for ch in chains[cc * 8:(cc + 1) * 8]])
                f.barrier()
            if RW_STOP in (2, 3):
                return

            with ExitStack() as es2:
                gs0 = sbt(es2, "gs0", [128, T]); gs1 = sbt(es2, "gs1", [32, T]); b_gs = Buf()
                rw = sbt(es2, "rw", [128, T + 2]); b_rw = Buf()
                for (dst, frow, nrows, tapc) in ((gs0, OFF_GD, 128, 8), (gs1, OFF_GD + 128, 32, 9)):
                    G([], [b_rw], lambda: nc.gpsimd.memset(rw[:], 0.0))
                    D(rw[0:nrows, 1:T + 1], zT[frow:frow + nrows, :], [], [b_rw])
                    w = lambda tap, tapc=tapc, nrows=nrows: cp[0:nrows, tapc * 3 + tap:tapc * 3 + tap + 1]
                    V([b_rw, b_cp], [b_gs], lambda: nc.vector.tensor_scalar(dst[0:nrows, :], rw[0:nrows, 1:T + 1], w(1), None, ALU.mult))
                    for (a, b) in ((0, L), (L, T)):
                        V([b_rw, b_cp, b_gs], [b_gs], lambda a=a, b=b: nc.vector.scalar_tensor_tensor(
                            dst[0:nrows, a + 1:b], rw[0:nrows, a + 1:b], w(0), dst[0:nrows, a + 1:b], ALU.mult, ALU.add))
                        V([b_rw, b_cp, b_gs], [b_gs], lambda a=a, b=b: nc.vector.scalar_tensor_tensor(
                            dst[0:nrows, a:b - 1], rw[0:nrows, a + 2:b + 1], w(2), dst[0:nrows, a:b - 1], ALU.mult, ALU.add))
                    A([b_gs], [b_gs], lambda: nc.scalar.activation(dst[0:nrows, :], dst[0:nrows, :], AF.Sigmoid))
                gb = sbt(es2, "gb", [128, 2, 256]); b_gb = Buf()
                D(gb[:, 0, :], a_lnx_g[l, :].partition_broadcast(128), [], [b_gb])
                D(gb[:, 1, :], a_lnx_b[l, :].partition_broadcast(128), [], [b_gb])
                pg = carve(es2, "pg", 1, 256)
                st4 = Rot(es2, "st4", 2, [128, 4, 6]); mv4 = Rot(es2, "mv4", 2, [128, 4, 2]); rs4 = Rot(es2, "rs4", 2, [128, 4])
                yn = Rot(es2, "yn", 2, [128, 256]); oo = Rot(es2, "oo", 2, [128, 256])
                for i in range(NT):
                    g_p, bg = pg.get()
                    P([b_gs, b_gup], [bg], lambda: nc.tensor.matmul(g_p[:], gs0[:, i * 128:(i + 1) * 128], gup0[:], start=True, stop=False))
                    P([b_gs, b_gup], [bg], lambda: nc.tensor.matmul(g_p[:], gs1[0:32, i * 128:(i + 1) * 128], gup1[:], start=False, stop=True))
                    s4, bs4 = st4.get(); m4, bm4 = mv4.get(); r4, br4 = rs4.get(); y_n, byn = yn.get(); o_t, bo = oo.get()
                    for h in range(4):
                        V([b_yacc[i]], [bs4], lambda h=h: nc.vector.bn_stats(s4[:, h, :], yacc[:, i, h * 64:(h + 1) * 64]))
                    for h in range(4):
                        V([bs4], [bm4], lambda h=h: nc.vector.bn_aggr(m4[:, h, :], s4[:, h, :]))
                    A([bm4], [br4], lambda: nc.scalar.activation(r4[:], m4[:, :, 1], AF.Sqrt, bias=eps_t[:, 1:2], scale=1.0))
                    V([br4], [br4], lambda: nc.vector.reciprocal(r4[:], r4[:]))
                    for h in range(4):
                        V([b_yacc[i], bm4, br4], [byn], lambda h=h: nc.vector.tensor_scalar(
                            y_n[:, h * 64:(h + 1) * 64], yacc[:, i, h * 64:(h + 1) * 64], m4[:, h, 0:1], r4[:, h:h + 1], ALU.subtract, ALU.mult))
                    G([byn, b_gb], [byn], lambda: nc.gpsimd.tensor_tensor(y_n[:], y_n[:], gb[:, 0, :], ALU.mult))
                    G([byn, b_gb], [byn], lambda: nc.gpsimd.tensor_tensor(y_n[:], y_n[:], gb[:, 1, :], ALU.add))
                    for h in range(4):
                        V([byn, b_bacc[i], b_Vt[i]], [byn], lambda h=h: nc.vector.scalar_tensor_tensor(
                            y_n[:, h * 64:(h + 1) * 64], Vt[:, i, h * 64:(h + 1) * 64], bacc[:, i, h:h + 1], y_n[:, h * 64:(h + 1) * 64], ALU.mult, ALU.add))
                    V([byn, bg], [bo], lambda: nc.vector.tensor_tensor(o_t[:], y_n[:], g_p[:], ALU.mult))
                    D(omix[i * 128:(i + 1) * 128, 0:256], o_t[:], [bo], [])
                f.barrier()

    def phase_attn_b(l):
        with_ctx = l < DEPTH - 1
        with ExitStack() as es:
            cosT = sbt(es, "cosT", [64, S]); sinT = sbt(es, "sinT", [64, S]); rPT = sbt(es, "rPT", [64, 64]); b_c = Buf()
            D(cosT[:], ccos[:, :], [], [b_c]); D(sinT[:], csin[:, :], [], [b_c]); D(rPT[:], cropeP[:, :], [], [b_c])
            mkf = sbt(es, "mkf", [128, 4, 128]); mkb = sbt(es, "mkb", [128, 4, 128], BF16); b_mk = Buf()
            D(mkf[:], cmasks[:, :, :], [], [b_mk])
            V([b_mk], [b_mk], lambda: nc.vector.tensor_copy(mkb[:], mkf[:]))
            esk = sbt(es, "esk", [128, 6]); b_esk = Buf()
            D(esk[:], b_sink[l, :].partition_broadcast(128), [], [b_esk])
            A([b_esk], [b_esk], lambda: nc.scalar.activation(esk[:], esk[:], AF.Exp))
            kTb = sbt(es, "kTb", [64, 2, T], BF16); b_k = [Buf() for _ in range(2)]
            qTb = sbt(es, "qTb", [64, 2, NT, 3, 128], BF16); b_q = [Buf() for _ in range(6)]
            Va = sbt(es, "Va", [128, 2, NT, 65], BF16); b_Va = Buf()
            G([], [b_Va], lambda: nc.gpsimd.memset(Va[:], 1.0))
            with ExitStack() as es2:
                vf = sbt(es2, "vf", [128, NT, 128]); b_vf = Buf()
                D(vf[:], vtok[:, 256:384].rearrange("(n p) f -> p n f", p=128), [], [b_vf])
                for g in range(2):
                    V([b_vf, b_Va], [b_Va], lambda g=g: nc.vector.tensor_copy(Va[:, g, :, 0:64], vf[:, :, g * 64:(g + 1) * 64]))
                raw = Rot(es2, "rawb", 2, [64, T])
                pr = Rot(es2, "pr", 2, [128, 512], psum=True)
                t1 = Rot(es2, "t1b", 2, [64, 512]); t2 = Rot(es2, "t2b", 2, [64, 512])
                for hd in range(8):
                    rt, br = raw.get()
                    frow = OFF_QB + 64 * hd if hd < 6 else OFF_KB + 64 * (hd - 6)
                    D(rt[:], zT[frow:frow + 64, :], [], [br])
                    if hd < 6:
                        g, i3 = hd // 3, hd % 3
                        dst_ctx = qTb[:, g, 0:2, i3, :]
                        wb_ = b_q[hd]
                    else:
                        g = hd - 6
                        dst_ctx = kTb[:, g, 0:L].rearrange("p (n t) -> p n t", t=128)
                        wb_ = b_k[g]
                    A([br], [wb_], lambda rt=rt, dst_ctx=dst_ctx: nc.scalar.copy(dst_ctx, rt[:, 0:L].rearrange("p (n t) -> p n t", t=128)))
                    for q4 in range(4):
                        cs_ = slice(L + q4 * 512, L + (q4 + 1) * 512); ps_ = slice(q4 * 512, (q4 + 1) * 512)
                        p_t, bp = pr.get(); a_t, ba = t1.get(); c_t, bc = t2.get()
                        P([br, b_c], [bp], lambda: nc.tensor.matmul(p_t[0:64, :], rPT[:], rt[:, cs_], start=True, stop=True))
                        V([br, b_c], [ba], lambda: nc.vector.tensor_tensor(a_t[:], rt[:, cs_], cosT[:, ps_], ALU.mult))
                        V([bp, b_c], [bc], lambda: nc.vector.tensor_tensor(c_t[:], p_t[0:64, :], sinT[:, ps_], ALU.mult))
                        if hd < 6:
                            dst = qTb[:, g, 2 + 4 * q4:2 + 4 * q4 + 4, i3, :]
                        else:
                            dst = kTb[:, g, cs_].rearrange("p (n t) -> p n t", t=128)
                        G([ba, bc], [wb_], lambda: nc.gpsimd.tensor_tensor(dst, a_t[:].rearrange("p (n t) -> p n t", t=128), c_t[:].rearrange("p (n t) -> p n t", t=128), ALU.add))
                f.barrier()
            psc = Rot(es, "psc", 5, [128, 512], psum=True)
            pov = Rot(es, "pov", 2, [128, 512], psum=True)
            pt = Rot(es, "ptb", 16, [128, 384], BF16)
            ob = Rot(es, "obb", 3, [128, 384])
            dn = Rot(es, "dnb", 4, [128, 3])
            items = [(n, g) for n in range(NT) if not (n < 2 and not with_ctx) for g in range(2)]
            obufs = {}

            def stage1(n, g):
                kbs = [(0, None), (1, None)]
                if n >= 2:
                    if n - 1 >= 2: kbs.append((n - 1, 3))
                    kbs.append((n, None))
                    if n + 1 < NT: kbs.append((n + 1, 1))
                pts = []
                for (m, mk) in kbs:
                    s_p, bs = psc.get(); p_t, bpt = pt.get()
                    P([b_k[g]] + b_q[3 * g:3 * g + 3], [bs], lambda: nc.tensor.matmul(
                        s_p[:, 0:384], kTb[:, g, m * 128:(m + 1) * 128], qTb[:, g, n, :, :].rearrange("p a t -> p (a t)"), start=True, stop=True))
                    A([bs], [bpt], lambda: nc.scalar.activation(p_t[:], s_p[:, 0:384], AF.Exp, scale=SCALE))
                    if mk is not None:
                        G([bpt, b_mk], [bpt], lambda: nc.gpsimd.tensor_tensor(
                            p_t[:].rearrange("p (a t) -> p a t", a=3), p_t[:].rearrange("p (a t) -> p a t", a=3),
                            mkb[:, mk, :].unsqueeze(1).broadcast_to([128, 3, 128]), ALU.mult))
                    pts.append((p_t, bpt, m))
                return pts

            def stage2(n, g, pts):
                if g == 0:
                    obufs[n] = ob.get()
                o_t, bo = obufs[n]
                o_pf, bop = pov.get()
                o_p = o_pf[:, 0:195].rearrange("p (a e) -> p a e", e=65)
                for i3 in range(3):
                    for q, (p_t, bpt, m) in enumerate(pts):
                        P([bpt, b_Va], [bop], lambda: nc.tensor.matmul(
                            o_p[:, i3, :], p_t[:, i3 * 128:(i3 + 1) * 128], Va[:, g, m, :], start=(q == 0), stop=(q == len(pts) - 1)))
                d_t, bd = dn.get()
                V([bop, b_esk], [bd], lambda: nc.vector.tensor_tensor(d_t[:], o_p[:, :, 64], esk[:, 3 * g:3 * g + 3], ALU.add))
                V([bd], [bd], lambda: nc.vector.reciprocal(d_t[:], d_t[:]))
                for i3 in range(3):
                    hc = (3 * g + i3) * 64
                    V([bop, bd], [bo], lambda: nc.vector.tensor_scalar(o_t[:, hc:hc + 64], o_p[:, i3, 0:64], d_t[:, i3:i3 + 1], None, ALU.mult))
                if g == 1:
                    D(omix[n * 128:(n + 1) * 128, 256:640], o_t[:], [bo], [])

            prev = None
            for it in items:
                cur = (it, stage1(*it))
                if prev is not None:
                    stage2(prev[0][0], prev[0][1], prev[1])
                prev = cur
            stage2(prev[0][0], prev[0][1], prev[1])
            f.barrier()

    def phase_attn_c(l):
        with_ctx = l < DEPTH - 1
        with ExitStack() as es:
            qT = sbt(es, "qTn", [64, 6, T], BF16); kTn = sbt(es, "kTn", [64, 6, T], BF16); b_qk = Buf()
            for h in range(6):
                D(qT[:, h, :], zT[OFF_QN + 64 * h:OFF_QN + 64 * h + 64, :], [], [b_qk], eng=POOL)
                D(kTn[:, h, :], zT[OFF_KN + 64 * h:OFF_KN + 64 * h + 64, :], [], [b_qk], eng=POOL)
            Va = sbt(es, "Van", [128, NT, 6, 65], BF16); b_Va = Buf()
            G([], [b_Va], lambda: nc.gpsimd.memset(Va[:], 1.0))
            with ExitStack() as es2:
                vf = sbt(es2, "vfn", [128, NT, 384]); b_vf = Buf()
                D(vf[:], vtok[:, 384:768].rearrange("(n p) f -> p n f", p=128), [], [b_vf])
                V([b_vf, b_Va], [b_Va], lambda: nc.vector.tensor_copy(Va[:, :, :, 0:64], vf[:].rearrange("p n (h e) -> p n h e", e=64)))
                f.barrier()
            cv = sbt(es, "cv", [128, 64]); b_cv = Buf()
            D(cv[:], ccolvalid[:, :], [], [b_cv])
            ebf = Rot(es, "ebf", 2, [128, 15, 64]); ebb = sbt(es, "ebb", [128, 6, 15, 64], BF16); b_eb = [Buf() for _ in range(6)]
            oC = sbt(es, "oC", [128, NT, 384]); b_oC = [Buf() for _ in range(NT)]
            psc = Rot(es, "pscn", 3, [128, 1024], psum=True)
            pov = carve(es, "povn", 2, 128)
            pt = Rot(es, "ptn", 4, [128, 896], BF16)
            dn = Rot(es, "dnn", 4, [128, 1])
            for h in range(6):
                e_f, bef = ebf.get()
                D(e_f[0:64, :, :], rpbtab[l, h, :, :, :], [], [bef])
                D(e_f[64:128, :, :], rpbtab[l, h, :, :, :], [], [bef])
                A([bef], [bef], lambda: nc.scalar.activation(e_f[:], e_f[:], AF.Exp))
                V([bef, b_cv], [b_eb[h]], lambda: nc.vector.tensor_tensor(ebb[:, h, :, :], e_f[:], cv[:].unsqueeze(1).broadcast_to([128, 15, 64]), ALU.mult))
            items = [(h, n) for h in range(6) for n in range(NT) if not (n < 2 and not with_ctx)]

            def keyblocks(n):
                kbs = [(0, None), (1, None)]
                if n >= 2:
                    nb = n - 2
                    for kb in range(16):
                        subs = []
                        anyv = False
                        for i in range(2):
                            for jq in range(2):
                                kr = 2 * kb + i; qr = 2 * nb + jq
                                rs_ = min(max(qr - 4, 0), 24)
                                ok = rs_ <= kr < rs_ + 8
                                subs.append((i, jq, kr - qr + 7 if ok else None))
                                anyv = anyv or ok
                        if anyv:
                            kbs.append((kb + 2, subs))
                return kbs

            def stage1(h, n):
                kbs = keyblocks(n)
                nk = len(kbs)
                s_p, bs = psc.get(); p_t, bpt = pt.get()
                for q, (m, subs) in enumerate(kbs):
                    P([b_qk], [bs], lambda: nc.tensor.matmul(s_p[:, q * 128:(q + 1) * 128], kTn[:, h, m * 128:(m + 1) * 128], qT[:, h, n * 128:(n + 1) * 128], start=True, stop=True))
                A([bs], [bpt], lambda: nc.scalar.activation(p_t[:, 0:nk * 128], s_p[:, 0:nk * 128], AF.Exp, scale=SCALE))
                cnt = 0
                for q, (m, subs) in enumerate(kbs):
                    if subs is None:
                        continue
                    for (i, jq, ro) in subs:
                        eng, ee = (G, nc.gpsimd) if cnt % 2 == 0 else (V, nc.vector)
                        cnt += 1
                        sl = p_t[64 * i:64 * i + 64, q * 128 + 64 * jq:q * 128 + 64 * jq + 64]
                        if ro is None:
                            eng([bpt], [bpt], lambda: ee.memset(sl, 0.0))
                        else:
                            eng([bpt, b_eb[h]], [bpt], lambda: ee.tensor_tensor(sl, sl, ebb[64 * i:64 * i + 64, h, ro, :], ALU.mult))
                return (p_t, bpt, kbs)

            def stage2(h, n, st):
                p_t, bpt, kbs = st
                o_p, bop = pov.get()
                for q, (m, subs) in enumerate(kbs):
                    P([bpt, b_Va], [bop], lambda: nc.tensor.matmul(o_p[:, 0:65], p_t[:, q * 128:(q + 1) * 128], Va[:, m, h, :], start=(q == 0), stop=(q == len(kbs) - 1)))
                d_t, bd = dn.get()
                V([bop], [bd], lambda: nc.vector.reciprocal(d_t[:], o_p[:, 64:65]))
                V([bop, bd], [b_oC[n]], lambda: nc.vector.tensor_scalar(oC[:, n, h * 64:(h + 1) * 64], o_p[:, 0:64], d_t[:, 0:1], None, ALU.mult))

            prev = None
            for it in items:
                cur = (it, stage1(*it))
                if prev is not None:
                    stage2(prev[0][0], prev[0][1], prev[1])
                prev = cur
            stage2(prev[0][0], prev[0][1], prev[1])
            for n in range(NT):
                if n < 2 and not with_ctx:
                    continue
                D(omix[n * 128:(n + 1) * 128, 640:1024], oC[:, n, :], [b_oC[n]], [])
            f.barrier()

    def phase_out_moe(l):
        with_ctx = l < DEPTH - 1
        tiles = list(range(NT)) if with_ctx else list(range(2, NT))
        with ExitStack() as es:
            h2T = sbt(es, "h2T", [128, 8, T], BF16); b_h2T = [Buf() for _ in range(NT)]
            Wr = sbt(es, "Wr", [128, NT, 32]); b_Wr = [Buf() for _ in range(NT)]
            with ExitStack() as es2:
                mods = sbt(es2, "mods2", [128, 2, 3 * DM]); b_mods = Buf()
                D(mods[:, 0, :], modrow[1, 2 * DM:5 * DM].partition_broadcast(128), [], [b_mods])
                D(mods[:, 1, :], modrow[0, 2 * DM:5 * DM].partition_broadcast(128), [], [b_mods])
                G([b_mods], [b_mods], lambda: nc.gpsimd.tensor_scalar(mods[:, :, 2 * DM:3 * DM], mods[:, :, 2 * DM:3 * DM], 1.0, None, ALU.add))
                lgb = sbt(es2, "lgb", [128, 2, DM]); b_lgb = Buf()
                D(lgb[:, 0, :], ln1_g[l, :].partition_broadcast(128), [], [b_lgb])
                D(lgb[:, 1, :], ln1_b[l, :].partition_broadcast(128), [], [b_lgb])
                wo = sbt(es2, "wo", [128, 8, DM], BF16); b_wo = Buf()
                D(wo[:], w_out[l, :, :].rearrange("(k p) n -> p k n", p=128), [], [b_wo], eng=POOL)
                wr = sbt(es2, "wr", [128, 8, 36]); b_wr = Buf()
                D(wr[:], w_r[l, :, :].rearrange("(k p) n -> p k n", p=128), [], [b_wr])
                brb = sbt(es2, "brb", [128, 36]); D(brb[:], b_r[l, :].partition_broadcast(128), [], [b_wr])
                om = Rot(es2, "om", 2, [128, DM]); omb = Rot(es2, "omb", 2, [128, DM], BF16)
                oT = Rot(es2, "oT", 2, [128, 8, 128], BF16)
                xt = Rot(es2, "xt2", 2, [128, DM]); tt = Rot(es2, "tt", 2, [128, DM]); x1 = Rot(es2, "x1", 2, [128, DM])
                hf = Rot(es2, "hf", 2, [128, DM]); hb = Rot(es2, "hb2", 2, [128, DM], BF16)
                hT32 = Rot(es2, "hT32", 2, [128, 8, 128])
                st = Rot(es2, "st2", 4, [128, 2, 6]); mv = Rot(es2, "mv2", 4, [128, 2]); rs = Rot(es2, "rs2", 4, [128, 1])
                pTb = Rot(es2, "pTb", 2, [128, 8, 128], BF16, psum=True)
                pO = Rot(es2, "pO", 1, [128, 1024], psum=True)
                pT32 = Rot(es2, "pT32", 1, [128, 8, 128], psum=True)
                pL = carve(es2, "pL", 1, 64)
                sm = Rot(es2, "sm", 2, [128, 160])
                for i in tiles:
                    w = 0 if i < 2 else 1
                    o_m, bom = om.get(); o_b, bob = omb.get(); o_T, boT = oT.get()
                    D(o_m[:], omix[i * 128:(i + 1) * 128, :], [], [bom])
                    A([bom], [bob], lambda: nc.scalar.copy(o_b[:], o_m[:]))
                    p_t, bp = pTb.get()
                    for k in range(8):
                        P([bob, b_identb], [bp], lambda k=k: nc.tensor.transpose(p_t[:, k, :], o_b[:, k * 128:(k + 1) * 128], identb[:]))
                    V([bp], [boT], lambda: nc.vector.tensor_copy(o_T[:], p_t[:]))
                    p_o, bpo = pO.get()
                    for hf_ in range(2):
                        for k in range(8):
                            P([boT, b_wo], [bpo], lambda k=k, hf_=hf_: nc.tensor.matmul(
                                p_o[:, hf_ * 512:(hf_ + 1) * 512], o_T[:, k, :], wo[:, k, hf_ * 512:(hf_ + 1) * 512], start=(k == 0), stop=(k == 7)))
                    x_t, bx = xt.get(); t_t, bt = tt.get(); x_1, bx1 = x1.get()
                    D(x_t[:], xsrc(l, i), [], [bx])
                    V([bpo, b_mods], [bt], lambda: nc.vector.tensor_tensor(t_t[:], p_o[:], mods[:, w, 0:DM], ALU.mult))
                    V([bx, bt], [bt], lambda: nc.vector.scalar_tensor_tensor(t_t[:], x_t[:], ALPHA, t_t[:], ALU.mult, ALU.add))
                    s_t, bs = st.get(); m_t, bmv = mv.get(); r_t, br = rs.get()
                    layer_norm_stats(None, t_t, bt, (s_t, bs, m_t, bmv, r_t, br))
                    V([bt, bmv, br], [bt], lambda: nc.vector.tensor_scalar(t_t[:], t_t[:], m_t[:, 0:1], r_t[:, 0:1], ALU.subtract, ALU.mult))
                    G([bt, b_lgb], [bt], lambda: nc.gpsimd.tensor_tensor(t_t[:], t_t[:], lgb[:, 0, :], ALU.mult))
                    G([bt, b_lgb], [bx1], lambda: nc.gpsimd.tensor_tensor(x_1[:], t_t[:], lgb[:, 1, :], ALU.add))
                    D(xs[i * 128:(i + 1) * 128, :], x_1[:], [bx1], [])
                    s_t, bs = st.get(); m_t, bmv = mv.get(); r_t, br = rs.get()
                    layer_norm_stats(None, x_1, bx1, (s_t, bs, m_t, bmv, r_t, br))
                    h_f, bhf = hf.get(); h_b, bhb = hb.get()
                    V([bx1, bmv, br], [bhf], lambda: nc.vector.tensor_scalar(h_f[:], x_1[:], m_t[:, 0:1], r_t[:, 0:1], ALU.subtract, ALU.mult))
                    G([bhf, b_mods], [bhf], lambda: nc.gpsimd.tensor_tensor(h_f[:], h_f[:], mods[:, w, 2 * DM:3 * DM], ALU.mult))
                    G([bhf, b_mods], [bhf], lambda: nc.gpsimd.tensor_tensor(h_f[:], h_f[:], mods[:, w, DM:2 * DM], ALU.add))
                    A([bhf], [bhb], lambda: nc.scalar.copy(h_b[:], h_f[:]))
                    p_t, bp = pTb.get()
                    for k in range(8):
                        P([bhb, b_identb], [bp], lambda k=k: nc.tensor.transpose(p_t[:, k, :], h_b[:, k * 128:(k + 1) * 128], identb[:]))
                    A([bp], [b_h2T[i]], lambda: nc.scalar.copy(h2T[:, :, i * 128:(i + 1) * 128], p_t[:]))
                    p32, bp32 = pT32.get()
                    for k in range(8):
                        P([bhf, b_ident], [bp32], lambda k=k: nc.tensor.transpose(p32[:, k, :], h_f[:, k * 128:(k + 1) * 128], ident[:]))
                    h32, bh32 = hT32.get()
                    V([bp32], [bh32], lambda: nc.vector.tensor_copy(h32[:], p32[:]))
                    p_l, bpl = pL.get()
                    for k in range(8):
                        P([bh32, b_wr], [bpl], lambda k=k: nc.tensor.matmul(p_l[:, 0:36], h32[:, k, :], wr[:, k, :], start=(k == 0), stop=(k == 7)))
                    s_, bsm = sm.get()
                    lg = s_[:, 0:36]; gmx = s_[:, 36:37]; ngm = s_[:, 37:38]; g1h = s_[:, 40:44]; e4 = s_[:, 44:48]
                    esum = s_[:, 48:49]; pgr = s_[:, 49:50]; pen = s_[:, 52:56]; m8 = s_[:, 56:64]; elm = s_[:, 64:96]
                    dd = s_[:, 96:97]; ee_ = s_[:, 97:98]; c1 = s_[:, 98:99]; c2 = s_[:, 99:100]
                    V([bpl, b_wr], [bsm], lambda: nc.vector.tensor_tensor(lg, p_l[:, 0:36], brb[:], ALU.add))
                    V([bsm], [bsm], lambda: nc.vector.tensor_reduce(gmx, s_[:, 0:4], AX.X, ALU.max))
                    V([bsm], [bsm], lambda: nc.vector.tensor_scalar(ngm, gmx, -1.0, None, ALU.mult))
                    V([bsm], [bsm], lambda: nc.vector.tensor_scalar(g1h, s_[:, 0:4], gmx, None, ALU.is_equal))
                    A([bsm], [bsm], lambda: nc.scalar.activation(e4, s_[:, 0:4], AF.Exp, bias=ngm, scale=1.0))
                    V([bsm], [bsm], lambda: nc.vector.tensor_reduce(esum, e4, AX.X, ALU.add))
                    V([bsm], [bsm], lambda: nc.vector.reciprocal(pgr, esum))
                    V([bsm], [bsm], lambda: nc.vector.tensor_scalar(pen, g1h, -1.0, 1e30, ALU.add, ALU.mult))
                    V([bsm], [bsm], lambda: nc.vector.tensor_tensor(elm.rearrange("p (g e) -> p g e", e=8), s_[:, 4:36].rearrange("p (g e) -> p g e", e=8),
                                                                    pen.unsqueeze(2).broadcast_to([128, 4, 8]), ALU.add))
                    V([bsm], [bsm], lambda: nc.vector.max(m8, elm))
                    V([bsm], [bsm], lambda: nc.vector.tensor_tensor(dd, m8[:, 1:2], m8[:, 0:1], ALU.subtract))
                    A([bsm], [bsm], lambda: nc.scalar.activation(ee_, dd, AF.Exp))
                    V([bsm], [bsm], lambda: nc.vector.tensor_scalar(c1, ee_, 1.0, None, ALU.add))
                    V([bsm], [bsm], lambda: nc.vector.reciprocal(c1, c1))
                    V([bsm], [bsm], lambda: nc.vector.tensor_tensor(c2, ee_, c1, ALU.mult))
                    V([bsm], [bsm], lambda: nc.vector.tensor_tensor(c1, c1, pgr, ALU.mult))
                    V([bsm], [bsm], lambda: nc.vector.tensor_tensor(c2, c2, pgr, ALU.mult))
                    V([bsm], [b_Wr[i]], lambda: nc.vector.tensor_scalar(Wr[:, i, :], elm, m8[:, 0:1], c1, ALU.is_equal, ALU.mult))
                    V([bsm], [bsm], lambda: nc.vector.tensor_scalar(s_[:, 100:132], elm, m8[:, 1:2], c2, ALU.is_equal, ALU.mult))
                    V([bsm, b_Wr[i]], [b_Wr[i]], lambda: nc.vector.tensor_tensor(Wr[:, i, :], Wr[:, i, :], s_[:, 100:132], ALU.add))
                f.barrier()
            if state.get("stop_before_moe"):
                return
            facc = sbt(es, "facc", [128, NT, DM]); b_f = [Buf() for _ in range(NT)]
            with ExitStack() as es2:
                wg = Rot(es2, "wg", 2, [128, 8, DE], BF16); wu = Rot(es2, "wu", 2, [128, 8, DE], BF16)
                wd = Rot(es2, "wd", 2, [128, 4, DM], BF16)
                pG = Rot(es2, "pG", 2, [128, 512], psum=True); pU = Rot(es2, "pU", 2, [128, 512], psum=True)
                pD = Rot(es2, "pD", 3, [128, 512], psum=True)
                sg = Rot(es2, "sg", 2, [128, 512]); aT = Rot(es2, "aT", 8, [128, 512], BF16)
                t0_ = tiles[0] * 128
                ntok = len(tiles) * 128
                blocks_ = [(t0_ + b * 512, min(512, ntok - b * 512)) for b in range((ntok + 511) // 512)]
                for e in range(NE):
                    g_w, bg = wg.get(); u_w, bu = wu.get(); d_w, bdw = wd.get()
                    D(g_w[:], w_gate[l, e, :, :].rearrange("(k p) n -> p k n", p=128), [], [bg], eng=POOL)
                    D(u_w[:], w_up[l, e, :, :].rearrange("(k p) n -> p k n", p=128), [], [bu], eng=POOL)
                    D(d_w[:], w_down[l, e, :, :].rearrange("(k p) n -> p k n", p=128), [], [bdw], eng=POOL)
                    for (tb0, tsz) in blocks_:
                        rb = [b_h2T[i] for i in range(tb0 // 128, (tb0 + tsz) // 128)]
                        acts = []
                        for fc in range(4):
                            g_p, bgp = pG.get(); u_p, bup = pU.get()
                            for k in range(8):
                                P(rb + [bg], [bgp], lambda k=k: nc.tensor.matmul(g_p[:, 0:tsz], g_w[:, k, fc * 128:(fc + 1) * 128], h2T[:, k, tb0:tb0 + tsz], start=(k == 0), stop=(k == 7)))
                            for k in range(8):
                                P(rb + [bu], [bup], lambda k=k: nc.tensor.matmul(u_p[:, 0:tsz], u_w[:, k, fc * 128:(fc + 1) * 128], h2T[:, k, tb0:tb0 + tsz], start=(k == 0), stop=(k == 7)))
                            s_g, bsg = sg.get(); a_T, baT = aT.get()
                            A([bgp], [bsg], lambda: nc.scalar.activation(s_g[:, 0:tsz], g_p[:, 0:tsz], AF.Silu))
                            V([bsg, bup], [baT], lambda: nc.vector.tensor_tensor(a_T[:, 0:tsz], s_g[:, 0:tsz], u_p[:, 0:tsz], ALU.mult))
                            acts.append((a_T, baT))
                        for q in range(tsz // 128):
                            ti = tb0 // 128 + q
                            for hf_ in range(2):
                                d_p, bdp = pD.get()
                                for fc in range(4):
                                    a_T, baT = acts[fc]
                                    P([baT, bdw], [bdp], lambda fc=fc, a_T=a_T: nc.tensor.matmul(
                                        d_p[:], a_T[:, q * 128:(q + 1) * 128], d_w[:, fc, hf_ * 512:(hf_ + 1) * 512], start=(fc == 0), stop=(fc == 3)))
                                fs = facc[:, ti, hf_ * 512:(hf_ + 1) * 512]
                                if e == 0:
                                    V([bdp, b_Wr[ti]], [b_f[ti]], lambda: nc.vector.tensor_scalar(fs, d_p[:], Wr[:, ti, e:e + 1], None, ALU.mult))
                                else:
                                    V([bdp, b_Wr[ti], b_f[ti]], [b_f[ti]], lambda: nc.vector.scalar_tensor_tensor(fs, d_p[:], Wr[:, ti, e:e + 1], fs, ALU.mult, ALU.add))
                f.barrier()
            with ExitStack() as es2:
                g2 = sbt(es2, "g2", [128, 2, DM]); b_g2 = Buf()
                D(g2[:, 0, :], modrow[1, 5 * DM:6 * DM].partition_broadcast(128), [], [b_g2])
                D(g2[:, 1, :], modrow[0, 5 * DM:6 * DM].partition_broadcast(128), [], [b_g2])
                lgb = sbt(es2, "lgb2", [128, 2, DM]); b_lgb = Buf()
                D(lgb[:, 0, :], ln2_g[l, :].partition_broadcast(128), [], [b_lgb])
                D(lgb[:, 1, :], ln2_b[l, :].partition_broadcast(128), [], [b_lgb])
                xt = Rot(es2, "xt3", 2, [128, DM]); tt = Rot(es2, "tt3", 2, [128, DM])
                st = Rot(es2, "st3", 2, [128, 2, 6]); mv = Rot(es2, "mv3", 2, [128, 2]); rs = Rot(es2, "rs3", 2, [128, 1])
                for i in tiles:
                    w = 0 if i < 2 else 1
                    x_t, bx = xt.get(); t_t, bt = tt.get()
                    D(x_t[:], xs[i * 128:(i + 1) * 128, :], [], [bx])
                    G([b_f[i], b_g2], [bt], lambda: nc.gpsimd.tensor_tensor(t_t[:], facc[:, i, :], g2[:, w, :], ALU.mult))
                    V([bx, bt], [bt], lambda: nc.vector.scalar_tensor_tensor(t_t[:], x_t[:], ALPHA, t_t[:], ALU.mult, ALU.add))
                    s_t, bs = st.get(); m_t, bmv = mv.get(); r_t, br = rs.get()
                    layer_norm_stats(None, t_t, bt, (s_t, bs, m_t, bmv, r_t, br))
                    V([bt, bmv, br], [bt], lambda: nc.vector.tensor_scalar(t_t[:], t_t[:], m_t[:, 0:1], r_t[:, 0:1], ALU.subtract, ALU.mult))
                    G([bt, b_lgb], [bt], lambda: nc.gpsimd.tensor_tensor(t_t[:], t_t[:], lgb[:, 0, :], ALU.mult))
                    G([bt, b_lgb], [bt], lambda: nc.gpsimd.tensor_tensor(t_t[:], t_t[:], lgb[:, 1, :], ALU.add))
                    if l == DEPTH - 1:
                        D(y_out[(i - 2) * 128:(i - 1) * 128, :], t_t[:], [bt], [])
                    else:
                        D(xs[i * 128:(i + 1) * 128, :], t_t[:], [bt], [])
                f.barrier()

    done = False
    for l in range(DEPTH):
        for tag, fn in (("mod", phase_mod), ("proj", phase_ln_proj), ("rwkv", phase_rwkv),
                        ("attb", phase_attn_b), ("attc", phase_attn_c), ("moe", phase_out_moe)):
            if STOP is not None and (l, tag) == tuple(STOP[:2]) and len(STOP) > 2:
                state["stop_before_moe"] = True
            fn(l)
            if STOP is not None and (l, tag) == tuple(STOP[:2]):
                done = True
                break
        if done:
            break
    f.barrier()
    print("n_inst", f.n_inst, flush=True)
    return nc


def _consts():
    p = np.arange(128)[:, None]; q = np.arange(128)[None, :]
    masks = np.stack([(p < q), (p <= q), (p > q), (p >= q)], 1).astype(np.float32)
    ident = np.eye(128, dtype=np.float32)
    block = (p // 64 == q // 64).astype(np.float32)
    hsel = (np.arange(128)[:, None] // 64 == np.arange(2)[None, :]).astype(np.float32)
    t = np.arange(S)
    pos = np.stack([t // 64, t % 64], -1).astype(np.float32)
    inv = (10000.0 ** (-np.arange(16, dtype=np.float32) / 16)).astype(np.float32)
    ang = pos[:, :, None] * inv
    cos = np.cos(ang).astype(np.float32); sin = np.sin(ang).astype(np.float32)
    cosT = np.zeros((64, S), np.float32); sinT = np.zeros((64, S), np.float32)
    for ax in range(2):
        for half in range(2):
            r0 = ax * 32 + half * 16
            cosT[r0:r0 + 16] = cos[:, ax, :].T
            sinT[r0:r0 + 16] = sin[:, ax, :].T
    Pm = np.zeros((64, 64), np.float32)
    for ax in range(2):
        for i in range(16):
            Pm[ax * 32 + i, ax * 32 + 16 + i] = -1.0
            Pm[ax * 32 + 16 + i, ax * 32 + i] = 1.0
    ropeP = np.ascontiguousarray(Pm.T)
    col = np.arange(64)
    cstart = np.clip(col - 8, 0, 48)
    cvalid = ((col[None, :] >= cstart[:, None]) & (col[None, :] < cstart[:, None] + 16)).astype(np.float32)
    colvalid = np.concatenate([cvalid.T, cvalid.T], 0)
    scan = np.ones((128, 512), np.float32); scan[:, ::128] = 0.0
    return dict(cmasks=masks, cident=ident, cblock=block, chsel=hsel, ccos=cosT, csin=sinT, cropeP=ropeP,
                ccolvalid=np.ascontiguousarray(colvalid), cscanmask=scan)


def _host_layout(inp):
    f32 = lambda a: np.ascontiguousarray(np.asarray(a, dtype=np.float32))
    sh = {}
    sh["w_mod"] = f32(inp["w_mod"]); sh["b_mod"] = f32(inp["b_mod"]); sh["w_in"] = f32(inp["w_in"]); sh["w_out"] = f32(inp["w_out"])
    sh["a_shift"] = f32(inp["a_shift"])
    sh["a_w_up"] = f32(np.asarray(inp["a_w_up"]).reshape(DEPTH, 128, 256))
    sh["a_a_up"] = f32(np.asarray(inp["a_a_up"]).reshape(DEPTH, 128, 256))
    sh["a_g_up"] = f32(inp["a_g_up"])
    cp = np.zeros((DEPTH, 128, 48), np.float32)
    ash = np.zeros((DEPTH, 3, 1280), np.float32); ash[:, :, :A_IN] = inp["a_shift"]
    for l in range(DEPTH):
        for ch in range(10):
            for tap in range(3):
                cp[l, :, ch * 3 + tap] = ash[l, tap, ch * 128:(ch + 1) * 128]
        for d in range(2):
            for j in range(2):
                cp[l, :, 30 + d * 2 + j] = inp["a_w0"][l, d, j * 128:(j + 1) * 128]
                cp[l, :, 34 + d * 2 + j] = inp["a_a0"][l, d, j * 128:(j + 1) * 128]
        for j in range(2):
            cp[l, :, 38 + j] = inp["a_k_k"][l, j * 128:(j + 1) * 128]
            cp[l, :, 40 + j] = inp["a_k_a"][l, j * 128:(j + 1) * 128]
            cp[l, :, 42 + j] = inp["a_r_k"][l, j * 128:(j + 1) * 128]
    sh["colpar"] = cp
    for k in ("a_r_k", "a_lnx_g", "a_lnx_b", "b_sink", "ln1_g", "ln1_b", "ln2_g", "ln2_b"):
        sh[k] = f32(inp[k])
    for k in ("w_gate", "w_up", "w_down"):
        sh[k] = f32(np.asarray(inp[k])[:, :NE_DECL])
    kc = np.arange(64)[:, None]; qc = np.arange(64)[None, :]
    idx = np.clip(kc - qc, -15, 15) + 15
    rpb = np.asarray(inp["c_rpb"], np.float32)
    tab = rpb[:, :, :, idx]
    sh["rpbtab"] = f32(tab.transpose(0, 1, 3, 2, 4))
    sh["w_r"] = f32(np.concatenate([inp["w_rg"], inp["w_re"]], -1))
    sh["b_r"] = f32(np.concatenate([inp["b_rg"], inp["b_re"]], -1))
    sh.update(_consts())
    return sh


_PROG = {}


def kernel(**inputs):
    inp = {k: np.asarray(v) for k, v in inputs.items()}
    shared = _host_layout(inp)
    if "nc" not in _PROG:
        _PROG["nc"] = build_program()
    nc = _PROG["nc"]
    in_maps = []
    for b in range(NCORES):
        m = dict(shared)
        m["x"] = np.ascontiguousarray(inp["x"][b], dtype=np.float32)
        m["ctx"] = np.ascontiguousarray(inp["ctx"][b], dtype=np.float32)
        cc = np.zeros((128, 8, 2), np.float32)
        cc[:, :, 0] = np.asarray(inp["c"][b], np.float32).reshape(8, 128).T
        cc[:, :, 1] = np.asarray(inp["c_ctx"], np.float32).reshape(8, 128).T
        m["ccol"] = cc
        in_maps.append(m)
    res = run_bass_kernel_spmd(nc, in_maps, core_ids=list(range(NCORES)))
    _PROG["res"] = res
    out = np.stack([np.asarray(r["y"], dtype=np.float32) for r in res.results], 0)
    return out
```

```python
from contextlib import ExitStack
import numpy as np
import concourse.bass as bass
import concourse.mybir as mybir
from concourse.bass_utils import run_bass_kernel_spmd

F32 = mybir.dt.float32
BF16 = mybir.dt.bfloat16
ALU = mybir.AluOpType
AF = mybir.ActivationFunctionType
AX = mybir.AxisListType

DM = 1024; S = 2048; L = 256; T = S + L; NT = T // 128; DEPTH = 2
HD = 64; A_IN = 1184; N_IN = 2976; NE = 32; DE = 512
ALPHA = (2.0 * DEPTH) ** 0.25
SCALE = HD ** -0.5
C0 = float(np.exp(-0.5))
LN_EPS = 1e-6; GN_EPS = 64e-5
OFF_R, OFF_K, OFF_V, OFF_WD, OFF_AD, OFF_GD = 0, 256, 512, 768, 896, 1024
OFF_QB, OFF_KB, OFF_VB = 1184, 1568, 1696
OFF_QN, OFF_KN, OFF_VN = 1824, 2208, 2592
EPOCH = 30000
DEBUG = False
RW_STOP = 0
NCORES = 8
NE_DECL = 32
STOP = None


class Buf:
    __slots__ = ("w", "r")

    def __init__(self):
        self.w = {}
        self.r = {}


class Eng:
    def __init__(self, fw, name, eng, dma_only=False):
        self.fw = fw; self.name = name; self.eng = eng
        self.sem = None; self.cnt = 0; self.epoch = 0; self.seen = {}
        self.dma_only = dma_only
        if not dma_only:
            self.new_sem()

    def new_sem(self):
        self.sem = self.fw.nc.alloc_semaphore(f"p_{self.name}_{self.epoch}")
        self.cnt = 0
        self.epoch += 1
        self.fw.all_sems.append(self)


class FW:
    def __init__(self, nc, n_dma_sems=24):
        self.nc = nc
        self.all_sems = []
        self.pe = Eng(self, "pe", nc.tensor)
        self.act = Eng(self, "act", nc.scalar)
        self.dve = Eng(self, "dve", nc.vector)
        self.pool = Eng(self, "pool", nc.gpsimd)
        self.sp = Eng(self, "sp", nc.sync, dma_only=True)
        self.dma_sems = [[nc.alloc_semaphore(f"d{i}"), 0] for i in range(n_dma_sems)]
        self.dma_rr = 0
        self.sw_sems = [[nc.alloc_semaphore(f"w{i}"), 0] for i in range(8)]
        self.sw_rr = 0
        self.n_inst = 0

    def _wait(self, E, sem, val):
        k = sem.name
        if E.seen.get(k, 0) >= val:
            return
        E.eng.wait_ge(sem, val)
        E.seen[k] = val

    def _deps(self, E, reads, writes, skip_self=True):
        need = {}
        for b in reads:
            for k, sv in b.w.items():
                if k not in need or need[k][1] < sv[1]:
                    need[k] = sv
        for b in writes:
            for d in (b.w, b.r):
                for k, sv in d.items():
                    if k not in need or need[k][1] < sv[1]:
                        need[k] = sv
        me = E.sem.name if (skip_self and E.sem is not None and E.name == 'pe') else None
        for k, (s, v) in need.items():
            if k == me:
                continue
            self._wait(E, s, v)

    def _mark(self, sem, val, reads, writes):
        k = sem.name
        for b in reads:
            o = b.r.get(k)
            if o is None or o[1] < val:
                b.r[k] = (sem, val)
        for b in writes:
            o = b.w.get(k)
            if o is None or o[1] < val:
                b.w[k] = (sem, val)

    def op(self, E, reads, writes, fn):
        if E.cnt >= EPOCH:
            E.new_sem()
        self._deps(E, reads, writes)
        ins = fn()
        E.cnt += 1
        ins.then_inc(E.sem, 1)
        self._mark(E.sem, E.cnt, reads, writes)
        self.n_inst += 1
        return ins

    def dma(self, E, out, in_, reads, writes, **kw):
        if E.name == "pool":
            slot = self.sw_sems[self.sw_rr]
            self.sw_rr = (self.sw_rr + 1) % len(self.sw_sems)
        else:
            slot = self.dma_sems[self.dma_rr]
            self.dma_rr = (self.dma_rr + 1) % len(self.dma_sems)
        sem, cnt = slot
        if cnt > 0:
            self._wait(E, sem, cnt)
        self._deps(E, reads, writes, skip_self=False)
        ins = E.eng.dma_start(out=out, in_=in_, **kw)
        slot[1] = cnt + 16
        ins.then_inc(sem, 16)
        self._mark(sem, slot[1], reads, writes)
        self.n_inst += 1
        return ins

    def barrier(self):
        cur = []
        for X in (self.pe, self.act, self.dve, self.pool):
            if X.cnt > 0:
                cur.append((X.sem, X.cnt))
        for sem, cnt in self.dma_sems + self.sw_sems:
            if cnt > 0:
                cur.append((sem, cnt))
        for E in (self.pe, self.act, self.dve, self.pool, self.sp):
            for sem, val in cur:
                if E.sem is not None and sem.name == E.sem.name:
                    continue
                self._wait(E, sem, val)


def build_program():
    nc = bass.Bass("TRN2", target_bir_lowering=False)
    f = FW(nc)
    PE, ACT, DVE, POOL, SP = f.pe, f.act, f.dve, f.pool, f.sp

    def din(name, shape):
        return nc.dram_tensor(name, list(shape), F32, kind="ExternalInput").ap()

    def scr(name, shape):
        return nc.dram_tensor(name, list(shape), F32, kind=("ExternalOutput" if DEBUG else "Internal")).ap()

    x_in = din("x", [S, DM]); ctx_in = din("ctx", [L, DM]); ccol = din("ccol", [128, 8, 2])
    w_mod = din("w_mod", [DEPTH, DM, 6 * DM]); b_mod = din("b_mod", [DEPTH, 6 * DM])
    w_in = din("w_in", [DEPTH, DM, N_IN]); w_out = din("w_out", [DEPTH, DM, DM])
    a_shift = din("a_shift", [DEPTH, 3, A_IN])
    a_w_up = din("a_w_up", [DEPTH, 128, 256]); a_a_up = din("a_a_up", [DEPTH, 128, 256])
    a_g_up = din("a_g_up", [DEPTH, 160, 256])
    colpar = din("colpar", [DEPTH, 128, 48])
    a_r_k = din("a_r_k", [DEPTH, 256]); a_lnx_g = din("a_lnx_g", [DEPTH, 256]); a_lnx_b = din("a_lnx_b", [DEPTH, 256])
    b_sink = din("b_sink", [DEPTH, 6])
    rpbtab = din("rpbtab", [DEPTH, 6, 64, 15, 64])
    ln1_g = din("ln1_g", [DEPTH, DM]); ln1_b = din("ln1_b", [DEPTH, DM])
    ln2_g = din("ln2_g", [DEPTH, DM]); ln2_b = din("ln2_b", [DEPTH, DM])
    w_r = din("w_r", [DEPTH, DM, 36]); b_r = din("b_r", [DEPTH, 36])
    w_gate = din("w_gate", [DEPTH, NE_DECL, DM, DE]); w_up = din("w_up", [DEPTH, NE_DECL, DM, DE])
    w_down = din("w_down", [DEPTH, NE_DECL, DE, DM])
    cmasks = din("cmasks", [128, 4, 128]); cident = din("cident", [128, 128])
    cblock = din("cblock", [128, 128]); chsel = din("chsel", [128, 2])
    ccos = din("ccos", [64, S]); csin = din("csin", [64, S]); cropeP = din("cropeP", [64, 64])
    ccolvalid = din("ccolvalid", [128, 64]); cscanmask = din("cscanmask", [128, 512])
    y_out = nc.dram_tensor("y", [S, DM], F32, kind="ExternalOutput").ap()

    xs = scr("xs", [T, DM]); modrow = scr("modrow", [2, 6 * DM])
    zT = scr("zT", [N_IN, T]); vtok = scr("vtok", [T, 768]); omix = scr("omix", [T, DM])
    dbgh = scr("dbgh", [T, DM]) if DEBUG else None
    dbgs = scr("dbgs", [T, 4]) if DEBUG else None

    state = {"stop": False}

    def xsrc(l, i):
        if l == 0:
            return ctx_in[i * 128:(i + 1) * 128, :] if i < 2 else x_in[(i - 2) * 128:(i - 1) * 128, :]
        return xs[i * 128:(i + 1) * 128, :]

    top = ExitStack()
    uid = [0]

    def sbt(es, name, shape, dt=F32):
        uid[0] += 1
        return es.enter_context(nc.sbuf_tensor(f"{name}_{uid[0]}", list(shape), dt))

    def pst(es, name, shape, dt=F32):
        uid[0] += 1
        return es.enter_context(nc.psum_tensor(f"{name}_{uid[0]}", list(shape), dt))
    ident = sbt(top, "ident", [128, 128]); b_ident = Buf()
    identb = sbt(top, "identb", [128, 128], BF16); b_identb = Buf()
    f.dma(SP, ident[:], cident[:, :], [], [b_ident])
    f.op(DVE, [b_ident], [b_identb], lambda: nc.vector.tensor_copy(identb[:], ident[:]))

    def V(r, w, fn): return f.op(DVE, r, w, fn)
    def A(r, w, fn): return f.op(ACT, r, w, fn)
    def G(r, w, fn): return f.op(POOL, r, w, fn)
    def P(r, w, fn): return f.op(PE, r, w, fn)
    def D(out, in_, r, w, eng=None, **kw): return f.dma(eng or SP, out, in_, r, w, **kw)

    class Rot:
        def __init__(self, es, name, n, shape, dt=F32, psum=False):
            mk = pst if psum else sbt
            self.t = [mk(es, f"{name}{i}", shape, dt) for i in range(n)]
            self.b = [Buf() for _ in range(n)]
            self.i = 0

        def get(self):
            k = self.i; self.i = (self.i + 1) % len(self.t)
            return self.t[k], self.b[k]

    class Slots:
        def __init__(self, aps):
            self.t = list(aps); self.b = [Buf() for _ in self.t]; self.i = 0

        def get(self):
            k = self.i; self.i = (self.i + 1) % len(self.t)
            return self.t[k], self.b[k]

    def carve(es, name, nbanks, width):
        per = 512 // width
        views = [[None] * nbanks for _ in range(per)]
        bufs = [Buf() for _ in range(nbanks)]
        for i in range(nbanks):
            bk = pst(es, f"{name}{i}", [128, 512])
            for q in range(per):
                views[q][i] = bk[:, q * width:(q + 1) * width]
        sl = Slots([views[q][i] for q in range(per) for i in range(nbanks)])
        sl.b = [bufs[i] for q in range(per) for i in range(nbanks)]
        return sl

    def layer_norm_stats(es_tiles, src_ap, b_src, tagbufs):
        st, b_st, mv, b_mv, rs, b_rs = tagbufs
        V([b_src], [b_st], lambda: nc.vector.bn_stats(st[:, 0, :], src_ap[:, 0:512]))
        V([b_src], [b_st], lambda: nc.vector.bn_stats(st[:, 1, :], src_ap[:, 512:1024]))
        V([b_st], [b_mv], lambda: nc.vector.bn_aggr(mv[:], st[:].rearrange("p a b -> p (a b)")))
        A([b_mv], [b_rs], lambda: nc.scalar.activation(rs[:], mv[:, 1:2], AF.Sqrt, bias=eps_t[:, 0:1], scale=1.0))
        V([b_rs], [b_rs], lambda: nc.vector.reciprocal(rs[:], rs[:]))

    eps_t = sbt(top, "eps_t", [128, 4]); b_eps = Buf()
    G([], [b_eps], lambda: nc.gpsimd.memset(eps_t[:, 0:1], LN_EPS))
    G([], [b_eps], lambda: nc.gpsimd.memset(eps_t[:, 1:2], GN_EPS))
    G([], [b_eps], lambda: nc.gpsimd.memset(eps_t[:, 2:3], 1e-12))
    G([], [b_eps], lambda: nc.gpsimd.memset(eps_t[:, 3:4], 1.0))
    f.barrier()

    def phase_mod(l):
        with ExitStack() as es:
            cr = sbt(es, "cr", [128, 8, 2]); b_cr = Buf()
            cs = sbt(es, "cs", [128, 8, 2]); b_cs = Buf()
            D(cr[:], ccol[:, :, :], [], [b_cr])
            A([b_cr], [b_cs], lambda: nc.scalar.activation(cs[:], cr[:], AF.Silu))
            bm2 = sbt(es, "bm2", [2, 6 * DM]); b_bm2 = Buf()
            D(bm2[:], b_mod[l, :].partition_broadcast(2), [], [b_bm2])
            msb = sbt(es, "msb", [2, 6 * DM]); b_msb = Buf()
            wm = Rot(es, "wm", 3, [128, 3072])
            acc = [pst(es, f"macc{j}", [128, 512]) for j in range(6)]
            b_acc = [Buf() for _ in range(6)]
            for hf in range(2):
                for k in range(8):
                    wt, bw = wm.get()
                    D(wt[:], w_mod[l, k * 128:(k + 1) * 128, hf * 3072:(hf + 1) * 3072], [], [bw])
                    for j in range(6):
                        P([bw, b_cs], [b_acc[j]], lambda j=j, wt=wt, k=k: nc.tensor.matmul(
                            acc[j][0:2, :], cs[:, k, :], wt[:, j * 512:(j + 1) * 512], start=(k == 0), stop=(k == 7)))
                for j in range(6):
                    c0 = hf * 3072 + j * 512
                    V([b_acc[j], b_bm2], [b_msb], lambda j=j, c0=c0: nc.vector.tensor_tensor(
                        msb[:, c0:c0 + 512], acc[j][0:2, :], bm2[:, c0:c0 + 512], ALU.add))
            D(modrow[:, :], msb[:], [b_msb], [])
            f.barrier()

    def phase_ln_proj(l):
        with ExitStack() as es:
            hT = sbt(es, "hT", [128, 8, T], BF16); b_hT = [Buf() for _ in range(NT)]
            with ExitStack() as es2:
                mods = sbt(es2, "mods", [128, 2, 2 * DM]); b_mods = Buf()
                D(mods[:, 0, :], modrow[1, 0:2 * DM].partition_broadcast(128), [], [b_mods])
                D(mods[:, 1, :], modrow[0, 0:2 * DM].partition_broadcast(128), [], [b_mods])
                G([b_mods], [b_mods], lambda: nc.gpsimd.tensor_scalar(mods[:, :, DM:2 * DM], mods[:, :, DM:2 * DM], 1.0, None, ALU.add))
                xt = Rot(es2, "xt", 2, [128, DM]); xn = Rot(es2, "xn", 2, [128, DM]); hb = Rot(es2, "hb", 2, [128, DM], BF16)
                st = Rot(es2, "st", 2, [128, 2, 6]); mv = Rot(es2, "mv", 2, [128, 2]); rs = Rot(es2, "rs", 2, [128, 1])
                pT = Rot(es2, "pT", 2, [128, 8, 128], BF16, psum=True)
                for i in range(NT):
                    x_t, bx = xt.get(); n_t, bn = xn.get(); h_t, bh = hb.get()
                    s_t, bs = st.get(); m_t, bmv = mv.get(); r_t, br = rs.get(); p_t, bp = pT.get()
                    D(x_t[:], xsrc(l, i), [], [bx])
                    layer_norm_stats(None, x_t, bx, (s_t, bs, m_t, bmv, r_t, br))
                    V([bx, bmv, br], [bn], lambda x_t=x_t, n_t=n_t, m_t=m_t, r_t=r_t: nc.vector.tensor_scalar(
                        n_t[:], x_t[:], m_t[:, 0:1], r_t[:, 0:1], ALU.subtract, ALU.mult))
                    w = 0 if i < 2 else 1
                    G([bn, b_mods], [bn], lambda n_t=n_t, w=w: nc.gpsimd.tensor_tensor(n_t[:], n_t[:], mods[:, w, DM:2 * DM], ALU.mult))
                    G([bn, b_mods], [bn], lambda n_t=n_t, w=w: nc.gpsimd.tensor_tensor(n_t[:], n_t[:], mods[:, w, 0:DM], ALU.add))
                    A([bn], [bh], lambda n_t=n_t, h_t=h_t: nc.scalar.copy(h_t[:], n_t[:]))
                    if DEBUG:
                        D(dbgh[i * 128:(i + 1) * 128, :], n_t[:], [bn], [])
                        D(dbgs[i * 128:(i + 1) * 128, 0:2], m_t[:], [bmv], [], allow_slow_non_contiguous=True)
                        D(dbgs[i * 128:(i + 1) * 128, 2:3], r_t[:], [br], [], allow_slow_non_contiguous=True)
                    for k in range(8):
                        P([bh, b_identb], [bp], lambda k=k, h_t=h_t, p_t=p_t: nc.tensor.transpose(p_t[:, k, :], h_t[:, k * 128:(k + 1) * 128], identb[:]))
                    A([bp], [b_hT[i]], lambda i=i, p_t=p_t: nc.scalar.copy(hT[:, :, i * 128:(i + 1) * 128], p_t[:]))
                f.barrier()
            wb = sbt(es, "wb", [128, 8, N_IN], BF16); b_wb = [Buf() for _ in range(8)]
            for k in range(8):
                D(wb[:, k, :], w_in[l, k * 128:(k + 1) * 128, :], [], [b_wb[k]], eng=POOL)
            pp = Rot(es, "pp", 4, [128, 512], psum=True)
            stg = Rot(es, "stg", 4, [128, 512])
            cnt = 0
            for fc in range(24):
                fsz = 128 if fc < 23 else 32
                for tb in range(5):
                    t0 = tb * 512; tsz = min(512, T - t0)
                    p_t, bp = pp.get(); s_t, bs = stg.get()
                    rb = [b_hT[i] for i in range(t0 // 128, (t0 + tsz) // 128)]
                    for k in range(8):
                        P(rb + [b_wb[k]], [bp], lambda k=k, p_t=p_t, fc=fc, fsz=fsz, t0=t0, tsz=tsz: nc.tensor.matmul(
                            p_t[0:fsz, 0:tsz], wb[:, k, fc * 128:fc * 128 + fsz], hT[:, k, t0:t0 + tsz], start=(k == 0), stop=(k == 7)))
                    if cnt % 2 == 0:
                        A([bp], [bs], lambda p_t=p_t, s_t=s_t, fsz=fsz, tsz=tsz: nc.scalar.copy(s_t[0:fsz, 0:tsz], p_t[0:fsz, 0:tsz]))
                    else:
                        V([bp], [bs], lambda p_t=p_t, s_t=s_t, fsz=fsz, tsz=tsz: nc.vector.tensor_copy(s_t[0:fsz, 0:tsz], p_t[0:fsz, 0:tsz]))
                    cnt += 1
                    D(zT[fc * 128:fc * 128 + fsz, t0:t0 + tsz], s_t[0:fsz, 0:tsz], [bs], [])
            vst = Rot(es, "vst", 2, [128, 768])
            for i in range(NT):
                p1, bp1 = pp.get(); p2, bp2 = pp.get(); s_t, bs = vst.get()
                for (pt_, bpt_, c0_, c1_, w0_, wn_) in ((p1, bp1, 0, 256, OFF_V, 256), (p1, bp1, 256, 384, OFF_VB, 128), (p2, bp2, 0, 384, OFF_VN, 384)):
                    for k in range(8):
                        P([b_hT[i], b_wb[k]], [bpt_], lambda k=k: nc.tensor.matmul(
                            pt_[:, c0_:c1_], hT[:, k, i * 128:(i + 1) * 128], wb[:, k, w0_:w0_ + wn_], start=(k == 0), stop=(k == 7)))
                A([bp1], [bs], lambda p1=p1, s_t=s_t: nc.scalar.copy(s_t[:, 0:384], p1[:, 0:384]))
                V([bp2], [bs], lambda p2=p2, s_t=s_t: nc.vector.tensor_copy(s_t[:, 384:768], p2[:, 0:384]))
                D(vtok[i * 128:(i + 1) * 128, :], s_t[:], [bs], [])
            f.barrier()

    def conv_rows(dst, b_dst, src, b_src, wcol, b_w, c0, n):
        V([b_src, b_w], [b_dst], lambda: nc.vector.tensor_scalar(dst[:, 0:n], src[:, 0:n], wcol[:, 1:2], None, ALU.mult))
        segs = []
        for (a, b) in ((0, L), (L, T)):
            lo = max(a, c0); hi = min(b, c0 + n)
            if lo < hi:
                segs.append((lo - c0, hi - c0, lo == a, hi == b))
        return segs

    def phase_rwkv(l):
        with ExitStack() as es:
            cp = sbt(es, "cp", [128, 48]); b_cp = Buf()
            D(cp[:], colpar[l, :, :], [], [b_cp])
            wup = sbt(es, "wup", [128, 256]); aup = sbt(es, "aup", [128, 256]); b_up = Buf()
            D(wup[:], a_w_up[l, :, :], [], [b_up]); D(aup[:], a_a_up[l, :, :], [], [b_up])
            gup0 = sbt(es, "gup0", [128, 256]); gup1 = sbt(es, "gup1", [32, 256]); b_gup = Buf()
            D(gup0[:], a_g_up[l, 0:128, :], [], [b_gup]); D(gup1[:], a_g_up[l, 128:160, :], [], [b_gup])
            msk = sbt(es, "msk", [128, 4, 128]); b_msk = Buf()
            D(msk[:], cmasks[:, :, :], [], [b_msk])
            blk = sbt(es, "blk", [128, 128]); hsel = sbt(es, "hsel", [128, 2]); b_cst = Buf()
            D(blk[:], cblock[:, :], [], [b_cst]); D(hsel[:], chsel[:, :], [], [b_cst])
            scm = sbt(es, "scm", [128, 512]); D(scm[:], cscanmask[:, :], [], [b_cst])
            vsh = sbt(es, "vsh", [128, 3, 256]); b_vsh = Buf()
            D(vsh[:], a_shift[l, :, OFF_V:OFF_V + 256].partition_broadcast(128), [], [b_vsh])
            yacc = sbt(es, "yacc", [128, NT, 256]); b_yacc = [Buf() for _ in range(NT)]
            bacc = sbt(es, "bacc", [128, NT, 4]); b_bacc = [Buf() for _ in range(NT)]
            Vt = sbt(es, "Vt", [128, NT, 256]); b_Vt = [Buf() for _ in range(NT)]
            Vtb = sbt(es, "Vtb", [128, NT, 256], BF16)
            G([], b_yacc, lambda: nc.gpsimd.memset(yacc[:], 0.0))
            G([], b_bacc, lambda: nc.gpsimd.memset(bacc[:], 0.0))
            Mst = [[sbt(es, f"M{d}{j}", [128, 64]) for j in range(2)] for d in range(2)]
            b_M = [[Buf() for j in range(2)] for d in range(2)]
            for d in range(2):
                for j in range(2):
                    G([], [b_M[d][j]], lambda d=d, j=j: nc.gpsimd.memset(Mst[d][j][:], 0.0))
            with ExitStack() as es2:
                vr = Rot(es2, "vr", 6, [128, 256])
                for i in range(NT):
                    tp, bp_ = vr.get(); tc, bc_ = vr.get(); tn, bn_ = vr.get()
                    first = i in (0, 2); last = i in (1, NT - 1)
                    r0 = i * 128
                    D(tc[:], vtok[r0:r0 + 128, 0:256], [], [bc_])
                    if first:
                        G([], [bp_], lambda tp=tp: nc.gpsimd.memset(tp[:], 0.0))
                        D(tp[1:128, :], vtok[r0:r0 + 127, 0:256], [], [bp_])
                    else:
                        D(tp[:], vtok[r0 - 1:r0 + 127, 0:256], [], [bp_])
                    if last:
                        G([], [bn_], lambda tn=tn: nc.gpsimd.memset(tn[:], 0.0))
                        D(tn[0:127, :], vtok[r0 + 1:r0 + 128, 0:256], [], [bn_])
                    else:
                        D(tn[:], vtok[r0 + 1:r0 + 129, 0:256], [], [bn_])
                    V([bc_, b_vsh], [b_Vt[i]], lambda i=i, tc=tc: nc.vector.tensor_tensor(Vt[:, i, :], tc[:], vsh[:, 1, :], ALU.mult))
                    G([bp_, b_vsh], [bp_], lambda tp=tp: nc.gpsimd.tensor_tensor(tp[:], tp[:], vsh[:, 0, :], ALU.mult))
                    G([bn_, b_vsh], [bn_], lambda tn=tn: nc.gpsimd.tensor_tensor(tn[:], tn[:], vsh[:, 2, :], ALU.mult))
                    V([bp_, b_Vt[i]], [b_Vt[i]], lambda i=i, tp=tp: nc.vector.tensor_tensor(Vt[:, i, :], Vt[:, i, :], tp[:], ALU.add))
                    V([bn_, b_Vt[i]], [b_Vt[i]], lambda i=i, tn=tn: nc.vector.tensor_tensor(Vt[:, i, :], Vt[:, i, :], tn[:], ALU.add))
                    A([b_Vt[i]], [b_Vt[i]], lambda i=i: nc.scalar.copy(Vtb[:, i, :], Vt[:, i, :]))
                f.barrier()
            if RW_STOP == 1:
                return

            blocks = [(0, 256)] + [(256 + 256 * q, 256) for q in range(8)]
            order = {0: list(range(9)), 1: [0] + list(range(8, 0, -1))}
            with ExitStack() as es2:
                BW = 256
                raw = Rot(es2, "raw", 3, [128, BW + 2])
                rT = [[sbt(es2, f"rT{d}{j}", [128, BW]) for j in range(2)] for d in range(2)]
                kT = [[sbt(es2, f"kT{d}{j}", [128, BW]) for j in range(2)] for d in range(2)]
                kkT = [[sbt(es2, f"kkT{d}{j}", [128, BW]) for j in range(2)] for d in range(2)]
                twd = [sbt(es2, f"twd{d}", [128, BW]) for d in range(2)]
                tad = [sbt(es2, f"tad{d}", [128, BW]) for d in range(2)]
                names = ["t0", "t1", "t2", "bb", "kd", "p1", "p2", "e1", "e2"]
                rows = {nm: [[sbt(es2, f"{nm}{d}{j}", [128, BW]) for j in range(2)] for d in range(2)] for nm in names}
                q12 = [[sbt(es2, f"q12{d}{j}", [128, 2, 2, 128]) for j in range(2)] for d in range(2)]
                tot = [[sbt(es2, f"tot{d}{j}", [128, 4]) for j in range(2)] for d in range(2)]
                gC = [[sbt(es2, f"gC{d}{j}", [128, 4]) for j in range(2)] for d in range(2)]
                b_row = [[Buf() for j in range(2)] for d in range(2)]
                b_sh = [Buf() for d in range(2)]
                tmp = Rot(es2, "tmp", 3, [128, BW])
                bnames = ["p1b", "p2b", "e1b", "e2b"]
                brows = {nm: [[sbt(es2, f"{nm}{d}{j}", [128, BW], BF16) for j in range(2)] for d in range(2)] for nm in bnames}
                q12b = [[sbt(es2, f"q12b{d}{j}", [128, 2, 2, 128], BF16) for j in range(2)] for d in range(2)]
                Mb = [[sbt(es2, f"Mb{d}{j}", [128, 64], BF16) for j in range(2)] for d in range(2)]
                for d in range(2):
                    for j in range(2):
                        G([], [b_M[d][j]], lambda d=d, j=j: nc.gpsimd.memset(Mb[d][j][:], 0.0))
                bankX = pst(es2, "bankX", [128, 512]); b_bankX = Buf()
                pM = [[bankX[:, (2 * d + j) * 64:(2 * d + j) * 64 + 64] for j in range(2)] for d in range(2)]
                b_pM = [[b_bankX for j in range(2)] for d in range(2)]
                ps5 = Slots([bankX[:, 256:512]]); ps5.b = [b_bankX]
                pN = Rot(es2, "pN", 1, [128, 512], psum=True)
                pW = Rot(es2, "pW", 2, [128, 1024], psum=True)
                pQ = carve(es2, "pQ", 1, 256)
                pEbank = pst(es2, "pEb", [128, 1024], BF16); b_pE = Buf()
                pE = Slots([pEbank[:, q * 256:(q + 1) * 256] for q in range(4)]); pE.b = [b_pE] * 4
                NCH = 16
                cA1 = [sbt(es2, f"cA1_{k}", [128, 256], BF16) for k in range(NCH)]
                cA2 = [sbt(es2, f"cA2_{k}", [128, 256], BF16) for k in range(NCH)]
                cbA = [(Buf(), Buf()) for k in range(NCH)]
                gPR = [Rot(es2, f"gPR{g}_", 3, [128, 4, 256], BF16) for g in range(4)]
                gPT = [Rot(es2, f"gPT{g}_", 3, [128, 4, 128], BF16) for g in range(4)]
                gSq = [Rot(es2, f"gSq{g}_", 2, [128, 4, 64], BF16) for g in range(4)]
                cE = [sbt(es2, f"cE_{k}", [128, 256], BF16) for k in range(NCH // 2)]
                cbE = [Buf() for k in range(NCH // 2)]

                def load_conv(dst, b_dst, frow, nrows, chunk, tap_chunk, c0, n):
                    rt, br = raw.get()
                    seq_lo, seq_hi = (0, L) if c0 < L else (L, T)
                    lo = max(seq_lo, c0 - 1); hi = min(seq_hi, c0 + n + 1)
                    if lo > c0 - 1 or hi < c0 + n + 1:
                        G([], [br], lambda rt=rt: nc.gpsimd.memset(rt[:], 0.0))
                    D(rt[0:nrows, lo - (c0 - 1):hi - (c0 - 1)], zT[frow:frow + nrows, lo:hi], [], [br])
                    w = lambda tap: cp[0:nrows, tap_chunk * 3 + tap:tap_chunk * 3 + tap + 1]
                    V([br, b_cp], [b_dst], lambda: nc.vector.tensor_scalar(dst[0:nrows, 0:n], rt[0:nrows, 1:n + 1], w(1), None, ALU.mult))
                    V([br, b_cp, b_dst], [b_dst], lambda: nc.vector.scalar_tensor_tensor(dst[0:nrows, 0:n], rt[0:nrows, 0:n], w(0), dst[0:nrows, 0:n], ALU.mult, ALU.add))
                    V([br, b_cp, b_dst], [b_dst], lambda: nc.vector.scalar_tensor_tensor(dst[0:nrows, 0:n], rt[0:nrows, 2:n + 2], w(2), dst[0:nrows, 0:n], ALU.mult, ALU.add))

                def prep_block(d, bi):
                    c0, n = blocks[bi]
                    nch = n // 128
                    load_conv(twd[d], b_sh[d], OFF_WD, 128, None, 6, c0, n)
                    A([b_sh[d]], [b_sh[d]], lambda: nc.scalar.activation(twd[d][:, 0:n], twd[d][:, 0:n], AF.Tanh))
                    load_conv(tad[d], b_sh[d], OFF_AD, 128, None, 7, c0, n)
                    hp = slice(64 * d, 64 * d + 64)
                    for j in range(2):
                        R = {nm: rows[nm][d][j] for nm in names}
                        br_ = b_row[d][j]
                        load_conv(rT[d][j], br_, OFF_R + 128 * j, 128, None, j, c0, n)
                        load_conv(kT[d][j], br_, OFF_K + 128 * j, 128, None, 2 + j, c0, n)
                        V([br_, b_cp], [br_], lambda: nc.vector.tensor_scalar(kkT[d][j][:, 0:n], kT[d][j][:, 0:n], cp[:, 38 + j:39 + j], None, ALU.mult))
                        tq, btq = tmp.get()
                        G([br_], [btq], lambda: nc.gpsimd.tensor_tensor(tq[:, 0:n], kkT[d][j][:, 0:n], kkT[d][j][:, 0:n], ALU.mult))
                        p5, bp5 = ps5.get()
                        P([btq, b_cst], [bp5], lambda: nc.tensor.matmul(p5[:, 0:n], blk[:], tq[:, 0:n], start=True, stop=True))
                        A([bp5], [btq], lambda: nc.scalar.activation(tq[:, 0:n], p5[:, 0:n], AF.Sqrt, bias=eps_t[:, 2:3], scale=1.0))
                        V([btq], [btq], lambda: nc.vector.reciprocal(tq[:, 0:n], tq[:, 0:n]))
                        V([btq, br_], [br_], lambda: nc.vector.tensor_tensor(kkT[d][j][:, 0:n], kkT[d][j][:, 0:n], tq[:, 0:n], ALU.mult))
                        p5, bp5 = ps5.get()
                        P([b_sh[d], b_up], [bp5], lambda: nc.tensor.matmul(p5[:, 0:n], wup[hp, j * 128:(j + 1) * 128], twd[d][hp, 0:n], start=True, stop=True))
                        A([bp5, b_cp], [br_], lambda: nc.scalar.activation(R["t0"][:, 0:n], p5[:, 0:n], AF.Sigmoid, bias=cp[:, 30 + d * 2 + j:31 + d * 2 + j], scale=1.0))
                        p5, bp5 = ps5.get()
                        P([b_sh[d], b_up], [bp5], lambda: nc.tensor.matmul(p5[:, 0:n], aup[hp, j * 128:(j + 1) * 128], tad[d][hp, 0:n], start=True, stop=True))
                        A([bp5, b_cp], [br_], lambda: nc.scalar.activation(R["bb"][:, 0:n], p5[:, 0:n], AF.Sigmoid, bias=cp[:, 34 + d * 2 + j:35 + d * 2 + j], scale=1.0))
                        V([br_, b_cp], [br_], lambda: nc.vector.tensor_scalar(R["kd"][:, 0:n], R["bb"][:, 0:n], -1.0, cp[:, 40 + j:41 + j], ALU.add, ALU.mult))
                        V([br_], [br_], lambda: nc.vector.scalar_tensor_tensor(R["kd"][:, 0:n], R["kd"][:, 0:n], 1.0, kT[d][j][:, 0:n], ALU.add, ALU.mult))
                        V([br_], [br_], lambda: nc.vector.tensor_tensor(R["bb"][:, 0:n], R["bb"][:, 0:n], kkT[d][j][:, 0:n], ALU.mult))
                        tq, btq = tmp.get()
                        V([br_, b_cp], [btq], lambda: nc.vector.scalar_tensor_tensor(tq[:, 0:n], rT[d][j][:, 0:n], cp[:, 42 + j:43 + j], R["kd"][:, 0:n], ALU.mult, ALU.mult))
                        for c in range(nch):
                            ti = c0 // 128 + c
                            pq, bpq = pQ.get()
                            P([btq, b_cst], [bpq], lambda c=c, pq=pq: nc.tensor.matmul(pq[:, 0:2], tq[:, c * 128:(c + 1) * 128], hsel[:], start=True, stop=True))
                            V([bpq, b_bacc[ti]], [b_bacc[ti]], lambda ti=ti, pq=pq: nc.vector.tensor_tensor(bacc[:, ti, 2 * j:2 * j + 2], bacc[:, ti, 2 * j:2 * j + 2], pq[:, 0:2], ALU.add))
                        V([br_, b_cst], [br_], lambda: nc.vector.tensor_tensor_scan(R["t1"][:, 0:n], scm[:, 0:n], R["t0"][:, 0:n], 0.0, ALU.mult, ALU.add))
                        V([br_], [br_], lambda: nc.vector.tensor_tensor(R["t0"][:, 0:n], R["t1"][:, 0:n], R["t0"][:, 0:n], ALU.subtract))
                        V([br_], [br_], lambda: nc.vector.tensor_copy(tot[d][j][:, 0:nch], R["t1"][:, 127:n:128]))
                        A([br_], [br_], lambda: nc.scalar.activation(gC[d][j][:, 0:nch], tot[d][j][:, 0:nch], AF.Exp, scale=-C0))
                        totb = tot[d][j][:, 0:nch].unsqueeze(2).broadcast_to([128, nch, 128])
                        v3 = lambda ap: ap[:, 0:n].rearrange("p (c t) -> p c t", t=128)
                        q1v = q12[d][j][:, 0:nch, 0, :]; q2v = q12[d][j][:, 0:nch, 1, :]
                        if d == 0:
                            V([br_], [br_], lambda: nc.vector.tensor_tensor(v3(R["t2"]), totb, v3(R["t1"]), ALU.subtract))
                            A([br_], [br_], lambda: nc.scalar.activation(q1v, v3(R["t0"]), AF.Exp, scale=-C0))
                            A([br_], [br_], lambda: nc.scalar.activation(q2v, v3(R["t1"]), AF.Exp, scale=-C0))
                            A([br_], [br_], lambda: nc.scalar.activation(R["p1"][:, 0:n], R["t1"][:, 0:n], AF.Exp, scale=C0))
                            A([br_], [br_], lambda: nc.scalar.activation(R["e1"][:, 0:n], R["t2"][:, 0:n], AF.Exp, scale=-C0))
                        else:
                            V([br_], [br_], lambda: nc.vector.tensor_tensor(v3(R["t2"]), totb, v3(R["t1"]), ALU.subtract))
                            V([br_], [br_], lambda: nc.vector.tensor_tensor(v3(R["t1"]), totb, v3(R["t0"]), ALU.subtract))
                            A([br_], [br_], lambda: nc.scalar.activation(q1v, v3(R["t2"]), AF.Exp, scale=-C0))
                            A([br_], [br_], lambda: nc.scalar.activation(q2v, v3(R["t1"]), AF.Exp, scale=-C0))
                            A([br_], [br_], lambda: nc.scalar.activation(R["p1"][:, 0:n], R["t1"][:, 0:n], AF.Exp, scale=C0))
                            A([br_], [br_], lambda: nc.scalar.activation(R["e1"][:, 0:n], R["t0"][:, 0:n], AF.Exp, scale=-C0))
                        BR = {nm: brows[nm][d][j] for nm in bnames}
                        q1b = q12b[d][j][:, 0:nch, 0, :]; q2b = q12b[d][j][:, 0:nch, 1, :]
                        V([br_], [br_], lambda: nc.vector.tensor_tensor(q1b, q1v, v3(kkT[d][j]), ALU.mult))
                        G([br_], [br_], lambda: nc.gpsimd.tensor_tensor(q2b, q2v, v3(rT[d][j]), ALU.mult))
                        V([br_], [br_], lambda: nc.vector.tensor_tensor(BR["p2b"][:, 0:n], R["p1"][:, 0:n], R["kd"][:, 0:n], ALU.mult))
                        G([br_], [br_], lambda: nc.gpsimd.tensor_tensor(BR["p1b"][:, 0:n], R["p1"][:, 0:n], R["bb"][:, 0:n], ALU.mult))
                        V([br_], [br_], lambda: nc.vector.tensor_tensor(BR["e2b"][:, 0:n], R["e1"][:, 0:n], R["bb"][:, 0:n], ALU.mult))
                        G([br_], [br_], lambda: nc.gpsimd.tensor_tensor(BR["e1b"][:, 0:n], R["e1"][:, 0:n], R["kd"][:, 0:n], ALU.mult))

                def score_chain(k, d, j, hh, bi, c):
                    cs_ = slice(c * 128, (c + 1) * 128)
                    br_ = b_row[d][j]
                    BR = {nm: brows[nm][d][j] for nm in bnames}
                    mT = msk[:, 0:2, :] if d == 0 else msk[:, 2:4, :]
                    mN = msk[:, 2, :] if d == 0 else msk[:, 0, :]
                    hp = slice(64 * hh, 64 * hh + 64)
                    ke = k // 2; gi = k // 4; m = k % 4
                    if hh == 0:
                        pe_, bpe = pE.get()
                        P([br_, b_identb], [bpe], lambda: nc.tensor.transpose(pe_[:, 0:128], BR["e1b"][:, cs_], identb[:]))
                        P([br_, b_identb], [bpe], lambda: nc.tensor.transpose(pe_[:, 128:256], BR["e2b"][:, cs_], identb[:]))
                        A([bpe], [cbE[ke]], lambda: nc.scalar.copy(cE[ke][:], pe_[:]))
                    yield
                    q12c = q12b[d][j][hp, c, :, :].rearrange("p a t -> p (a t)")
                    q1c = q12b[d][j][hp, c, 0, :]
                    A1m, A2m = cA1[k], cA2[k]; bA1, bA2 = cbA[k]
                    a1f, ba1 = pW.get(); a1 = a1f[:, 0:256]
                    P([br_], [ba1], lambda: nc.tensor.matmul(a1, BR["p1b"][hp, cs_], q12c, start=True, stop=True))
                    V([ba1, b_msk], [bA1], lambda: nc.vector.tensor_tensor(A1m[:].rearrange("p (a t) -> p a t", a=2), a1.rearrange("p (a t) -> p a t", a=2), mT, ALU.mult))
                    yield
                    a2f, ba2 = pW.get(); a2 = a2f[:, 0:256]
                    P([br_], [ba2], lambda: nc.tensor.matmul(a2, BR["p2b"][hp, cs_], q12c, start=True, stop=True))
                    V([ba2, b_msk], [bA2], lambda: nc.vector.tensor_tensor(A2m[:].rearrange("p (a t) -> p a t", a=2), a2.rearrange("p (a t) -> p a t", a=2), mT, ALU.mult))
                    yield
                    anf, ban = pN.get(); an = anf[:, 0:128]
                    P([br_], [ban], lambda: nc.tensor.matmul(an, q1c, BR["p1b"][hp, cs_], start=True, stop=True))
                    PR, bPR = gcur[gi][0]; PT, bPT = gcur[gi][1]
                    V([ban, b_msk], [bPT], lambda: nc.vector.scalar_tensor_tensor(PT[:, m, :], an, -1.0, mN, ALU.mult, ALU.mult))
                    G([bA1], [bPR], lambda: nc.gpsimd.tensor_scalar(PR[:, m, 0:128], A1m[:, 0:128], -1.0, None, ALU.mult))
                    G([b_identb], [bPR], lambda: nc.gpsimd.tensor_copy(PR[:, m, 128:256], identb[:]))
                    yield

                def neumann_group(gi):
                    (PR, bPR), (PT, bPT) = gcur[gi]
                    for lev in range(7):
                        if lev < 6:
                            w1, bw1 = pW.get()
                            w1v = w1[:].rearrange("p (m c) -> p m c", c=256)
                            for m in range(4):
                                P([bPT, bPR], [bw1], lambda: nc.tensor.matmul(w1[:, m * 256:(m + 1) * 256], PT[:, m, :], PR[:, m, :], start=True, stop=True))
                            PRn, bPRn = gPR[gi].get()
                            A([bw1], [bPRn], lambda: nc.scalar.copy(PRn[:, :, 0:128], w1v[:, :, 0:128]))
                            V([bw1, bPR], [bPRn], lambda: nc.vector.tensor_tensor(PRn[:, :, 128:256], w1v[:, :, 128:256], PR[:, :, 128:256], ALU.add))
                            w2, bw2 = pN.get()
                            for m in range(4):
                                P([bPT, bPR], [bw2], lambda: nc.tensor.matmul(w2[:, m * 128:(m + 1) * 128], PR[:, m, 0:128], PT[:, m, :], start=True, stop=True))
                            PTn, bPTn = gPT[gi].get()
                            w2v = w2[:].rearrange("p (m c) -> p m c", c=128)
                            if (gi + lev) % 2 == 0:
                                A([bw2], [bPTn], lambda: nc.scalar.copy(PTn[:], w2v))
                            else:
                                V([bw2], [bPTn], lambda: nc.vector.tensor_copy(PTn[:], w2v))
                            PR, bPR, PT, bPT = PRn, bPRn, PTn, bPTn
                        else:
                            w2, bw2 = pN.get()
                            for m in range(4):
                                P([bPT, bPR], [bw2], lambda: nc.tensor.matmul(w2[:, m * 128:(m + 1) * 128], PT[:, m, :], PR[:, m, 128:256], start=True, stop=True))
                            PRn, bPRn = gPR[gi].get()
                            w2v = w2[:].rearrange("p (m c) -> p m c", c=128)
                            V([bw2, bPR], [bPRn], lambda: nc.vector.tensor_tensor(PRn[:, :, 128:256], w2v, PR[:, :, 128:256], ALU.add))
                            PR, bPR = PRn, bPRn
                        yield
                    gR[gi] = (PR, bPR)

                def seq_group(gi, d, bi, c):
                    c0, n = blocks[bi]
                    ti = c0 // 128 + c
                    PRf, bR = gR[gi]
                    hps = [slice(0, 64), slice(64, 128)]
                    ppg, bpp = pQ.get()
                    for m in range(4):
                        j, hh = m // 2, m % 2; k = 4 * gi + m; hp = hps[hh]
                        hcol = slice((2 * j + hh) * 64, (2 * j + hh) * 64 + 64)
                        P([b_row[d][j], b_M[d][j]], [bpp], lambda: nc.tensor.matmul(ppg[:, m * 64:(m + 1) * 64], q12b[d][j][hp, c, 0, :], Mb[d][j][hp, :], start=True, stop=False))
                        P([cbA[k][1], b_Vt[ti]], [bpp], lambda: nc.tensor.matmul(ppg[:, m * 64:(m + 1) * 64], cA2[k][:, 0:128], Vtb[:, ti, hcol], start=False, stop=True))
                    Psb, bPsb = gSq[gi].get()
                    A([bpp], [bPsb], lambda: nc.scalar.copy(Psb[:], ppg[:].rearrange("p (m c) -> p m c", c=64)))
                    yield
                    upg, bup = pQ.get()
                    for m in range(4):
                        P([bR, bPsb], [bup], lambda: nc.tensor.matmul(upg[:, m * 64:(m + 1) * 64], PRf[:, m, 128:256], Psb[:, m, :], start=True, stop=True))
                    nU, bnU = gSq[gi].get()
                    A([bup], [bnU], lambda: nc.scalar.mul(nU[:], upg[:].rearrange("p (m c) -> p m c", c=64), -1.0))
                    yield
                    ypg, byp = pQ.get()
                    for m in range(4):
                        j, hh = m // 2, m % 2; k = 4 * gi + m; hp = hps[hh]
                        hcol = slice((2 * j + hh) * 64, (2 * j + hh) * 64 + 64)
                        P([b_row[d][j], b_M[d][j]], [byp], lambda: nc.tensor.matmul(ypg[:, m * 64:(m + 1) * 64], q12b[d][j][hp, c, 1, :], Mb[d][j][hp, :], start=True, stop=False))
                        P([cbA[k][1], b_Vt[ti]], [byp], lambda: nc.tensor.matmul(ypg[:, m * 64:(m + 1) * 64], cA2[k][:, 128:256], Vtb[:, ti, hcol], start=False, stop=False))
                        P([cbA[k][0], bnU], [byp], lambda: nc.tensor.matmul(ypg[:, m * 64:(m + 1) * 64], cA1[k][:, 128:256], nU[:, m, :], start=False, stop=True))
                    V([byp, b_yacc[ti]], [b_yacc[ti]], lambda: nc.vector.tensor_tensor(yacc[:, ti, :], yacc[:, ti, :], ypg[:], ALU.add))
                    yield
                    for m in range(4):
                        j, hh = m // 2, m % 2; k = 4 * gi + m; hp = hps[hh]
                        hcol = slice((2 * j + hh) * 64, (2 * j + hh) * 64 + 64)
                        E_, bE = cE[k // 2], cbE[k // 2]
                        pm = pM[d][j]
                        P([bE, b_Vt[ti]], [b_pM[d][j]], lambda: nc.tensor.matmul(pm[hp, :], E_[:, 64 * hh:64 * hh + 64], Vtb[:, ti, hcol], start=True, stop=False))
                        P([bE, bnU], [b_pM[d][j]], lambda: nc.tensor.matmul(pm[hp, :], E_[:, 128 + 64 * hh:128 + 64 * hh + 64], nU[:, m, :], start=False, stop=True))
                    for j in range(2):
                        V([b_pM[d][j], b_M[d][j], b_row[d][j]], [b_M[d][j]], lambda: nc.vector.scalar_tensor_tensor(
                            Mst[d][j][:], Mst[d][j][:], gC[d][j][:, c:c + 1], pM[d][j][:], ALU.mult, ALU.add))
                        G([b_M[d][j]], [b_M[d][j]], lambda: nc.gpsimd.tensor_copy(Mb[d][j][:], Mst[d][j][:]))
                    yield

                def run_rr(gens):
                    gens = list(gens)
                    while gens:
                        nxt = []
                        for g in gens:
                            try:
                                next(g); nxt.append(g)
                            except StopIteration:
                                pass
                        gens = nxt

                gR = {}; gcur = {}
                for step in range(9):
                    if RW_STOP in (2, 3) and step > 0:
                        break
                    for d in range(2):
                        prep_block(d, order[d][step])
                    if RW_STOP == 2:
                        break
                    chains = []; groups = []
                    for cc in range(2):
                        for d in range(2):
                            bi = order[d][step]
                            c = cc if d == 0 else 1 - cc
                            groups.append((len(groups), d, bi, c))
                            for j in range(2):
                                for hh in range(2):
                                    chains.append((len(chains), d, j, hh, bi, c))
                    for gi in range(4):
                        gcur[gi] = (gPR[gi].get(), gPT[gi].get())
                    run_rr([score_chain(*ch) for ch in chains])
                    run_rr([neumann_group(gi) for gi in range(4)])
                    for cc in range(2):
                        run_rr([seq_group(*g) for g in groups[cc * 2:(cc + 1) * 2]])
                f.barrier()
            if RW_STOP in (2, 3):
                return

            with ExitStack() as es2:
                gs0 = sbt(es2, "gs0", [128, T]); gs1 = sbt(es2, "gs1", [32, T]); b_gs = Buf()
                rw = sbt(es2, "rw", [128, T + 2]); b_rw = Buf()
                for (dst, frow, nrows, tapc) in ((gs0, OFF_GD, 128, 8), (gs1, OFF_GD + 128, 32, 9)):
                    G([], [b_rw], lambda: nc.gpsimd.memset(rw[:], 0.0))
                    D(rw[0:nrows, 1:T + 1], zT[frow:frow + nrows, :], [], [b_rw])
                    w = lambda tap, tapc=tapc, nrows=nrows: cp[0:nrows, tapc * 3 + tap:tapc * 3 + tap + 1]
                    V([b_rw, b_cp], [b_gs], lambda: nc.vector.tensor_scalar(dst[0:nrows, :], rw[0:nrows, 1:T + 1], w(1), None, ALU.mult))
                    for (a, b) in ((0, L), (L, T)):
                        V([b_rw, b_cp, b_gs], [b_gs], lambda a=a, b=b: nc.vector.scalar_tensor_tensor(
                            dst[0:nrows, a + 1:b], rw[0:nrows, a + 1:b], w(0), dst[0:nrows, a + 1:b], ALU.mult, ALU.add))
                        V([b_rw, b_cp, b_gs], [b_gs], lambda a=a, b=b: nc.vector.scalar_tensor_tensor(
                            dst[0:nrows, a:b - 1], rw[0:nrows, a + 2:b + 1], w(2), dst[0:nrows, a:b - 1], ALU.mult, ALU.add))
                    A([b_gs], [b_gs], lambda: nc.scalar.activation(dst[0:nrows, :], dst[0:nrows, :], AF.Sigmoid))
                gb = sbt(es2, "gb", [128, 2, 256]); b_gb = Buf()
                D(gb[:, 0, :], a_lnx_g[l, :].partition_broadcast(128), [], [b_gb])
                D(gb[:, 1, :], a_lnx_b[l, :].partition_broadcast(128), [], [b_gb])
                pg = carve(es2, "pg", 1, 256)
                st4 = Rot(es2, "st4", 2, [128, 4, 6]); mv4 = Rot(es2, "mv4", 2, [128, 4, 2]); rs4 = Rot(es2, "rs4", 2, [128, 4])
                yn = Rot(es2, "yn", 2, [128, 256]); oo = Rot(es2, "oo", 2, [128, 256])
                for i in range(NT):
                    g_p, bg = pg.get()
                    P([b_gs, b_gup], [bg], lambda: nc.tensor.matmul(g_p[:], gs0[:, i * 128:(i + 1) * 128], gup0[:], start=True, stop=False))
                    P([b_gs, b_gup], [bg], lambda: nc.tensor.matmul(g_p[:], gs1[0:32, i * 128:(i + 1) * 128], gup1[:], start=False, stop=True))
                    s4, bs4 = st4.get(); m4, bm4 = mv4.get(); r4, br4 = rs4.get(); y_n, byn = yn.get(); o_t, bo = oo.get()
                    for h in range(4):
                        V([b_yacc[i]], [bs4], lambda h=h: nc.vector.bn_stats(s4[:, h, :], yacc[:, i, h * 64:(h + 1) * 64]))
                    for h in range(4):
                        V([bs4], [bm4], lambda h=h: nc.vector.bn_aggr(m4[:, h, :], s4[:, h, :]))
                    A([bm4], [br4], lambda: nc.scalar.activation(r4[:], m4[:, :, 1], AF.Sqrt, bias=eps_t[:, 1:2], scale=1.0))
                    V([br4], [br4], lambda: nc.vector.reciprocal(r4[:], r4[:]))
                    for h in range(4):
                        V([b_yacc[i], bm4, br4], [byn], lambda h=h: nc.vector.tensor_scalar(
                            y_n[:, h * 64:(h + 1) * 64], yacc[:, i, h * 64:(h + 1) * 64], m4[:, h, 0:1], r4[:, h:h + 1], ALU.subtract, ALU.mult))
                    G([byn, b_gb], [byn], lambda: nc.gpsimd.tensor_tensor(y_n[:], y_n[:], gb[:, 0, :], ALU.mult))
                    G([byn, b_gb], [byn], lambda: nc.gpsimd.tensor_tensor(y_n[:], y_n[:], gb[:, 1, :], ALU.add))
                    for h in range(4):
                        V([byn, b_bacc[i], b_Vt[i]], [byn], lambda h=h: nc.vector.scalar_tensor_tensor(
                            y_n[:, h * 64:(h + 1) * 64], Vt[:, i, h * 64:(h + 1) * 64], bacc[:, i, h:h + 1], y_n[:, h * 64:(h + 1) * 64], ALU.mult, ALU.add))
                    V([byn, bg], [bo], lambda: nc.vector.tensor_tensor(o_t[:], y_n[:], g_p[:], ALU.mult))
                    D(omix[i * 128:(i + 1) * 128, 0:256], o_t[:], [bo], [])
                f.barrier()

    def phase_attn_b(l):
        with_ctx = l < DEPTH - 1
        with ExitStack() as es:
            cosT = sbt(es, "cosT", [64, S]); sinT = sbt(es, "sinT", [64, S]); rPT = sbt(es, "rPT", [64, 64]); b_c = Buf()
            D(cosT[:], ccos[:, :], [], [b_c]); D(sinT[:], csin[:, :], [], [b_c]); D(rPT[:], cropeP[:, :], [], [b_c])
            mkf = sbt(es, "mkf", [128, 4, 128]); mkb = sbt(es, "mkb", [128, 4, 128], BF16); b_mk = Buf()
            D(mkf[:], cmasks[:, :, :], [], [b_mk])
            V([b_mk], [b_mk], lambda: nc.vector.tensor_copy(mkb[:], mkf[:]))
            esk = sbt(es, "esk", [128, 6]); b_esk = Buf()
            D(esk[:], b_sink[l, :].partition_broadcast(128), [], [b_esk])
            A([b_esk], [b_esk], lambda: nc.scalar.activation(esk[:], esk[:], AF.Exp))
            kTb = sbt(es, "kTb", [64, 2, T], BF16); b_k = [Buf() for _ in range(2)]
            qTb = sbt(es, "qTb", [64, 2, NT, 3, 128], BF16); b_q = [Buf() for _ in range(6)]
            Va = sbt(es, "Va", [128, 2, NT, 65], BF16); b_Va = Buf()
            G([], [b_Va], lambda: nc.gpsimd.memset(Va[:], 1.0))
            with ExitStack() as es2:
                vf = sbt(es2, "vf", [128, NT, 128]); b_vf = Buf()
                D(vf[:], vtok[:, 256:384].rearrange("(n p) f -> p n f", p=128), [], [b_vf])
                for g in range(2):
                    V([b_vf, b_Va], [b_Va], lambda g=g: nc.vector.tensor_copy(Va[:, g, :, 0:64], vf[:, :, g * 64:(g + 1) * 64]))
                raw = Rot(es2, "rawb", 2, [64, T])
                pr = Rot(es2, "pr", 2, [128, 512], psum=True)
                t1 = Rot(es2, "t1b", 2, [64, 512]); t2 = Rot(es2, "t2b", 2, [64, 512])
                for hd in range(8):
                    rt, br = raw.get()
                    frow = OFF_QB + 64 * hd if hd < 6 else OFF_KB + 64 * (hd - 6)
                    D(rt[:], zT[frow:frow + 64, :], [], [br])
                    if hd < 6:
                        g, i3 = hd // 3, hd % 3
                        dst_ctx = qTb[:, g, 0:2, i3, :]
                        wb_ = b_q[hd]
                    else:
                        g = hd - 6
                        dst_ctx = kTb[:, g, 0:L].rearrange("p (n t) -> p n t", t=128)
                        wb_ = b_k[g]
                    A([br], [wb_], lambda rt=rt, dst_ctx=dst_ctx: nc.scalar.copy(dst_ctx, rt[:, 0:L].rearrange("p (n t) -> p n t", t=128)))
                    for q4 in range(4):
                        cs_ = slice(L + q4 * 512, L + (q4 + 1) * 512); ps_ = slice(q4 * 512, (q4 + 1) * 512)
                        p_t, bp = pr.get(); a_t, ba = t1.get(); c_t, bc = t2.get()
                        P([br, b_c], [bp], lambda: nc.tensor.matmul(p_t[0:64, :], rPT[:], rt[:, cs_], start=True, stop=True))
                        V([br, b_c], [ba], lambda: nc.vector.tensor_tensor(a_t[:], rt[:, cs_], cosT[:, ps_], ALU.mult))
                        V([bp, b_c], [bc], lambda: nc.vector.tensor_tensor(c_t[:], p_t[0:64, :], sinT[:, ps_], ALU.mult))
                        if hd < 6:
                            dst = qTb[:, g, 2 + 4 * q4:2 + 4 * q4 + 4, i3, :]
                        else:
                            dst = kTb[:, g, cs_].rearrange("p (n t) -> p n t", t=128)
                        G([ba, bc], [wb_], lambda: nc.gpsimd.tensor_tensor(dst, a_t[:].rearrange("p (n t) -> p n t", t=128), c_t[:].rearrange("p (n t) -> p n t", t=128), ALU.add))
                f.barrier()
            psc = Rot(es, "psc", 5, [128, 512], psum=True)
            pov = Rot(es, "pov", 2, [128, 512], psum=True)
            pt = Rot(es, "ptb", 16, [128, 384], BF16)
            ob = Rot(es, "obb", 3, [128, 384])
            dn = Rot(es, "dnb", 4, [128, 3])
            items = [(n, g) for n in range(NT) if not (n < 2 and not with_ctx) for g in range(2)]
            obufs = {}

            def stage1(n, g):
                kbs = [(0, None), (1, None)]
                if n >= 2:
                    if n - 1 >= 2: kbs.append((n - 1, 3))
                    kbs.append((n, None))
                    if n + 1 < NT: kbs.append((n + 1, 1))
                pts = []
                for (m, mk) in kbs:
                    s_p, bs = psc.get(); p_t, bpt = pt.get()
                    P([b_k[g]] + b_q[3 * g:3 * g + 3], [bs], lambda: nc.tensor.matmul(
                        s_p[:, 0:384], kTb[:, g, m * 128:(m + 1) * 128], qTb[:, g, n, :, :].rearrange("p a t -> p (a t)"), start=True, stop=True))
                    A([bs], [bpt], lambda: nc.scalar.activation(p_t[:], s_p[:, 0:384], AF.Exp, scale=SCALE))
                    if mk is not None:
                        G([bpt, b_mk], [bpt], lambda: nc.gpsimd.tensor_tensor(
                            p_t[:].rearrange("p (a t) -> p a t", a=3), p_t[:].rearrange("p (a t) -> p a t", a=3),
                            mkb[:, mk, :].unsqueeze(1).broadcast_to([128, 3, 128]), ALU.mult))
                    pts.append((p_t, bpt, m))
                return pts

            def stage2(n, g, pts):
                if g == 0:
                    obufs[n] = ob.get()
                o_t, bo = obufs[n]
                o_pf, bop = pov.get()
                o_p = o_pf[:, 0:195].rearrange("p (a e) -> p a e", e=65)
                for i3 in range(3):
                    for q, (p_t, bpt, m) in enumerate(pts):
                        P([bpt, b_Va], [bop], lambda: nc.tensor.matmul(
                            o_p[:, i3, :], p_t[:, i3 * 128:(i3 + 1) * 128], Va[:, g, m, :], start=(q == 0), stop=(q == len(pts) - 1)))
                d_t, bd = dn.get()
                V([bop, b_esk], [bd], lambda: nc.vector.tensor_tensor(d_t[:], o_p[:, :, 64], esk[:, 3 * g:3 * g + 3], ALU.add))
                V([bd], [bd], lambda: nc.vector.reciprocal(d_t[:], d_t[:]))
                for i3 in range(3):
                    hc = (3 * g + i3) * 64
                    V([bop, bd], [bo], lambda: nc.vector.tensor_scalar(o_t[:, hc:hc + 64], o_p[:, i3, 0:64], d_t[:, i3:i3 + 1], None, ALU.mult))
                if g == 1:
                    D(omix[n * 128:(n + 1) * 128, 256:640], o_t[:], [bo], [])

            prev = None
            for it in items:
                cur = (it, stage1(*it))
                if prev is not None:
                    stage2(prev[0][0], prev[0][1], prev[1])
                prev = cur
            stage2(prev[0][0], prev[0][1], prev[1])
            f.barrier()

    def phase_attn_c(l):
        with_ctx = l < DEPTH - 1
        with ExitStack() as es:
            qT = sbt(es, "qTn", [64, 6, T], BF16); kTn = sbt(es, "kTn", [64, 6, T], BF16); b_qk = Buf()
            for h in range(6):
                D(qT[:, h, :], zT[OFF_QN + 64 * h:OFF_QN + 64 * h + 64, :], [], [b_qk], eng=POOL)
                D(kTn[:, h, :], zT[OFF_KN + 64 * h:OFF_KN + 64 * h + 64, :], [], [b_qk], eng=POOL)
            Va = sbt(es, "Van", [128, NT, 6, 65], BF16); b_Va = Buf()
            G([], [b_Va], lambda: nc.gpsimd.memset(Va[:], 1.0))
            with ExitStack() as es2:
                vf = sbt(es2, "vfn", [128, NT, 384]); b_vf = Buf()
                D(vf[:], vtok[:, 384:768].rearrange("(n p) f -> p n f", p=128), [], [b_vf])
                V([b_vf, b_Va], [b_Va], lambda: nc.vector.tensor_copy(Va[:, :, :, 0:64], vf[:].rearrange("p n (h e) -> p n h e", e=64)))
                f.barrier()
            cv = sbt(es, "cv", [128, 64]); b_cv = Buf()
            D(cv[:], ccolvalid[:, :], [], [b_cv])
            ebf = Rot(es, "ebf", 2, [128, 15, 64]); ebb = sbt(es, "ebb", [128, 6, 15, 64], BF16); b_eb = [Buf() for _ in range(6)]
            oC = sbt(es, "oC", [128, NT, 384]); b_oC = [Buf() for _ in range(NT)]
            psc = Rot(es, "pscn", 3, [128, 1024], psum=True)
            pov = carve(es, "povn", 2, 128)
            pt = Rot(es, "ptn", 4, [128, 896], BF16)
            dn = Rot(es, "dnn", 4, [128, 1])
            for h in range(6):
                e_f, bef = ebf.get()
                D(e_f[0:64, :, :], rpbtab[l, h, :, :, :], [], [bef])
                D(e_f[64:128, :, :], rpbtab[l, h, :, :, :], [], [bef])
                A([bef], [bef], lambda: nc.scalar.activation(e_f[:], e_f[:], AF.Exp))
                V([bef, b_cv], [b_eb[h]], lambda: nc.vector.tensor_tensor(ebb[:, h, :, :], e_f[:], cv[:].unsqueeze(1).broadcast_to([128, 15, 64]), ALU.mult))
            items = [(h, n) for h in range(6) for n in range(NT) if not (n < 2 and not with_ctx)]

            def keyblocks(n):
                kbs = [(0, None), (1, None)]
                if n >= 2:
                    nb = n - 2
                    for kb in range(16):
                        subs = []
                        anyv = False
                        for i in range(2):
                            for jq in range(2):
                                kr = 2 * kb + i; qr = 2 * nb + jq
                                rs_ = min(max(qr - 4, 0), 24)
                                ok = rs_ <= kr < rs_ + 8
                                subs.append((i, jq, kr - qr + 7 if ok else None))
                                anyv = anyv or ok
                        if anyv:
                            kbs.append((kb + 2, subs))
                return kbs

            def stage1(h, n):
                kbs = keyblocks(n)
                nk = len(kbs)
                s_p, bs = psc.get(); p_t, bpt = pt.get()
                for q, (m, subs) in enumerate(kbs):
                    P([b_qk], [bs], lambda: nc.tensor.matmul(s_p[:, q * 128:(q + 1) * 128], kTn[:, h, m * 128:(m + 1) * 128], qT[:, h, n * 128:(n + 1) * 128], start=True, stop=True))
                A([bs], [bpt], lambda: nc.scalar.activation(p_t[:, 0:nk * 128], s_p[:, 0:nk * 128], AF.Exp, scale=SCALE))
                cnt = 0
                for q, (m, subs) in enumerate(kbs):
                    if subs is None:
                        continue
                    for (i, jq, ro) in subs:
                        eng, ee = (G, nc.gpsimd) if cnt % 2 == 0 else (V, nc.vector)
                        cnt += 1
                        sl = p_t[64 * i:64 * i + 64, q * 128 + 64 * jq:q * 128 + 64 * jq + 64]
                        if ro is None:
                            eng([bpt], [bpt], lambda: ee.memset(sl, 0.0))
                        else:
                            eng([bpt, b_eb[h]], [bpt], lambda: ee.tensor_tensor(sl, sl, ebb[64 * i:64 * i + 64, h, ro, :], ALU.mult))
                return (p_t, bpt, kbs)

            def stage2(h, n, st):
                p_t, bpt, kbs = st
                o_p, bop = pov.get()
                for q, (m, subs) in enumerate(kbs):
                    P([bpt, b_Va], [bop], lambda: nc.tensor.matmul(o_p[:, 0:65], p_t[:, q * 128:(q + 1) * 128], Va[:, m, h, :], start=(q == 0), stop=(q == len(kbs) - 1)))
                d_t, bd = dn.get()
                V([bop], [bd], lambda: nc.vector.reciprocal(d_t[:], o_p[:, 64:65]))
                V([bop, bd], [b_oC[n]], lambda: nc.vector.tensor_scalar(oC[:, n, h * 64:(h + 1) * 64], o_p[:, 0:64], d_t[:, 0:1], None, ALU.mult))

            prev = None
            for it in items:
                cur = (it, stage1(*it))
                if prev is not None:
                    stage2(prev[0][0], prev[0][1], prev[1])
                prev = cur
            stage2(prev[0][0], prev[0][1], prev[1])
            for n in range(NT):
                if n < 2 and not with_ctx:
                    continue
                D(omix[n * 128:(n + 1) * 128, 640:1024], oC[:, n, :], [b_oC[n]], [])
            f.barrier()

    def phase_out_moe(l):
        with_ctx = l < DEPTH - 1
        tiles = list(range(NT)) if with_ctx else list(range(2, NT))
        with ExitStack() as es:
            h2T = sbt(es, "h2T", [128, 8, T], BF16); b_h2T = [Buf() for _ in range(NT)]
            Wr = sbt(es, "Wr", [128, NT, 32]); b_Wr = [Buf() for _ in range(NT)]
            with ExitStack() as es2:
                mods = sbt(es2, "mods2", [128, 2, 3 * DM]); b_mods = Buf()
                D(mods[:, 0, :], modrow[1, 2 * DM:5 * DM].partition_broadcast(128), [], [b_mods])
                D(mods[:, 1, :], modrow[0, 2 * DM:5 * DM].partition_broadcast(128), [], [b_mods])
                G([b_mods], [b_mods], lambda: nc.gpsimd.tensor_scalar(mods[:, :, 2 * DM:3 * DM], mods[:, :, 2 * DM:3 * DM], 1.0, None, ALU.add))
                lgb = sbt(es2, "lgb", [128, 2, DM]); b_lgb = Buf()
                D(lgb[:, 0, :], ln1_g[l, :].partition_broadcast(128), [], [b_lgb])
                D(lgb[:, 1, :], ln1_b[l, :].partition_broadcast(128), [], [b_lgb])
                wo = sbt(es2, "wo", [128, 8, DM], BF16); b_wo = Buf()
                D(wo[:], w_out[l, :, :].rearrange("(k p) n -> p k n", p=128), [], [b_wo], eng=POOL)
                wr = sbt(es2, "wr", [128, 8, 36]); b_wr = Buf()
                D(wr[:], w_r[l, :, :].rearrange("(k p) n -> p k n", p=128), [], [b_wr])
                brb = sbt(es2, "brb", [128, 36]); D(brb[:], b_r[l, :].partition_broadcast(128), [], [b_wr])
                om = Rot(es2, "om", 2, [128, DM]); omb = Rot(es2, "omb", 2, [128, DM], BF16)
                oT = Rot(es2, "oT", 2, [128, 8, 128], BF16)
                xt = Rot(es2, "xt2", 2, [128, DM]); tt = Rot(es2, "tt", 2, [128, DM]); x1 = Rot(es2, "x1", 2, [128, DM])
                hf = Rot(es2, "hf", 2, [128, DM]); hb = Rot(es2, "hb2", 2, [128, DM], BF16)
                hT32 = Rot(es2, "hT32", 2, [128, 8, 128])
                st = Rot(es2, "st2", 4, [128, 2, 6]); mv = Rot(es2, "mv2", 4, [128, 2]); rs = Rot(es2, "rs2", 4, [128, 1])
                pTb = Rot(es2, "pTb", 2, [128, 8, 128], BF16, psum=True)
                pO = Rot(es2, "pO", 1, [128, 1024], psum=True)
                pT32 = Rot(es2, "pT32", 1, [128, 8, 128], psum=True)
                pL = carve(es2, "pL", 1, 64)
                sm = Rot(es2, "sm", 2, [128, 160])
                for i in tiles:
                    w = 0 if i < 2 else 1
                    o_m, bom = om.get(); o_b, bob = omb.get(); o_T, boT = oT.get()
                    D(o_m[:], omix[i * 128:(i + 1) * 128, :], [], [bom])
                    A([bom], [bob], lambda: nc.scalar.copy(o_b[:], o_m[:]))
                    p_t, bp = pTb.get()
                    for k in range(8):
                        P([bob, b_identb], [bp], lambda k=k: nc.tensor.transpose(p_t[:, k, :], o_b[:, k * 128:(k + 1) * 128], identb[:]))
                    V([bp], [boT], lambda: nc.vector.tensor_copy(o_T[:], p_t[:]))
                    p_o, bpo = pO.get()
                    for hf_ in range(2):
                        for k in range(8):
                            P([boT, b_wo], [bpo], lambda k=k, hf_=hf_: nc.tensor.matmul(
                                p_o[:, hf_ * 512:(hf_ + 1) * 512], o_T[:, k, :], wo[:, k, hf_ * 512:(hf_ + 1) * 512], start=(k == 0), stop=(k == 7)))
                    x_t, bx = xt.get(); t_t, bt = tt.get(); x_1, bx1 = x1.get()
                    D(x_t[:], xsrc(l, i), [], [bx])
                    V([bpo, b_mods], [bt], lambda: nc.vector.tensor_tensor(t_t[:], p_o[:], mods[:, w, 0:DM], ALU.mult))
                    V([bx, bt], [bt], lambda: nc.vector.scalar_tensor_tensor(t_t[:], x_t[:], ALPHA, t_t[:], ALU.mult, ALU.add))
                    s_t, bs = st.get(); m_t, bmv = mv.get(); r_t, br = rs.get()
                    layer_norm_stats(None, t_t, bt, (s_t, bs, m_t, bmv, r_t, br))
                    V([bt, bmv, br], [bt], lambda: nc.vector.tensor_scalar(t_t[:], t_t[:], m_t[:, 0:1], r_t[:, 0:1], ALU.subtract, ALU.mult))
                    G([bt, b_lgb], [bt], lambda: nc.gpsimd.tensor_tensor(t_t[:], t_t[:], lgb[:, 0, :], ALU.mult))
                    G([bt, b_lgb], [bx1], lambda: nc.gpsimd.tensor_tensor(x_1[:], t_t[:], lgb[:, 1, :], ALU.add))
                    D(xs[i * 128:(i + 1) * 128, :], x_1[:], [bx1], [])
                    s_t, bs = st.get(); m_t, bmv = mv.get(); r_t, br = rs.get()
                    layer_norm_stats(None, x_1, bx1, (s_t, bs, m_t, bmv, r_t, br))
                    h_f, bhf = hf.get(); h_b, bhb = hb.get()
                    V([bx1, bmv, br], [bhf], lambda: nc.vector.tensor_scalar(h_f[:], x_1[:], m_t[:, 0:1], r_t[:, 0:1], ALU.subtract, ALU.mult))
                    G([bhf, b_mods], [bhf], lambda: nc.gpsimd.tensor_tensor(h_f[:], h_f[:], mods[:, w, 2 * DM:3 * DM], ALU.mult))
                    G([bhf, b_mods], [bhf], lambda: nc.gpsimd.tensor_tensor(h_f[:], h_f[:], mods[:, w, DM:2 * DM], ALU.add))
                    A([bhf], [bhb], lambda: nc.scalar.copy(h_b[:], h_f[:]))
                    p_t, bp = pTb.get()
                    for k in range(8):
                        P([bhb, b_identb], [bp], lambda k=k: nc.tensor.transpose(p_t[:, k, :], h_b[:, k * 128:(k + 1) * 128], identb[:]))
                    A([bp], [b_h2T[i]], lambda: nc.scalar.copy(h2T[:, :, i * 128:(i + 1) * 128], p_t[:]))
                    p32, bp32 = pT32.get()
                    for k in range(8):
                        P([bhf, b_ident], [bp32], lambda k=k: nc.tensor.transpose(p32[:, k, :], h_f[:, k * 128:(k + 1) * 128], ident[:]))
                    h32, bh32 = hT32.get()
                    V([bp32], [bh32], lambda: nc.vector.tensor_copy(h32[:], p32[:]))
                    p_l, bpl = pL.get()
                    for k in range(8):
                        P([bh32, b_wr], [bpl], lambda k=k: nc.tensor.matmul(p_l[:, 0:36], h32[:, k, :], wr[:, k, :], start=(k == 0), stop=(k == 7)))
                    s_, bsm = sm.get()
                    lg = s_[:, 0:36]; gmx = s_[:, 36:37]; ngm = s_[:, 37:38]; g1h = s_[:, 40:44]; e4 = s_[:, 44:48]
                    esum = s_[:, 48:49]; pgr = s_[:, 49:50]; pen = s_[:, 52:56]; m8 = s_[:, 56:64]; elm = s_[:, 64:96]
                    dd = s_[:, 96:97]; ee_ = s_[:, 97:98]; c1 = s_[:, 98:99]; c2 = s_[:, 99:100]
                    V([bpl, b_wr], [bsm], lambda: nc.vector.tensor_tensor(lg, p_l[:, 0:36], brb[:], ALU.add))
                    V([bsm], [bsm], lambda: nc.vector.tensor_reduce(gmx, s_[:, 0:4], AX.X, ALU.max))
                    V([bsm], [bsm], lambda: nc.vector.tensor_scalar(ngm, gmx, -1.0, None, ALU.mult))
                    V([bsm], [bsm], lambda: nc.vector.tensor_scalar(g1h, s_[:, 0:4], gmx, None, ALU.is_equal))
                    A([bsm], [bsm], lambda: nc.scalar.activation(e4, s_[:, 0:4], AF.Exp, bias=ngm, scale=1.0))
                    V([bsm], [bsm], lambda: nc.vector.tensor_reduce(esum, e4, AX.X, ALU.add))
                    V([bsm], [bsm], lambda: nc.vector.reciprocal(pgr, esum))
                    V([bsm], [bsm], lambda: nc.vector.tensor_scalar(pen, g1h, -1.0, 1e30, ALU.add, ALU.mult))
                    V([bsm], [bsm], lambda: nc.vector.tensor_tensor(elm.rearrange("p (g e) -> p g e", e=8), s_[:, 4:36].rearrange("p (g e) -> p g e", e=8),
                                                                    pen.unsqueeze(2).broadcast_to([128, 4, 8]), ALU.add))
                    V([bsm], [bsm], lambda: nc.vector.max(m8, elm))
                    V([bsm], [bsm], lambda: nc.vector.tensor_tensor(dd, m8[:, 1:2], m8[:, 0:1], ALU.subtract))
                    A([bsm], [bsm], lambda: nc.scalar.activation(ee_, dd, AF.Exp))
                    V([bsm], [bsm], lambda: nc.vector.tensor_scalar(c1, ee_, 1.0, None, ALU.add))
                    V([bsm], [bsm], lambda: nc.vector.reciprocal(c1, c1))
                    V([bsm], [bsm], lambda: nc.vector.tensor_tensor(c2, ee_, c1, ALU.mult))
                    V([bsm], [bsm], lambda: nc.vector.tensor_tensor(c1, c1, pgr, ALU.mult))
                    V([bsm], [bsm], lambda: nc.vector.tensor_tensor(c2, c2, pgr, ALU.mult))
                    V([bsm], [b_Wr[i]], lambda: nc.vector.tensor_scalar(Wr[:, i, :], elm, m8[:, 0:1], c1, ALU.is_equal, ALU.mult))
                    V([bsm], [bsm], lambda: nc.vector.tensor_scalar(s_[:, 100:132], elm, m8[:, 1:2], c2, ALU.is_equal, ALU.mult))
                    V([bsm, b_Wr[i]], [b_Wr[i]], lambda: nc.vector.tensor_tensor(Wr[:, i, :], Wr[:, i, :], s_[:, 100:132], ALU.add))
                f.barrier()
            if state.get("stop_before_moe"):
                return
            facc = sbt(es, "facc", [128, NT, DM]); b_f = [Buf() for _ in range(NT)]
            with ExitStack() as es2:
                wg = Rot(es2, "wg", 2, [128, 8, DE], BF16); wu = Rot(es2, "wu", 2, [128, 8, DE], BF16)
                wd = Rot(es2, "wd", 2, [128, 4, DM], BF16)
                pG = Rot(es2, "pG", 2, [128, 512], psum=True); pU = Rot(es2, "pU", 2, [128, 512], psum=True)
                pD = Rot(es2, "pD", 3, [128, 512], psum=True)
                sg = Rot(es2, "sg", 2, [128, 512]); aT = Rot(es2, "aT", 8, [128, 512], BF16)
                t0_ = tiles[0] * 128
                ntok = len(tiles) * 128
                blocks_ = [(t0_ + b * 512, min(512, ntok - b * 512)) for b in range((ntok + 511) // 512)]
                for e in range(NE):
                    g_w, bg = wg.get(); u_w, bu = wu.get(); d_w, bdw = wd.get()
                    D(g_w[:], w_gate[l, e, :, :].rearrange("(k p) n -> p k n", p=128), [], [bg], eng=POOL)
                    D(u_w[:], w_up[l, e, :, :].rearrange("(k p) n -> p k n", p=128), [], [bu], eng=POOL)
                    D(d_w[:], w_down[l, e, :, :].rearrange("(k p) n -> p k n", p=128), [], [bdw], eng=POOL)
                    for (tb0, tsz) in blocks_:
                        rb = [b_h2T[i] for i in range(tb0 // 128, (tb0 + tsz) // 128)]
                        acts = []
                        for fc in range(4):
                            g_p, bgp = pG.get(); u_p, bup = pU.get()
                            for k in range(8):
                                P(rb + [bg], [bgp], lambda k=k: nc.tensor.matmul(g_p[:, 0:tsz], g_w[:, k, fc * 128:(fc + 1) * 128], h2T[:, k, tb0:tb0 + tsz], start=(k == 0), stop=(k == 7)))
                            for k in range(8):
                                P(rb + [bu], [bup], lambda k=k: nc.tensor.matmul(u_p[:, 0:tsz], u_w[:, k, fc * 128:(fc + 1) * 128], h2T[:, k, tb0:tb0 + tsz], start=(k == 0), stop=(k == 7)))
                            s_g, bsg = sg.get(); a_T, baT = aT.get()
                            A([bgp], [bsg], lambda: nc.scalar.activation(s_g[:, 0:tsz], g_p[:, 0:tsz], AF.Silu))
                            V([bsg, bup], [baT], lambda: nc.vector.tensor_tensor(a_T[:, 0:tsz], s_g[:, 0:tsz], u_p[:, 0:tsz], ALU.mult))
                            acts.append((a_T, baT))
                        for q in range(tsz // 128):
                            ti = tb0 // 128 + q
                            for hf_ in range(2):
                                d_p, bdp = pD.get()
                                for fc in range(4):
                                    a_T, baT = acts[fc]
                                    P([baT, bdw], [bdp], lambda fc=fc, a_T=a_T: nc.tensor.matmul(
                                        d_p[:], a_T[:, q * 128:(q + 1) * 128], d_w[:, fc, hf_ * 512:(hf_ + 1) * 512], start=(fc == 0), stop=(fc == 3)))
                                fs = facc[:, ti, hf_ * 512:(hf_ + 1) * 512]
                                if e == 0:
                                    V([bdp, b_Wr[ti]], [b_f[ti]], lambda: nc.vector.tensor_scalar(fs, d_p[:], Wr[:, ti, e:e + 1], None, ALU.mult))
                                else:
                                    V([bdp, b_Wr[ti], b_f[ti]], [b_f[ti]], lambda: nc.vector.scalar_tensor_tensor(fs, d_p[:], Wr[:, ti, e:e + 1], fs, ALU.mult, ALU.add))
                f.barrier()
            with ExitStack() as es2:
                g2 = sbt(es2, "g2", [128, 2, DM]); b_g2 = Buf()
                D(g2[:, 0, :], modrow[1, 5 * DM:6 * DM].partition_broadcast(128), [], [b_g2])
                D(g2[:, 1, :], modrow[0, 5 * DM:6 * DM].partition_broadcast(128), [], [b_g2])
                lgb = sbt(es2, "lgb2", [128, 2, DM]); b_lgb = Buf()
                D(lgb[:, 0, :], ln2_g[l, :].partition_broadcast(128), [], [b_lgb])
                D(lgb[:, 1, :], ln2_b[l, :].partition_broadcast(128), [], [b_lgb])
                xt = Rot(es2, "xt3", 2, [128, DM]); tt = Rot(es2, "tt3", 2, [128, DM])
                st = Rot(es2, "st3", 2, [128, 2, 6]); mv = Rot(es2, "mv3", 2, [128, 2]); rs = Rot(es2, "rs3", 2, [128, 1])
                for i in tiles:
                    w = 0 if i < 2 else 1
                    x_t, bx = xt.get(); t_t, bt = tt.get()
                    D(x_t[:], xs[i * 128:(i + 1) * 128, :], [], [bx])
                    G([b_f[i], b_g2], [bt], lambda: nc.gpsimd.tensor_tensor(t_t[:], facc[:, i, :], g2[:, w, :], ALU.mult))
                    V([bx, bt], [bt], lambda: nc.vector.scalar_tensor_tensor(t_t[:], x_t[:], ALPHA, t_t[:], ALU.mult, ALU.add))
                    s_t, bs = st.get(); m_t, bmv = mv.get(); r_t, br = rs.get()
                    layer_norm_stats(None, t_t, bt, (s_t, bs, m_t, bmv, r_t, br))
                    V([bt, bmv, br], [bt], lambda: nc.vector.tensor_scalar(t_t[:], t_t[:], m_t[:, 0:1], r_t[:, 0:1], ALU.subtract, ALU.mult))
                    G([bt, b_lgb], [bt], lambda: nc.gpsimd.tensor_tensor(t_t[:], t_t[:], lgb[:, 0, :], ALU.mult))
                    G([bt, b_lgb], [bt], lambda: nc.gpsimd.tensor_tensor(t_t[:], t_t[:], lgb[:, 1, :], ALU.add))
                    if l == DEPTH - 1:
                        D(y_out[(i - 2) * 128:(i - 1) * 128, :], t_t[:], [bt], [])
                    else:
                        D(xs[i * 128:(i + 1) * 128, :], t_t[:], [bt], [])
                f.barrier()

    done = False
    for l in range(DEPTH):
        for tag, fn in (("mod", phase_mod), ("proj", phase_ln_proj), ("rwkv", phase_rwkv),
                        ("attb", phase_attn_b), ("attc", phase_attn_c), ("moe", phase_out_moe)):
            if STOP is not None and (l, tag) == tuple(STOP[:2]) and len(STOP) > 2:
                state["stop_before_moe"] = True
            fn(l)
            if STOP is not None and (l, tag) == tuple(STOP[:2]):
                done = True
                break
        if done:
            break
    f.barrier()
    print("n_inst", f.n_inst, flush=True)
    return nc


def _consts():
    p = np.arange(128)[:, None]; q = np.arange(128)[None, :]
    masks = np.stack([(p < q), (p <= q), (p > q), (p >= q)], 1).astype(np.float32)
    ident = np.eye(128, dtype=np.float32)
    block = (p // 64 == q // 64).astype(np.float32)
    hsel = (np.arange(128)[:, None] // 64 == np.arange(2)[None, :]).astype(np.float32)
    t = np.arange(S)
    pos = np.stack([t // 64, t % 64], -1).astype(np.float32)
    inv = (10000.0 ** (-np.arange(16, dtype=np.float32) / 16)).astype(np.float32)
    ang = pos[:, :, None] * inv
    cos = np.cos(ang).astype(np.float32); sin = np.sin(ang).astype(np.float32)
    cosT = np.zeros((64, S), np.float32); sinT = np.zeros((64, S), np.float32)
    for ax in range(2):
        for half in range(2):
            r0 = ax * 32 + half * 16
            cosT[r0:r0 + 16] = cos[:, ax, :].T
            sinT[r0:r0 + 16] = sin[:, ax, :].T
    Pm = np.zeros((64, 64), np.float32)
    for ax in range(2):
        for i in range(16):
            Pm[ax * 32 + i, ax * 32 + 16 + i] = -1.0
            Pm[ax * 32 + 16 + i, ax * 32 + i] = 1.0
    ropeP = np.ascontiguousarray(Pm.T)
    col = np.arange(64)
    cstart = np.clip(col - 8, 0, 48)
    cvalid = ((col[None, :] >= cstart[:, None]) & (col[None, :] < cstart[:, None] + 16)).astype(np.float32)
    colvalid = np.concatenate([cvalid.T, cvalid.T], 0)
    scan = np.ones((128, 512), np.float32); scan[:, ::128] = 0.0
    return dict(cmasks=masks, cident=ident, cblock=block, chsel=hsel, ccos=cosT, csin=sinT, cropeP=ropeP,
                ccolvalid=np.ascontiguousarray(colvalid), cscanmask=scan)


def _host_layout(inp):
    f32 = lambda a: np.ascontiguousarray(np.asarray(a, dtype=np.float32))
    sh = {}
    sh["w_mod"] = f32(inp["w_mod"]); sh["b_mod"] = f32(inp["b_mod"]); sh["w_in"] = f32(inp["w_in"]); sh["w_out"] = f32(inp["w_out"])
    sh["a_shift"] = f32(inp["a_shift"])
    sh["a_w_up"] = f32(np.asarray(inp["a_w_up"]).reshape(DEPTH, 128, 256))
    sh["a_a_up"] = f32(np.asarray(inp["a_a_up"]).reshape(DEPTH, 128, 256))
    sh["a_g_up"] = f32(inp["a_g_up"])
    cp = np.zeros((DEPTH, 128, 48), np.float32)
    ash = np.zeros((DEPTH, 3, 1280), np.float32); ash[:, :, :A_IN] = inp["a_shift"]
    for l in range(DEPTH):
        for ch in range(10):
            for tap in range(3):
                cp[l, :, ch * 3 + tap] = ash[l, tap, ch * 128:(ch + 1) * 128]
        for d in range(2):
            for j in range(2):
                cp[l, :, 30 + d * 2 + j] = inp["a_w0"][l, d, j * 128:(j + 1) * 128]
                cp[l, :, 34 + d * 2 + j] = inp["a_a0"][l, d, j * 128:(j + 1) * 128]
        for j in range(2):
            cp[l, :, 38 + j] = inp["a_k_k"][l, j * 128:(j + 1) * 128]
            cp[l, :, 40 + j] = inp["a_k_a"][l, j * 128:(j + 1) * 128]
            cp[l, :, 42 + j] = inp["a_r_k"][l, j * 128:(j + 1) * 128]
    sh["colpar"] = cp
    for k in ("a_r_k", "a_lnx_g", "a_lnx_b", "b_sink", "ln1_g", "ln1_b", "ln2_g", "ln2_b"):
        sh[k] = f32(inp[k])
    for k in ("w_gate", "w_up", "w_down"):
        sh[k] = f32(np.asarray(inp[k])[:, :NE_DECL])
    kc = np.arange(64)[:, None]; qc = np.arange(64)[None, :]
    idx = np.clip(kc - qc, -15, 15) + 15
    rpb = np.asarray(inp["c_rpb"], np.float32)
    tab = rpb[:, :, :, idx]
    sh["rpbtab"] = f32(tab.transpose(0, 1, 3, 2, 4))
    sh["w_r"] = f32(np.concatenate([inp["w_rg"], inp["w_re"]], -1))
    sh["b_r"] = f32(np.concatenate([inp["b_rg"], inp["b_re"]], -1))
    sh.update(_consts())
    return sh


_PROG = {}


def kernel(**inputs):
    inp = {k: np.asarray(v) for k, v in inputs.items()}
    shared = _host_layout(inp)
    if "nc" not in _PROG:
        _PROG["nc"] = build_program()
    nc = _PROG["nc"]
    in_maps = []
    for b in range(NCORES):
        m = dict(shared)
        m["x"] = np.ascontiguousarray(inp["x"][b], dtype=np.float32)
        m["ctx"] = np.ascontiguousarray(inp["ctx"][b], dtype=np.float32)
        cc = np.zeros((128, 8, 2), np.float32)
        cc[:, :, 0] = np.asarray(inp["c"][b], np.float32).reshape(8, 128).T
        cc[:, :, 1] = np.asarray(inp["c_ctx"], np.float32).reshape(8, 128).T
        m["ccol"] = cc
        in_maps.append(m)
    res = run_bass_kernel_spmd(nc, in_maps, core_ids=list(range(NCORES)))
    _PROG["res"] = res
    out = np.stack([np.asarray(r["y"], dtype=np.float32) for r in res.results], 0)
    return out
```

```python
from contextlib import ExitStack
import numpy as np
import concourse.bass as bass
import concourse.mybir as mybir
from concourse.bass_utils import run_bass_kernel_spmd

F32 = mybir.dt.float32
BF16 = mybir.dt.bfloat16
ALU = mybir.AluOpType
AF = mybir.ActivationFunctionType
AX = mybir.AxisListType

DM = 1024; S = 2048; L = 256; T = S + L; NT = T // 128; DEPTH = 2
HD = 64; A_IN = 1184; N_IN = 2976; NE = 32; DE = 512
ALPHA = (2.0 * DEPTH) ** 0.25
SCALE = HD ** -0.5
C0 = float(np.exp(-0.5))
LN_EPS = 1e-6; GN_EPS = 64e-5
OFF_R, OFF_K, OFF_V, OFF_WD, OFF_AD, OFF_GD = 0, 256, 512, 768, 896, 1024
OFF_QB, OFF_KB, OFF_VB = 1184, 1568, 1696
OFF_QN, OFF_KN, OFF_VN = 1824, 2208, 2592
EPOCH = 30000
DEBUG = False
RW_STOP = 0
NCORES = 8
NE_DECL = 32
STOP = None


class Buf:
    __slots__ = ("w", "r")

    def __init__(self):
        self.w = {}
        self.r = {}


class Eng:
    def __init__(self, fw, name, eng, dma_only=False):
        self.fw = fw; self.name = name; self.eng = eng
        self.sem = None; self.cnt = 0; self.epoch = 0; self.seen = {}
        self.dma_only = dma_only
        if not dma_only:
            self.new_sem()

    def new_sem(self):
        self.sem = self.fw.nc.alloc_semaphore(f"p_{self.name}_{self.epoch}")
        self.cnt = 0
        self.epoch += 1
        self.fw.all_sems.append(self)


class FW:
    def __init__(self, nc, n_dma_sems=24):
        self.nc = nc
        self.all_sems = []
        self.pe = Eng(self, "pe", nc.tensor)
        self.act = Eng(self, "act", nc.scalar)
        self.dve = Eng(self, "dve", nc.vector)
        self.pool = Eng(self, "pool", nc.gpsimd)
        self.sp = Eng(self, "sp", nc.sync, dma_only=True)
        self.dma_sems = [[nc.alloc_semaphore(f"d{i}"), 0] for i in range(n_dma_sems)]
        self.dma_rr = 0
        self.sw_sems = [[nc.alloc_semaphore(f"w{i}"), 0] for i in range(8)]
        self.sw_rr = 0
        self.n_inst = 0

    def _wait(self, E, sem, val):
        k = sem.name
        if E.seen.get(k, 0) >= val:
            return
        E.eng.wait_ge(sem, val)
        E.seen[k] = val

    def _deps(self, E, reads, writes, skip_self=True):
        need = {}
        for b in reads:
            for k, sv in b.w.items():
                if k not in need or need[k][1] < sv[1]:
                    need[k] = sv
        for b in writes:
            for d in (b.w, b.r):
                for k, sv in d.items():
                    if k not in need or need[k][1] < sv[1]:
                        need[k] = sv
        me = E.sem.name if (skip_self and E.sem is not None and E.name == 'pe') else None
        for k, (s, v) in need.items():
            if k == me:
                continue
            self._wait(E, s, v)

    def _mark(self, sem, val, reads, writes):
        k = sem.name
        for b in reads:
            o = b.r.get(k)
            if o is None or o[1] < val:
                b.r[k] = (sem, val)
        for b in writes:
            o = b.w.get(k)
            if o is None or o[1] < val:
                b.w[k] = (sem, val)

    def op(self, E, reads, writes, fn):
        if E.cnt >= EPOCH:
            E.new_sem()
        self._deps(E, reads, writes)
        ins = fn()
        E.cnt += 1
        ins.then_inc(E.sem, 1)
        self._mark(E.sem, E.cnt, reads, writes)
        self.n_inst += 1
        return ins

    def dma(self, E, out, in_, reads, writes, **kw):
        if E.name == "pool":
            slot = self.sw_sems[self.sw_rr]
            self.sw_rr = (self.sw_rr + 1) % len(self.sw_sems)
        else:
            slot = self.dma_sems[self.dma_rr]
            self.dma_rr = (self.dma_rr + 1) % len(self.dma_sems)
        sem, cnt = slot
        if cnt > 0:
            self._wait(E, sem, cnt)
        self._deps(E, reads, writes, skip_self=False)
        ins = E.eng.dma_start(out=out, in_=in_, **kw)
        slot[1] = cnt + 16
        ins.then_inc(sem, 16)
        self._mark(sem, slot[1], reads, writes)
        self.n_inst += 1
        return ins

    def barrier(self):
        cur = []
        for X in (self.pe, self.act, self.dve, self.pool):
            if X.cnt > 0:
                cur.append((X.sem, X.cnt))
        for sem, cnt in self.dma_sems + self.sw_sems:
            if cnt > 0:
                cur.append((sem, cnt))
        for E in (self.pe, self.act, self.dve, self.pool, self.sp):
            for sem, val in cur:
                if E.sem is not None and sem.name == E.sem.name:
                    continue
                self._wait(E, sem, val)


def build_program():
    nc = bass.Bass("TRN2", target_bir_lowering=False)
    f = FW(nc)
    PE, ACT, DVE, POOL, SP = f.pe, f.act, f.dve, f.pool, f.sp

    def din(name, shape):
        return nc.dram_tensor(name, list(shape), F32, kind="ExternalInput").ap()

    def scr(name, shape):
        return nc.dram_tensor(name, list(shape), F32, kind=("ExternalOutput" if DEBUG else "Internal")).ap()

    x_in = din("x", [S, DM]); ctx_in = din("ctx", [L, DM]); ccol = din("ccol", [128, 8, 2])
    w_mod = din("w_mod", [DEPTH, DM, 6 * DM]); b_mod = din("b_mod", [DEPTH, 6 * DM])
    w_in = din("w_in", [DEPTH, DM, N_IN]); w_out = din("w_out", [DEPTH, DM, DM])
    a_shift = din("a_shift", [DEPTH, 3, A_IN])
    a_w_up = din("a_w_up", [DEPTH, 128, 256]); a_a_up = din("a_a_up", [DEPTH, 128, 256])
    a_g_up = din("a_g_up", [DEPTH, 160, 256])
    colpar = din("colpar", [DEPTH, 128, 48])
    a_r_k = din("a_r_k", [DEPTH, 256]); a_lnx_g = din("a_lnx_g", [DEPTH, 256]); a_lnx_b = din("a_lnx_b", [DEPTH, 256])
    b_sink = din("b_sink", [DEPTH, 6])
    rpbtab = din("rpbtab", [DEPTH, 6, 64, 15, 64])
    ln1_g = din("ln1_g", [DEPTH, DM]); ln1_b = din("ln1_b", [DEPTH, DM])
    ln2_g = din("ln2_g", [DEPTH, DM]); ln2_b = din("ln2_b", [DEPTH, DM])
    w_r = din("w_r", [DEPTH, DM, 36]); b_r = din("b_r", [DEPTH, 36])
    w_gate = din("w_gate", [DEPTH, NE_DECL, DM, DE]); w_up = din("w_up", [DEPTH, NE_DECL, DM, DE])
    w_down = din("w_down", [DEPTH, NE_DECL, DE, DM])
    cmasks = din("cmasks", [128, 4, 128]); cident = din("cident", [128, 128])
    cblock = din("cblock", [128, 128]); chsel = din("chsel", [128, 2])
    ccos = din("ccos", [64, S]); csin = din("csin", [64, S]); cropeP = din("cropeP", [64, 64])
    ccolvalid = din("ccolvalid", [128, 64]); cscanmask = din("cscanmask", [128, 512])
    y_out = nc.dram_tensor("y", [S, DM], F32, kind="ExternalOutput").ap()

    xs = scr("xs", [T, DM]); modrow = scr("modrow", [2, 6 * DM])
    zT = scr("zT", [N_IN, T]); vtok = scr("vtok", [T, 768]); omix = scr("omix", [T, DM])
    dbgh = scr("dbgh", [T, DM]) if DEBUG else None
    dbgs = scr("dbgs", [T, 4]) if DEBUG else None

    state = {"stop": False}

    def xsrc(l, i):
        if l == 0:
            return ctx_in[i * 128:(i + 1) * 128, :] if i < 2 else x_in[(i - 2) * 128:(i - 1) * 128, :]
        return xs[i * 128:(i + 1) * 128, :]

    top = ExitStack()
    uid = [0]

    def sbt(es, name, shape, dt=F32):
        uid[0] += 1
        return es.enter_context(nc.sbuf_tensor(f"{name}_{uid[0]}", list(shape), dt))

    def pst(es, name, shape, dt=F32):
        uid[0] += 1
        return es.enter_context(nc.psum_tensor(f"{name}_{uid[0]}", list(shape), dt))
    ident = sbt(top, "ident", [128, 128]); b_ident = Buf()
    identb = sbt(top, "identb", [128, 128], BF16); b_identb = Buf()
    f.dma(SP, ident[:], cident[:, :], [], [b_ident])
    f.op(DVE, [b_ident], [b_identb], lambda: nc.vector.tensor_copy(identb[:], ident[:]))

    def V(r, w, fn): return f.op(DVE, r, w, fn)
    def A(r, w, fn): return f.op(ACT, r, w, fn)
    def G(r, w, fn): return f.op(POOL, r, w, fn)
    def P(r, w, fn): return f.op(PE, r, w, fn)
    def D(out, in_, r, w, eng=None, **kw): return f.dma(eng or SP, out, in_, r, w, **kw)

    class Rot:
        def __init__(self, es, name, n, shape, dt=F32, psum=False):
            mk = pst if psum else sbt
            self.t = [mk(es, f"{name}{i}", shape, dt) for i in range(n)]
            self.b = [Buf() for _ in range(n)]
            self.i = 0

        def get(self):
            k = self.i; self.i = (self.i + 1) % len(self.t)
            return self.t[k], self.b[k]

    class Slots:
        def __init__(self, aps):
            self.t = list(aps); self.b = [Buf() for _ in self.t]; self.i = 0

        def get(self):
            k = self.i; self.i = (self.i + 1) % len(self.t)
            return self.t[k], self.b[k]

    def carve(es, name, nbanks, width):
        per = 512 // width
        views = [[None] * nbanks for _ in range(per)]
        bufs = [Buf() for _ in range(nbanks)]
        for i in range(nbanks):
            bk = pst(es, f"{name}{i}", [128, 512])
            for q in range(per):
                views[q][i] = bk[:, q * width:(q + 1) * width]
        sl = Slots([views[q][i] for q in range(per) for i in range(nbanks)])
        sl.b = [bufs[i] for q in range(per) for i in range(nbanks)]
        return sl

    def layer_norm_stats(es_tiles, src_ap, b_src, tagbufs):
        st, b_st, mv, b_mv, rs, b_rs = tagbufs
        V([b_src], [b_st], lambda: nc.vector.bn_stats(st[:, 0, :], src_ap[:, 0:512]))
        V([b_src], [b_st], lambda: nc.vector.bn_stats(st[:, 1, :], src_ap[:, 512:1024]))
        V([b_st], [b_mv], lambda: nc.vector.bn_aggr(mv[:], st[:].rearrange("p a b -> p (a b)")))
        A([b_mv], [b_rs], lambda: nc.scalar.activation(rs[:], mv[:, 1:2], AF.Sqrt, bias=eps_t[:, 0:1], scale=1.0))
        V([b_rs], [b_rs], lambda: nc.vector.reciprocal(rs[:], rs[:]))

    eps_t = sbt(top, "eps_t", [128, 4]); b_eps = Buf()
    G([], [b_eps], lambda: nc.gpsimd.memset(eps_t[:, 0:1], LN_EPS))
    G([], [b_eps], lambda: nc.gpsimd.memset(eps_t[:, 1:2], GN_EPS))
    G([], [b_eps], lambda: nc.gpsimd.memset(eps_t[:, 2:3], 1e-12))
    G([], [b_eps], lambda: nc.gpsimd.memset(eps_t[:, 3:4], 1.0))
    f.barrier()

    def phase_mod(l):
        with ExitStack() as es:
            cr = sbt(es, "cr", [128, 8, 2]); b_cr = Buf()
            cs = sbt(es, "cs", [128, 8, 2]); b_cs = Buf()
            D(cr[:], ccol[:, :, :], [], [b_cr])
            A([b_cr], [b_cs], lambda: nc.scalar.activation(cs[:], cr[:], AF.Silu))
            bm2 = sbt(es, "bm2", [2, 6 * DM]); b_bm2 = Buf()
            D(bm2[:], b_mod[l, :].partition_broadcast(2), [], [b_bm2])
            msb = sbt(es, "msb", [2, 6 * DM]); b_msb = Buf()
            wm = Rot(es, "wm", 3, [128, 3072])
            acc = [pst(es, f"macc{j}", [128, 512]) for j in range(6)]
            b_acc = [Buf() for _ in range(6)]
            for hf in range(2):
                for k in range(8):
                    wt, bw = wm.get()
                    D(wt[:], w_mod[l, k * 128:(k + 1) * 128, hf * 3072:(hf + 1) * 3072], [], [bw])
                    for j in range(6):
                        P([bw, b_cs], [b_acc[j]], lambda j=j, wt=wt, k=k: nc.tensor.matmul(
                            acc[j][0:2, :], cs[:, k, :], wt[:, j * 512:(j + 1) * 512], start=(k == 0), stop=(k == 7)))
                for j in range(6):
                    c0 = hf * 3072 + j * 512
                    V([b_acc[j], b_bm2], [b_msb], lambda j=j, c0=c0: nc.vector.tensor_tensor(
                        msb[:, c0:c0 + 512], acc[j][0:2, :], bm2[:, c0:c0 + 512], ALU.add))
            D(modrow[:, :], msb[:], [b_msb], [])
            f.barrier()

    def phase_ln_proj(l):
        with ExitStack() as es:
            hT = sbt(es, "hT", [128, 8, T], BF16); b_hT = [Buf() for _ in range(NT)]
            with ExitStack() as es2:
                mods = sbt(es2, "mods", [128, 2, 2 * DM]); b_mods = Buf()
                D(mods[:, 0, :], modrow[1, 0:2 * DM].partition_broadcast(128), [], [b_mods])
                D(mods[:, 1, :], modrow[0, 0:2 * DM].partition_broadcast(128), [], [b_mods])
                G([b_mods], [b_mods], lambda: nc.gpsimd.tensor_scalar(mods[:, :, DM:2 * DM], mods[:, :, DM:2 * DM], 1.0, None, ALU.add))
                xt = Rot(es2, "xt", 2, [128, DM]); xn = Rot(es2, "xn", 2, [128, DM]); hb = Rot(es2, "hb", 2, [128, DM], BF16)
                st = Rot(es2, "st", 2, [128, 2, 6]); mv = Rot(es2, "mv", 2, [128, 2]); rs = Rot(es2, "rs", 2, [128, 1])
                pT = Rot(es2, "pT", 2, [128, 8, 128], BF16, psum=True)
                for i in range(NT):
                    x_t, bx = xt.get(); n_t, bn = xn.get(); h_t, bh = hb.get()
                    s_t, bs = st.get(); m_t, bmv = mv.get(); r_t, br = rs.get(); p_t, bp = pT.get()
                    D(x_t[:], xsrc(l, i), [], [bx])
                    layer_norm_stats(None, x_t, bx, (s_t, bs, m_t, bmv, r_t, br))
                    V([bx, bmv, br], [bn], lambda x_t=x_t, n_t=n_t, m_t=m_t, r_t=r_t: nc.vector.tensor_scalar(
                        n_t[:], x_t[:], m_t[:, 0:1], r_t[:, 0:1], ALU.subtract, ALU.mult))
                    w = 0 if i < 2 else 1
                    G([bn, b_mods], [bn], lambda n_t=n_t, w=w: nc.gpsimd.tensor_tensor(n_t[:], n_t[:], mods[:, w, DM:2 * DM], ALU.mult))
                    G([bn, b_mods], [bn], lambda n_t=n_t, w=w: nc.gpsimd.tensor_tensor(n_t[:], n_t[:], mods[:, w, 0:DM], ALU.add))
                    A([bn], [bh], lambda n_t=n_t, h_t=h_t: nc.scalar.copy(h_t[:], n_t[:]))
                    if DEBUG:
                        D(dbgh[i * 128:(i + 1) * 128, :], n_t[:], [bn], [])
                        D(dbgs[i * 128:(i + 1) * 128, 0:2], m_t[:], [bmv], [], allow_slow_non_contiguous=True)
                        D(dbgs[i * 128:(i + 1) * 128, 2:3], r_t[:], [br], [], allow_slow_non_contiguous=True)
                    for k in range(8):
                        P([bh, b_identb], [bp], lambda k=k, h_t=h_t, p_t=p_t: nc.tensor.transpose(p_t[:, k, :], h_t[:, k * 128:(k + 1) * 128], identb[:]))
                    A([bp], [b_hT[i]], lambda i=i, p_t=p_t: nc.scalar.copy(hT[:, :, i * 128:(i + 1) * 128], p_t[:]))
                f.barrier()
            wb = sbt(es, "wb", [128, 8, N_IN], BF16); b_wb = [Buf() for _ in range(8)]
            for k in range(8):
                D(wb[:, k, :], w_in[l, k * 128:(k + 1) * 128, :], [], [b_wb[k]], eng=POOL)
            pp = Rot(es, "pp", 4, [128, 512], psum=True)
            stg = Rot(es, "stg", 4, [128, 512])
            cnt = 0
            for fc in range(24):
                fsz = 128 if fc < 23 else 32
                for tb in range(5):
                    t0 = tb * 512; tsz = min(512, T - t0)
                    p_t, bp = pp.get(); s_t, bs = stg.get()
                    rb = [b_hT[i] for i in range(t0 // 128, (t0 + tsz) // 128)]
                    for k in range(8):
                        P(rb + [b_wb[k]], [bp], lambda k=k, p_t=p_t, fc=fc, fsz=fsz, t0=t0, tsz=tsz: nc.tensor.matmul(
                            p_t[0:fsz, 0:tsz], wb[:, k, fc * 128:fc * 128 + fsz], hT[:, k, t0:t0 + tsz], start=(k == 0), stop=(k == 7)))
                    if cnt % 2 == 0:
                        A([bp], [bs], lambda p_t=p_t, s_t=s_t, fsz=fsz, tsz=tsz: nc.scalar.copy(s_t[0:fsz, 0:tsz], p_t[0:fsz, 0:tsz]))
                    else:
                        V([bp], [bs], lambda p_t=p_t, s_t=s_t, fsz=fsz, tsz=tsz: nc.vector.tensor_copy(s_t[0:fsz, 0:tsz], p_t[0:fsz, 0:tsz]))
                    cnt += 1
                    D(zT[fc * 128:fc * 128 + fsz, t0:t0 + tsz], s_t[0:fsz, 0:tsz], [bs], [])
            vst = Rot(es, "vst", 2, [128, 768])
            for i in range(NT):
                p1, bp1 = pp.get(); p2, bp2 = pp.get(); s_t, bs = vst.get()
                for (pt_, bpt_, c0_, c1_, w0_, wn_) in ((p1, bp1, 0, 256, OFF_V, 256), (p1, bp1, 256, 384, OFF_VB, 128), (p2, bp2, 0, 384, OFF_VN, 384)):
                    for k in range(8):
                        P([b_hT[i], b_wb[k]], [bpt_], lambda k=k: nc.tensor.matmul(
                            pt_[:, c0_:c1_], hT[:, k, i * 128:(i + 1) * 128], wb[:, k, w0_:w0_ + wn_], start=(k == 0), stop=(k == 7)))
                A([bp1], [bs], lambda p1=p1, s_t=s_t: nc.scalar.copy(s_t[:, 0:384], p1[:, 0:384]))
                V([bp2], [bs], lambda p2=p2, s_t=s_t: nc.vector.tensor_copy(s_t[:, 384:768], p2[:, 0:384]))
                D(vtok[i * 128:(i + 1) * 128, :], s_t[:], [bs], [])
            f.barrier()

    def conv_rows(dst, b_dst, src, b_src, wcol, b_w, c0, n):
        V([b_src, b_w], [b_dst], lambda: nc.vector.tensor_scalar(dst[:, 0:n], src[:, 0:n], wcol[:, 1:2], None, ALU.mult))
        segs = []
        for (a, b) in ((0, L), (L, T)):
            lo = max(a, c0); hi = min(b, c0 + n)
            if lo < hi:
                segs.append((lo - c0, hi - c0, lo == a, hi == b))
        return segs

    def phase_rwkv(l):
        with ExitStack() as es:
            cp = sbt(es, "cp", [128, 48]); b_cp = Buf()
            D(cp[:], colpar[l, :, :], [], [b_cp])
            wup = sbt(es, "wup", [128, 256]); aup = sbt(es, "aup", [128, 256]); b_up = Buf()
            D(wup[:], a_w_up[l, :, :], [], [b_up]); D(aup[:], a_a_up[l, :, :], [], [b_up])
            gup0 = sbt(es, "gup0", [128, 256]); gup1 = sbt(es, "gup1", [32, 256]); b_gup = Buf()
            D(gup0[:], a_g_up[l, 0:128, :], [], [b_gup]); D(gup1[:], a_g_up[l, 128:160, :], [], [b_gup])
            msk = sbt(es, "msk", [128, 4, 128]); b_msk = Buf()
            D(msk[:], cmasks[:, :, :], [], [b_msk])
            blk = sbt(es, "blk", [128, 128]); hsel = sbt(es, "hsel", [128, 2]); b_cst = Buf()
            D(blk[:], cblock[:, :], [], [b_cst]); D(hsel[:], chsel[:, :], [], [b_cst])
            scm = sbt(es, "scm", [128, 512]); D(scm[:], cscanmask[:, :], [], [b_cst])
            vsh = sbt(es, "vsh", [128, 3, 256]); b_vsh = Buf()
            D(vsh[:], a_shift[l, :, OFF_V:OFF_V + 256].partition_broadcast(128), [], [b_vsh])
            yacc = sbt(es, "yacc", [128, NT, 256]); b_yacc = [Buf() for _ in range(NT)]
            bacc = sbt(es, "bacc", [128, NT, 4]); b_bacc = [Buf() for _ in range(NT)]
            Vt = sbt(es, "Vt", [128, NT, 256]); b_Vt = [Buf() for _ in range(NT)]
            Vtb = sbt(es, "Vtb", [128, NT, 256], BF16)
            G([], b_yacc, lambda: nc.gpsimd.memset(yacc[:], 0.0))
            G([], b_bacc, lambda: nc.gpsimd.memset(bacc[:], 0.0))
            Mst = [[sbt(es, f"M{d}{j}", [128, 64]) for j in range(2)] for d in range(2)]
            b_M = [[Buf() for j in range(2)] for d in range(2)]
            for d in range(2):
                for j in range(2):
                    G([], [b_M[d][j]], lambda d=d, j=j: nc.gpsimd.memset(Mst[d][j][:], 0.0))
            with ExitStack() as es2:
                vr = Rot(es2, "vr", 6, [128, 256])
                for i in range(NT):
                    tp, bp_ = vr.get(); tc, bc_ = vr.get(); tn, bn_ = vr.get()
                    first = i in (0, 2); last = i in (1, NT - 1)
                    r0 = i * 128
                    D(tc[:], vtok[r0:r0 + 128, 0:256], [], [bc_])
                    if first:
                        G([], [bp_], lambda tp=tp: nc.gpsimd.memset(tp[:], 0.0))
                        D(tp[1:128, :], vtok[r0:r0 + 127, 0:256], [], [bp_])
                    else:
                        D(tp[:], vtok[r0 - 1:r0 + 127, 0:256], [], [bp_])
                    if last:
                        G([], [bn_], lambda tn=tn: nc.gpsimd.memset(tn[:], 0.0))
                        D(tn[0:127, :], vtok[r0 + 1:r0 + 128, 0:256], [], [bn_])
                    else:
                        D(tn[:], vtok[r0 + 1:r0 + 129, 0:256], [], [bn_])
                    V([bc_, b_vsh], [b_Vt[i]], lambda i=i, tc=tc: nc.vector.tensor_tensor(Vt[:, i, :], tc[:], vsh[:, 1, :], ALU.mult))
                    G([bp_, b_vsh], [bp_], lambda tp=tp: nc.gpsimd.tensor_tensor(tp[:], tp[:], vsh[:, 0, :], ALU.mult))
                    G([bn_, b_vsh], [bn_], lambda tn=tn: nc.gpsimd.tensor_tensor(tn[:], tn[:], vsh[:, 2, :], ALU.mult))
                    V([bp_, b_Vt[i]], [b_Vt[i]], lambda i=i, tp=tp: nc.vector.tensor_tensor(Vt[:, i, :], Vt[:, i, :], tp[:], ALU.add))
                    V([bn_, b_Vt[i]], [b_Vt[i]], lambda i=i, tn=tn: nc.vector.tensor_tensor(Vt[:, i, :], Vt[:, i, :], tn[:], ALU.add))
                    A([b_Vt[i]], [b_Vt[i]], lambda i=i: nc.scalar.copy(Vtb[:, i, :], Vt[:, i, :]))
                f.barrier()
            if RW_STOP == 1:
                return

            blocks = [(0, 256)] + [(256 + 256 * q, 256) for q in range(8)]
            order = {0: list(range(9)), 1: [0] + list(range(8, 0, -1))}
            with ExitStack() as es2:
                BW = 256
                rawS = [[(sbt(es2, f"rawS{d}{q}", [128, BW + 2]), Buf()) for q in range(2)] for d in range(2)]
                rawC = [[[(sbt(es2, f"rawC{d}{j}{q}", [128, BW + 2]), Buf()) for q in range(2)] for j in range(2)] for d in range(2)]
                tmpC = [[[(sbt(es2, f"tmpC{d}{j}{q}", [128, BW]), Buf()) for q in range(2)] for j in range(2)] for d in range(2)]
                rT = [[sbt(es2, f"rT{d}{j}", [128, BW]) for j in range(2)] for d in range(2)]
                kT = [[sbt(es2, f"kT{d}{j}", [128, BW]) for j in range(2)] for d in range(2)]
                kkT = [[sbt(es2, f"kkT{d}{j}", [128, BW]) for j in range(2)] for d in range(2)]
                twd = [sbt(es2, f"twd{d}", [128, BW]) for d in range(2)]
                tad = [sbt(es2, f"tad{d}", [128, BW]) for d in range(2)]
                names = ["t0", "t1", "t2", "bb", "kd", "p1", "e1"]
                rows = {nm: [[sbt(es2, f"{nm}{d}{j}", [128, BW]) for j in range(2)] for d in range(2)] for nm in names}
                q12 = [[sbt(es2, f"q12{d}{j}", [128, 2, 2, 128]) for j in range(2)] for d in range(2)]
                tot = [[sbt(es2, f"tot{d}{j}", [128, 4]) for j in range(2)] for d in range(2)]
                gC = [[sbt(es2, f"gC{d}{j}", [128, 4]) for j in range(2)] for d in range(2)]
                b_row = [[Buf() for j in range(2)] for d in range(2)]
                b_sh = [Buf() for d in range(2)]
                bnames = ["p1b", "p2b", "e1b", "e2b"]
                brows = {nm: [[sbt(es2, f"{nm}{d}{j}", [128, BW], BF16) for j in range(2)] for d in range(2)] for nm in bnames}
                q12b = [[sbt(es2, f"q12b{d}{j}", [128, 2, 2, 128], BF16) for j in range(2)] for d in range(2)]
                Mb = [[sbt(es2, f"Mb{d}{j}", [128, 64], BF16) for j in range(2)] for d in range(2)]
                for d in range(2):
                    for j in range(2):
                        G([], [b_M[d][j]], lambda d=d, j=j: nc.gpsimd.memset(Mb[d][j][:], 0.0))
                bankX = pst(es2, "bankX", [128, 512]); b_bankX = Buf()
                pM = [[bankX[:, (2 * d + j) * 64:(2 * d + j) * 64 + 64] for j in range(2)] for d in range(2)]
                b_pM = [[b_bankX for j in range(2)] for d in range(2)]
                ps5 = Slots([bankX[:, 256:512]]); ps5.b = [b_bankX]
                pN = Rot(es2, "pN", 1, [128, 512], psum=True)
                pW = Rot(es2, "pW", 2, [128, 1024], psum=True)
                pQ = carve(es2, "pQ", 1, 256)
                pEbank = pst(es2, "pEb", [128, 1024], BF16); b_pE = Buf()
                pE = Slots([pEbank[:, q * 256:(q + 1) * 256] for q in range(4)]); pE.b = [b_pE] * 4
                NCH = 16
                cA1 = [sbt(es2, f"cA1_{k}", [128, 256], BF16) for k in range(NCH)]
                cA2 = [sbt(es2, f"cA2_{k}", [128, 256], BF16) for k in range(NCH)]
                cbA = [(Buf(), Buf()) for k in range(NCH)]
                gPR = [Rot(es2, f"gPR{g}_", 3, [128, 4, 256], BF16) for g in range(4)]
                gPT = [Rot(es2, f"gPT{g}_", 3, [128, 4, 128], BF16) for g in range(4)]
                gSq = [Rot(es2, f"gSq{g}_", 2, [128, 4, 64], BF16) for g in range(4)]
                cE = [sbt(es2, f"cE_{k}", [128, 256], BF16) for k in range(NCH // 2)]
                cbE = [Buf() for k in range(NCH // 2)]

                def load_conv(dst, b_dst, frow, nrows, rawt, tap_chunk, c0, n):
                    rt, br = rawt
                    seq_lo, seq_hi = (0, L) if c0 < L else (L, T)
                    lo = max(seq_lo, c0 - 1); hi = min(seq_hi, c0 + n + 1)
                    if lo > c0 - 1 or hi < c0 + n + 1:
                        G([], [br], lambda rt=rt: nc.gpsimd.memset(rt[:], 0.0))
                    D(rt[0:nrows, lo - (c0 - 1):hi - (c0 - 1)], zT[frow:frow + nrows, lo:hi], [], [br])
                    w = lambda tap: cp[0:nrows, tap_chunk * 3 + tap:tap_chunk * 3 + tap + 1]
                    V([br, b_cp], [b_dst], lambda: nc.vector.tensor_scalar(dst[0:nrows, 0:n], rt[0:nrows, 1:n + 1], w(1), None, ALU.mult))
                    V([br, b_cp, b_dst], [b_dst], lambda: nc.vector.scalar_tensor_tensor(dst[0:nrows, 0:n], rt[0:nrows, 0:n], w(0), dst[0:nrows, 0:n], ALU.mult, ALU.add))
                    V([br, b_cp, b_dst], [b_dst], lambda: nc.vector.scalar_tensor_tensor(dst[0:nrows, 0:n], rt[0:nrows, 2:n + 2], w(2), dst[0:nrows, 0:n], ALU.mult, ALU.add))

                def prep_shared(d, bi):
                    c0, n = blocks[bi]
                    load_conv(twd[d], b_sh[d], OFF_WD, 128, rawS[d][0], 6, c0, n)
                    A([b_sh[d]], [b_sh[d]], lambda: nc.scalar.activation(twd[d][:, 0:n], twd[d][:, 0:n], AF.Tanh))
                    load_conv(tad[d], b_sh[d], OFF_AD, 128, rawS[d][1], 7, c0, n)

                def prep_combo(d, j, bi):
                    c0, n = blocks[bi]
                    nch = n // 128
                    hp = slice(64 * d, 64 * d + 64)
                    tmpi = [0]

                    def tmp_get():
                        t = tmpC[d][j][tmpi[0] % 2]; tmpi[0] += 1
                        return t
                    if True:
                        R = {nm: rows[nm][d][j] for nm in names}
                        br_ = b_row[d][j]
                        load_conv(rT[d][j], br_, OFF_R + 128 * j, 128, rawC[d][j][0], j, c0, n)
                        yield
                        load_conv(kT[d][j], br_, OFF_K + 128 * j, 128, rawC[d][j][1], 2 + j, c0, n)
                        yield
                        V([br_, b_cp], [br_], lambda: nc.vector.tensor_scalar(kkT[d][j][:, 0:n], kT[d][j][:, 0:n], cp[:, 38 + j:39 + j], None, ALU.mult))
                        tq, btq = tmp_get()
                        G([br_], [btq], lambda: nc.gpsimd.tensor_tensor(tq[:, 0:n], kkT[d][j][:, 0:n], kkT[d][j][:, 0:n], ALU.mult))
                        p5, bp5 = ps5.get()
                        P([btq, b_cst], [bp5], lambda: nc.tensor.matmul(p5[:, 0:n], blk[:], tq[:, 0:n], start=True, stop=True))
                        A([bp5], [btq], lambda: nc.scalar.activation(tq[:, 0:n], p5[:, 0:n], AF.Sqrt, bias=eps_t[:, 2:3], scale=1.0))
                        yield
                        V([btq], [btq], lambda: nc.vector.reciprocal(tq[:, 0:n], tq[:, 0:n]))
                        V([btq, br_], [br_], lambda: nc.vector.tensor_tensor(kkT[d][j][:, 0:n], kkT[d][j][:, 0:n], tq[:, 0:n], ALU.mult))
                        yield
                        p5, bp5 = ps5.get()
                        P([b_sh[d], b_up], [bp5], lambda: nc.tensor.matmul(p5[:, 0:n], wup[hp, j * 128:(j + 1) * 128], twd[d][hp, 0:n], start=True, stop=True))
                        A([bp5, b_cp], [br_], lambda: nc.scalar.activation(R["t0"][:, 0:n], p5[:, 0:n], AF.Sigmoid, bias=cp[:, 30 + d * 2 + j:31 + d * 2 + j], scale=1.0))
                        yield
                        p5, bp5 = ps5.get()
                        P([b_sh[d], b_up], [bp5], lambda: nc.tensor.matmul(p5[:, 0:n], aup[hp, j * 128:(j + 1) * 128], tad[d][hp, 0:n], start=True, stop=True))
                        A([bp5, b_cp], [br_], lambda: nc.scalar.activation(R["bb"][:, 0:n], p5[:, 0:n], AF.Sigmoid, bias=cp[:, 34 + d * 2 + j:35 + d * 2 + j], scale=1.0))
                        yield
                        V([br_, b_cp], [br_], lambda: nc.vector.tensor_scalar(R["kd"][:, 0:n], R["bb"][:, 0:n], -1.0, cp[:, 40 + j:41 + j], ALU.add, ALU.mult))
                        V([br_], [br_], lambda: nc.vector.scalar_tensor_tensor(R["kd"][:, 0:n], R["kd"][:, 0:n], 1.0, kT[d][j][:, 0:n], ALU.add, ALU.mult))
                        V([br_], [br_], lambda: nc.vector.tensor_tensor(R["bb"][:, 0:n], R["bb"][:, 0:n], kkT[d][j][:, 0:n], ALU.mult))
                        yield
                        tq, btq = tmp_get()
                        V([br_, b_cp], [btq], lambda: nc.vector.scalar_tensor_tensor(tq[:, 0:n], rT[d][j][:, 0:n], cp[:, 42 + j:43 + j], R["kd"][:, 0:n], ALU.mult, ALU.mult))
                        for c in range(nch):
                            ti = c0 // 128 + c
                            pq, bpq = pQ.get()
                            P([btq, b_cst], [bpq], lambda c=c, pq=pq: nc.tensor.matmul(pq[:, 0:2], tq[:, c * 128:(c + 1) * 128], hsel[:], start=True, stop=True))
                            V([bpq, b_bacc[ti]], [b_bacc[ti]], lambda ti=ti, pq=pq: nc.vector.tensor_tensor(bacc[:, ti, 2 * j:2 * j + 2], bacc[:, ti, 2 * j:2 * j + 2], pq[:, 0:2], ALU.add))
                        yield
                        V([br_, b_cst], [br_], lambda: nc.vector.tensor_tensor_scan(R["t1"][:, 0:n], scm[:, 0:n], R["t0"][:, 0:n], 0.0, ALU.mult, ALU.add))
                        V([br_], [br_], lambda: nc.vector.tensor_tensor(R["t0"][:, 0:n], R["t1"][:, 0:n], R["t0"][:, 0:n], ALU.subtract))
                        V([br_], [br_], lambda: nc.vector.tensor_copy(tot[d][j][:, 0:nch], R["t1"][:, 127:n:128]))
                        A([br_], [br_], lambda: nc.scalar.activation(gC[d][j][:, 0:nch], tot[d][j][:, 0:nch], AF.Exp, scale=-C0))
                        yield
                        totb = tot[d][j][:, 0:nch].unsqueeze(2).broadcast_to([128, nch, 128])
                        v3 = lambda ap: ap[:, 0:n].rearrange("p (c t) -> p c t", t=128)
                        q1v = q12[d][j][:, 0:nch, 0, :]; q2v = q12[d][j][:, 0:nch, 1, :]
                        if d == 0:
                            V([br_], [br_], lambda: nc.vector.tensor_tensor(v3(R["t2"]), totb, v3(R["t1"]), ALU.subtract))
                            A([br_], [br_], lambda: nc.scalar.activation(q1v, v3(R["t0"]), AF.Exp, scale=-C0))
                            A([br_], [br_], lambda: nc.scalar.activation(q2v, v3(R["t1"]), AF.Exp, scale=-C0))
                            A([br_], [br_], lambda: nc.scalar.activation(R["p1"][:, 0:n], R["t1"][:, 0:n], AF.Exp, scale=C0))
                            A([br_], [br_], lambda: nc.scalar.activation(R["e1"][:, 0:n], R["t2"][:, 0:n], AF.Exp, scale=-C0))
                        else:
                            V([br_], [br_], lambda: nc.vector.tensor_tensor(v3(R["t2"]), totb, v3(R["t1"]), ALU.subtract))
                            V([br_], [br_], lambda: nc.vector.tensor_tensor(v3(R["t1"]), totb, v3(R["t0"]), ALU.subtract))
                            A([br_], [br_], lambda: nc.scalar.activation(q1v, v3(R["t2"]), AF.Exp, scale=-C0))
                            A([br_], [br_], lambda: nc.scalar.activation(q2v, v3(R["t1"]), AF.Exp, scale=-C0))
                            A([br_], [br_], lambda: nc.scalar.activation(R["p1"][:, 0:n], R["t1"][:, 0:n], AF.Exp, scale=C0))
                            A([br_], [br_], lambda: nc.scalar.activation(R["e1"][:, 0:n], R["t0"][:, 0:n], AF.Exp, scale=-C0))
                        yield
                        BR = {nm: brows[nm][d][j] for nm in bnames}
                        q1b = q12b[d][j][:, 0:nch, 0, :]; q2b = q12b[d][j][:, 0:nch, 1, :]
                        V([br_], [br_], lambda: nc.vector.tensor_tensor(q1b, q1v, v3(kkT[d][j]), ALU.mult))
                        G([br_], [br_], lambda: nc.gpsimd.tensor_tensor(q2b, q2v, v3(rT[d][j]), ALU.mult))
                        yield
                        V([br_], [br_], lambda: nc.vector.tensor_tensor(BR["p2b"][:, 0:n], R["p1"][:, 0:n], R["kd"][:, 0:n], ALU.mult))
                        G([br_], [br_], lambda: nc.gpsimd.tensor_tensor(BR["p1b"][:, 0:n], R["p1"][:, 0:n], R["bb"][:, 0:n], ALU.mult))
                        V([br_], [br_], lambda: nc.vector.tensor_tensor(BR["e2b"][:, 0:n], R["e1"][:, 0:n], R["bb"][:, 0:n], ALU.mult))
                        G([br_], [br_], lambda: nc.gpsimd.tensor_tensor(BR["e1b"][:, 0:n], R["e1"][:, 0:n], R["kd"][:, 0:n], ALU.mult))

                def score_chain(k, d, j, hh, bi, c):
                    cs_ = slice(c * 128, (c + 1) * 128)
                    br_ = b_row[d][j]
                    BR = {nm: brows[nm][d][j] for nm in bnames}
                    mT = msk[:, 0:2, :] if d == 0 else msk[:, 2:4, :]
                    mN = msk[:, 2, :] if d == 0 else msk[:, 0, :]
                    hp = slice(64 * hh, 64 * hh + 64)
                    ke = k // 2; gi = k // 4; m = k % 4
                    if hh == 0:
                        pe_, bpe = pE.get()
                        P([br_, b_identb], [bpe], lambda: nc.tensor.transpose(pe_[:, 0:128], BR["e1b"][:, cs_], identb[:]))
                        P([br_, b_identb], [bpe], lambda: nc.tensor.transpose(pe_[:, 128:256], BR["e2b"][:, cs_], identb[:]))
                        A([bpe], [cbE[ke]], lambda: nc.scalar.copy(cE[ke][:], pe_[:]))
                    yield
                    q12c = q12b[d][j][hp, c, :, :].rearrange("p a t -> p (a t)")
                    q1c = q12b[d][j][hp, c, 0, :]
                    A1m, A2m = cA1[k], cA2[k]; bA1, bA2 = cbA[k]
                    a1f, ba1 = pW.get(); a1 = a1f[:, 0:256]
                    P([br_], [ba1], lambda: nc.tensor.matmul(a1, BR["p1b"][hp, cs_], q12c, start=True, stop=True))
                    V([ba1, b_msk], [bA1], lambda: nc.vector.tensor_tensor(A1m[:].rearrange("p (a t) -> p a t", a=2), a1.rearrange("p (a t) -> p a t", a=2), mT, ALU.mult))
                    PR, bPR = gcur[gi][0]; PT, bPT = gcur[gi][1]
                    V([ba1, b_msk], [bPR], lambda: nc.vector.scalar_tensor_tensor(PR[:, m, 0:128], a1[:, 0:128], -1.0, mT[:, 0, :], ALU.mult, ALU.mult))
                    A([b_identb], [bPR], lambda: nc.scalar.copy(PR[:, m, 128:256], identb[:]))
                    yield
                    a2f, ba2 = pW.get(); a2 = a2f[:, 0:256]
                    P([br_], [ba2], lambda: nc.tensor.matmul(a2, BR["p2b"][hp, cs_], q12c, start=True, stop=True))
                    V([ba2, b_msk], [bA2], lambda: nc.vector.tensor_tensor(A2m[:].rearrange("p (a t) -> p a t", a=2), a2.rearrange("p (a t) -> p a t", a=2), mT, ALU.mult))
                    yield
                    anf, ban = pN.get(); an = anf[:, 0:128]
                    P([br_], [ban], lambda: nc.tensor.matmul(an, q1c, BR["p1b"][hp, cs_], start=True, stop=True))
                    V([ban, b_msk], [bPT], lambda: nc.vector.scalar_tensor_tensor(PT[:, m, :], an, -1.0, mN, ALU.mult, ALU.mult))
                    yield

                def neumann_group(gi):
                    (PR, bPR), (PT, bPT) = gcur[gi]
                    for lev in range(7):
                        if lev < 6:
                            w1, bw1 = pW.get()
                            w1v = w1[:].rearrange("p (m c) -> p m c", c=256)
                            for m in range(4):
                                P([bPT, bPR], [bw1], lambda: nc.tensor.matmul(w1[:, m * 256:(m + 1) * 256], PT[:, m, :], PR[:, m, :], start=True, stop=True))
                            PRn, bPRn = gPR[gi].get()
                            A([bw1], [bPRn], lambda: nc.scalar.copy(PRn[:, :, 0:128], w1v[:, :, 0:128]))
                            V([bw1, bPR], [bPRn], lambda: nc.vector.tensor_tensor(PRn[:, :, 128:256], w1v[:, :, 128:256], PR[:, :, 128:256], ALU.add))
                            w2, bw2 = pN.get()
                            for m in range(4):
                                P([bPT, bPR], [bw2], lambda: nc.tensor.matmul(w2[:, m * 128:(m + 1) * 128], PR[:, m, 0:128], PT[:, m, :], start=True, stop=True))
                            PTn, bPTn = gPT[gi].get()
                            w2v = w2[:].rearrange("p (m c) -> p m c", c=128)
                            if (gi + lev) % 2 == 0:
                                A([bw2], [bPTn], lambda: nc.scalar.copy(PTn[:], w2v))
                            else:
                                V([bw2], [bPTn], lambda: nc.vector.tensor_copy(PTn[:], w2v))
                            PR, bPR, PT, bPT = PRn, bPRn, PTn, bPTn
                        else:
                            w2, bw2 = pN.get()
                            for m in range(4):
                                P([bPT, bPR], [bw2], lambda: nc.tensor.matmul(w2[:, m * 128:(m + 1) * 128], PT[:, m, :], PR[:, m, 128:256], start=True, stop=True))
                            PRn, bPRn = gPR[gi].get()
                            w2v = w2[:].rearrange("p (m c) -> p m c", c=128)
                            V([bw2, bPR], [bPRn], lambda: nc.vector.tensor_tensor(PRn[:, :, 128:256], w2v, PR[:, :, 128:256], ALU.add))
                            PR, bPR = PRn, bPRn
                        yield
                    gR[gi] = (PR, bPR)

                def seq_group(gi, d, bi, c):
                    c0, n = blocks[bi]
                    ti = c0 // 128 + c
                    PRf, bR = gR[gi]
                    hps = [slice(0, 64), slice(64, 128)]
                    ppg, bpp = pQ.get()
                    for m in range(4):
                        j, hh = m // 2, m % 2; k = 4 * gi + m; hp = hps[hh]
                        hcol = slice((2 * j + hh) * 64, (2 * j + hh) * 64 + 64)
                        P([b_row[d][j], b_M[d][j]], [bpp], lambda: nc.tensor.matmul(ppg[:, m * 64:(m + 1) * 64], q12b[d][j][hp, c, 0, :], Mb[d][j][hp, :], start=True, stop=False))
                        P([cbA[k][1], b_Vt[ti]], [bpp], lambda: nc.tensor.matmul(ppg[:, m * 64:(m + 1) * 64], cA2[k][:, 0:128], Vtb[:, ti, hcol], start=False, stop=True))
                    Psb, bPsb = gSq[gi].get()
                    A([bpp], [bPsb], lambda: nc.scalar.copy(Psb[:], ppg[:].rearrange("p (m c) -> p m c", c=64)))
                    yield
                    upg, bup = pQ.get()
                    for m in range(4):
                        P([bR, bPsb], [bup], lambda: nc.tensor.matmul(upg[:, m * 64:(m + 1) * 64], PRf[:, m, 128:256], Psb[:, m, :], start=True, stop=True))
                    nU, bnU = gSq[gi].get()
                    A([bup], [bnU], lambda: nc.scalar.mul(nU[:], upg[:].rearrange("p (m c) -> p m c", c=64), -1.0))
                    yield
                    ypg, byp = pQ.get()
                    for m in range(4):
                        j, hh = m // 2, m % 2; k = 4 * gi + m; hp = hps[hh]
                        hcol = slice((2 * j + hh) * 64, (2 * j + hh) * 64 + 64)
                        P([b_row[d][j], b_M[d][j]], [byp], lambda: nc.tensor.matmul(ypg[:, m * 64:(m + 1) * 64], q12b[d][j][hp, c, 1, :], Mb[d][j][hp, :], start=True, stop=False))
                        P([cbA[k][1], b_Vt[ti]], [byp], lambda: nc.tensor.matmul(ypg[:, m * 64:(m + 1) * 64], cA2[k][:, 128:256], Vtb[:, ti, hcol], start=False, stop=False))
                        P([cbA[k][0], bnU], [byp], lambda: nc.tensor.matmul(ypg[:, m * 64:(m + 1) * 64], cA1[k][:, 128:256], nU[:, m, :], start=False, stop=True))
                    V([byp, b_yacc[ti]], [b_yacc[ti]], lambda: nc.vector.tensor_tensor(yacc[:, ti, :], yacc[:, ti, :], ypg[:], ALU.add))
                    yield
                    for m in range(4):
                        j, hh = m // 2, m % 2; k = 4 * gi + m; hp = hps[hh]
                        hcol = slice((2 * j + hh) * 64, (2 * j + hh) * 64 + 64)
                        E_, bE = cE[k // 2], cbE[k // 2]
                        pm = pM[d][j]
                        P([bE, b_Vt[ti]], [b_pM[d][j]], lambda: nc.tensor.matmul(pm[hp, :], E_[:, 64 * hh:64 * hh + 64], Vtb[:, ti, hcol], start=True, stop=False))
                        P([bE, bnU], [b_pM[d][j]], lambda: nc.tensor.matmul(pm[hp, :], E_[:, 128 + 64 * hh:128 + 64 * hh + 64], nU[:, m, :], start=False, stop=True))
                    for j in range(2):
                        V([b_pM[d][j], b_M[d][j], b_row[d][j]], [b_M[d][j]], lambda: nc.vector.scalar_tensor_tensor(
                            Mst[d][j][:], Mst[d][j][:], gC[d][j][:, c:c + 1], pM[d][j][:], ALU.mult, ALU.add))
                        G([b_M[d][j]], [b_M[d][j]], lambda: nc.gpsimd.tensor_copy(Mb[d][j][:], Mst[d][j][:]))
                    yield

                def run_rr(gens):
                    gens = list(gens)
                    while gens:
                        nxt = []
                        for g in gens:
                            try:
                                next(g); nxt.append(g)
                            except StopIteration:
                                pass
                        gens = nxt

                gR = {}; gcur = {}
                for step in range(9):
                    if RW_STOP in (2, 3) and step > 0:
                        break
                    for d in range(2):
                        prep_shared(d, order[d][step])
                    run_rr([prep_combo(d, j, order[d][step]) for d in range(2) for j in range(2)])
                    if RW_STOP == 2:
                        break
                    chains = []; groups = []
                    for cc in range(2):
                        for d in range(2):
                            bi = order[d][step]
                            c = cc if d == 0 else 1 - cc
                            groups.append((len(groups), d, bi, c))
                            for j in range(2):
                                for hh in range(2):
                                    chains.append((len(chains), d, j, hh, bi, c))
                    for gi in range(4):
                        gcur[gi] = (gPR[gi].get(), gPT[gi].get())
                    run_rr([score_chain(*ch) for ch in chains])
                    run_rr([neumann_group(gi) for gi in range(4)])
                    for cc in range(2):
                        run_rr([seq_group(*g) for g in groups[cc * 2:(cc + 1) * 2]])
                f.barrier()
            if RW_STOP in (2, 3):
                return

            with ExitStack() as es2:
                gs0 = sbt(es2, "gs0", [128, T]); gs1 = sbt(es2, "gs1", [32, T]); b_gs = Buf()
                rw = sbt(es2, "rw", [128, T + 2]); b_rw = Buf()
                for (dst, frow, nrows, tapc) in ((gs0, OFF_GD, 128, 8), (gs1, OFF_GD + 128, 32, 9)):
                    G([], [b_rw], lambda: nc.gpsimd.memset(rw[:], 0.0))
                    D(rw[0:nrows, 1:T + 1], zT[frow:frow + nrows, :], [], [b_rw])
                    w = lambda tap, tapc=tapc, nrows=nrows: cp[0:nrows, tapc * 3 + tap:tapc * 3 + tap + 1]
                    V([b_rw, b_cp], [b_gs], lambda: nc.vector.tensor_scalar(dst[0:nrows, :], rw[0:nrows, 1:T + 1], w(1), None, ALU.mult))
                    for (a, b) in ((0, L), (L, T)):
                        V([b_rw, b_cp, b_gs], [b_gs], lambda a=a, b=b: nc.vector.scalar_tensor_tensor(
                            dst[0:nrows, a + 1:b], rw[0:nrows, a + 1:b], w(0), dst[0:nrows, a + 1:b], ALU.mult, ALU.add))
                        V([b_rw, b_cp, b_gs], [b_gs], lambda a=a, b=b: nc.vector.scalar_tensor_tensor(
                            dst[0:nrows, a:b - 1], rw[0:nrows, a + 2:b + 1], w(2), dst[0:nrows, a:b - 1], ALU.mult, ALU.add))
                    A([b_gs], [b_gs], lambda: nc.scalar.activation(dst[0:nrows, :], dst[0:nrows, :], AF.Sigmoid))
                gb = sbt(es2, "gb", [128, 2, 256]); b_gb = Buf()
                D(gb[:, 0, :], a_lnx_g[l, :].partition_broadcast(128), [], [b_gb])
                D(gb[:, 1, :], a_lnx_b[l, :].partition_broadcast(128), [], [b_gb])
                pg = carve(es2, "pg", 1, 256)
                st4 = Rot(es2, "st4", 2, [128, 4, 6]); mv4 = Rot(es2, "mv4", 2, [128, 4, 2]); rs4 = Rot(es2, "rs4", 2, [128, 4])
                yn = Rot(es2, "yn", 2, [128, 256]); oo = Rot(es2, "oo", 2, [128, 256])
                for i in range(NT):
                    g_p, bg = pg.get()
                    P([b_gs, b_gup], [bg], lambda: nc.tensor.matmul(g_p[:], gs0[:, i * 128:(i + 1) * 128], gup0[:], start=True, stop=False))
                    P([b_gs, b_gup], [bg], lambda: nc.tensor.matmul(g_p[:], gs1[0:32, i * 128:(i + 1) * 128], gup1[:], start=False, stop=True))
                    s4, bs4 = st4.get(); m4, bm4 = mv4.get(); r4, br4 = rs4.get(); y_n, byn = yn.get(); o_t, bo = oo.get()
                    for h in range(4):
                        V([b_yacc[i]], [bs4], lambda h=h: nc.vector.bn_stats(s4[:, h, :], yacc[:, i, h * 64:(h + 1) * 64]))
                    for h in range(4):
                        V([bs4], [bm4], lambda h=h: nc.vector.bn_aggr(m4[:, h, :], s4[:, h, :]))
                    A([bm4], [br4], lambda: nc.scalar.activation(r4[:], m4[:, :, 1], AF.Sqrt, bias=eps_t[:, 1:2], scale=1.0))
                    V([br4], [br4], lambda: nc.vector.reciprocal(r4[:], r4[:]))
                    for h in range(4):
                        V([b_yacc[i], bm4, br4], [byn], lambda h=h: nc.vector.tensor_scalar(
                            y_n[:, h * 64:(h + 1) * 64], yacc[:, i, h * 64:(h + 1) * 64], m4[:, h, 0:1], r4[:, h:h + 1], ALU.subtract, ALU.mult))
                    G([byn, b_gb], [byn], lambda: nc.gpsimd.tensor_tensor(y_n[:], y_n[:], gb[:, 0, :], ALU.mult))
                    G([byn, b_gb], [byn], lambda: nc.gpsimd.tensor_tensor(y_n[:], y_n[:], gb[:, 1, :], ALU.add))
                    for h in range(4):
                        V([byn, b_bacc[i], b_Vt[i]], [byn], lambda h=h: nc.vector.scalar_tensor_tensor(
                            y_n[:, h * 64:(h + 1) * 64], Vt[:, i, h * 64:(h + 1) * 64], bacc[:, i, h:h + 1], y_n[:, h * 64:(h + 1) * 64], ALU.mult, ALU.add))
                    V([byn, bg], [bo], lambda: nc.vector.tensor_tensor(o_t[:], y_n[:], g_p[:], ALU.mult))
                    D(omix[i * 128:(i + 1) * 128, 0:256], o_t[:], [bo], [])
                f.barrier()

    def phase_attn_b(l):
        with_ctx = l < DEPTH - 1
        with ExitStack() as es:
            cosT = sbt(es, "cosT", [64, S]); sinT = sbt(es, "sinT", [64, S]); rPT = sbt(es, "rPT", [64, 64]); b_c = Buf()
            D(cosT[:], ccos[:, :], [], [b_c]); D(sinT[:], csin[:, :], [], [b_c]); D(rPT[:], cropeP[:, :], [], [b_c])
            mkf = sbt(es, "mkf", [128, 4, 128]); mkb = sbt(es, "mkb", [128, 4, 128], BF16); b_mk = Buf()
            D(mkf[:], cmasks[:, :, :], [], [b_mk])
            V([b_mk], [b_mk], lambda: nc.vector.tensor_copy(mkb[:], mkf[:]))
            esk = sbt(es, "esk", [128, 6]); b_esk = Buf()
            D(esk[:], b_sink[l, :].partition_broadcast(128), [], [b_esk])
            A([b_esk], [b_esk], lambda: nc.scalar.activation(esk[:], esk[:], AF.Exp))
            kTb = sbt(es, "kTb", [64, 2, T], BF16); b_k = [Buf() for _ in range(2)]
            qTb = sbt(es, "qTb", [64, 2, NT, 3, 128], BF16); b_q = [Buf() for _ in range(6)]
            Va = sbt(es, "Va", [128, 2, NT, 65], BF16); b_Va = Buf()
            G([], [b_Va], lambda: nc.gpsimd.memset(Va[:], 1.0))
            with ExitStack() as es2:
                vf = sbt(es2, "vf", [128, NT, 128]); b_vf = Buf()
                D(vf[:], vtok[:, 256:384].rearrange("(n p) f -> p n f", p=128), [], [b_vf])
                for g in range(2):
                    V([b_vf, b_Va], [b_Va], lambda g=g: nc.vector.tensor_copy(Va[:, g, :, 0:64], vf[:, :, g * 64:(g + 1) * 64]))
                raw = Rot(es2, "rawb", 2, [64, T])
                pr = Rot(es2, "pr", 2, [128, 512], psum=True)
                t1 = Rot(es2, "t1b", 2, [64, 512]); t2 = Rot(es2, "t2b", 2, [64, 512])
                for hd in range(8):
                    rt, br = raw.get()
                    frow = OFF_QB + 64 * hd if hd < 6 else OFF_KB + 64 * (hd - 6)
                    D(rt[:], zT[frow:frow + 64, :], [], [br])
                    if hd < 6:
                        g, i3 = hd // 3, hd % 3
                        dst_ctx = qTb[:, g, 0:2, i3, :]
                        wb_ = b_q[hd]
                    else:
                        g = hd - 6
                        dst_ctx = kTb[:, g, 0:L].rearrange("p (n t) -> p n t", t=128)
                        wb_ = b_k[g]
                    A([br], [wb_], lambda rt=rt, dst_ctx=dst_ctx: nc.scalar.copy(dst_ctx, rt[:, 0:L].rearrange("p (n t) -> p n t", t=128)))
                    for q4 in range(4):
                        cs_ = slice(L + q4 * 512, L + (q4 + 1) * 512); ps_ = slice(q4 * 512, (q4 + 1) * 512)
                        p_t, bp = pr.get(); a_t, ba = t1.get(); c_t, bc = t2.get()
                        P([br, b_c], [bp], lambda: nc.tensor.matmul(p_t[0:64, :], rPT[:], rt[:, cs_], start=True, stop=True))
                        V([br, b_c], [ba], lambda: nc.vector.tensor_tensor(a_t[:], rt[:, cs_], cosT[:, ps_], ALU.mult))
                        V([bp, b_c], [bc], lambda: nc.vector.tensor_tensor(c_t[:], p_t[0:64, :], sinT[:, ps_], ALU.mult))
                        if hd < 6:
                            dst = qTb[:, g, 2 + 4 * q4:2 + 4 * q4 + 4, i3, :]
                        else:
                            dst = kTb[:, g, cs_].rearrange("p (n t) -> p n t", t=128)
                        G([ba, bc], [wb_], lambda: nc.gpsimd.tensor_tensor(dst, a_t[:].rearrange("p (n t) -> p n t", t=128), c_t[:].rearrange("p (n t) -> p n t", t=128), ALU.add))
                f.barrier()
            psc = Rot(es, "psc", 5, [128, 512], psum=True)
            pov = Rot(es, "pov", 2, [128, 512], psum=True)
            pt = Rot(es, "ptb", 16, [128, 384], BF16)
            ob = Rot(es, "obb", 3, [128, 384])
            dn = Rot(es, "dnb", 4, [128, 3])
            items = [(n, g) for n in range(NT) if not (n < 2 and not with_ctx) for g in range(2)]
            obufs = {}

            def stage1(n, g):
                kbs = [(0, None), (1, None)]
                if n >= 2:
                    if n - 1 >= 2: kbs.append((n - 1, 3))
                    kbs.append((n, None))
                    if n + 1 < NT: kbs.append((n + 1, 1))
                pts = []
                for (m, mk) in kbs:
                    s_p, bs = psc.get(); p_t, bpt = pt.get()
                    P([b_k[g]] + b_q[3 * g:3 * g + 3], [bs], lambda: nc.tensor.matmul(
                        s_p[:, 0:384], kTb[:, g, m * 128:(m + 1) * 128], qTb[:, g, n, :, :].rearrange("p a t -> p (a t)"), start=True, stop=True))
                    A([bs], [bpt], lambda: nc.scalar.activation(p_t[:], s_p[:, 0:384], AF.Exp, scale=SCALE))
                    if mk is not None:
                        G([bpt, b_mk], [bpt], lambda: nc.gpsimd.tensor_tensor(
                            p_t[:].rearrange("p (a t) -> p a t", a=3), p_t[:].rearrange("p (a t) -> p a t", a=3),
                            mkb[:, mk, :].unsqueeze(1).broadcast_to([128, 3, 128]), ALU.mult))
                    pts.append((p_t, bpt, m))
                return pts

            def stage2(n, g, pts):
                if g == 0:
                    obufs[n] = ob.get()
                o_t, bo = obufs[n]
                o_pf, bop = pov.get()
                o_p = o_pf[:, 0:195].rearrange("p (a e) -> p a e", e=65)
                for i3 in range(3):
                    for q, (p_t, bpt, m) in enumerate(pts):
                        P([bpt, b_Va], [bop], lambda: nc.tensor.matmul(
                            o_p[:, i3, :], p_t[:, i3 * 128:(i3 + 1) * 128], Va[:, g, m, :], start=(q == 0), stop=(q == len(pts) - 1)))
                d_t, bd = dn.get()
                V([bop, b_esk], [bd], lambda: nc.vector.tensor_tensor(d_t[:], o_p[:, :, 64], esk[:, 3 * g:3 * g + 3], ALU.add))
                V([bd], [bd], lambda: nc.vector.reciprocal(d_t[:], d_t[:]))
                for i3 in range(3):
                    hc = (3 * g + i3) * 64
                    V([bop, bd], [bo], lambda: nc.vector.tensor_scalar(o_t[:, hc:hc + 64], o_p[:, i3, 0:64], d_t[:, i3:i3 + 1], None, ALU.mult))
                if g == 1:
                    D(omix[n * 128:(n + 1) * 128, 256:640], o_t[:], [bo], [])

            prev = None
            for it in items:
                cur = (it, stage1(*it))
                if prev is not None:
                    stage2(prev[0][0], prev[0][1], prev[1])
                prev = cur
            stage2(prev[0][0], prev[0][1], prev[1])
            f.barrier()

    def phase_attn_c(l):
        with_ctx = l < DEPTH - 1
        with ExitStack() as es:
            qT = sbt(es, "qTn", [64, 6, T], BF16); kTn = sbt(es, "kTn", [64, 6, T], BF16); b_qk = Buf()
            for h in range(6):
                D(qT[:, h, :], zT[OFF_QN + 64 * h:OFF_QN + 64 * h + 64, :], [], [b_qk], eng=POOL)
                D(kTn[:, h, :], zT[OFF_KN + 64 * h:OFF_KN + 64 * h + 64, :], [], [b_qk], eng=POOL)
            Va = sbt(es, "Van", [128, NT, 6, 65], BF16); b_Va = Buf()
            G([], [b_Va], lambda: nc.gpsimd.memset(Va[:], 1.0))
            with ExitStack() as es2:
                vf = sbt(es2, "vfn", [128, NT, 384]); b_vf = Buf()
                D(vf[:], vtok[:, 384:768].rearrange("(n p) f -> p n f", p=128), [], [b_vf])
                V([b_vf, b_Va], [b_Va], lambda: nc.vector.tensor_copy(Va[:, :, :, 0:64], vf[:].rearrange("p n (h e) -> p n h e", e=64)))
                f.barrier()
            cv = sbt(es, "cv", [128, 64]); b_cv = Buf()
            D(cv[:], ccolvalid[:, :], [], [b_cv])
            ebf = Rot(es, "ebf", 2, [128, 15, 64]); ebb = sbt(es, "ebb", [128, 6, 15, 64], BF16); b_eb = [Buf() for _ in range(6)]
            oC = sbt(es, "oC", [128, NT, 384]); b_oC = [Buf() for _ in range(NT)]
            psc = Rot(es, "pscn", 3, [128, 1024], psum=True)
            pov = carve(es, "povn", 2, 128)
            pt = Rot(es, "ptn", 4, [128, 896], BF16)
            dn = Rot(es, "dnn", 4, [128, 1])
            for h in range(6):
                e_f, bef = ebf.get()
                D(e_f[0:64, :, :], rpbtab[l, h, :, :, :], [], [bef])
                D(e_f[64:128, :, :], rpbtab[l, h, :, :, :], [], [bef])
                A([bef], [bef], lambda: nc.scalar.activation(e_f[:], e_f[:], AF.Exp))
                V([bef, b_cv], [b_eb[h]], lambda: nc.vector.tensor_tensor(ebb[:, h, :, :], e_f[:], cv[:].unsqueeze(1).broadcast_to([128, 15, 64]), ALU.mult))
            items = [(h, n) for h in range(6) for n in range(NT) if not (n < 2 and not with_ctx)]

            def keyblocks(n):
                kbs = [(0, None), (1, None)]
                if n >= 2:
                    nb = n - 2
                    for kb in range(16):
                        subs = []
                        anyv = False
                        for i in range(2):
                            for jq in range(2):
                                kr = 2 * kb + i; qr = 2 * nb + jq
                                rs_ = min(max(qr - 4, 0), 24)
                                ok = rs_ <= kr < rs_ + 8
                                subs.append((i, jq, kr - qr + 7 if ok else None))
                                anyv = anyv or ok
                        if anyv:
                            kbs.append((kb + 2, subs))
                return kbs

            def stage1(h, n):
                kbs = keyblocks(n)
                nk = len(kbs)
                s_p, bs = psc.get(); p_t, bpt = pt.get()
                for q, (m, subs) in enumerate(kbs):
                    P([b_qk], [bs], lambda: nc.tensor.matmul(s_p[:, q * 128:(q + 1) * 128], kTn[:, h, m * 128:(m + 1) * 128], qT[:, h, n * 128:(n + 1) * 128], start=True, stop=True))
                A([bs], [bpt], lambda: nc.scalar.activation(p_t[:, 0:nk * 128], s_p[:, 0:nk * 128], AF.Exp, scale=SCALE))
                cnt = 0
                for q, (m, subs) in enumerate(kbs):
                    if subs is None:
                        continue
                    for (i, jq, ro) in subs:
                        eng, ee = (G, nc.gpsimd) if cnt % 2 == 0 else (V, nc.vector)
                        cnt += 1
                        sl = p_t[64 * i:64 * i + 64, q * 128 + 64 * jq:q * 128 + 64 * jq + 64]
                        if ro is None:
                            eng([bpt], [bpt], lambda: ee.memset(sl, 0.0))
                        else:
                            eng([bpt, b_eb[h]], [bpt], lambda: ee.tensor_tensor(sl, sl, ebb[64 * i:64 * i + 64, h, ro, :], ALU.mult))
                return (p_t, bpt, kbs)

            def stage2(h, n, st):
                p_t, bpt, kbs = st
                o_p, bop = pov.get()
                for q, (m, subs) in enumerate(kbs):
                    P([bpt, b_Va], [bop], lambda: nc.tensor.matmul(o_p[:, 0:65], p_t[:, q * 128:(q + 1) * 128], Va[:, m, h, :], start=(q == 0), stop=(q == len(kbs) - 1)))
                d_t, bd = dn.get()
                V([bop], [bd], lambda: nc.vector.reciprocal(d_t[:], o_p[:, 64:65]))
                V([bop, bd], [b_oC[n]], lambda: nc.vector.tensor_scalar(oC[:, n, h * 64:(h + 1) * 64], o_p[:, 0:64], d_t[:, 0:1], None, ALU.mult))

            prev = None
            for it in items:
                cur = (it, stage1(*it))
                if prev is not None:
                    stage2(prev[0][0], prev[0][1], prev[1])
                prev = cur
            stage2(prev[0][0], prev[0][1], prev[1])
            for n in range(NT):
                if n < 2 and not with_ctx:
                    continue
                D(omix[n * 128:(n + 1) * 128, 640:1024], oC[:, n, :], [b_oC[n]], [])
            f.barrier()

    def phase_out_moe(l):
        with_ctx = l < DEPTH - 1
        tiles = list(range(NT)) if with_ctx else list(range(2, NT))
        with ExitStack() as es:
            h2T = sbt(es, "h2T", [128, 8, T], BF16); b_h2T = [Buf() for _ in range(NT)]
            Wr = sbt(es, "Wr", [128, NT, 32]); b_Wr = [Buf() for _ in range(NT)]
            with ExitStack() as es2:
                mods = sbt(es2, "mods2", [128, 2, 3 * DM]); b_mods = Buf()
                D(mods[:, 0, :], modrow[1, 2 * DM:5 * DM].partition_broadcast(128), [], [b_mods])
                D(mods[:, 1, :], modrow[0, 2 * DM:5 * DM].partition_broadcast(128), [], [b_mods])
                G([b_mods], [b_mods], lambda: nc.gpsimd.tensor_scalar(mods[:, :, 2 * DM:3 * DM], mods[:, :, 2 * DM:3 * DM], 1.0, None, ALU.add))
                lgb = sbt(es2, "lgb", [128, 2, DM]); b_lgb = Buf()
                D(lgb[:, 0, :], ln1_g[l, :].partition_broadcast(128), [], [b_lgb])
                D(lgb[:, 1, :], ln1_b[l, :].partition_broadcast(128), [], [b_lgb])
                wo = sbt(es2, "wo", [128, 8, DM], BF16); b_wo = Buf()
                D(wo[:], w_out[l, :, :].rearrange("(k p) n -> p k n", p=128), [], [b_wo], eng=POOL)
                wr = sbt(es2, "wr", [128, 8, 36]); b_wr = Buf()
                D(wr[:], w_r[l, :, :].rearrange("(k p) n -> p k n", p=128), [], [b_wr])
                brb = sbt(es2, "brb", [128, 36]); D(brb[:], b_r[l, :].partition_broadcast(128), [], [b_wr])
                om = Rot(es2, "om", 2, [128, DM]); omb = Rot(es2, "omb", 2, [128, DM], BF16)
                oT = Rot(es2, "oT", 2, [128, 8, 128], BF16)
                xt = Rot(es2, "xt2", 2, [128, DM]); tt = Rot(es2, "tt", 2, [128, DM]); x1 = Rot(es2, "x1", 2, [128, DM])
                hf = Rot(es2, "hf", 2, [128, DM]); hb = Rot(es2, "hb2", 2, [128, DM], BF16)
                hT32 = Rot(es2, "hT32", 2, [128, 8, 128])
                st = Rot(es2, "st2", 4, [128, 2, 6]); mv = Rot(es2, "mv2", 4, [128, 2]); rs = Rot(es2, "rs2", 4, [128, 1])
                pTb = Rot(es2, "pTb", 2, [128, 8, 128], BF16, psum=True)
                pO = Rot(es2, "pO", 1, [128, 1024], psum=True)
                pT32 = Rot(es2, "pT32", 1, [128, 8, 128], psum=True)
                pL = carve(es2, "pL", 1, 64)
                sm = Rot(es2, "sm", 2, [128, 160])
                for i in tiles:
                    w = 0 if i < 2 else 1
                    o_m, bom = om.get(); o_b, bob = omb.get(); o_T, boT = oT.get()
                    D(o_m[:], omix[i * 128:(i + 1) * 128, :], [], [bom])
                    A([bom], [bob], lambda: nc.scalar.copy(o_b[:], o_m[:]))
                    p_t, bp = pTb.get()
                    for k in range(8):
                        P([bob, b_identb], [bp], lambda k=k: nc.tensor.transpose(p_t[:, k, :], o_b[:, k * 128:(k + 1) * 128], identb[:]))
                    V([bp], [boT], lambda: nc.vector.tensor_copy(o_T[:], p_t[:]))
                    p_o, bpo = pO.get()
                    for hf_ in range(2):
                        for k in range(8):
                            P([boT, b_wo], [bpo], lambda k=k, hf_=hf_: nc.tensor.matmul(
                                p_o[:, hf_ * 512:(hf_ + 1) * 512], o_T[:, k, :], wo[:, k, hf_ * 512:(hf_ + 1) * 512], start=(k == 0), stop=(k == 7)))
                    x_t, bx = xt.get(); t_t, bt = tt.get(); x_1, bx1 = x1.get()
                    D(x_t[:], xsrc(l, i), [], [bx])
                    V([bpo, b_mods], [bt], lambda: nc.vector.tensor_tensor(t_t[:], p_o[:], mods[:, w, 0:DM], ALU.mult))
                    V([bx, bt], [bt], lambda: nc.vector.scalar_tensor_tensor(t_t[:], x_t[:], ALPHA, t_t[:], ALU.mult, ALU.add))
                    s_t, bs = st.get(); m_t, bmv = mv.get(); r_t, br = rs.get()
                    layer_norm_stats(None, t_t, bt, (s_t, bs, m_t, bmv, r_t, br))
                    V([bt, bmv, br], [bt], lambda: nc.vector.tensor_scalar(t_t[:], t_t[:], m_t[:, 0:1], r_t[:, 0:1], ALU.subtract, ALU.mult))
                    G([bt, b_lgb], [bt], lambda: nc.gpsimd.tensor_tensor(t_t[:], t_t[:], lgb[:, 0, :], ALU.mult))
                    G([bt, b_lgb], [bx1], lambda: nc.gpsimd.tensor_tensor(x_1[:], t_t[:], lgb[:, 1, :], ALU.add))
                    D(xs[i * 128:(i + 1) * 128, :], x_1[:], [bx1], [])
                    s_t, bs = st.get(); m_t, bmv = mv.get(); r_t, br = rs.get()
                    layer_norm_stats(None, x_1, bx1, (s_t, bs, m_t, bmv, r_t, br))
                    h_f, bhf = hf.get(); h_b, bhb = hb.get()
                    V([bx1, bmv, br], [bhf], lambda: nc.vector.tensor_scalar(h_f[:], x_1[:], m_t[:, 0:1], r_t[:, 0:1], ALU.subtract, ALU.mult))
                    G([bhf, b_mods], [bhf], lambda: nc.gpsimd.tensor_tensor(h_f[:], h_f[:], mods[:, w, 2 * DM:3 * DM], ALU.mult))
                    G([bhf, b_mods], [bhf], lambda: nc.gpsimd.tensor_tensor(h_f[:], h_f[:], mods[:, w, DM:2 * DM], ALU.add))
                    A([bhf], [bhb], lambda: nc.scalar.copy(h_b[:], h_f[:]))
                    p_t, bp = pTb.get()
                    for k in range(8):
                        P([bhb, b_identb], [bp], lambda k=k: nc.tensor.transpose(p_t[:, k, :], h_b[:, k * 128:(k + 1) * 128], identb[:]))
                    A([bp], [b_h2T[i]], lambda: nc.scalar.copy(h2T[:, :, i * 128:(i + 1) * 128], p_t[:]))
                    p32, bp32 = pT32.get()
                    for k in range(8):
                        P([bhf, b_ident], [bp32], lambda k=k: nc.tensor.transpose(p32[:, k, :], h_f[:, k * 128:(k + 1) * 128], ident[:]))
                    h32, bh32 = hT32.get()
                    V([bp32], [bh32], lambda: nc.vector.tensor_copy(h32[:], p32[:]))
                    p_l, bpl = pL.get()
                    for k in range(8):
                        P([bh32, b_wr], [bpl], lambda k=k: nc.tensor.matmul(p_l[:, 0:36], h32[:, k, :], wr[:, k, :], start=(k == 0), stop=(k == 7)))
                    s_, bsm = sm.get()
                    lg = s_[:, 0:36]; gmx = s_[:, 36:37]; ngm = s_[:, 37:38]; g1h = s_[:, 40:44]; e4 = s_[:, 44:48]
                    esum = s_[:, 48:49]; pgr = s_[:, 49:50]; pen = s_[:, 52:56]; m8 = s_[:, 56:64]; elm = s_[:, 64:96]
                    dd = s_[:, 96:97]; ee_ = s_[:, 97:98]; c1 = s_[:, 98:99]; c2 = s_[:, 99:100]
                    V([bpl, b_wr], [bsm], lambda: nc.vector.tensor_tensor(lg, p_l[:, 0:36], brb[:], ALU.add))
                    V([bsm], [bsm], lambda: nc.vector.tensor_reduce(gmx, s_[:, 0:4], AX.X, ALU.max))
                    V([bsm], [bsm], lambda: nc.vector.tensor_scalar(ngm, gmx, -1.0, None, ALU.mult))
                    V([bsm], [bsm], lambda: nc.vector.tensor_scalar(g1h, s_[:, 0:4], gmx, None, ALU.is_equal))
                    A([bsm], [bsm], lambda: nc.scalar.activation(e4, s_[:, 0:4], AF.Exp, bias=ngm, scale=1.0))
                    V([bsm], [bsm], lambda: nc.vector.tensor_reduce(esum, e4, AX.X, ALU.add))
                    V([bsm], [bsm], lambda: nc.vector.reciprocal(pgr, esum))
                    V([bsm], [bsm], lambda: nc.vector.tensor_scalar(pen, g1h, -1.0, 1e30, ALU.add, ALU.mult))
                    V([bsm], [bsm], lambda: nc.vector.tensor_tensor(elm.rearrange("p (g e) -> p g e", e=8), s_[:, 4:36].rearrange("p (g e) -> p g e", e=8),
                                                                    pen.unsqueeze(2).broadcast_to([128, 4, 8]), ALU.add))
                    V([bsm], [bsm], lambda: nc.vector.max(m8, elm))
                    V([bsm], [bsm], lambda: nc.vector.tensor_tensor(dd, m8[:, 1:2], m8[:, 0:1], ALU.subtract))
                    A([bsm], [bsm], lambda: nc.scalar.activation(ee_, dd, AF.Exp))
                    V([bsm], [bsm], lambda: nc.vector.tensor_scalar(c1, ee_, 1.0, None, ALU.add))
                    V([bsm], [bsm], lambda: nc.vector.reciprocal(c1, c1))
                    V([bsm], [bsm], lambda: nc.vector.tensor_tensor(c2, ee_, c1, ALU.mult))
                    V([bsm], [bsm], lambda: nc.vector.tensor_tensor(c1, c1, pgr, ALU.mult))
                    V([bsm], [bsm], lambda: nc.vector.tensor_tensor(c2, c2, pgr, ALU.mult))
                    V([bsm], [b_Wr[i]], lambda: nc.vector.tensor_scalar(Wr[:, i, :], elm, m8[:, 0:1], c1, ALU.is_equal, ALU.mult))
                    V([bsm], [bsm], lambda: nc.vector.tensor_scalar(s_[:, 100:132], elm, m8[:, 1:2], c2, ALU.is_equal, ALU.mult))
                    V([bsm, b_Wr[i]], [b_Wr[i]], lambda: nc.vector.tensor_tensor(Wr[:, i, :], Wr[:, i, :], s_[:, 100:132], ALU.add))
                f.barrier()
            if state.get("stop_before_moe"):
                return
            facc = sbt(es, "facc", [128, NT, DM]); b_f = [Buf() for _ in range(NT)]
            with ExitStack() as es2:
                wg = Rot(es2, "wg", 2, [128, 8, DE], BF16); wu = Rot(es2, "wu", 2, [128, 8, DE], BF16)
                wd = Rot(es2, "wd", 2, [128, 4, DM], BF16)
                pG = Rot(es2, "pG", 2, [128, 512], psum=True); pU = Rot(es2, "pU", 2, [128, 512], psum=True)
                pD = Rot(es2, "pD", 3, [128, 512], psum=True)
                sg = Rot(es2, "sg", 2, [128, 512]); aT = Rot(es2, "aT", 8, [128, 512], BF16)
                t0_ = tiles[0] * 128
                ntok = len(tiles) * 128
                blocks_ = [(t0_ + b * 512, min(512, ntok - b * 512)) for b in range((ntok + 511) // 512)]
                for e in range(NE):
                    g_w, bg = wg.get(); u_w, bu = wu.get(); d_w, bdw = wd.get()
                    D(g_w[:], w_gate[l, e, :, :].rearrange("(k p) n -> p k n", p=128), [], [bg], eng=POOL)
                    D(u_w[:], w_up[l, e, :, :].rearrange("(k p) n -> p k n", p=128), [], [bu], eng=POOL)
                    D(d_w[:], w_down[l, e, :, :].rearrange("(k p) n -> p k n", p=128), [], [bdw], eng=POOL)
                    for (tb0, tsz) in blocks_:
                        rb = [b_h2T[i] for i in range(tb0 // 128, (tb0 + tsz) // 128)]
                        acts = []
                        for fc in range(4):
                            g_p, bgp = pG.get(); u_p, bup = pU.get()
                            for k in range(8):
                                P(rb + [bg], [bgp], lambda k=k: nc.tensor.matmul(g_p[:, 0:tsz], g_w[:, k, fc * 128:(fc + 1) * 128], h2T[:, k, tb0:tb0 + tsz], start=(k == 0), stop=(k == 7)))
                            for k in range(8):
                                P(rb + [bu], [bup], lambda k=k: nc.tensor.matmul(u_p[:, 0:tsz], u_w[:, k, fc * 128:(fc + 1) * 128], h2T[:, k, tb0:tb0 + tsz], start=(k == 0), stop=(k == 7)))
                            s_g, bsg = sg.get(); a_T, baT = aT.get()
                            A([bgp], [bsg], lambda: nc.scalar.activation(s_g[:, 0:tsz], g_p[:, 0:tsz], AF.Silu))
                            V([bsg, bup], [baT], lambda: nc.vector.tensor_tensor(a_T[:, 0:tsz], s_g[:, 0:tsz], u_p[:, 0:tsz], ALU.mult))
                            acts.append((a_T, baT))
                        for q in range(tsz // 128):
                            ti = tb0 // 128 + q
                            for hf_ in range(2):
                                d_p, bdp = pD.get()
                                for fc in range(4):
                                    a_T, baT = acts[fc]
                                    P([baT, bdw], [bdp], lambda fc=fc, a_T=a_T: nc.tensor.matmul(
                                        d_p[:], a_T[:, q * 128:(q + 1) * 128], d_w[:, fc, hf_ * 512:(hf_ + 1) * 512], start=(fc == 0), stop=(fc == 3)))
                                fs = facc[:, ti, hf_ * 512:(hf_ + 1) * 512]
                                if e == 0:
                                    V([bdp, b_Wr[ti]], [b_f[ti]], lambda: nc.vector.tensor_scalar(fs, d_p[:], Wr[:, ti, e:e + 1], None, ALU.mult))
                                else:
                                    V([bdp, b_Wr[ti], b_f[ti]], [b_f[ti]], lambda: nc.vector.scalar_tensor_tensor(fs, d_p[:], Wr[:, ti, e:e + 1], fs, ALU.mult, ALU.add))
                f.barrier()
            with ExitStack() as es2:
                g2 = sbt(es2, "g2", [128, 2, DM]); b_g2 = Buf()
                D(g2[:, 0, :], modrow[1, 5 * DM:6 * DM].partition_broadcast(128), [], [b_g2])
                D(g2[:, 1, :], modrow[0, 5 * DM:6 * DM].partition_broadcast(128), [], [b_g2])
                lgb = sbt(es2, "lgb2", [128, 2, DM]); b_lgb = Buf()
                D(lgb[:, 0, :], ln2_g[l, :].partition_broadcast(128), [], [b_lgb])
                D(lgb[:, 1, :], ln2_b[l, :].partition_broadcast(128), [], [b_lgb])
                xt = Rot(es2, "xt3", 2, [128, DM]); tt = Rot(es2, "tt3", 2, [128, DM])
                st = Rot(es2, "st3", 2, [128, 2, 6]); mv = Rot(es2, "mv3", 2, [128, 2]); rs = Rot(es2, "rs3", 2, [128, 1])
                for i in tiles:
                    w = 0 if i < 2 else 1
                    x_t, bx = xt.get(); t_t, bt = tt.get()
                    D(x_t[:], xs[i * 128:(i + 1) * 128, :], [], [bx])
                    G([b_f[i], b_g2], [bt], lambda: nc.gpsimd.tensor_tensor(t_t[:], facc[:, i, :], g2[:, w, :], ALU.mult))
                    V([bx, bt], [bt], lambda: nc.vector.scalar_tensor_tensor(t_t[:], x_t[:], ALPHA, t_t[:], ALU.mult, ALU.add))
                    s_t, bs = st.get(); m_t, bmv = mv.get(); r_t, br = rs.get()
                    layer_norm_stats(None, t_t, bt, (s_t, bs, m_t, bmv, r_t, br))
                    V([bt, bmv, br], [bt], lambda: nc.vector.tensor_scalar(t_t[:], t_t[:], m_t[:, 0:1], r_t[:, 0:1], ALU.subtract, ALU.mult))
                    G([bt, b_lgb], [bt], lambda: nc.gpsimd.tensor_tensor(t_t[:], t_t[:], lgb[:, 0, :], ALU.mult))
                    G([bt, b_lgb], [bt], lambda: nc.gpsimd.tensor_tensor(t_t[:], t_t[:], lgb[:, 1, :], ALU.add))
                    if l == DEPTH - 1:
                        D(y_out[(i - 2) * 128:(i - 1) * 128, :], t_t[:], [bt], [])
                    else:
                        D(xs[i * 128:(i + 1) * 128, :], t_t[:], [bt], [])
                f.barrier()

    done = False
    for l in range(DEPTH):
        for tag, fn in (("mod", phase_mod), ("proj", phase_ln_proj), ("rwkv", phase_rwkv),
                        ("attb", phase_attn_b), ("attc", phase_attn_c), ("moe", phase_out_moe)):
            if STOP is not None and (l, tag) == tuple(STOP[:2]) and len(STOP) > 2:
                state["stop_before_moe"] = True
            fn(l)
            if STOP is not None and (l, tag) == tuple(STOP[:2]):
                done = True
                break
        if done:
            break
    f.barrier()
    print("n_inst", f.n_inst, flush=True)
    return nc


def _consts():
    p = np.arange(128)[:, None]; q = np.arange(128)[None, :]
    masks = np.stack([(p < q), (p <= q), (p > q), (p >= q)], 1).astype(np.float32)
    ident = np.eye(128, dtype=np.float32)
    block = (p // 64 == q // 64).astype(np.float32)
    hsel = (np.arange(128)[:, None] // 64 == np.arange(2)[None, :]).astype(np.float32)
    t = np.arange(S)
    pos = np.stack([t // 64, t % 64], -1).astype(np.float32)
    inv = (10000.0 ** (-np.arange(16, dtype=np.float32) / 16)).astype(np.float32)
    ang = pos[:, :, None] * inv
    cos = np.cos(ang).astype(np.float32); sin = np.sin(ang).astype(np.float32)
    cosT = np.zeros((64, S), np.float32); sinT = np.zeros((64, S), np.float32)
    for ax in range(2):
        for half in range(2):
            r0 = ax * 32 + half * 16
            cosT[r0:r0 + 16] = cos[:, ax, :].T
            sinT[r0:r0 + 16] = sin[:, ax, :].T
    Pm = np.zeros((64, 64), np.float32)
    for ax in range(2):
        for i in range(16):
            Pm[ax * 32 + i, ax * 32 + 16 + i] = -1.0
            Pm[ax * 32 + 16 + i, ax * 32 + i] = 1.0
    ropeP = np.ascontiguousarray(Pm.T)
    col = np.arange(64)
    cstart = np.clip(col - 8, 0, 48)
    cvalid = ((col[None, :] >= cstart[:, None]) & (col[None, :] < cstart[:, None] + 16)).astype(np.float32)
    colvalid = np.concatenate([cvalid.T, cvalid.T], 0)
    scan = np.ones((128, 512), np.float32); scan[:, ::128] = 0.0
    return dict(cmasks=masks, cident=ident, cblock=block, chsel=hsel, ccos=cosT, csin=sinT, cropeP=ropeP,
                ccolvalid=np.ascontiguousarray(colvalid), cscanmask=scan)


def _host_layout(inp):
    f32 = lambda a: np.ascontiguousarray(np.asarray(a, dtype=np.float32))
    sh = {}
    sh["w_mod"] = f32(inp["w_mod"]); sh["b_mod"] = f32(inp["b_mod"]); sh["w_in"] = f32(inp["w_in"]); sh["w_out"] = f32(inp["w_out"])
    sh["a_shift"] = f32(inp["a_shift"])
    sh["a_w_up"] = f32(np.asarray(inp["a_w_up"]).reshape(DEPTH, 128, 256))
    sh["a_a_up"] = f32(np.asarray(inp["a_a_up"]).reshape(DEPTH, 128, 256))
    sh["a_g_up"] = f32(inp["a_g_up"])
    cp = np.zeros((DEPTH, 128, 48), np.float32)
    ash = np.zeros((DEPTH, 3, 1280), np.float32); ash[:, :, :A_IN] = inp["a_shift"]
    for l in range(DEPTH):
        for ch in range(10):
            for tap in range(3):
                cp[l, :, ch * 3 + tap] = ash[l, tap, ch * 128:(ch + 1) * 128]
        for d in range(2):
            for j in range(2):
                cp[l, :, 30 + d * 2 + j] = inp["a_w0"][l, d, j * 128:(j + 1) * 128]
                cp[l, :, 34 + d * 2 + j] = inp["a_a0"][l, d, j * 128:(j + 1) * 128]
        for j in range(2):
            cp[l, :, 38 + j] = inp["a_k_k"][l, j * 128:(j + 1) * 128]
            cp[l, :, 40 + j] = inp["a_k_a"][l, j * 128:(j + 1) * 128]
            cp[l, :, 42 + j] = inp["a_r_k"][l, j * 128:(j + 1) * 128]
    sh["colpar"] = cp
    for k in ("a_r_k", "a_lnx_g", "a_lnx_b", "b_sink", "ln1_g", "ln1_b", "ln2_g", "ln2_b"):
        sh[k] = f32(inp[k])
    for k in ("w_gate", "w_up", "w_down"):
        sh[k] = f32(np.asarray(inp[k])[:, :NE_DECL])
    kc = np.arange(64)[:, None]; qc = np.arange(64)[None, :]
    idx = np.clip(kc - qc, -15, 15) + 15
    rpb = np.asarray(inp["c_rpb"], np.float32)
    tab = rpb[:, :, :, idx]
    sh["rpbtab"] = f32(tab.transpose(0, 1, 3, 2, 4))
    sh["w_r"] = f32(np.concatenate([inp["w_rg"], inp["w_re"]], -1))
    sh["b_r"] = f32(np.concatenate([inp["b_rg"], inp["b_re"]], -1))
    sh.update(_consts())
    return sh


_PROG = {}


def kernel(**inputs):
    inp = {k: np.asarray(v) for k, v in inputs.items()}
    shared = _host_layout(inp)
    if "nc" not in _PROG:
        _PROG["nc"] = build_program()
    nc = _PROG["nc"]
    in_maps = []
    for b in range(NCORES):
        m = dict(shared)
        m["x"] = np.ascontiguousarray(inp["x"][b], dtype=np.float32)
        m["ctx"] = np.ascontiguousarray(inp["ctx"][b], dtype=np.float32)
        cc = np.zeros((128, 8, 2), np.float32)
        cc[:, :, 0] = np.asarray(inp["c"][b], np.float32).reshape(8, 128).T
        cc[:, :, 1] = np.asarray(inp["c_ctx"], np.float32).reshape(8, 128).T
        m["ccol"] = cc
        in_maps.append(m)
    res = run_bass_kernel_spmd(nc, in_maps, core_ids=list(range(NCORES)))
    _PROG["res"] = res
    out = np.stack([np.asarray(r["y"], dtype=np.float32) for r in res.results], 0)
    return out
```

```python
from contextlib import ExitStack
import numpy as np
import concourse.bass as bass
import concourse.mybir as mybir
from concourse.bass_utils import run_bass_kernel_spmd

F32 = mybir.dt.float32
BF16 = mybir.dt.bfloat16
ALU = mybir.AluOpType
AF = mybir.ActivationFunctionType
AX = mybir.AxisListType

DM = 1024; S = 2048; L = 256; T = S + L; NT = T // 128; DEPTH = 2
HD = 64; A_IN = 1184; N_IN = 2976; NE = 32; DE = 512
ALPHA = (2.0 * DEPTH) ** 0.25
SCALE = HD ** -0.5
C0 = float(np.exp(-0.5))
LN_EPS = 1e-6; GN_EPS = 64e-5
OFF_R, OFF_K, OFF_V, OFF_WD, OFF_AD, OFF_GD = 0, 256, 512, 768, 896, 1024
OFF_QB, OFF_KB, OFF_VB = 1184, 1568, 1696
OFF_QN, OFF_KN, OFF_VN = 1824, 2208, 2592
EPOCH = 30000
DEBUG = False
RW_STOP = 0
NCORES = 8
NE_DECL = 32
STOP = None


class Buf:
    __slots__ = ("w", "r")

    def __init__(self):
        self.w = {}
        self.r = {}


class Eng:
    def __init__(self, fw, name, eng, dma_only=False):
        self.fw = fw; self.name = name; self.eng = eng
        self.sem = None; self.cnt = 0; self.epoch = 0; self.seen = {}
        self.dma_only = dma_only
        if not dma_only:
            self.new_sem()

    def new_sem(self):
        self.sem = self.fw.nc.alloc_semaphore(f"p_{self.name}_{self.epoch}")
        self.cnt = 0
        self.epoch += 1
        self.fw.all_sems.append(self)


class FW:
    def __init__(self, nc, n_dma_sems=24):
        self.nc = nc
        self.all_sems = []
        self.pe = Eng(self, "pe", nc.tensor)
        self.act = Eng(self, "act", nc.scalar)
        self.dve = Eng(self, "dve", nc.vector)
        self.pool = Eng(self, "pool", nc.gpsimd)
        self.sp = Eng(self, "sp", nc.sync, dma_only=True)
        self.dma_sems = [[nc.alloc_semaphore(f"d{i}"), 0] for i in range(n_dma_sems)]
        self.dma_rr = 0
        self.sw_sems = [[nc.alloc_semaphore(f"w{i}"), 0] for i in range(8)]
        self.sw_rr = 0
        self.n_inst = 0

    def _wait(self, E, sem, val):
        k = sem.name
        if E.seen.get(k, 0) >= val:
            return
        E.eng.wait_ge(sem, val)
        E.seen[k] = val

    def _deps(self, E, reads, writes, skip_self=True):
        need = {}
        for b in reads:
            for k, sv in b.w.items():
                if k not in need or need[k][1] < sv[1]:
                    need[k] = sv
        for b in writes:
            for d in (b.w, b.r):
                for k, sv in d.items():
                    if k not in need or need[k][1] < sv[1]:
                        need[k] = sv
        me = E.sem.name if (skip_self and E.sem is not None and E.name == 'pe') else None
        for k, (s, v) in need.items():
            if k == me:
                continue
            self._wait(E, s, v)

    def _mark(self, sem, val, reads, writes):
        k = sem.name
        for b in reads:
            o = b.r.get(k)
            if o is None or o[1] < val:
                b.r[k] = (sem, val)
        for b in writes:
            o = b.w.get(k)
            if o is None or o[1] < val:
                b.w[k] = (sem, val)

    def op(self, E, reads, writes, fn):
        if E.cnt >= EPOCH:
            E.new_sem()
        self._deps(E, reads, writes)
        ins = fn()
        E.cnt += 1
        ins.then_inc(E.sem, 1)
        self._mark(E.sem, E.cnt, reads, writes)
        self.n_inst += 1
        return ins

    def dma(self, E, out, in_, reads, writes, **kw):
        if E.name == "pool":
            slot = self.sw_sems[self.sw_rr]
            self.sw_rr = (self.sw_rr + 1) % len(self.sw_sems)
        else:
            slot = self.dma_sems[self.dma_rr]
            self.dma_rr = (self.dma_rr + 1) % len(self.dma_sems)
        sem, cnt = slot
        if cnt > 0:
            self._wait(E, sem, cnt)
        self._deps(E, reads, writes, skip_self=False)
        ins = E.eng.dma_start(out=out, in_=in_, **kw)
        slot[1] = cnt + 16
        ins.then_inc(sem, 16)
        self._mark(sem, slot[1], reads, writes)
        self.n_inst += 1
        return ins

    def barrier(self):
        cur = []
        for X in (self.pe, self.act, self.dve, self.pool):
            if X.cnt > 0:
                cur.append((X.sem, X.cnt))
        for sem, cnt in self.dma_sems + self.sw_sems:
            if cnt > 0:
                cur.append((sem, cnt))
        for E in (self.pe, self.act, self.dve, self.pool, self.sp):
            for sem, val in cur:
                if E.sem is not None and sem.name == E.sem.name:
                    continue
                self._wait(E, sem, val)


def build_program():
    nc = bass.Bass("TRN2", target_bir_lowering=False)
    f = FW(nc)
    PE, ACT, DVE, POOL, SP = f.pe, f.act, f.dve, f.pool, f.sp

    def din(name, shape):
        return nc.dram_tensor(name, list(shape), F32, kind="ExternalInput").ap()

    def scr(name, shape):
        return nc.dram_tensor(name, list(shape), F32, kind=("ExternalOutput" if DEBUG else "Internal")).ap()

    x_in = din("x", [S, DM]); ctx_in = din("ctx", [L, DM]); ccol = din("ccol", [128, 8, 2])
    w_mod = din("w_mod", [DEPTH, DM, 6 * DM]); b_mod = din("b_mod", [DEPTH, 6 * DM])
    w_in = din("w_in", [DEPTH, DM, N_IN]); w_out = din("w_out", [DEPTH, DM, DM])
    a_shift = din("a_shift", [DEPTH, 3, A_IN])
    a_w_up = din("a_w_up", [DEPTH, 128, 256]); a_a_up = din("a_a_up", [DEPTH, 128, 256])
    a_g_up = din("a_g_up", [DEPTH, 160, 256])
    colpar = din("colpar", [DEPTH, 128, 48])
    a_r_k = din("a_r_k", [DEPTH, 256]); a_lnx_g = din("a_lnx_g", [DEPTH, 256]); a_lnx_b = din("a_lnx_b", [DEPTH, 256])
    b_sink = din("b_sink", [DEPTH, 6])
    rpbtab = din("rpbtab", [DEPTH, 6, 64, 15, 64])
    ln1_g = din("ln1_g", [DEPTH, DM]); ln1_b = din("ln1_b", [DEPTH, DM])
    ln2_g = din("ln2_g", [DEPTH, DM]); ln2_b = din("ln2_b", [DEPTH, DM])
    w_r = din("w_r", [DEPTH, DM, 36]); b_r = din("b_r", [DEPTH, 36])
    w_gate = din("w_gate", [DEPTH, NE_DECL, DM, DE]); w_up = din("w_up", [DEPTH, NE_DECL, DM, DE])
    w_down = din("w_down", [DEPTH, NE_DECL, DE, DM])
    cmasks = din("cmasks", [128, 4, 128]); cident = din("cident", [128, 128])
    cblock = din("cblock", [128, 128]); chsel = din("chsel", [128, 2])
    ccos = din("ccos", [64, S]); csin = din("csin", [64, S]); cropeP = din("cropeP", [64, 64])
    ccolvalid = din("ccolvalid", [128, 64]); cscanmask = din("cscanmask", [128, 512])
    y_out = nc.dram_tensor("y", [S, DM], F32, kind="ExternalOutput").ap()

    xs = scr("xs", [T, DM]); modrow = scr("modrow", [2, 6 * DM])
    zT = scr("zT", [N_IN, T]); vtok = scr("vtok", [T, 768]); omix = scr("omix", [T, DM])
    dbgh = scr("dbgh", [T, DM]) if DEBUG else None
    dbgs = scr("dbgs", [T, 4]) if DEBUG else None

    state = {"stop": False}

    def xsrc(l, i):
        if l == 0:
            return ctx_in[i * 128:(i + 1) * 128, :] if i < 2 else x_in[(i - 2) * 128:(i - 1) * 128, :]
        return xs[i * 128:(i + 1) * 128, :]

    top = ExitStack()
    uid = [0]

    def sbt(es, name, shape, dt=F32):
        uid[0] += 1
        return es.enter_context(nc.sbuf_tensor(f"{name}_{uid[0]}", list(shape), dt))

    def pst(es, name, shape, dt=F32):
        uid[0] += 1
        return es.enter_context(nc.psum_tensor(f"{name}_{uid[0]}", list(shape), dt))
    ident = sbt(top, "ident", [128, 128]); b_ident = Buf()
    identb = sbt(top, "identb", [128, 128], BF16); b_identb = Buf()
    f.dma(SP, ident[:], cident[:, :], [], [b_ident])
    f.op(DVE, [b_ident], [b_identb], lambda: nc.vector.tensor_copy(identb[:], ident[:]))

    def V(r, w, fn): return f.op(DVE, r, w, fn)
    def A(r, w, fn): return f.op(ACT, r, w, fn)
    def G(r, w, fn): return f.op(POOL, r, w, fn)
    def P(r, w, fn): return f.op(PE, r, w, fn)
    def D(out, in_, r, w, eng=None, **kw): return f.dma(eng or SP, out, in_, r, w, **kw)

    class Rot:
        def __init__(self, es, name, n, shape, dt=F32, psum=False):
            mk = pst if psum else sbt
            self.t = [mk(es, f"{name}{i}", shape, dt) for i in range(n)]
            self.b = [Buf() for _ in range(n)]
            self.i = 0

        def get(self):
            k = self.i; self.i = (self.i + 1) % len(self.t)
            return self.t[k], self.b[k]

    class Slots:
        def __init__(self, aps):
            self.t = list(aps); self.b = [Buf() for _ in self.t]; self.i = 0

        def get(self):
            k = self.i; self.i = (self.i + 1) % len(self.t)
            return self.t[k], self.b[k]

    def carve(es, name, nbanks, width):
        per = 512 // width
        views = [[None] * nbanks for _ in range(per)]
        bufs = [Buf() for _ in range(nbanks)]
        for i in range(nbanks):
            bk = pst(es, f"{name}{i}", [128, 512])
            for q in range(per):
                views[q][i] = bk[:, q * width:(q + 1) * width]
        sl = Slots([views[q][i] for q in range(per) for i in range(nbanks)])
        sl.b = [bufs[i] for q in range(per) for i in range(nbanks)]
        return sl

    def layer_norm_stats(es_tiles, src_ap, b_src, tagbufs):
        st, b_st, mv, b_mv, rs, b_rs = tagbufs
        V([b_src], [b_st], lambda: nc.vector.bn_stats(st[:, 0, :], src_ap[:, 0:512]))
        V([b_src], [b_st], lambda: nc.vector.bn_stats(st[:, 1, :], src_ap[:, 512:1024]))
        V([b_st], [b_mv], lambda: nc.vector.bn_aggr(mv[:], st[:].rearrange("p a b -> p (a b)")))
        A([b_mv], [b_rs], lambda: nc.scalar.activation(rs[:], mv[:, 1:2], AF.Sqrt, bias=eps_t[:, 0:1], scale=1.0))
        V([b_rs], [b_rs], lambda: nc.vector.reciprocal(rs[:], rs[:]))

    eps_t = sbt(top, "eps_t", [128, 4]); b_eps = Buf()
    G([], [b_eps], lambda: nc.gpsimd.memset(eps_t[:, 0:1], LN_EPS))
    G([], [b_eps], lambda: nc.gpsimd.memset(eps_t[:, 1:2], GN_EPS))
    G([], [b_eps], lambda: nc.gpsimd.memset(eps_t[:, 2:3], 1e-12))
    G([], [b_eps], lambda: nc.gpsimd.memset(eps_t[:, 3:4], 1.0))
    f.barrier()

    def phase_mod(l):
        with ExitStack() as es:
            cr = sbt(es, "cr", [128, 8, 2]); b_cr = Buf()
            cs = sbt(es, "cs", [128, 8, 2]); b_cs = Buf()
            D(cr[:], ccol[:, :, :], [], [b_cr])
            A([b_cr], [b_cs], lambda: nc.scalar.activation(cs[:], cr[:], AF.Silu))
            bm2 = sbt(es, "bm2", [2, 6 * DM]); b_bm2 = Buf()
            D(bm2[:], b_mod[l, :].partition_broadcast(2), [], [b_bm2])
            msb = sbt(es, "msb", [2, 6 * DM]); b_msb = Buf()
            wm = Rot(es, "wm", 3, [128, 3072])
            acc = [pst(es, f"macc{j}", [128, 512]) for j in range(6)]
            b_acc = [Buf() for _ in range(6)]
            for hf in range(2):
                for k in range(8):
                    wt, bw = wm.get()
                    D(wt[:], w_mod[l, k * 128:(k + 1) * 128, hf * 3072:(hf + 1) * 3072], [], [bw])
                    for j in range(6):
                        P([bw, b_cs], [b_acc[j]], lambda j=j, wt=wt, k=k: nc.tensor.matmul(
                            acc[j][0:2, :], cs[:, k, :], wt[:, j * 512:(j + 1) * 512], start=(k == 0), stop=(k == 7)))
                for j in range(6):
                    c0 = hf * 3072 + j * 512
                    V([b_acc[j], b_bm2], [b_msb], lambda j=j, c0=c0: nc.vector.tensor_tensor(
                        msb[:, c0:c0 + 512], acc[j][0:2, :], bm2[:, c0:c0 + 512], ALU.add))
            D(modrow[:, :], msb[:], [b_msb], [])
            f.barrier()

    def phase_ln_proj(l):
        with ExitStack() as es:
            hT = sbt(es, "hT", [128, 8, T], BF16); b_hT = [Buf() for _ in range(NT)]
            with ExitStack() as es2:
                mods = sbt(es2, "mods", [128, 2, 2 * DM]); b_mods = Buf()
                D(mods[:, 0, :], modrow[1, 0:2 * DM].partition_broadcast(128), [], [b_mods])
                D(mods[:, 1, :], modrow[0, 0:2 * DM].partition_broadcast(128), [], [b_mods])
                G([b_mods], [b_mods], lambda: nc.gpsimd.tensor_scalar(mods[:, :, DM:2 * DM], mods[:, :, DM:2 * DM], 1.0, None, ALU.add))
                xt = Rot(es2, "xt", 2, [128, DM]); xn = Rot(es2, "xn", 2, [128, DM]); hb = Rot(es2, "hb", 2, [128, DM], BF16)
                st = Rot(es2, "st", 2, [128, 2, 6]); mv = Rot(es2, "mv", 2, [128, 2]); rs = Rot(es2, "rs", 2, [128, 1])
                pT = Rot(es2, "pT", 2, [128, 8, 128], BF16, psum=True)
                for i in range(NT):
                    x_t, bx = xt.get(); n_t, bn = xn.get(); h_t, bh = hb.get()
                    s_t, bs = st.get(); m_t, bmv = mv.get(); r_t, br = rs.get(); p_t, bp = pT.get()
                    D(x_t[:], xsrc(l, i), [], [bx])
                    layer_norm_stats(None, x_t, bx, (s_t, bs, m_t, bmv, r_t, br))
                    V([bx, bmv, br], [bn], lambda x_t=x_t, n_t=n_t, m_t=m_t, r_t=r_t: nc.vector.tensor_scalar(
                        n_t[:], x_t[:], m_t[:, 0:1], r_t[:, 0:1], ALU.subtract, ALU.mult))
                    w = 0 if i < 2 else 1
                    G([bn, b_mods], [bn], lambda n_t=n_t, w=w: nc.gpsimd.tensor_tensor(n_t[:], n_t[:], mods[:, w, DM:2 * DM], ALU.mult))
                    G([bn, b_mods], [bn], lambda n_t=n_t, w=w: nc.gpsimd.tensor_tensor(n_t[:], n_t[:], mods[:, w, 0:DM], ALU.add))
                    A([bn], [bh], lambda n_t=n_t, h_t=h_t: nc.scalar.copy(h_t[:], n_t[:]))
                    if DEBUG:
                        D(dbgh[i * 128:(i + 1) * 128, :], n_t[:], [bn], [])
                        D(dbgs[i * 128:(i + 1) * 128, 0:2], m_t[:], [bmv], [], allow_slow_non_contiguous=True)
                        D(dbgs[i * 128:(i + 1) * 128, 2:3], r_t[:], [br], [], allow_slow_non_contiguous=True)
                    for k in range(8):
                        P([bh, b_identb], [bp], lambda k=k, h_t=h_t, p_t=p_t: nc.tensor.transpose(p_t[:, k, :], h_t[:, k * 128:(k + 1) * 128], identb[:]))
                    A([bp], [b_hT[i]], lambda i=i, p_t=p_t: nc.scalar.copy(hT[:, :, i * 128:(i + 1) * 128], p_t[:]))
                f.barrier()
            wb = sbt(es, "wb", [128, 8, N_IN], BF16); b_wb = [Buf() for _ in range(8)]
            for k in range(8):
                D(wb[:, k, :], w_in[l, k * 128:(k + 1) * 128, :], [], [b_wb[k]], eng=POOL)
            pp = Rot(es, "pp", 4, [128, 512], psum=True)
            stg = Rot(es, "stg", 4, [128, 512])
            cnt = 0
            for fc in range(24):
                fsz = 128 if fc < 23 else 32
                for tb in range(5):
                    t0 = tb * 512; tsz = min(512, T - t0)
                    p_t, bp = pp.get(); s_t, bs = stg.get()
                    rb = [b_hT[i] for i in range(t0 // 128, (t0 + tsz) // 128)]
                    for k in range(8):
                        P(rb + [b_wb[k]], [bp], lambda k=k, p_t=p_t, fc=fc, fsz=fsz, t0=t0, tsz=tsz: nc.tensor.matmul(
                            p_t[0:fsz, 0:tsz], wb[:, k, fc * 128:fc * 128 + fsz], hT[:, k, t0:t0 + tsz], start=(k == 0), stop=(k == 7)))
                    if cnt % 2 == 0:
                        A([bp], [bs], lambda p_t=p_t, s_t=s_t, fsz=fsz, tsz=tsz: nc.scalar.copy(s_t[0:fsz, 0:tsz], p_t[0:fsz, 0:tsz]))
                    else:
                        V([bp], [bs], lambda p_t=p_t, s_t=s_t, fsz=fsz, tsz=tsz: nc.vector.tensor_copy(s_t[0:fsz, 0:tsz], p_t[0:fsz, 0:tsz]))
                    cnt += 1
                    D(zT[fc * 128:fc * 128 + fsz, t0:t0 + tsz], s_t[0:fsz, 0:tsz], [bs], [])
            vst = Rot(es, "vst", 2, [128, 768])
            for i in range(NT):
                p1, bp1 = pp.get(); p2, bp2 = pp.get(); s_t, bs = vst.get()
                for (pt_, bpt_, c0_, c1_, w0_, wn_) in ((p1, bp1, 0, 256, OFF_V, 256), (p1, bp1, 256, 384, OFF_VB, 128), (p2, bp2, 0, 384, OFF_VN, 384)):
                    for k in range(8):
                        P([b_hT[i], b_wb[k]], [bpt_], lambda k=k: nc.tensor.matmul(
                            pt_[:, c0_:c1_], hT[:, k, i * 128:(i + 1) * 128], wb[:, k, w0_:w0_ + wn_], start=(k == 0), stop=(k == 7)))
                A([bp1], [bs], lambda p1=p1, s_t=s_t: nc.scalar.copy(s_t[:, 0:384], p1[:, 0:384]))
                V([bp2], [bs], lambda p2=p2, s_t=s_t: nc.vector.tensor_copy(s_t[:, 384:768], p2[:, 0:384]))
                D(vtok[i * 128:(i + 1) * 128, :], s_t[:], [bs], [])
            f.barrier()

    def conv_rows(dst, b_dst, src, b_src, wcol, b_w, c0, n):
        V([b_src, b_w], [b_dst], lambda: nc.vector.tensor_scalar(dst[:, 0:n], src[:, 0:n], wcol[:, 1:2], None, ALU.mult))
        segs = []
        for (a, b) in ((0, L), (L, T)):
            lo = max(a, c0); hi = min(b, c0 + n)
            if lo < hi:
                segs.append((lo - c0, hi - c0, lo == a, hi == b))
        return segs

    def phase_rwkv(l):
        with ExitStack() as es:
            cp = sbt(es, "cp", [128, 48]); b_cp = Buf()
            D(cp[:], colpar[l, :, :], [], [b_cp])
            wup = sbt(es, "wup", [128, 256]); aup = sbt(es, "aup", [128, 256]); b_up = Buf()
            D(wup[:], a_w_up[l, :, :], [], [b_up]); D(aup[:], a_a_up[l, :, :], [], [b_up])
            gup0 = sbt(es, "gup0", [128, 256]); gup1 = sbt(es, "gup1", [32, 256]); b_gup = Buf()
            D(gup0[:], a_g_up[l, 0:128, :], [], [b_gup]); D(gup1[:], a_g_up[l, 128:160, :], [], [b_gup])
            msk = sbt(es, "msk", [128, 4, 128]); b_msk = Buf()
            D(msk[:], cmasks[:, :, :], [], [b_msk])
            blk = sbt(es, "blk", [128, 128]); hsel = sbt(es, "hsel", [128, 2]); b_cst = Buf()
            D(blk[:], cblock[:, :], [], [b_cst]); D(hsel[:], chsel[:, :], [], [b_cst])
            scm = sbt(es, "scm", [128, 512]); D(scm[:], cscanmask[:, :], [], [b_cst])
            vsh = sbt(es, "vsh", [128, 3, 256]); b_vsh = Buf()
            D(vsh[:], a_shift[l, :, OFF_V:OFF_V + 256].partition_broadcast(128), [], [b_vsh])
            yacc = sbt(es, "yacc", [128, NT, 256]); b_yacc = [Buf() for _ in range(NT)]
            bacc = sbt(es, "bacc", [128, NT, 4]); b_bacc = [Buf() for _ in range(NT)]
            Vt = sbt(es, "Vt", [128, NT, 256]); b_Vt = [Buf() for _ in range(NT)]
            Vtb = sbt(es, "Vtb", [128, NT, 256], BF16)
            G([], b_yacc, lambda: nc.gpsimd.memset(yacc[:], 0.0))
            G([], b_bacc, lambda: nc.gpsimd.memset(bacc[:], 0.0))
            Mst = [[sbt(es, f"M{d}{j}", [128, 64]) for j in range(2)] for d in range(2)]
            b_M = [[Buf() for j in range(2)] for d in range(2)]
            for d in range(2):
                for j in range(2):
                    G([], [b_M[d][j]], lambda d=d, j=j: nc.gpsimd.memset(Mst[d][j][:], 0.0))
            with ExitStack() as es2:
                vr = Rot(es2, "vr", 6, [128, 256])
                for i in range(NT):
                    tp, bp_ = vr.get(); tc, bc_ = vr.get(); tn, bn_ = vr.get()
                    first = i in (0, 2); last = i in (1, NT - 1)
                    r0 = i * 128
                    D(tc[:], vtok[r0:r0 + 128, 0:256], [], [bc_])
                    if first:
                        G([], [bp_], lambda tp=tp: nc.gpsimd.memset(tp[:], 0.0))
                        D(tp[1:128, :], vtok[r0:r0 + 127, 0:256], [], [bp_])
                    else:
                        D(tp[:], vtok[r0 - 1:r0 + 127, 0:256], [], [bp_])
                    if last:
                        G([], [bn_], lambda tn=tn: nc.gpsimd.memset(tn[:], 0.0))
                        D(tn[0:127, :], vtok[r0 + 1:r0 + 128, 0:256], [], [bn_])
                    else:
                        D(tn[:], vtok[r0 + 1:r0 + 129, 0:256], [], [bn_])
                    V([bc_, b_vsh], [b_Vt[i]], lambda i=i, tc=tc: nc.vector.tensor_tensor(Vt[:, i, :], tc[:], vsh[:, 1, :], ALU.mult))
                    G([bp_, b_vsh], [bp_], lambda tp=tp: nc.gpsimd.tensor_tensor(tp[:], tp[:], vsh[:, 0, :], ALU.mult))
                    G([bn_, b_vsh], [bn_], lambda tn=tn: nc.gpsimd.tensor_tensor(tn[:], tn[:], vsh[:, 2, :], ALU.mult))
                    V([bp_, b_Vt[i]], [b_Vt[i]], lambda i=i, tp=tp: nc.vector.tensor_tensor(Vt[:, i, :], Vt[:, i, :], tp[:], ALU.add))
                    V([bn_, b_Vt[i]], [b_Vt[i]], lambda i=i, tn=tn: nc.vector.tensor_tensor(Vt[:, i, :], Vt[:, i, :], tn[:], ALU.add))
                    A([b_Vt[i]], [b_Vt[i]], lambda i=i: nc.scalar.copy(Vtb[:, i, :], Vt[:, i, :]))
                f.barrier()
            if RW_STOP == 1:
                return

            blocks = [(0, 256)] + [(256 + 256 * q, 256) for q in range(8)]
            order = {0: list(range(9)), 1: [0] + list(range(8, 0, -1))}
            with ExitStack() as es2:
                BW = 256
                rawS = [[(sbt(es2, f"rawS{d}{q}", [128, BW + 2]), Buf()) for q in range(2)] for d in range(2)]
                rawC = [[[(sbt(es2, f"rawC{d}{j}{q}", [128, BW + 2]), Buf()) for q in range(2)] for j in range(2)] for d in range(2)]
                tmpC = [[[(sbt(es2, f"tmpC{d}{j}{q}", [128, BW]), Buf()) for q in range(2)] for j in range(2)] for d in range(2)]
                rT = [[sbt(es2, f"rT{d}{j}", [128, BW]) for j in range(2)] for d in range(2)]
                kT = [[sbt(es2, f"kT{d}{j}", [128, BW]) for j in range(2)] for d in range(2)]
                kkT = [[sbt(es2, f"kkT{d}{j}", [128, BW]) for j in range(2)] for d in range(2)]
                twd = [sbt(es2, f"twd{d}", [128, BW]) for d in range(2)]
                tad = [sbt(es2, f"tad{d}", [128, BW]) for d in range(2)]
                names = ["t0", "t1", "t2", "bb", "kd", "p1", "e1"]
                rows = {nm: [[sbt(es2, f"{nm}{d}{j}", [128, BW]) for j in range(2)] for d in range(2)] for nm in names}
                q12 = [[sbt(es2, f"q12{d}{j}", [128, 2, 2, 128]) for j in range(2)] for d in range(2)]
                tot = [[sbt(es2, f"tot{d}{j}", [128, 4]) for j in range(2)] for d in range(2)]
                gC = [[sbt(es2, f"gC{d}{j}", [128, 4]) for j in range(2)] for d in range(2)]
                b_row = [[Buf() for j in range(2)] for d in range(2)]
                b_sh = [Buf() for d in range(2)]
                bnames = ["p1b", "p2b", "e1b", "e2b"]
                brows = {nm: [[sbt(es2, f"{nm}{d}{j}", [128, BW], BF16) for j in range(2)] for d in range(2)] for nm in bnames}
                q12b = [[sbt(es2, f"q12b{d}{j}", [128, 2, 2, 128], BF16) for j in range(2)] for d in range(2)]
                Mb = [[sbt(es2, f"Mb{d}{j}", [128, 64], BF16) for j in range(2)] for d in range(2)]
                for d in range(2):
                    for j in range(2):
                        G([], [b_M[d][j]], lambda d=d, j=j: nc.gpsimd.memset(Mb[d][j][:], 0.0))
                bankX = pst(es2, "bankX", [128, 512]); b_bankX = Buf()
                pM = [[bankX[:, (2 * d + j) * 64:(2 * d + j) * 64 + 64] for j in range(2)] for d in range(2)]
                b_pM = [[b_bankX for j in range(2)] for d in range(2)]
                ps5 = Slots([bankX[:, 256:512]]); ps5.b = [b_bankX]
                pN = Rot(es2, "pN", 1, [128, 512], psum=True)
                pW = Rot(es2, "pW", 2, [128, 1024], psum=True)
                pQ = carve(es2, "pQ", 1, 256)
                pEbank = pst(es2, "pEb", [128, 1024], BF16); b_pE = Buf()
                pE = Slots([pEbank[:, q * 256:(q + 1) * 256] for q in range(4)]); pE.b = [b_pE] * 4
                NCH = 16
                cA1 = [sbt(es2, f"cA1_{k}", [128, 256], BF16) for k in range(NCH)]
                cA2 = [sbt(es2, f"cA2_{k}", [128, 256], BF16) for k in range(NCH)]
                cbA = [(Buf(), Buf()) for k in range(NCH)]
                gPR = [Rot(es2, f"gPR{g}_", 3, [128, 4, 256], BF16) for g in range(4)]
                gPT = [Rot(es2, f"gPT{g}_", 3, [128, 4, 128], BF16) for g in range(4)]
                gSq = [Rot(es2, f"gSq{g}_", 2, [128, 4, 64], BF16) for g in range(4)]
                cE = [sbt(es2, f"cE_{k}", [128, 256], BF16) for k in range(NCH // 2)]
                cbE = [Buf() for k in range(NCH // 2)]

                def load_conv(dst, b_dst, frow, nrows, rawt, tap_chunk, c0, n):
                    rt, br = rawt
                    seq_lo, seq_hi = (0, L) if c0 < L else (L, T)
                    lo = max(seq_lo, c0 - 1); hi = min(seq_hi, c0 + n + 1)
                    if lo > c0 - 1 or hi < c0 + n + 1:
                        G([], [br], lambda rt=rt: nc.gpsimd.memset(rt[:], 0.0))
                    D(rt[0:nrows, lo - (c0 - 1):hi - (c0 - 1)], zT[frow:frow + nrows, lo:hi], [], [br])
                    w = lambda tap: cp[0:nrows, tap_chunk * 3 + tap:tap_chunk * 3 + tap + 1]
                    V([br, b_cp], [b_dst], lambda: nc.vector.tensor_scalar(dst[0:nrows, 0:n], rt[0:nrows, 1:n + 1], w(1), None, ALU.mult))
                    V([br, b_cp, b_dst], [b_dst], lambda: nc.vector.scalar_tensor_tensor(dst[0:nrows, 0:n], rt[0:nrows, 0:n], w(0), dst[0:nrows, 0:n], ALU.mult, ALU.add))
                    V([br, b_cp, b_dst], [b_dst], lambda: nc.vector.scalar_tensor_tensor(dst[0:nrows, 0:n], rt[0:nrows, 2:n + 2], w(2), dst[0:nrows, 0:n], ALU.mult, ALU.add))

                def prep_shared(d, bi):
                    c0, n = blocks[bi]
                    load_conv(twd[d], b_sh[d], OFF_WD, 128, rawS[d][0], 6, c0, n)
                    A([b_sh[d]], [b_sh[d]], lambda: nc.scalar.activation(twd[d][:, 0:n], twd[d][:, 0:n], AF.Tanh))
                    load_conv(tad[d], b_sh[d], OFF_AD, 128, rawS[d][1], 7, c0, n)

                def prep_combo(d, j, bi):
                    c0, n = blocks[bi]
                    nch = n // 128
                    hp = slice(64 * d, 64 * d + 64)
                    tmpi = [0]

                    def tmp_get():
                        t = tmpC[d][j][tmpi[0] % 2]; tmpi[0] += 1
                        return t
                    if True:
                        R = {nm: rows[nm][d][j] for nm in names}
                        br_ = b_row[d][j]
                        load_conv(rT[d][j], br_, OFF_R + 128 * j, 128, rawC[d][j][0], j, c0, n)
                        yield
                        load_conv(kT[d][j], br_, OFF_K + 128 * j, 128, rawC[d][j][1], 2 + j, c0, n)
                        yield
                        V([br_, b_cp], [br_], lambda: nc.vector.tensor_scalar(kkT[d][j][:, 0:n], kT[d][j][:, 0:n], cp[:, 38 + j:39 + j], None, ALU.mult))
                        tq, btq = tmp_get()
                        G([br_], [btq], lambda: nc.gpsimd.tensor_tensor(tq[:, 0:n], kkT[d][j][:, 0:n], kkT[d][j][:, 0:n], ALU.mult))
                        p5, bp5 = ps5.get()
                        P([btq, b_cst], [bp5], lambda: nc.tensor.matmul(p5[:, 0:n], blk[:], tq[:, 0:n], start=True, stop=True))
                        A([bp5], [btq], lambda: nc.scalar.activation(tq[:, 0:n], p5[:, 0:n], AF.Sqrt, bias=eps_t[:, 2:3], scale=1.0))
                        yield
                        V([btq], [btq], lambda: nc.vector.reciprocal(tq[:, 0:n], tq[:, 0:n]))
                        V([btq, br_], [br_], lambda: nc.vector.tensor_tensor(kkT[d][j][:, 0:n], kkT[d][j][:, 0:n], tq[:, 0:n], ALU.mult))
                        yield
                        p5, bp5 = ps5.get()
                        P([b_sh[d], b_up], [bp5], lambda: nc.tensor.matmul(p5[:, 0:n], wup[hp, j * 128:(j + 1) * 128], twd[d][hp, 0:n], start=True, stop=True))
                        A([bp5, b_cp], [br_], lambda: nc.scalar.activation(R["t0"][:, 0:n], p5[:, 0:n], AF.Sigmoid, bias=cp[:, 30 + d * 2 + j:31 + d * 2 + j], scale=1.0))
                        yield
                        p5, bp5 = ps5.get()
                        P([b_sh[d], b_up], [bp5], lambda: nc.tensor.matmul(p5[:, 0:n], aup[hp, j * 128:(j + 1) * 128], tad[d][hp, 0:n], start=True, stop=True))
                        A([bp5, b_cp], [br_], lambda: nc.scalar.activation(R["bb"][:, 0:n], p5[:, 0:n], AF.Sigmoid, bias=cp[:, 34 + d * 2 + j:35 + d * 2 + j], scale=1.0))
                        yield
                        V([br_, b_cp], [br_], lambda: nc.vector.tensor_scalar(R["kd"][:, 0:n], R["bb"][:, 0:n], -1.0, cp[:, 40 + j:41 + j], ALU.add, ALU.mult))
                        V([br_], [br_], lambda: nc.vector.scalar_tensor_tensor(R["kd"][:, 0:n], R["kd"][:, 0:n], 1.0, kT[d][j][:, 0:n], ALU.add, ALU.mult))
                        V([br_], [br_], lambda: nc.vector.tensor_tensor(R["bb"][:, 0:n], R["bb"][:, 0:n], kkT[d][j][:, 0:n], ALU.mult))
                        yield
                        tq, btq = tmp_get()
                        V([br_, b_cp], [btq], lambda: nc.vector.scalar_tensor_tensor(tq[:, 0:n], rT[d][j][:, 0:n], cp[:, 42 + j:43 + j], R["kd"][:, 0:n], ALU.mult, ALU.mult))
                        for c in range(nch):
                            ti = c0 // 128 + c
                            pq, bpq = pQ.get()
                            P([btq, b_cst], [bpq], lambda c=c, pq=pq: nc.tensor.matmul(pq[:, 0:2], tq[:, c * 128:(c + 1) * 128], hsel[:], start=True, stop=True))
                            V([bpq, b_bacc[ti]], [b_bacc[ti]], lambda ti=ti, pq=pq: nc.vector.tensor_tensor(bacc[:, ti, 2 * j:2 * j + 2], bacc[:, ti, 2 * j:2 * j + 2], pq[:, 0:2], ALU.add))
                        yield
                        V([br_, b_cst], [br_], lambda: nc.vector.tensor_tensor_scan(R["t1"][:, 0:n], scm[:, 0:n], R["t0"][:, 0:n], 0.0, ALU.mult, ALU.add))
                        V([br_], [br_], lambda: nc.vector.tensor_tensor(R["t0"][:, 0:n], R["t1"][:, 0:n], R["t0"][:, 0:n], ALU.subtract))
                        V([br_], [br_], lambda: nc.vector.tensor_copy(tot[d][j][:, 0:nch], R["t1"][:, 127:n:128]))
                        A([br_], [br_], lambda: nc.scalar.activation(gC[d][j][:, 0:nch], tot[d][j][:, 0:nch], AF.Exp, scale=-C0))
                        yield
                        totb = tot[d][j][:, 0:nch].unsqueeze(2).broadcast_to([128, nch, 128])
                        v3 = lambda ap: ap[:, 0:n].rearrange("p (c t) -> p c t", t=128)
                        q1v = q12[d][j][:, 0:nch, 0, :]; q2v = q12[d][j][:, 0:nch, 1, :]
                        if d == 0:
                            V([br_], [br_], lambda: nc.vector.tensor_tensor(v3(R["t2"]), totb, v3(R["t1"]), ALU.subtract))
                            A([br_], [br_], lambda: nc.scalar.activation(q1v, v3(R["t0"]), AF.Exp, scale=-C0))
                            A([br_], [br_], lambda: nc.scalar.activation(q2v, v3(R["t1"]), AF.Exp, scale=-C0))
                            A([br_], [br_], lambda: nc.scalar.activation(R["p1"][:, 0:n], R["t1"][:, 0:n], AF.Exp, scale=C0))
                            A([br_], [br_], lambda: nc.scalar.activation(R["e1"][:, 0:n], R["t2"][:, 0:n], AF.Exp, scale=-C0))
                        else:
                            V([br_], [br_], lambda: nc.vector.tensor_tensor(v3(R["t2"]), totb, v3(R["t1"]), ALU.subtract))
                            V([br_], [br_], lambda: nc.vector.tensor_tensor(v3(R["t1"]), totb, v3(R["t0"]), ALU.subtract))
                            A([br_], [br_], lambda: nc.scalar.activation(q1v, v3(R["t2"]), AF.Exp, scale=-C0))
                            A([br_], [br_], lambda: nc.scalar.activation(q2v, v3(R["t1"]), AF.Exp, scale=-C0))
                            A([br_], [br_], lambda: nc.scalar.activation(R["p1"][:, 0:n], R["t1"][:, 0:n], AF.Exp, scale=C0))
                            A([br_], [br_], lambda: nc.scalar.activation(R["e1"][:, 0:n], R["t0"][:, 0:n], AF.Exp, scale=-C0))
                        yield
                        BR = {nm: brows[nm][d][j] for nm in bnames}
                        q1b = q12b[d][j][:, 0:nch, 0, :]; q2b = q12b[d][j][:, 0:nch, 1, :]
                        V([br_], [br_], lambda: nc.vector.tensor_tensor(q1b, q1v, v3(kkT[d][j]), ALU.mult))
                        G([br_], [br_], lambda: nc.gpsimd.tensor_tensor(q2b, q2v, v3(rT[d][j]), ALU.mult))
                        yield
                        V([br_], [br_], lambda: nc.vector.tensor_tensor(BR["p2b"][:, 0:n], R["p1"][:, 0:n], R["kd"][:, 0:n], ALU.mult))
                        G([br_], [br_], lambda: nc.gpsimd.tensor_tensor(BR["p1b"][:, 0:n], R["p1"][:, 0:n], R["bb"][:, 0:n], ALU.mult))
                        V([br_], [br_], lambda: nc.vector.tensor_tensor(BR["e2b"][:, 0:n], R["e1"][:, 0:n], R["bb"][:, 0:n], ALU.mult))
                        G([br_], [br_], lambda: nc.gpsimd.tensor_tensor(BR["e1b"][:, 0:n], R["e1"][:, 0:n], R["kd"][:, 0:n], ALU.mult))

                def score_chain(k, d, j, hh, bi, c):
                    cs_ = slice(c * 128, (c + 1) * 128)
                    br_ = b_row[d][j]
                    BR = {nm: brows[nm][d][j] for nm in bnames}
                    mT = msk[:, 0:2, :] if d == 0 else msk[:, 2:4, :]
                    mN = msk[:, 2, :] if d == 0 else msk[:, 0, :]
                    hp = slice(64 * hh, 64 * hh + 64)
                    ke = k // 2; gi = k // 4; m = k % 4
                    if hh == 0:
                        pe_, bpe = pE.get()
                        P([br_, b_identb], [bpe], lambda: nc.tensor.transpose(pe_[:, 0:128], BR["e1b"][:, cs_], identb[:]))
                        P([br_, b_identb], [bpe], lambda: nc.tensor.transpose(pe_[:, 128:256], BR["e2b"][:, cs_], identb[:]))
                        A([bpe], [cbE[ke]], lambda: nc.scalar.copy(cE[ke][:], pe_[:]))
                    yield
                    q12c = q12b[d][j][hp, c, :, :].rearrange("p a t -> p (a t)")
                    q1c = q12b[d][j][hp, c, 0, :]
                    A1m, A2m = cA1[k], cA2[k]; bA1, bA2 = cbA[k]
                    a1f, ba1 = pW.get(); a1 = a1f[:, 0:256]
                    P([br_], [ba1], lambda: nc.tensor.matmul(a1, BR["p1b"][hp, cs_], q12c, start=True, stop=True))
                    V([ba1, b_msk], [bA1], lambda: nc.vector.tensor_tensor(A1m[:].rearrange("p (a t) -> p a t", a=2), a1.rearrange("p (a t) -> p a t", a=2), mT, ALU.mult))
                    PR, bPR = gcur[gi][0]; PT, bPT = gcur[gi][1]
                    V([ba1, b_msk], [bPR], lambda: nc.vector.scalar_tensor_tensor(PR[:, m, 0:128], a1[:, 0:128], -1.0, mT[:, 0, :], ALU.mult, ALU.mult))
                    A([b_identb], [bPR], lambda: nc.scalar.copy(PR[:, m, 128:256], identb[:]))
                    yield
                    a2f, ba2 = pW.get(); a2 = a2f[:, 0:256]
                    P([br_], [ba2], lambda: nc.tensor.matmul(a2, BR["p2b"][hp, cs_], q12c, start=True, stop=True))
                    V([ba2, b_msk], [bA2], lambda: nc.vector.tensor_tensor(A2m[:].rearrange("p (a t) -> p a t", a=2), a2.rearrange("p (a t) -> p a t", a=2), mT, ALU.mult))
                    yield
                    anf, ban = pN.get(); an = anf[:, 0:128]
                    P([br_], [ban], lambda: nc.tensor.matmul(an, q1c, BR["p1b"][hp, cs_], start=True, stop=True))
                    V([ban, b_msk], [bPT], lambda: nc.vector.scalar_tensor_tensor(PT[:, m, :], an, -1.0, mN, ALU.mult, ALU.mult))
                    yield

                def neumann_group(gi):
                    (PR, bPR), (PT, bPT) = gcur[gi]
                    for lev in range(7):
                        if lev < 6:
                            w1, bw1 = pW.get()
                            w1v = w1[:].rearrange("p (m c) -> p m c", c=256)
                            for m in range(4):
                                P([bPT, bPR], [bw1], lambda: nc.tensor.matmul(w1[:, m * 256:(m + 1) * 256], PT[:, m, :], PR[:, m, :], start=True, stop=True))
                            PRn, bPRn = gPR[gi].get()
                            A([bw1], [bPRn], lambda: nc.scalar.copy(PRn[:, :, 0:128], w1v[:, :, 0:128]))
                            V([bw1, bPR], [bPRn], lambda: nc.vector.tensor_tensor(PRn[:, :, 128:256], w1v[:, :, 128:256], PR[:, :, 128:256], ALU.add))
                            w2, bw2 = pN.get()
                            for m in range(4):
                                P([bPT, bPR], [bw2], lambda: nc.tensor.matmul(w2[:, m * 128:(m + 1) * 128], PR[:, m, 0:128], PT[:, m, :], start=True, stop=True))
                            PTn, bPTn = gPT[gi].get()
                            w2v = w2[:].rearrange("p (m c) -> p m c", c=128)
                            if (gi + lev) % 2 == 0:
                                A([bw2], [bPTn], lambda: nc.scalar.copy(PTn[:], w2v))
                            else:
                                V([bw2], [bPTn], lambda: nc.vector.tensor_copy(PTn[:], w2v))
                            PR, bPR, PT, bPT = PRn, bPRn, PTn, bPTn
                        else:
                            w2, bw2 = pN.get()
                            for m in range(4):
                                P([bPT, bPR], [bw2], lambda: nc.tensor.matmul(w2[:, m * 128:(m + 1) * 128], PT[:, m, :], PR[:, m, 128:256], start=True, stop=True))
                            PRn, bPRn = gPR[gi].get()
                            w2v = w2[:].rearrange("p (m c) -> p m c", c=128)
                            V([bw2, bPR], [bPRn], lambda: nc.vector.tensor_tensor(PRn[:, :, 128:256], w2v, PR[:, :, 128:256], ALU.add))
                            PR, bPR = PRn, bPRn
                        yield
                    gR[gi] = (PR, bPR)

                def seq_group(gi, d, bi, c):
                    c0, n = blocks[bi]
                    ti = c0 // 128 + c
                    PRf, bR = gR[gi]
                    hps = [slice(0, 64), slice(64, 128)]
                    ppg, bpp = pQ.get()
                    for m in range(4):
                        j, hh = m // 2, m % 2; k = 4 * gi + m; hp = hps[hh]
                        hcol = slice((2 * j + hh) * 64, (2 * j + hh) * 64 + 64)
                        P([b_row[d][j], b_M[d][j]], [bpp], lambda: nc.tensor.matmul(ppg[:, m * 64:(m + 1) * 64], q12b[d][j][hp, c, 0, :], Mb[d][j][hp, :], start=True, stop=False))
                        P([cbA[k][1], b_Vt[ti]], [bpp], lambda: nc.tensor.matmul(ppg[:, m * 64:(m + 1) * 64], cA2[k][:, 0:128], Vtb[:, ti, hcol], start=False, stop=True))
                    Psb, bPsb = gSq[gi].get()
                    A([bpp], [bPsb], lambda: nc.scalar.copy(Psb[:], ppg[:].rearrange("p (m c) -> p m c", c=64)))
                    yield
                    upg, bup = pQ.get()
                    for m in range(4):
                        P([bR, bPsb], [bup], lambda: nc.tensor.matmul(upg[:, m * 64:(m + 1) * 64], PRf[:, m, 128:256], Psb[:, m, :], start=True, stop=True))
                    nU, bnU = gSq[gi].get()
                    A([bup], [bnU], lambda: nc.scalar.mul(nU[:], upg[:].rearrange("p (m c) -> p m c", c=64), -1.0))
                    yield
                    ypg, byp = pQ.get()
                    for m in range(4):
                        j, hh = m // 2, m % 2; k = 4 * gi + m; hp = hps[hh]
                        hcol = slice((2 * j + hh) * 64, (2 * j + hh) * 64 + 64)
                        P([b_row[d][j], b_M[d][j]], [byp], lambda: nc.tensor.matmul(ypg[:, m * 64:(m + 1) * 64], q12b[d][j][hp, c, 1, :], Mb[d][j][hp, :], start=True, stop=False))
                        P([cbA[k][1], b_Vt[ti]], [byp], lambda: nc.tensor.matmul(ypg[:, m * 64:(m + 1) * 64], cA2[k][:, 128:256], Vtb[:, ti, hcol], start=False, stop=False))
                        P([cbA[k][0], bnU], [byp], lambda: nc.tensor.matmul(ypg[:, m * 64:(m + 1) * 64], cA1[k][:, 128:256], nU[:, m, :], start=False, stop=True))
                    V([byp, b_yacc[ti]], [b_yacc[ti]], lambda: nc.vector.tensor_tensor(yacc[:, ti, :], yacc[:, ti, :], ypg[:], ALU.add))
                    yield
                    for m in range(4):
                        j, hh = m // 2, m % 2; k = 4 * gi + m; hp = hps[hh]
                        hcol = slice((2 * j + hh) * 64, (2 * j + hh) * 64 + 64)
                        E_, bE = cE[k // 2], cbE[k // 2]
                        pm = pM[d][j]
                        P([bE, b_Vt[ti]], [b_pM[d][j]], lambda: nc.tensor.matmul(pm[hp, :], E_[:, 64 * hh:64 * hh + 64], Vtb[:, ti, hcol], start=True, stop=False))
                        P([bE, bnU], [b_pM[d][j]], lambda: nc.tensor.matmul(pm[hp, :], E_[:, 128 + 64 * hh:128 + 64 * hh + 64], nU[:, m, :], start=False, stop=True))
                    for j in range(2):
                        V([b_pM[d][j], b_M[d][j], b_row[d][j]], [b_M[d][j]], lambda: nc.vector.scalar_tensor_tensor(
                            Mst[d][j][:], Mst[d][j][:], gC[d][j][:, c:c + 1], pM[d][j][:], ALU.mult, ALU.add))
                        G([b_M[d][j]], [b_M[d][j]], lambda: nc.gpsimd.tensor_copy(Mb[d][j][:], Mst[d][j][:]))
                    yield

                def run_rr(gens):
                    gens = list(gens)
                    while gens:
                        nxt = []
                        for g in gens:
                            try:
                                next(g); nxt.append(g)
                            except StopIteration:
                                pass
                        gens = nxt

                gR = {}; gcur = {}
                for step in range(9):
                    if RW_STOP in (2, 3) and step > 0:
                        break
                    for d in range(2):
                        prep_shared(d, order[d][step])
                    run_rr([prep_combo(d, j, order[d][step]) for d in range(2) for j in range(2)])
                    if RW_STOP == 2:
                        break
                    chains = []; groups = []
                    for cc in range(2):
                        for d in range(2):
                            bi = order[d][step]
                            c = cc if d == 0 else 1 - cc
                            groups.append((len(groups), d, bi, c))
                            for j in range(2):
                                for hh in range(2):
                                    chains.append((len(chains), d, j, hh, bi, c))
                    for gi in range(4):
                        gcur[gi] = (gPR[gi].get(), gPT[gi].get())
                    run_rr([score_chain(*ch) for ch in chains])
                    run_rr([neumann_group(gi) for gi in range(4)])
                    for cc in range(2):
                        run_rr([seq_group(*g) for g in groups[cc * 2:(cc + 1) * 2]])
                f.barrier()
            if RW_STOP in (2, 3):
                return

            with ExitStack() as es2:
                gs0 = sbt(es2, "gs0", [128, T]); gs1 = sbt(es2, "gs1", [32, T]); b_gs = Buf()
                rw = sbt(es2, "rw", [128, T + 2]); b_rw = Buf()
                for (dst, frow, nrows, tapc) in ((gs0, OFF_GD, 128, 8), (gs1, OFF_GD + 128, 32, 9)):
                    G([], [b_rw], lambda: nc.gpsimd.memset(rw[:], 0.0))
                    D(rw[0:nrows, 1:T + 1], zT[frow:frow + nrows, :], [], [b_rw])
                    w = lambda tap, tapc=tapc, nrows=nrows: cp[0:nrows, tapc * 3 + tap:tapc * 3 + tap + 1]
                    V([b_rw, b_cp], [b_gs], lambda: nc.vector.tensor_scalar(dst[0:nrows, :], rw[0:nrows, 1:T + 1], w(1), None, ALU.mult))
                    for (a, b) in ((0, L), (L, T)):
                        V([b_rw, b_cp, b_gs], [b_gs], lambda a=a, b=b: nc.vector.scalar_tensor_tensor(
                            dst[0:nrows, a + 1:b], rw[0:nrows, a + 1:b], w(0), dst[0:nrows, a + 1:b], ALU.mult, ALU.add))
                        V([b_rw, b_cp, b_gs], [b_gs], lambda a=a, b=b: nc.vector.scalar_tensor_tensor(
                            dst[0:nrows, a:b - 1], rw[0:nrows, a + 2:b + 1], w(2), dst[0:nrows, a:b - 1], ALU.mult, ALU.add))
                    A([b_gs], [b_gs], lambda: nc.scalar.activation(dst[0:nrows, :], dst[0:nrows, :], AF.Sigmoid))
                gb = sbt(es2, "gb", [128, 2, 256]); b_gb = Buf()
                D(gb[:, 0, :], a_lnx_g[l, :].partition_broadcast(128), [], [b_gb])
                D(gb[:, 1, :], a_lnx_b[l, :].partition_broadcast(128), [], [b_gb])
                pg = carve(es2, "pg", 1, 256)
                st4 = Rot(es2, "st4", 2, [128, 4, 6]); mv4 = Rot(es2, "mv4", 2, [128, 4, 2]); rs4 = Rot(es2, "rs4", 2, [128, 4])
                yn = Rot(es2, "yn", 2, [128, 256]); oo = Rot(es2, "oo", 2, [128, 256])
                for i in range(NT):
                    g_p, bg = pg.get()
                    P([b_gs, b_gup], [bg], lambda: nc.tensor.matmul(g_p[:], gs0[:, i * 128:(i + 1) * 128], gup0[:], start=True, stop=False))
                    P([b_gs, b_gup], [bg], lambda: nc.tensor.matmul(g_p[:], gs1[0:32, i * 128:(i + 1) * 128], gup1[:], start=False, stop=True))
                    s4, bs4 = st4.get(); m4, bm4 = mv4.get(); r4, br4 = rs4.get(); y_n, byn = yn.get(); o_t, bo = oo.get()
                    for h in range(4):
                        V([b_yacc[i]], [bs4], lambda h=h: nc.vector.bn_stats(s4[:, h, :], yacc[:, i, h * 64:(h + 1) * 64]))
                    for h in range(4):
                        V([bs4], [bm4], lambda h=h: nc.vector.bn_aggr(m4[:, h, :], s4[:, h, :]))
                    A([bm4], [br4], lambda: nc.scalar.activation(r4[:], m4[:, :, 1], AF.Sqrt, bias=eps_t[:, 1:2], scale=1.0))
                    V([br4], [br4], lambda: nc.vector.reciprocal(r4[:], r4[:]))
                    for h in range(4):
                        V([b_yacc[i], bm4, br4], [byn], lambda h=h: nc.vector.tensor_scalar(
                            y_n[:, h * 64:(h + 1) * 64], yacc[:, i, h * 64:(h + 1) * 64], m4[:, h, 0:1], r4[:, h:h + 1], ALU.subtract, ALU.mult))
                    G([byn, b_gb], [byn], lambda: nc.gpsimd.tensor_tensor(y_n[:], y_n[:], gb[:, 0, :], ALU.mult))
                    G([byn, b_gb], [byn], lambda: nc.gpsimd.tensor_tensor(y_n[:], y_n[:], gb[:, 1, :], ALU.add))
                    for h in range(4):
                        V([byn, b_bacc[i], b_Vt[i]], [byn], lambda h=h: nc.vector.scalar_tensor_tensor(
                            y_n[:, h * 64:(h + 1) * 64], Vt[:, i, h * 64:(h + 1) * 64], bacc[:, i, h:h + 1], y_n[:, h * 64:(h + 1) * 64], ALU.mult, ALU.add))
                    V([byn, bg], [bo], lambda: nc.vector.tensor_tensor(o_t[:], y_n[:], g_p[:], ALU.mult))
                    D(omix[i * 128:(i + 1) * 128, 0:256], o_t[:], [bo], [])
                f.barrier()

    def phase_attn_b(l):
        with_ctx = l < DEPTH - 1
        with ExitStack() as es:
            cosT = sbt(es, "cosT", [64, S]); sinT = sbt(es, "sinT", [64, S]); rPT = sbt(es, "rPT", [64, 64]); b_c = Buf()
            D(cosT[:], ccos[:, :], [], [b_c]); D(sinT[:], csin[:, :], [], [b_c]); D(rPT[:], cropeP[:, :], [], [b_c])
            mkf = sbt(es, "mkf", [128, 4, 128]); mkb = sbt(es, "mkb", [128, 4, 128], BF16); b_mk = Buf()
            D(mkf[:], cmasks[:, :, :], [], [b_mk])
            V([b_mk], [b_mk], lambda: nc.vector.tensor_copy(mkb[:], mkf[:]))
            esk = sbt(es, "esk", [128, 6]); b_esk = Buf()
            D(esk[:], b_sink[l, :].partition_broadcast(128), [], [b_esk])
            A([b_esk], [b_esk], lambda: nc.scalar.activation(esk[:], esk[:], AF.Exp))
            kTb = sbt(es, "kTb", [64, 2, T], BF16); b_k = [Buf() for _ in range(2)]
            qTb = sbt(es, "qTb", [64, 2, NT, 3, 128], BF16); b_q = [Buf() for _ in range(6)]
            Va = sbt(es, "Va", [128, 2, NT, 65], BF16); b_Va = Buf()
            G([], [b_Va], lambda: nc.gpsimd.memset(Va[:], 1.0))
            with ExitStack() as es2:
                vf = sbt(es2, "vf", [128, NT, 128]); b_vf = Buf()
                D(vf[:], vtok[:, 256:384].rearrange("(n p) f -> p n f", p=128), [], [b_vf])
                for g in range(2):
                    V([b_vf, b_Va], [b_Va], lambda g=g: nc.vector.tensor_copy(Va[:, g, :, 0:64], vf[:, :, g * 64:(g + 1) * 64]))
                raw = Rot(es2, "rawb", 2, [64, T])
                pr = Rot(es2, "pr", 2, [128, 512], psum=True)
                t1 = Rot(es2, "t1b", 2, [64, 512]); t2 = Rot(es2, "t2b", 2, [64, 512])
                for hd in range(8):
                    rt, br = raw.get()
                    frow = OFF_QB + 64 * hd if hd < 6 else OFF_KB + 64 * (hd - 6)
                    D(rt[:], zT[frow:frow + 64, :], [], [br])
                    if hd < 6:
                        g, i3 = hd // 3, hd % 3
                        dst_ctx = qTb[:, g, 0:2, i3, :]
                        wb_ = b_q[hd]
                    else:
                        g = hd - 6
                        dst_ctx = kTb[:, g, 0:L].rearrange("p (n t) -> p n t", t=128)
                        wb_ = b_k[g]
                    A([br], [wb_], lambda rt=rt, dst_ctx=dst_ctx: nc.scalar.copy(dst_ctx, rt[:, 0:L].rearrange("p (n t) -> p n t", t=128)))
                    for q4 in range(4):
                        cs_ = slice(L + q4 * 512, L + (q4 + 1) * 512); ps_ = slice(q4 * 512, (q4 + 1) * 512)
                        p_t, bp = pr.get(); a_t, ba = t1.get(); c_t, bc = t2.get()
                        P([br, b_c], [bp], lambda: nc.tensor.matmul(p_t[0:64, :], rPT[:], rt[:, cs_], start=True, stop=True))
                        V([br, b_c], [ba], lambda: nc.vector.tensor_tensor(a_t[:], rt[:, cs_], cosT[:, ps_], ALU.mult))
                        V([bp, b_c], [bc], lambda: nc.vector.tensor_tensor(c_t[:], p_t[0:64, :], sinT[:, ps_], ALU.mult))
                        if hd < 6:
                            dst = qTb[:, g, 2 + 4 * q4:2 + 4 * q4 + 4, i3, :]
                        else:
                            dst = kTb[:, g, cs_].rearrange("p (n t) -> p n t", t=128)
                        G([ba, bc], [wb_], lambda: nc.gpsimd.tensor_tensor(dst, a_t[:].rearrange("p (n t) -> p n t", t=128), c_t[:].rearrange("p (n t) -> p n t", t=128), ALU.add))
                f.barrier()
            psc = Rot(es, "psc", 5, [128, 512], psum=True)
            pov = Rot(es, "pov", 2, [128, 512], psum=True)
            pt = Rot(es, "ptb", 16, [128, 384], BF16)
            ob = Rot(es, "obb", 3, [128, 384])
            dn = Rot(es, "dnb", 4, [128, 3])
            items = [(n, g) for n in range(NT) if not (n < 2 and not with_ctx) for g in range(2)]
            obufs = {}

            def stage1(n, g):
                kbs = [(0, None), (1, None)]
                if n >= 2:
                    if n - 1 >= 2: kbs.append((n - 1, 3))
                    kbs.append((n, None))
                    if n + 1 < NT: kbs.append((n + 1, 1))
                pts = []
                for (m, mk) in kbs:
                    s_p, bs = psc.get(); p_t, bpt = pt.get()
                    P([b_k[g]] + b_q[3 * g:3 * g + 3], [bs], lambda: nc.tensor.matmul(
                        s_p[:, 0:384], kTb[:, g, m * 128:(m + 1) * 128], qTb[:, g, n, :, :].rearrange("p a t -> p (a t)"), start=True, stop=True))
                    A([bs], [bpt], lambda: nc.scalar.activation(p_t[:], s_p[:, 0:384], AF.Exp, scale=SCALE))
                    if mk is not None:
                        G([bpt, b_mk], [bpt], lambda: nc.gpsimd.tensor_tensor(
                            p_t[:].rearrange("p (a t) -> p a t", a=3), p_t[:].rearrange("p (a t) -> p a t", a=3),
                            mkb[:, mk, :].unsqueeze(1).broadcast_to([128, 3, 128]), ALU.mult))
                    pts.append((p_t, bpt, m))
                return pts

            def stage2(n, g, pts):
                if g == 0:
                    obufs[n] = ob.get()
                o_t, bo = obufs[n]
                o_pf, bop = pov.get()
                o_p = o_pf[:, 0:195].rearrange("p (a e) -> p a e", e=65)
                for i3 in range(3):
                    for q, (p_t, bpt, m) in enumerate(pts):
                        P([bpt, b_Va], [bop], lambda: nc.tensor.matmul(
                            o_p[:, i3, :], p_t[:, i3 * 128:(i3 + 1) * 128], Va[:, g, m, :], start=(q == 0), stop=(q == len(pts) - 1)))
                d_t, bd = dn.get()
                V([bop, b_esk], [bd], lambda: nc.vector.tensor_tensor(d_t[:], o_p[:, :, 64], esk[:, 3 * g:3 * g + 3], ALU.add))
                V([bd], [bd], lambda: nc.vector.reciprocal(d_t[:], d_t[:]))
                for i3 in range(3):
                    hc = (3 * g + i3) * 64
                    V([bop, bd], [bo], lambda: nc.vector.tensor_scalar(o_t[:, hc:hc + 64], o_p[:, i3, 0:64], d_t[:, i3:i3 + 1], None, ALU.mult))
                if g == 1:
                    D(omix[n * 128:(n + 1) * 128, 256:640], o_t[:], [bo], [])

            prev = None
            for it in items:
                cur = (it, stage1(*it))
                if prev is not None:
                    stage2(prev[0][0], prev[0][1], prev[1])
                prev = cur
            stage2(prev[0][0], prev[0][1], prev[1])
            f.barrier()

    def phase_attn_c(l):
        with_ctx = l < DEPTH - 1
        with ExitStack() as es:
            qT = sbt(es, "qTn", [64, 6, T], BF16); kTn = sbt(es, "kTn", [64, 6, T], BF16); b_qk = Buf()
            for h in range(6):
                D(qT[:, h, :], zT[OFF_QN + 64 * h:OFF_QN + 64 * h + 64, :], [], [b_qk], eng=POOL)
                D(kTn[:, h, :], zT[OFF_KN + 64 * h:OFF_KN + 64 * h + 64, :], [], [b_qk], eng=POOL)
            Va = sbt(es, "Van", [128, NT, 6, 65], BF16); b_Va = Buf()
            G([], [b_Va], lambda: nc.gpsimd.memset(Va[:], 1.0))
            with ExitStack() as es2:
                vf = sbt(es2, "vfn", [128, NT, 384]); b_vf = Buf()
                D(vf[:], vtok[:, 384:768].rearrange("(n p) f -> p n f", p=128), [], [b_vf])
                V([b_vf, b_Va], [b_Va], lambda: nc.vector.tensor_copy(Va[:, :, :, 0:64], vf[:].rearrange("p n (h e) -> p n h e", e=64)))
                f.barrier()
            cv = sbt(es, "cv", [128, 64]); b_cv = Buf()
            D(cv[:], ccolvalid[:, :], [], [b_cv])
            ebf = Rot(es, "ebf", 2, [128, 15, 64]); ebb = sbt(es, "ebb", [128, 6, 15, 64], BF16); b_eb = [Buf() for _ in range(6)]
            oC = sbt(es, "oC", [128, NT, 384]); b_oC = [Buf() for _ in range(NT)]
            psc = Rot(es, "pscn", 3, [128, 1024], psum=True)
            pov = carve(es, "povn", 2, 128)
            pt = Rot(es, "ptn", 4, [128, 896], BF16)
            dn = Rot(es, "dnn", 4, [128, 1])
            for h in range(6):
                e_f, bef = ebf.get()
                D(e_f[0:64, :, :], rpbtab[l, h, :, :, :], [], [bef])
                D(e_f[64:128, :, :], rpbtab[l, h, :, :, :], [], [bef])
                A([bef], [bef], lambda: nc.scalar.activation(e_f[:], e_f[:], AF.Exp))
                V([bef, b_cv], [b_eb[h]], lambda: nc.vector.tensor_tensor(ebb[:, h, :, :], e_f[:], cv[:].unsqueeze(1).broadcast_to([128, 15, 64]), ALU.mult))
            items = [(h, n) for h in range(6) for n in range(NT) if not (n < 2 and not with_ctx)]

            def keyblocks(n):
                kbs = [(0, None), (1, None)]
                if n >= 2:
                    nb = n - 2
                    for kb in range(16):
                        subs = []
                        anyv = False
                        for i in range(2):
                            for jq in range(2):
                                kr = 2 * kb + i; qr = 2 * nb + jq
                                rs_ = min(max(qr - 4, 0), 24)
                                ok = rs_ <= kr < rs_ + 8
                                subs.append((i, jq, kr - qr + 7 if ok else None))
                                anyv = anyv or ok
                        if anyv:
                            kbs.append((kb + 2, subs))
                return kbs

            def stage1(h, n):
                kbs = keyblocks(n)
                nk = len(kbs)
                s_p, bs = psc.get(); p_t, bpt = pt.get()
                for q, (m, subs) in enumerate(kbs):
                    P([b_qk], [bs], lambda: nc.tensor.matmul(s_p[:, q * 128:(q + 1) * 128], kTn[:, h, m * 128:(m + 1) * 128], qT[:, h, n * 128:(n + 1) * 128], start=True, stop=True))
                A([bs], [bpt], lambda: nc.scalar.activation(p_t[:, 0:nk * 128], s_p[:, 0:nk * 128], AF.Exp, scale=SCALE))
                cnt = 0
                for q, (m, subs) in enumerate(kbs):
                    if subs is None:
                        continue
                    for (i, jq, ro) in subs:
                        eng, ee = (G, nc.gpsimd) if cnt % 2 == 0 else (V, nc.vector)
                        cnt += 1
                        sl = p_t[64 * i:64 * i + 64, q * 128 + 64 * jq:q * 128 + 64 * jq + 64]
                        if ro is None:
                            eng([bpt], [bpt], lambda: ee.memset(sl, 0.0))
                        else:
                            eng([bpt, b_eb[h]], [bpt], lambda: ee.tensor_tensor(sl, sl, ebb[64 * i:64 * i + 64, h, ro, :], ALU.mult))
                return (p_t, bpt, kbs)

            def stage2(h, n, st):
                p_t, bpt, kbs = st
                o_p, bop = pov.get()
                for q, (m, subs) in enumerate(kbs):
                    P([bpt, b_Va], [bop], lambda: nc.tensor.matmul(o_p[:, 0:65], p_t[:, q * 128:(q + 1) * 128], Va[:, m, h, :], start=(q == 0), stop=(q == len(kbs) - 1)))
                d_t, bd = dn.get()
                V([bop], [bd], lambda: nc.vector.reciprocal(d_t[:], o_p[:, 64:65]))
                V([bop, bd], [b_oC[n]], lambda: nc.vector.tensor_scalar(oC[:, n, h * 64:(h + 1) * 64], o_p[:, 0:64], d_t[:, 0:1], None, ALU.mult))

            prev = None
            for it in items:
                cur = (it, stage1(*it))
                if prev is not None:
                    stage2(prev[0][0], prev[0][1], prev[1])
                prev = cur
            stage2(prev[0][0], prev[0][1], prev[1])
            for n in range(NT):
                if n < 2 and not with_ctx:
                    continue
                D(omix[n * 128:(n + 1) * 128, 640:1024], oC[:, n, :], [b_oC[n]], [])
            f.barrier()

    def phase_out_moe(l):
        with_ctx = l < DEPTH - 1
        tiles = list(range(NT)) if with_ctx else list(range(2, NT))
        with ExitStack() as es:
            h2T = sbt(es, "h2T", [128, 8, T], BF16); b_h2T = [Buf() for _ in range(NT)]
            Wr = sbt(es, "Wr", [128, NT, 32]); b_Wr = [Buf() for _ in range(NT)]
            with ExitStack() as es2:
                mods = sbt(es2, "mods2", [128, 2, 3 * DM]); b_mods = Buf()
                D(mods[:, 0, :], modrow[1, 2 * DM:5 * DM].partition_broadcast(128), [], [b_mods])
                D(mods[:, 1, :], modrow[0, 2 * DM:5 * DM].partition_broadcast(128), [], [b_mods])
                G([b_mods], [b_mods], lambda: nc.gpsimd.tensor_scalar(mods[:, :, 2 * DM:3 * DM], mods[:, :, 2 * DM:3 * DM], 1.0, None, ALU.add))
                lgb = sbt(es2, "lgb", [128, 2, DM]); b_lgb = Buf()
                D(lgb[:, 0, :], ln1_g[l, :].partition_broadcast(128), [], [b_lgb])
                D(lgb[:, 1, :], ln1_b[l, :].partition_broadcast(128), [], [b_lgb])
                wo = sbt(es2, "wo", [128, 8, DM], BF16); b_wo = Buf()
                D(wo[:], w_out[l, :, :].rearrange("(k p) n -> p k n", p=128), [], [b_wo], eng=POOL)
                wr = sbt(es2, "wr", [128, 8, 36]); b_wr = Buf()
                D(wr[:], w_r[l, :, :].rearrange("(k p) n -> p k n", p=128), [], [b_wr])
                brb = sbt(es2, "brb", [128, 36]); D(brb[:], b_r[l, :].partition_broadcast(128), [], [b_wr])
                om = Rot(es2, "om", 2, [128, DM]); omb = Rot(es2, "omb", 2, [128, DM], BF16)
                oT = Rot(es2, "oT", 2, [128, 8, 128], BF16)
                xt = Rot(es2, "xt2", 2, [128, DM]); tt = Rot(es2, "tt", 2, [128, DM]); x1 = Rot(es2, "x1", 3, [128, DM])
                hf = Rot(es2, "hf", 2, [128, DM]); hb = Rot(es2, "hb2", 2, [128, DM], BF16)
                hT32 = Rot(es2, "hT32", 2, [128, 8, 128])
                st = Rot(es2, "st2", 4, [128, 2, 6]); mv = Rot(es2, "mv2", 4, [128, 2]); rs = Rot(es2, "rs2", 4, [128, 1])
                pTb = Rot(es2, "pTb", 2, [128, 8, 128], BF16, psum=True)
                pO = Rot(es2, "pO", 1, [128, 1024], psum=True)
                pT32 = Rot(es2, "pT32", 1, [128, 8, 128], psum=True)
                pL = carve(es2, "pL", 1, 64)
                sm = Rot(es2, "sm", 2, [128, 160])
                def tile_gen(i):
                    w = 0 if i < 2 else 1
                    o_m, bom = om.get(); o_b, bob = omb.get(); o_T, boT = oT.get()
                    D(o_m[:], omix[i * 128:(i + 1) * 128, :], [], [bom])
                    A([bom], [bob], lambda: nc.scalar.copy(o_b[:], o_m[:]))
                    p_t, bp = pTb.get()
                    for k in range(8):
                        P([bob, b_identb], [bp], lambda k=k: nc.tensor.transpose(p_t[:, k, :], o_b[:, k * 128:(k + 1) * 128], identb[:]))
                    V([bp], [boT], lambda: nc.vector.tensor_copy(o_T[:], p_t[:]))
                    p_o, bpo = pO.get()
                    for hf_ in range(2):
                        for k in range(8):
                            P([boT, b_wo], [bpo], lambda k=k, hf_=hf_: nc.tensor.matmul(
                                p_o[:, hf_ * 512:(hf_ + 1) * 512], o_T[:, k, :], wo[:, k, hf_ * 512:(hf_ + 1) * 512], start=(k == 0), stop=(k == 7)))
                    x_t, bx = xt.get(); t_t, bt = tt.get(); x_1, bx1 = x1.get()
                    D(x_t[:], xsrc(l, i), [], [bx])
                    V([bpo, b_mods], [bt], lambda: nc.vector.tensor_tensor(t_t[:], p_o[:], mods[:, w, 0:DM], ALU.mult))
                    V([bx, bt], [bt], lambda: nc.vector.scalar_tensor_tensor(t_t[:], x_t[:], ALPHA, t_t[:], ALU.mult, ALU.add))
                    s_t, bs = st.get(); m_t, bmv = mv.get(); r_t, br = rs.get()
                    layer_norm_stats(None, t_t, bt, (s_t, bs, m_t, bmv, r_t, br))
                    V([bt, bmv, br], [bt], lambda: nc.vector.tensor_scalar(t_t[:], t_t[:], m_t[:, 0:1], r_t[:, 0:1], ALU.subtract, ALU.mult))
                    G([bt, b_lgb], [bt], lambda: nc.gpsimd.tensor_tensor(t_t[:], t_t[:], lgb[:, 0, :], ALU.mult))
                    G([bt, b_lgb], [bx1], lambda: nc.gpsimd.tensor_tensor(x_1[:], t_t[:], lgb[:, 1, :], ALU.add))
                    D(xs[i * 128:(i + 1) * 128, :], x_1[:], [bx1], [])
                    yield
                    s_t, bs = st.get(); m_t, bmv = mv.get(); r_t, br = rs.get()
                    layer_norm_stats(None, x_1, bx1, (s_t, bs, m_t, bmv, r_t, br))
                    h_f, bhf = hf.get(); h_b, bhb = hb.get()
                    V([bx1, bmv, br], [bhf], lambda: nc.vector.tensor_scalar(h_f[:], x_1[:], m_t[:, 0:1], r_t[:, 0:1], ALU.subtract, ALU.mult))
                    G([bhf, b_mods], [bhf], lambda: nc.gpsimd.tensor_tensor(h_f[:], h_f[:], mods[:, w, 2 * DM:3 * DM], ALU.mult))
                    G([bhf, b_mods], [bhf], lambda: nc.gpsimd.tensor_tensor(h_f[:], h_f[:], mods[:, w, DM:2 * DM], ALU.add))
                    A([bhf], [bhb], lambda: nc.scalar.copy(h_b[:], h_f[:]))
                    p_t, bp = pTb.get()
                    for k in range(8):
                        P([bhb, b_identb], [bp], lambda k=k: nc.tensor.transpose(p_t[:, k, :], h_b[:, k * 128:(k + 1) * 128], identb[:]))
                    A([bp], [b_h2T[i]], lambda: nc.scalar.copy(h2T[:, :, i * 128:(i + 1) * 128], p_t[:]))
                    p32, bp32 = pT32.get()
                    for k in range(8):
                        P([bhf, b_ident], [bp32], lambda k=k: nc.tensor.transpose(p32[:, k, :], h_f[:, k * 128:(k + 1) * 128], ident[:]))
                    h32, bh32 = hT32.get()
                    V([bp32], [bh32], lambda: nc.vector.tensor_copy(h32[:], p32[:]))
                    p_l, bpl = pL.get()
                    for k in range(8):
                        P([bh32, b_wr], [bpl], lambda k=k: nc.tensor.matmul(p_l[:, 0:36], h32[:, k, :], wr[:, k, :], start=(k == 0), stop=(k == 7)))
                    yield
                    s_, bsm = sm.get()
                    lg = s_[:, 0:36]; gmx = s_[:, 36:37]; ngm = s_[:, 37:38]; g1h = s_[:, 40:44]; e4 = s_[:, 44:48]
                    esum = s_[:, 48:49]; pgr = s_[:, 49:50]; pen = s_[:, 52:56]; m8 = s_[:, 56:64]; elm = s_[:, 64:96]
                    dd = s_[:, 96:97]; ee_ = s_[:, 97:98]; c1 = s_[:, 98:99]; c2 = s_[:, 99:100]
                    V([bpl, b_wr], [bsm], lambda: nc.vector.tensor_tensor(lg, p_l[:, 0:36], brb[:], ALU.add))
                    V([bsm], [bsm], lambda: nc.vector.tensor_reduce(gmx, s_[:, 0:4], AX.X, ALU.max))
                    V([bsm], [bsm], lambda: nc.vector.tensor_scalar(ngm, gmx, -1.0, None, ALU.mult))
                    V([bsm], [bsm], lambda: nc.vector.tensor_scalar(g1h, s_[:, 0:4], gmx, None, ALU.is_equal))
                    A([bsm], [bsm], lambda: nc.scalar.activation(e4, s_[:, 0:4], AF.Exp, bias=ngm, scale=1.0))
                    V([bsm], [bsm], lambda: nc.vector.tensor_reduce(esum, e4, AX.X, ALU.add))
                    V([bsm], [bsm], lambda: nc.vector.reciprocal(pgr, esum))
                    V([bsm], [bsm], lambda: nc.vector.tensor_scalar(pen, g1h, -1.0, 1e30, ALU.add, ALU.mult))
                    V([bsm], [bsm], lambda: nc.vector.tensor_tensor(elm.rearrange("p (g e) -> p g e", e=8), s_[:, 4:36].rearrange("p (g e) -> p g e", e=8),
                                                                    pen.unsqueeze(2).broadcast_to([128, 4, 8]), ALU.add))
                    V([bsm], [bsm], lambda: nc.vector.max(m8, elm))
                    V([bsm], [bsm], lambda: nc.vector.tensor_tensor(dd, m8[:, 1:2], m8[:, 0:1], ALU.subtract))
                    A([bsm], [bsm], lambda: nc.scalar.activation(ee_, dd, AF.Exp))
                    V([bsm], [bsm], lambda: nc.vector.tensor_scalar(c1, ee_, 1.0, None, ALU.add))
                    V([bsm], [bsm], lambda: nc.vector.reciprocal(c1, c1))
                    V([bsm], [bsm], lambda: nc.vector.tensor_tensor(c2, ee_, c1, ALU.mult))
                    V([bsm], [bsm], lambda: nc.vector.tensor_tensor(c1, c1, pgr, ALU.mult))
                    V([bsm], [bsm], lambda: nc.vector.tensor_tensor(c2, c2, pgr, ALU.mult))
                    V([bsm], [b_Wr[i]], lambda: nc.vector.tensor_scalar(Wr[:, i, :], elm, m8[:, 0:1], c1, ALU.is_equal, ALU.mult))
                    V([bsm], [bsm], lambda: nc.vector.tensor_scalar(s_[:, 100:132], elm, m8[:, 1:2], c2, ALU.is_equal, ALU.mult))
                    V([bsm, b_Wr[i]], [b_Wr[i]], lambda: nc.vector.tensor_tensor(Wr[:, i, :], Wr[:, i, :], s_[:, 100:132], ALU.add))

                active = []
                for i in list(tiles) + [None, None]:
                    for g in list(active):
                        try:
                            next(g)
                        except StopIteration:
                            active.remove(g)
                    if i is not None:
                        g = tile_gen(i)
                        next(g)
                        active.append(g)
                for g in active:
                    for _ in g:
                        pass
                f.barrier()
            if state.get("stop_before_moe"):
                return
            facc = sbt(es, "facc", [128, NT, DM]); b_f = [Buf() for _ in range(NT)]
            with ExitStack() as es2:
                wg = Rot(es2, "wg", 2, [128, 8, DE], BF16); wu = Rot(es2, "wu", 2, [128, 8, DE], BF16)
                wd = Rot(es2, "wd", 2, [128, 4, DM], BF16)
                pG = Rot(es2, "pG", 2, [128, 512], psum=True); pU = Rot(es2, "pU", 2, [128, 512], psum=True)
                pD = Rot(es2, "pD", 3, [128, 512], psum=True)
                sg = Rot(es2, "sg", 2, [128, 512]); aT = Rot(es2, "aT", 8, [128, 512], BF16)
                t0_ = tiles[0] * 128
                ntok = len(tiles) * 128
                blocks_ = [(t0_ + b * 512, min(512, ntok - b * 512)) for b in range((ntok + 511) // 512)]
                for e in range(NE):
                    g_w, bg = wg.get(); u_w, bu = wu.get(); d_w, bdw = wd.get()
                    D(g_w[:], w_gate[l, e, :, :].rearrange("(k p) n -> p k n", p=128), [], [bg], eng=POOL)
                    D(u_w[:], w_up[l, e, :, :].rearrange("(k p) n -> p k n", p=128), [], [bu], eng=POOL)
                    D(d_w[:], w_down[l, e, :, :].rearrange("(k p) n -> p k n", p=128), [], [bdw], eng=POOL)
                    for (tb0, tsz) in blocks_:
                        rb = [b_h2T[i] for i in range(tb0 // 128, (tb0 + tsz) // 128)]
                        acts = []
                        for fc in range(4):
                            g_p, bgp = pG.get(); u_p, bup = pU.get()
                            for k in range(8):
                                P(rb + [bg], [bgp], lambda k=k: nc.tensor.matmul(g_p[:, 0:tsz], g_w[:, k, fc * 128:(fc + 1) * 128], h2T[:, k, tb0:tb0 + tsz], start=(k == 0), stop=(k == 7)))
                            for k in range(8):
                                P(rb + [bu], [bup], lambda k=k: nc.tensor.matmul(u_p[:, 0:tsz], u_w[:, k, fc * 128:(fc + 1) * 128], h2T[:, k, tb0:tb0 + tsz], start=(k == 0), stop=(k == 7)))
                            s_g, bsg = sg.get(); a_T, baT = aT.get()
                            A([bgp], [bsg], lambda: nc.scalar.activation(s_g[:, 0:tsz], g_p[:, 0:tsz], AF.Silu))
                            V([bsg, bup], [baT], lambda: nc.vector.tensor_tensor(a_T[:, 0:tsz], s_g[:, 0:tsz], u_p[:, 0:tsz], ALU.mult))
                            acts.append((a_T, baT))
                        for q in range(tsz // 128):
                            ti = tb0 // 128 + q
                            for hf_ in range(2):
                                d_p, bdp = pD.get()
                                for fc in range(4):
                                    a_T, baT = acts[fc]
                                    P([baT, bdw], [bdp], lambda fc=fc, a_T=a_T: nc.tensor.matmul(
                                        d_p[:], a_T[:, q * 128:(q + 1) * 128], d_w[:, fc, hf_ * 512:(hf_ + 1) * 512], start=(fc == 0), stop=(fc == 3)))
                                fs = facc[:, ti, hf_ * 512:(hf_ + 1) * 512]
                                if e == 0:
                                    V([bdp, b_Wr[ti]], [b_f[ti]], lambda: nc.vector.tensor_scalar(fs, d_p[:], Wr[:, ti, e:e + 1], None, ALU.mult))
                                else:
                                    V([bdp, b_Wr[ti], b_f[ti]], [b_f[ti]], lambda: nc.vector.scalar_tensor_tensor(fs, d_p[:], Wr[:, ti, e:e + 1], fs, ALU.mult, ALU.add))
                f.barrier()
            with ExitStack() as es2:
                g2 = sbt(es2, "g2", [128, 2, DM]); b_g2 = Buf()
                D(g2[:, 0, :], modrow[1, 5 * DM:6 * DM].partition_broadcast(128), [], [b_g2])
                D(g2[:, 1, :], modrow[0, 5 * DM:6 * DM].partition_broadcast(128), [], [b_g2])
                lgb = sbt(es2, "lgb2", [128, 2, DM]); b_lgb = Buf()
                D(lgb[:, 0, :], ln2_g[l, :].partition_broadcast(128), [], [b_lgb])
                D(lgb[:, 1, :], ln2_b[l, :].partition_broadcast(128), [], [b_lgb])
                xt = Rot(es2, "xt3", 2, [128, DM]); tt = Rot(es2, "tt3", 2, [128, DM])
                st = Rot(es2, "st3", 2, [128, 2, 6]); mv = Rot(es2, "mv3", 2, [128, 2]); rs = Rot(es2, "rs3", 2, [128, 1])
                for i in tiles:
                    w = 0 if i < 2 else 1
                    x_t, bx = xt.get(); t_t, bt = tt.get()
                    D(x_t[:], xs[i * 128:(i + 1) * 128, :], [], [bx])
                    G([b_f[i], b_g2], [bt], lambda: nc.gpsimd.tensor_tensor(t_t[:], facc[:, i, :], g2[:, w, :], ALU.mult))
                    V([bx, bt], [bt], lambda: nc.vector.scalar_tensor_tensor(t_t[:], x_t[:], ALPHA, t_t[:], ALU.mult, ALU.add))
                    s_t, bs = st.get(); m_t, bmv = mv.get(); r_t, br = rs.get()
                    layer_norm_stats(None, t_t, bt, (s_t, bs, m_t, bmv, r_t, br))
                    V([bt, bmv, br], [bt], lambda: nc.vector.tensor_scalar(t_t[:], t_t[:], m_t[:, 0:1], r_t[:, 0:1], ALU.subtract, ALU.mult))
                    G([bt, b_lgb], [bt], lambda: nc.gpsimd.tensor_tensor(t_t[:], t_t[:], lgb[:, 0, :], ALU.mult))
                    G([bt, b_lgb], [bt], lambda: nc.gpsimd.tensor_tensor(t_t[:], t_t[:], lgb[:, 1, :], ALU.add))
                    if l == DEPTH - 1:
                        D(y_out[(i - 2) * 128:(i - 1) * 128, :], t_t[:], [bt], [])
                    else:
                        D(xs[i * 128:(i + 1) * 128, :], t_t[:], [bt], [])
                f.barrier()

    done = False
    for l in range(DEPTH):
        for tag, fn in (("mod", phase_mod), ("proj", phase_ln_proj), ("rwkv", phase_rwkv),
                        ("attb", phase_attn_b), ("attc", phase_attn_c), ("moe", phase_out_moe)):
            if STOP is not None and (l, tag) == tuple(STOP[:2]) and len(STOP) > 2:
                state["stop_before_moe"] = True
            fn(l)
            if STOP is not None and (l, tag) == tuple(STOP[:2]):
                done = True
                break
        if done:
            break
    f.barrier()
    print("n_inst", f.n_inst, flush=True)
    return nc


def _consts():
    p = np.arange(128)[:, None]; q = np.arange(128)[None, :]
    masks = np.stack([(p < q), (p <= q), (p > q), (p >= q)], 1).astype(np.float32)
    ident = np.eye(128, dtype=np.float32)
    block = (p // 64 == q // 64).astype(np.float32)
    hsel = (np.arange(128)[:, None] // 64 == np.arange(2)[None, :]).astype(np.float32)
    t = np.arange(S)
    pos = np.stack([t // 64, t % 64], -1).astype(np.float32)
    inv = (10000.0 ** (-np.arange(16, dtype=np.float32) / 16)).astype(np.float32)
    ang = pos[:, :, None] * inv
    cos = np.cos(ang).astype(np.float32); sin = np.sin(ang).astype(np.float32)
    cosT = np.zeros((64, S), np.float32); sinT = np.zeros((64, S), np.float32)
    for ax in range(2):
        for half in range(2):
            r0 = ax * 32 + half * 16
            cosT[r0:r0 + 16] = cos[:, ax, :].T
            sinT[r0:r0 + 16] = sin[:, ax, :].T
    Pm = np.zeros((64, 64), np.float32)
    for ax in range(2):
        for i in range(16):
            Pm[ax * 32 + i, ax * 32 + 16 + i] = -1.0
            Pm[ax * 32 + 16 + i, ax * 32 + i] = 1.0
    ropeP = np.ascontiguousarray(Pm.T)
    col = np.arange(64)
    cstart = np.clip(col - 8, 0, 48)
    cvalid = ((col[None, :] >= cstart[:, None]) & (col[None, :] < cstart[:, None] + 16)).astype(np.float32)
    colvalid = np.concatenate([cvalid.T, cvalid.T], 0)
    scan = np.ones((128, 512), np.float32); scan[:, ::128] = 0.0
    return dict(cmasks=masks, cident=ident, cblock=block, chsel=hsel, ccos=cosT, csin=sinT, cropeP=ropeP,
                ccolvalid=np.ascontiguousarray(colvalid), cscanmask=scan)


def _host_layout(inp):
    f32 = lambda a: np.ascontiguousarray(np.asarray(a, dtype=np.float32))
    sh = {}
    sh["w_mod"] = f32(inp["w_mod"]); sh["b_mod"] = f32(inp["b_mod"]); sh["w_in"] = f32(inp["w_in"]); sh["w_out"] = f32(inp["w_out"])
    sh["a_shift"] = f32(inp["a_shift"])
    sh["a_w_up"] = f32(np.asarray(inp["a_w_up"]).reshape(DEPTH, 128, 256))
    sh["a_a_up"] = f32(np.asarray(inp["a_a_up"]).reshape(DEPTH, 128, 256))
    sh["a_g_up"] = f32(inp["a_g_up"])
    cp = np.zeros((DEPTH, 128, 48), np.float32)
    ash = np.zeros((DEPTH, 3, 1280), np.float32); ash[:, :, :A_IN] = inp["a_shift"]
    for l in range(DEPTH):
        for ch in range(10):
            for tap in range(3):
                cp[l, :, ch * 3 + tap] = ash[l, tap, ch * 128:(ch + 1) * 128]
        for d in range(2):
            for j in range(2):
                cp[l, :, 30 + d * 2 + j] = inp["a_w0"][l, d, j * 128:(j + 1) * 128]
                cp[l, :, 34 + d * 2 + j] = inp["a_a0"][l, d, j * 128:(j + 1) * 128]
        for j in range(2):
            cp[l, :, 38 + j] = inp["a_k_k"][l, j * 128:(j + 1) * 128]
            cp[l, :, 40 + j] = inp["a_k_a"][l, j * 128:(j + 1) * 128]
            cp[l, :, 42 + j] = inp["a_r_k"][l, j * 128:(j + 1) * 128]
    sh["colpar"] = cp
    for k in ("a_r_k", "a_lnx_g", "a_lnx_b", "b_sink", "ln1_g", "ln1_b", "ln2_g", "ln2_b"):
        sh[k] = f32(inp[k])
    for k in ("w_gate", "w_up", "w_down"):
        sh[k] = f32(np.asarray(inp[k])[:, :NE_DECL])
    kc = np.arange(64)[:, None]; qc = np.arange(64)[None, :]
    idx = np.clip(kc - qc, -15, 15) + 15
    rpb = np.asarray(inp["c_rpb"], np.float32)
    tab = rpb[:, :, :, idx]
    sh["rpbtab"] = f32(tab.transpose(0, 1, 3, 2, 4))
    sh["w_r"] = f32(np.concatenate([inp["w_rg"], inp["w_re"]], -1))
    sh["b_r"] = f32(np.concatenate([inp["b_rg"], inp["b_re"]], -1))
    sh.update(_consts())
    return sh


_PROG = {}


def kernel(**inputs):
    inp = {k: np.asarray(v) for k, v in inputs.items()}
    shared = _host_layout(inp)
    if "nc" not in _PROG:
        _PROG["nc"] = build_program()
    nc = _PROG["nc"]
    in_maps = []
    for b in range(NCORES):
        m = dict(shared)
        m["x"] = np.ascontiguousarray(inp["x"][b], dtype=np.float32)
        m["ctx"] = np.ascontiguousarray(inp["ctx"][b], dtype=np.float32)
        cc = np.zeros((128, 8, 2), np.float32)
        cc[:, :, 0] = np.asarray(inp["c"][b], np.float32).reshape(8, 128).T
        cc[:, :, 1] = np.asarray(inp["c_ctx"], np.float32).reshape(8, 128).T
        m["ccol"] = cc
        in_maps.append(m)
    res = run_bass_kernel_spmd(nc, in_maps, core_ids=list(range(NCORES)))
    _PROG["res"] = res
    out = np.stack([np.asarray(r["y"], dtype=np.float32) for r in res.results], 0)
    return out
```

```python
from contextlib import ExitStack
import numpy as np
import concourse.bass as bass
import concourse.mybir as mybir
from concourse.bass_utils import run_bass_kernel_spmd

F32 = mybir.dt.float32
BF16 = mybir.dt.bfloat16
ALU = mybir.AluOpType
AF = mybir.ActivationFunctionType
AX = mybir.AxisListType

DM = 1024; S = 2048; L = 256; T = S + L; NT = T // 128; DEPTH = 2
HD = 64; A_IN = 1184; N_IN = 2976; NE = 32; DE = 512
ALPHA = (2.0 * DEPTH) ** 0.25
SCALE = HD ** -0.5
C0 = float(np.exp(-0.5))
LN_EPS = 1e-6; GN_EPS = 64e-5
OFF_R, OFF_K, OFF_V, OFF_WD, OFF_AD, OFF_GD = 0, 256, 512, 768, 896, 1024
OFF_QB, OFF_KB, OFF_VB = 1184, 1568, 1696
OFF_QN, OFF_KN, OFF_VN = 1824, 2208, 2592
EPOCH = 30000
DEBUG = False
RW_STOP = 0
NCORES = 8
NE_DECL = 32
STOP = None


class Buf:
    __slots__ = ("w", "r")

    def __init__(self):
        self.w = {}
        self.r = {}


class Eng:
    def __init__(self, fw, name, eng, dma_only=False):
        self.fw = fw; self.name = name; self.eng = eng
        self.sem = None; self.cnt = 0; self.epoch = 0; self.seen = {}
        self.dma_only = dma_only
        if not dma_only:
            self.new_sem()

    def new_sem(self):
        self.sem = self.fw.nc.alloc_semaphore(f"p_{self.name}_{self.epoch}")
        self.cnt = 0
        self.epoch += 1
        self.fw.all_sems.append(self)


class FW:
    def __init__(self, nc, n_dma_sems=24):
        self.nc = nc
        self.all_sems = []
        self.pe = Eng(self, "pe", nc.tensor)
        self.act = Eng(self, "act", nc.scalar)
        self.dve = Eng(self, "dve", nc.vector)
        self.pool = Eng(self, "pool", nc.gpsimd)
        self.sp = Eng(self, "sp", nc.sync, dma_only=True)
        self.dma_sems = [[nc.alloc_semaphore(f"d{i}"), 0] for i in range(n_dma_sems)]
        self.dma_rr = 0
        self.sw_sems = [[nc.alloc_semaphore(f"w{i}"), 0] for i in range(8)]
        self.sw_rr = 0
        self.n_inst = 0

    def _wait(self, E, sem, val):
        k = sem.name
        if E.seen.get(k, 0) >= val:
            return
        E.eng.wait_ge(sem, val)
        E.seen[k] = val

    def _deps(self, E, reads, writes, skip_self=True):
        need = {}
        for b in reads:
            for k, sv in b.w.items():
                if k not in need or need[k][1] < sv[1]:
                    need[k] = sv
        for b in writes:
            for d in (b.w, b.r):
                for k, sv in d.items():
                    if k not in need or need[k][1] < sv[1]:
                        need[k] = sv
        me = E.sem.name if (skip_self and E.sem is not None and E.name == 'pe') else None
        for k, (s, v) in need.items():
            if k == me:
                continue
            self._wait(E, s, v)

    def _mark(self, sem, val, reads, writes):
        k = sem.name
        for b in reads:
            o = b.r.get(k)
            if o is None or o[1] < val:
                b.r[k] = (sem, val)
        for b in writes:
            o = b.w.get(k)
            if o is None or o[1] < val:
                b.w[k] = (sem, val)

    def op(self, E, reads, writes, fn):
        if E.cnt >= EPOCH:
            E.new_sem()
        self._deps(E, reads, writes)
        ins = fn()
        E.cnt += 1
        ins.then_inc(E.sem, 1)
        self._mark(E.sem, E.cnt, reads, writes)
        self.n_inst += 1
        return ins

    def dma(self, E, out, in_, reads, writes, **kw):
        if E.name == "pool":
            slot = self.sw_sems[self.sw_rr]
            self.sw_rr = (self.sw_rr + 1) % len(self.sw_sems)
        else:
            slot = self.dma_sems[self.dma_rr]
            self.dma_rr = (self.dma_rr + 1) % len(self.dma_sems)
        sem, cnt = slot
        if cnt > 0:
            self._wait(E, sem, cnt)
        self._deps(E, reads, writes, skip_self=False)
        ins = E.eng.dma_start(out=out, in_=in_, **kw)
        slot[1] = cnt + 16
        ins.then_inc(sem, 16)
        self._mark(sem, slot[1], reads, writes)
        self.n_inst += 1
        return ins

    def barrier(self):
        cur = []
        for X in (self.pe, self.act, self.dve, self.pool):
            if X.cnt > 0:
                cur.append((X.sem, X.cnt))
        for sem, cnt in self.dma_sems + self.sw_sems:
            if cnt > 0:
                cur.append((sem, cnt))
        for E in (self.pe, self.act, self.dve, self.pool, self.sp):
            for sem, val in cur:
                if E.sem is not None and sem.name == E.sem.name:
                    continue
                self._wait(E, sem, val)


def build_program():
    nc = bass.Bass("TRN2", target_bir_lowering=False)
    f = FW(nc)
    PE, ACT, DVE, POOL, SP = f.pe, f.act, f.dve, f.pool, f.sp

    def din(name, shape):
        return nc.dram_tensor(name, list(shape), F32, kind="ExternalInput").ap()

    def scr(name, shape):
        return nc.dram_tensor(name, list(shape), F32, kind=("ExternalOutput" if DEBUG else "Internal")).ap()

    x_in = din("x", [S, DM]); ctx_in = din("ctx", [L, DM]); ccol = din("ccol", [128, 8, 2])
    w_mod = din("w_mod", [DEPTH, DM, 6 * DM]); b_mod = din("b_mod", [DEPTH, 6 * DM])
    w_in = din("w_in", [DEPTH, DM, N_IN]); w_out = din("w_out", [DEPTH, DM, DM])
    a_shift = din("a_shift", [DEPTH, 3, A_IN])
    a_w_up = din("a_w_up", [DEPTH, 128, 256]); a_a_up = din("a_a_up", [DEPTH, 128, 256])
    a_g_up = din("a_g_up", [DEPTH, 160, 256])
    colpar = din("colpar", [DEPTH, 128, 48])
    a_r_k = din("a_r_k", [DEPTH, 256]); a_lnx_g = din("a_lnx_g", [DEPTH, 256]); a_lnx_b = din("a_lnx_b", [DEPTH, 256])
    b_sink = din("b_sink", [DEPTH, 6])
    rpbtab = din("rpbtab", [DEPTH, 6, 64, 15, 64])
    ln1_g = din("ln1_g", [DEPTH, DM]); ln1_b = din("ln1_b", [DEPTH, DM])
    ln2_g = din("ln2_g", [DEPTH, DM]); ln2_b = din("ln2_b", [DEPTH, DM])
    w_r = din("w_r", [DEPTH, DM, 36]); b_r = din("b_r", [DEPTH, 36])
    w_gate = din("w_gate", [DEPTH, NE_DECL, DM, DE]); w_up = din("w_up", [DEPTH, NE_DECL, DM, DE])
    w_down = din("w_down", [DEPTH, NE_DECL, DE, DM])
    cmasks = din("cmasks", [128, 4, 128]); cident = din("cident", [128, 128])
    cblock = din("cblock", [128, 128]); chsel = din("chsel", [128, 2])
    ccos = din("ccos", [64, S]); csin = din("csin", [64, S]); cropeP = din("cropeP", [64, 64])
    ccolvalid = din("ccolvalid", [128, 64]); cscanmask = din("cscanmask", [128, 512])
    y_out = nc.dram_tensor("y", [S, DM], F32, kind="ExternalOutput").ap()

    xs = scr("xs", [T, DM]); modrow = scr("modrow", [2, 6 * DM])
    zT = scr("zT", [N_IN, T]); vtok = scr("vtok", [T, 768]); omix = scr("omix", [T, DM])
    dbgh = scr("dbgh", [T, DM]) if DEBUG else None
    dbgs = scr("dbgs", [T, 4]) if DEBUG else None

    state = {"stop": False}

    def xsrc(l, i):
        if l == 0:
            return ctx_in[i * 128:(i + 1) * 128, :] if i < 2 else x_in[(i - 2) * 128:(i - 1) * 128, :]
        return xs[i * 128:(i + 1) * 128, :]

    top = ExitStack()
    uid = [0]

    def sbt(es, name, shape, dt=F32):
        uid[0] += 1
        return es.enter_context(nc.sbuf_tensor(f"{name}_{uid[0]}", list(shape), dt))

    def pst(es, name, shape, dt=F32):
        uid[0] += 1
        return es.enter_context(nc.psum_tensor(f"{name}_{uid[0]}", list(shape), dt))
    ident = sbt(top, "ident", [128, 128]); b_ident = Buf()
    identb = sbt(top, "identb", [128, 128], BF16); b_identb = Buf()
    f.dma(SP, ident[:], cident[:, :], [], [b_ident])
    f.op(DVE, [b_ident], [b_identb], lambda: nc.vector.tensor_copy(identb[:], ident[:]))

    def V(r, w, fn): return f.op(DVE, r, w, fn)
    def A(r, w, fn): return f.op(ACT, r, w, fn)
    def G(r, w, fn): return f.op(POOL, r, w, fn)
    def P(r, w, fn): return f.op(PE, r, w, fn)
    def D(out, in_, r, w, eng=None, **kw): return f.dma(eng or SP, out, in_, r, w, **kw)

    class Rot:
        def __init__(self, es, name, n, shape, dt=F32, psum=False):
            mk = pst if psum else sbt
            self.t = [mk(es, f"{name}{i}", shape, dt) for i in range(n)]
            self.b = [Buf() for _ in range(n)]
            self.i = 0

        def get(self):
            k = self.i; self.i = (self.i + 1) % len(self.t)
            return self.t[k], self.b[k]

    class Slots:
        def __init__(self, aps):
            self.t = list(aps); self.b = [Buf() for _ in self.t]; self.i = 0

        def get(self):
            k = self.i; self.i = (self.i + 1) % len(self.t)
            return self.t[k], self.b[k]

    def carve(es, name, nbanks, width):
        per = 512 // width
        views = [[None] * nbanks for _ in range(per)]
        bufs = [Buf() for _ in range(nbanks)]
        for i in range(nbanks):
            bk = pst(es, f"{name}{i}", [128, 512])
            for q in range(per):
                views[q][i] = bk[:, q * width:(q + 1) * width]
        sl = Slots([views[q][i] for q in range(per) for i in range(nbanks)])
        sl.b = [bufs[i] for q in range(per) for i in range(nbanks)]
        return sl

    def layer_norm_stats(es_tiles, src_ap, b_src, tagbufs):
        st, b_st, mv, b_mv, rs, b_rs = tagbufs
        V([b_src], [b_st], lambda: nc.vector.bn_stats(st[:, 0, :], src_ap[:, 0:512]))
        V([b_src], [b_st], lambda: nc.vector.bn_stats(st[:, 1, :], src_ap[:, 512:1024]))
        V([b_st], [b_mv], lambda: nc.vector.bn_aggr(mv[:], st[:].rearrange("p a b -> p (a b)")))
        A([b_mv], [b_rs], lambda: nc.scalar.activation(rs[:], mv[:, 1:2], AF.Sqrt, bias=eps_t[:, 0:1], scale=1.0))
        V([b_rs], [b_rs], lambda: nc.vector.reciprocal(rs[:], rs[:]))

    eps_t = sbt(top, "eps_t", [128, 4]); b_eps = Buf()
    G([], [b_eps], lambda: nc.gpsimd.memset(eps_t[:, 0:1], LN_EPS))
    G([], [b_eps], lambda: nc.gpsimd.memset(eps_t[:, 1:2], GN_EPS))
    G([], [b_eps], lambda: nc.gpsimd.memset(eps_t[:, 2:3], 1e-12))
    G([], [b_eps], lambda: nc.gpsimd.memset(eps_t[:, 3:4], 1.0))
    f.barrier()

    def phase_mod(l):
        with ExitStack() as es:
            cr = sbt(es, "cr", [128, 8, 2]); b_cr = Buf()
            cs = sbt(es, "cs", [128, 8, 2]); b_cs = Buf()
            D(cr[:], ccol[:, :, :], [], [b_cr])
            A([b_cr], [b_cs], lambda: nc.scalar.activation(cs[:], cr[:], AF.Silu))
            bm2 = sbt(es, "bm2", [2, 6 * DM]); b_bm2 = Buf()
            D(bm2[:], b_mod[l, :].partition_broadcast(2), [], [b_bm2])
            msb = sbt(es, "msb", [2, 6 * DM]); b_msb = Buf()
            wm = Rot(es, "wm", 3, [128, 3072])
            acc = [pst(es, f"macc{j}", [128, 512]) for j in range(6)]
            b_acc = [Buf() for _ in range(6)]
            for hf in range(2):
                for k in range(8):
                    wt, bw = wm.get()
                    D(wt[:], w_mod[l, k * 128:(k + 1) * 128, hf * 3072:(hf + 1) * 3072], [], [bw])
                    for j in range(6):
                        P([bw, b_cs], [b_acc[j]], lambda j=j, wt=wt, k=k: nc.tensor.matmul(
                            acc[j][0:2, :], cs[:, k, :], wt[:, j * 512:(j + 1) * 512], start=(k == 0), stop=(k == 7)))
                for j in range(6):
                    c0 = hf * 3072 + j * 512
                    V([b_acc[j], b_bm2], [b_msb], lambda j=j, c0=c0: nc.vector.tensor_tensor(
                        msb[:, c0:c0 + 512], acc[j][0:2, :], bm2[:, c0:c0 + 512], ALU.add))
            D(modrow[:, :], msb[:], [b_msb], [])
            f.barrier()

    def phase_ln_proj(l):
        with ExitStack() as es:
            hT = sbt(es, "hT", [128, 8, T], BF16); b_hT = [Buf() for _ in range(NT)]
            with ExitStack() as es2:
                mods = sbt(es2, "mods", [128, 2, 2 * DM]); b_mods = Buf()
                D(mods[:, 0, :], modrow[1, 0:2 * DM].partition_broadcast(128), [], [b_mods])
                D(mods[:, 1, :], modrow[0, 0:2 * DM].partition_broadcast(128), [], [b_mods])
                G([b_mods], [b_mods], lambda: nc.gpsimd.tensor_scalar(mods[:, :, DM:2 * DM], mods[:, :, DM:2 * DM], 1.0, None, ALU.add))
                xt = Rot(es2, "xt", 2, [128, DM]); xn = Rot(es2, "xn", 2, [128, DM]); hb = Rot(es2, "hb", 2, [128, DM], BF16)
                st = Rot(es2, "st", 2, [128, 2, 6]); mv = Rot(es2, "mv", 2, [128, 2]); rs = Rot(es2, "rs", 2, [128, 1])
                pT = Rot(es2, "pT", 2, [128, 8, 128], BF16, psum=True)
                for i in range(NT):
                    x_t, bx = xt.get(); n_t, bn = xn.get(); h_t, bh = hb.get()
                    s_t, bs = st.get(); m_t, bmv = mv.get(); r_t, br = rs.get(); p_t, bp = pT.get()
                    D(x_t[:], xsrc(l, i), [], [bx])
                    layer_norm_stats(None, x_t, bx, (s_t, bs, m_t, bmv, r_t, br))
                    V([bx, bmv, br], [bn], lambda x_t=x_t, n_t=n_t, m_t=m_t, r_t=r_t: nc.vector.tensor_scalar(
                        n_t[:], x_t[:], m_t[:, 0:1], r_t[:, 0:1], ALU.subtract, ALU.mult))
                    w = 0 if i < 2 else 1
                    G([bn, b_mods], [bn], lambda n_t=n_t, w=w: nc.gpsimd.tensor_tensor(n_t[:], n_t[:], mods[:, w, DM:2 * DM], ALU.mult))
                    G([bn, b_mods], [bn], lambda n_t=n_t, w=w: nc.gpsimd.tensor_tensor(n_t[:], n_t[:], mods[:, w, 0:DM], ALU.add))
                    A([bn], [bh], lambda n_t=n_t, h_t=h_t: nc.scalar.copy(h_t[:], n_t[:]))
                    if DEBUG:
                        D(dbgh[i * 128:(i + 1) * 128, :], n_t[:], [bn], [])
                        D(dbgs[i * 128:(i + 1) * 128, 0:2], m_t[:], [bmv], [], allow_slow_non_contiguous=True)
                        D(dbgs[i * 128:(i + 1) * 128, 2:3], r_t[:], [br], [], allow_slow_non_contiguous=True)
                    for k in range(8):
                        P([bh, b_identb], [bp], lambda k=k, h_t=h_t, p_t=p_t: nc.tensor.transpose(p_t[:, k, :], h_t[:, k * 128:(k + 1) * 128], identb[:]))
                    A([bp], [b_hT[i]], lambda i=i, p_t=p_t: nc.scalar.copy(hT[:, :, i * 128:(i + 1) * 128], p_t[:]))
                f.barrier()
            wb = sbt(es, "wb", [128, 8, N_IN], BF16); b_wb = [Buf() for _ in range(8)]
            for k in range(8):
                D(wb[:, k, :], w_in[l, k * 128:(k + 1) * 128, :], [], [b_wb[k]], eng=POOL)
            pp = Rot(es, "pp", 4, [128, 512], psum=True)
            stg = Rot(es, "stg", 4, [128, 512])
            cnt = 0
            for fc in range(24):
                fsz = 128 if fc < 23 else 32
                for tb in range(5):
                    t0 = tb * 512; tsz = min(512, T - t0)
                    p_t, bp = pp.get(); s_t, bs = stg.get()
                    rb = [b_hT[i] for i in range(t0 // 128, (t0 + tsz) // 128)]
                    for k in range(8):
                        P(rb + [b_wb[k]], [bp], lambda k=k, p_t=p_t, fc=fc, fsz=fsz, t0=t0, tsz=tsz: nc.tensor.matmul(
                            p_t[0:fsz, 0:tsz], wb[:, k, fc * 128:fc * 128 + fsz], hT[:, k, t0:t0 + tsz], start=(k == 0), stop=(k == 7)))
                    if cnt % 2 == 0:
                        A([bp], [bs], lambda p_t=p_t, s_t=s_t, fsz=fsz, tsz=tsz: nc.scalar.copy(s_t[0:fsz, 0:tsz], p_t[0:fsz, 0:tsz]))
                    else:
                        V([bp], [bs], lambda p_t=p_t, s_t=s_t, fsz=fsz, tsz=tsz: nc.vector.tensor_copy(s_t[0:fsz, 0:tsz], p_t[0:fsz, 0:tsz]))
                    cnt += 1
                    D(zT[fc * 128:fc * 128 + fsz, t0:t0 + tsz], s_t[0:fsz, 0:tsz], [bs], [])
            vst = Rot(es, "vst", 2, [128, 768])
            for i in range(NT):
                p1, bp1 = pp.get(); p2, bp2 = pp.get(); s_t, bs = vst.get()
                for (pt_, bpt_, c0_, c1_, w0_, wn_) in ((p1, bp1, 0, 256, OFF_V, 256), (p1, bp1, 256, 384, OFF_VB, 128), (p2, bp2, 0, 384, OFF_VN, 384)):
                    for k in range(8):
                        P([b_hT[i], b_wb[k]], [bpt_], lambda k=k: nc.tensor.matmul(
                            pt_[:, c0_:c1_], hT[:, k, i * 128:(i + 1) * 128], wb[:, k, w0_:w0_ + wn_], start=(k == 0), stop=(k == 7)))
                A([bp1], [bs], lambda p1=p1, s_t=s_t: nc.scalar.copy(s_t[:, 0:384], p1[:, 0:384]))
                V([bp2], [bs], lambda p2=p2, s_t=s_t: nc.vector.tensor_copy(s_t[:, 384:768], p2[:, 0:384]))
                D(vtok[i * 128:(i + 1) * 128, :], s_t[:], [bs], [])
            f.barrier()

    def conv_rows(dst, b_dst, src, b_src, wcol, b_w, c0, n):
        V([b_src, b_w], [b_dst], lambda: nc.vector.tensor_scalar(dst[:, 0:n], src[:, 0:n], wcol[:, 1:2], None, ALU.mult))
        segs = []
        for (a, b) in ((0, L), (L, T)):
            lo = max(a, c0); hi = min(b, c0 + n)
            if lo < hi:
                segs.append((lo - c0, hi - c0, lo == a, hi == b))
        return segs

    def phase_rwkv(l):
        with ExitStack() as es:
            cp = sbt(es, "cp", [128, 48]); b_cp = Buf()
            D(cp[:], colpar[l, :, :], [], [b_cp])
            wup = sbt(es, "wup", [128, 256]); aup = sbt(es, "aup", [128, 256]); b_up = Buf()
            D(wup[:], a_w_up[l, :, :], [], [b_up]); D(aup[:], a_a_up[l, :, :], [], [b_up])
            gup0 = sbt(es, "gup0", [128, 256]); gup1 = sbt(es, "gup1", [32, 256]); b_gup = Buf()
            D(gup0[:], a_g_up[l, 0:128, :], [], [b_gup]); D(gup1[:], a_g_up[l, 128:160, :], [], [b_gup])
            msk = sbt(es, "msk", [128, 4, 128]); b_msk = Buf()
            D(msk[:], cmasks[:, :, :], [], [b_msk])
            blk = sbt(es, "blk", [128, 128]); hsel = sbt(es, "hsel", [128, 2]); b_cst = Buf()
            D(blk[:], cblock[:, :], [], [b_cst]); D(hsel[:], chsel[:, :], [], [b_cst])
            scm = sbt(es, "scm", [128, 512]); D(scm[:], cscanmask[:, :], [], [b_cst])
            vsh = sbt(es, "vsh", [128, 3, 256]); b_vsh = Buf()
            D(vsh[:], a_shift[l, :, OFF_V:OFF_V + 256].partition_broadcast(128), [], [b_vsh])
            yacc = sbt(es, "yacc", [128, NT, 256]); b_yacc = [Buf() for _ in range(NT)]
            bacc = sbt(es, "bacc", [128, NT, 4]); b_bacc = [Buf() for _ in range(NT)]
            Vt = sbt(es, "Vt", [128, NT, 256]); b_Vt = [Buf() for _ in range(NT)]
            Vtb = sbt(es, "Vtb", [128, NT, 256], BF16)
            G([], b_yacc, lambda: nc.gpsimd.memset(yacc[:], 0.0))
            G([], b_bacc, lambda: nc.gpsimd.memset(bacc[:], 0.0))
            Mst = [[sbt(es, f"M{d}{j}", [128, 64]) for j in range(2)] for d in range(2)]
            b_M = [[Buf() for j in range(2)] for d in range(2)]
            for d in range(2):
                for j in range(2):
                    G([], [b_M[d][j]], lambda d=d, j=j: nc.gpsimd.memset(Mst[d][j][:], 0.0))
            with ExitStack() as es2:
                vr = Rot(es2, "vr", 6, [128, 256])
                for i in range(NT):
                    tp, bp_ = vr.get(); tc, bc_ = vr.get(); tn, bn_ = vr.get()
                    first = i in (0, 2); last = i in (1, NT - 1)
                    r0 = i * 128
                    D(tc[:], vtok[r0:r0 + 128, 0:256], [], [bc_])
                    if first:
                        G([], [bp_], lambda tp=tp: nc.gpsimd.memset(tp[:], 0.0))
                        D(tp[1:128, :], vtok[r0:r0 + 127, 0:256], [], [bp_])
                    else:
                        D(tp[:], vtok[r0 - 1:r0 + 127, 0:256], [], [bp_])
                    if last:
                        G([], [bn_], lambda tn=tn: nc.gpsimd.memset(tn[:], 0.0))
                        D(tn[0:127, :], vtok[r0 + 1:r0 + 128, 0:256], [], [bn_])
                    else:
                        D(tn[:], vtok[r0 + 1:r0 + 129, 0:256], [], [bn_])
                    V([bc_, b_vsh], [b_Vt[i]], lambda i=i, tc=tc: nc.vector.tensor_tensor(Vt[:, i, :], tc[:], vsh[:, 1, :], ALU.mult))
                    G([bp_, b_vsh], [bp_], lambda tp=tp: nc.gpsimd.tensor_tensor(tp[:], tp[:], vsh[:, 0, :], ALU.mult))
                    G([bn_, b_vsh], [bn_], lambda tn=tn: nc.gpsimd.tensor_tensor(tn[:], tn[:], vsh[:, 2, :], ALU.mult))
                    V([bp_, b_Vt[i]], [b_Vt[i]], lambda i=i, tp=tp: nc.vector.tensor_tensor(Vt[:, i, :], Vt[:, i, :], tp[:], ALU.add))
                    V([bn_, b_Vt[i]], [b_Vt[i]], lambda i=i, tn=tn: nc.vector.tensor_tensor(Vt[:, i, :], Vt[:, i, :], tn[:], ALU.add))
                    A([b_Vt[i]], [b_Vt[i]], lambda i=i: nc.scalar.copy(Vtb[:, i, :], Vt[:, i, :]))
                f.barrier()
            if RW_STOP == 1:
                return

            blocks = [(0, 256)] + [(256 + 256 * q, 256) for q in range(8)]
            order = {0: list(range(9)), 1: [0] + list(range(8, 0, -1))}
            with ExitStack() as es2:
                BW = 256
                rawS = [[(sbt(es2, f"rawS{d}{q}", [128, BW + 2]), Buf()) for q in range(2)] for d in range(2)]
                rawC = [[[(sbt(es2, f"rawC{d}{j}{q}", [128, BW + 2]), Buf()) for q in range(2)] for j in range(2)] for d in range(2)]
                tmpC = [[[(sbt(es2, f"tmpC{d}{j}{q}", [128, BW]), Buf()) for q in range(2)] for j in range(2)] for d in range(2)]
                rT = [[sbt(es2, f"rT{d}{j}", [128, BW]) for j in range(2)] for d in range(2)]
                kT = [[sbt(es2, f"kT{d}{j}", [128, BW]) for j in range(2)] for d in range(2)]
                kkT = [[sbt(es2, f"kkT{d}{j}", [128, BW]) for j in range(2)] for d in range(2)]
                twd = [sbt(es2, f"twd{d}", [128, BW]) for d in range(2)]
                tad = [sbt(es2, f"tad{d}", [128, BW]) for d in range(2)]
                names = ["t0", "t1", "t2", "bb", "kd", "p1", "e1"]
                rows = {nm: [[sbt(es2, f"{nm}{d}{j}", [128, BW]) for j in range(2)] for d in range(2)] for nm in names}
                q12 = [[sbt(es2, f"q12{d}{j}", [128, 2, 2, 128]) for j in range(2)] for d in range(2)]
                tot = [[sbt(es2, f"tot{d}{j}", [128, 4]) for j in range(2)] for d in range(2)]
                gC = [[sbt(es2, f"gC{d}{j}", [128, 4]) for j in range(2)] for d in range(2)]
                b_row = [[Buf() for j in range(2)] for d in range(2)]
                b_sh = [Buf() for d in range(2)]
                bnames = ["p1b", "p2b", "e1b", "e2b"]
                brows = {nm: [[sbt(es2, f"{nm}{d}{j}", [128, BW], BF16) for j in range(2)] for d in range(2)] for nm in bnames}
                q12b = [[sbt(es2, f"q12b{d}{j}", [128, 2, 2, 128], BF16) for j in range(2)] for d in range(2)]
                Mb = [[sbt(es2, f"Mb{d}{j}", [128, 64], BF16) for j in range(2)] for d in range(2)]
                for d in range(2):
                    for j in range(2):
                        G([], [b_M[d][j]], lambda d=d, j=j: nc.gpsimd.memset(Mb[d][j][:], 0.0))
                bankX = pst(es2, "bankX", [128, 512]); b_bankX = Buf()
                pM = [[bankX[:, (2 * d + j) * 64:(2 * d + j) * 64 + 64] for j in range(2)] for d in range(2)]
                b_pM = [[b_bankX for j in range(2)] for d in range(2)]
                ps5 = Slots([bankX[:, 256:512]]); ps5.b = [b_bankX]
                pN = Rot(es2, "pN", 1, [128, 512], psum=True)
                pW = Rot(es2, "pW", 2, [128, 1024], psum=True)
                pQ = carve(es2, "pQ", 1, 256)
                pEbank = pst(es2, "pEb", [128, 1024], BF16); b_pE = Buf()
                pE = Slots([pEbank[:, q * 256:(q + 1) * 256] for q in range(4)]); pE.b = [b_pE] * 4
                NCH = 16
                cA1 = [sbt(es2, f"cA1_{k}", [128, 256], BF16) for k in range(NCH)]
                cA2 = [sbt(es2, f"cA2_{k}", [128, 256], BF16) for k in range(NCH)]
                cbA = [(Buf(), Buf()) for k in range(NCH)]
                gPR = [Rot(es2, f"gPR{g}_", 3, [128, 4, 256], BF16) for g in range(4)]
                gPT = [Rot(es2, f"gPT{g}_", 3, [128, 4, 128], BF16) for g in range(4)]
                gSq = [Rot(es2, f"gSq{g}_", 2, [128, 4, 64], BF16) for g in range(4)]
                cE = [sbt(es2, f"cE_{k}", [128, 256], BF16) for k in range(NCH // 2)]
                cbE = [Buf() for k in range(NCH // 2)]

                def load_conv(dst, b_dst, frow, nrows, rawt, tap_chunk, c0, n):
                    rt, br = rawt
                    seq_lo, seq_hi = (0, L) if c0 < L else (L, T)
                    lo = max(seq_lo, c0 - 1); hi = min(seq_hi, c0 + n + 1)
                    if lo > c0 - 1 or hi < c0 + n + 1:
                        G([], [br], lambda rt=rt: nc.gpsimd.memset(rt[:], 0.0))
                    D(rt[0:nrows, lo - (c0 - 1):hi - (c0 - 1)], zT[frow:frow + nrows, lo:hi], [], [br])
                    w = lambda tap: cp[0:nrows, tap_chunk * 3 + tap:tap_chunk * 3 + tap + 1]
                    V([br, b_cp], [b_dst], lambda: nc.vector.tensor_scalar(dst[0:nrows, 0:n], rt[0:nrows, 1:n + 1], w(1), None, ALU.mult))
                    V([br, b_cp, b_dst], [b_dst], lambda: nc.vector.scalar_tensor_tensor(dst[0:nrows, 0:n], rt[0:nrows, 0:n], w(0), dst[0:nrows, 0:n], ALU.mult, ALU.add))
                    V([br, b_cp, b_dst], [b_dst], lambda: nc.vector.scalar_tensor_tensor(dst[0:nrows, 0:n], rt[0:nrows, 2:n + 2], w(2), dst[0:nrows, 0:n], ALU.mult, ALU.add))

                def prep_shared(d, bi):
                    c0, n = blocks[bi]
                    load_conv(twd[d], b_sh[d], OFF_WD, 128, rawS[d][0], 6, c0, n)
                    A([b_sh[d]], [b_sh[d]], lambda: nc.scalar.activation(twd[d][:, 0:n], twd[d][:, 0:n], AF.Tanh))
                    load_conv(tad[d], b_sh[d], OFF_AD, 128, rawS[d][1], 7, c0, n)

                def prep_combo(d, j, bi):
                    c0, n = blocks[bi]
                    nch = n // 128
                    hp = slice(64 * d, 64 * d + 64)
                    tmpi = [0]

                    def tmp_get():
                        t = tmpC[d][j][tmpi[0] % 2]; tmpi[0] += 1
                        return t
                    if True:
                        R = {nm: rows[nm][d][j] for nm in names}
                        br_ = b_row[d][j]
                        load_conv(rT[d][j], br_, OFF_R + 128 * j, 128, rawC[d][j][0], j, c0, n)
                        yield
                        load_conv(kT[d][j], br_, OFF_K + 128 * j, 128, rawC[d][j][1], 2 + j, c0, n)
                        yield
                        V([br_, b_cp], [br_], lambda: nc.vector.tensor_scalar(kkT[d][j][:, 0:n], kT[d][j][:, 0:n], cp[:, 38 + j:39 + j], None, ALU.mult))
                        tq, btq = tmp_get()
                        G([br_], [btq], lambda: nc.gpsimd.tensor_tensor(tq[:, 0:n], kkT[d][j][:, 0:n], kkT[d][j][:, 0:n], ALU.mult))
                        p5, bp5 = ps5.get()
                        P([btq, b_cst], [bp5], lambda: nc.tensor.matmul(p5[:, 0:n], blk[:], tq[:, 0:n], start=True, stop=True))
                        A([bp5], [btq], lambda: nc.scalar.activation(tq[:, 0:n], p5[:, 0:n], AF.Sqrt, bias=eps_t[:, 2:3], scale=1.0))
                        yield
                        V([btq], [btq], lambda: nc.vector.reciprocal(tq[:, 0:n], tq[:, 0:n]))
                        V([btq, br_], [br_], lambda: nc.vector.tensor_tensor(kkT[d][j][:, 0:n], kkT[d][j][:, 0:n], tq[:, 0:n], ALU.mult))
                        yield
                        p5, bp5 = ps5.get()
                        P([b_sh[d], b_up], [bp5], lambda: nc.tensor.matmul(p5[:, 0:n], wup[hp, j * 128:(j + 1) * 128], twd[d][hp, 0:n], start=True, stop=True))
                        A([bp5, b_cp], [br_], lambda: nc.scalar.activation(R["t0"][:, 0:n], p5[:, 0:n], AF.Sigmoid, bias=cp[:, 30 + d * 2 + j:31 + d * 2 + j], scale=1.0))
                        yield
                        p5, bp5 = ps5.get()
                        P([b_sh[d], b_up], [bp5], lambda: nc.tensor.matmul(p5[:, 0:n], aup[hp, j * 128:(j + 1) * 128], tad[d][hp, 0:n], start=True, stop=True))
                        A([bp5, b_cp], [br_], lambda: nc.scalar.activation(R["bb"][:, 0:n], p5[:, 0:n], AF.Sigmoid, bias=cp[:, 34 + d * 2 + j:35 + d * 2 + j], scale=1.0))
                        yield
                        V([br_, b_cp], [br_], lambda: nc.vector.tensor_scalar(R["kd"][:, 0:n], R["bb"][:, 0:n], -1.0, cp[:, 40 + j:41 + j], ALU.add, ALU.mult))
                        V([br_], [br_], lambda: nc.vector.scalar_tensor_tensor(R["kd"][:, 0:n], R["kd"][:, 0:n], 1.0, kT[d][j][:, 0:n], ALU.add, ALU.mult))
                        V([br_], [br_], lambda: nc.vector.tensor_tensor(R["bb"][:, 0:n], R["bb"][:, 0:n], kkT[d][j][:, 0:n], ALU.mult))
                        yield
                        tq, btq = tmp_get()
                        V([br_, b_cp], [btq], lambda: nc.vector.scalar_tensor_tensor(tq[:, 0:n], rT[d][j][:, 0:n], cp[:, 42 + j:43 + j], R["kd"][:, 0:n], ALU.mult, ALU.mult))
                        for c in range(nch):
                            ti = c0 // 128 + c
                            pq, bpq = pQ.get()
                            P([btq, b_cst], [bpq], lambda c=c, pq=pq: nc.tensor.matmul(pq[:, 0:2], tq[:, c * 128:(c + 1) * 128], hsel[:], start=True, stop=True))
                            V([bpq, b_bacc[ti]], [b_bacc[ti]], lambda ti=ti, pq=pq: nc.vector.tensor_tensor(bacc[:, ti, 2 * j:2 * j + 2], bacc[:, ti, 2 * j:2 * j + 2], pq[:, 0:2], ALU.add))
                        yield
                        V([br_, b_cst], [br_], lambda: nc.vector.tensor_tensor_scan(R["t1"][:, 0:n], scm[:, 0:n], R["t0"][:, 0:n], 0.0, ALU.mult, ALU.add))
                        V([br_], [br_], lambda: nc.vector.tensor_tensor(R["t0"][:, 0:n], R["t1"][:, 0:n], R["t0"][:, 0:n], ALU.subtract))
                        V([br_], [br_], lambda: nc.vector.tensor_copy(tot[d][j][:, 0:nch], R["t1"][:, 127:n:128]))
                        A([br_], [br_], lambda: nc.scalar.activation(gC[d][j][:, 0:nch], tot[d][j][:, 0:nch], AF.Exp, scale=-C0))
                        yield
                        totb = tot[d][j][:, 0:nch].unsqueeze(2).broadcast_to([128, nch, 128])
                        v3 = lambda ap: ap[:, 0:n].rearrange("p (c t) -> p c t", t=128)
                        q1v = q12[d][j][:, 0:nch, 0, :]; q2v = q12[d][j][:, 0:nch, 1, :]
                        if d == 0:
                            V([br_], [br_], lambda: nc.vector.tensor_tensor(v3(R["t2"]), totb, v3(R["t1"]), ALU.subtract))
                            A([br_], [br_], lambda: nc.scalar.activation(q1v, v3(R["t0"]), AF.Exp, scale=-C0))
                            A([br_], [br_], lambda: nc.scalar.activation(q2v, v3(R["t1"]), AF.Exp, scale=-C0))
                            A([br_], [br_], lambda: nc.scalar.activation(R["p1"][:, 0:n], R["t1"][:, 0:n], AF.Exp, scale=C0))
                            A([br_], [br_], lambda: nc.scalar.activation(R["e1"][:, 0:n], R["t2"][:, 0:n], AF.Exp, scale=-C0))
                        else:
                            V([br_], [br_], lambda: nc.vector.tensor_tensor(v3(R["t2"]), totb, v3(R["t1"]), ALU.subtract))
                            V([br_], [br_], lambda: nc.vector.tensor_tensor(v3(R["t1"]), totb, v3(R["t0"]), ALU.subtract))
                            A([br_], [br_], lambda: nc.scalar.activation(q1v, v3(R["t2"]), AF.Exp, scale=-C0))
                            A([br_], [br_], lambda: nc.scalar.activation(q2v, v3(R["t1"]), AF.Exp, scale=-C0))
                            A([br_], [br_], lambda: nc.scalar.activation(R["p1"][:, 0:n], R["t1"][:, 0:n], AF.Exp, scale=C0))
                            A([br_], [br_], lambda: nc.scalar.activation(R["e1"][:, 0:n], R["t0"][:, 0:n], AF.Exp, scale=-C0))
                        yield
                        BR = {nm: brows[nm][d][j] for nm in bnames}
                        q1b = q12b[d][j][:, 0:nch, 0, :]; q2b = q12b[d][j][:, 0:nch, 1, :]
                        V([br_], [br_], lambda: nc.vector.tensor_tensor(q1b, q1v, v3(kkT[d][j]), ALU.mult))
                        G([br_], [br_], lambda: nc.gpsimd.tensor_tensor(q2b, q2v, v3(rT[d][j]), ALU.mult))
                        yield
                        V([br_], [br_], lambda: nc.vector.tensor_tensor(BR["p2b"][:, 0:n], R["p1"][:, 0:n], R["kd"][:, 0:n], ALU.mult))
                        G([br_], [br_], lambda: nc.gpsimd.tensor_tensor(BR["p1b"][:, 0:n], R["p1"][:, 0:n], R["bb"][:, 0:n], ALU.mult))
                        V([br_], [br_], lambda: nc.vector.tensor_tensor(BR["e2b"][:, 0:n], R["e1"][:, 0:n], R["bb"][:, 0:n], ALU.mult))
                        G([br_], [br_], lambda: nc.gpsimd.tensor_tensor(BR["e1b"][:, 0:n], R["e1"][:, 0:n], R["kd"][:, 0:n], ALU.mult))

                def score_chain(k, d, j, hh, bi, c):
                    cs_ = slice(c * 128, (c + 1) * 128)
                    br_ = b_row[d][j]
                    BR = {nm: brows[nm][d][j] for nm in bnames}
                    mT = msk[:, 0:2, :] if d == 0 else msk[:, 2:4, :]
                    mN = msk[:, 2, :] if d == 0 else msk[:, 0, :]
                    hp = slice(64 * hh, 64 * hh + 64)
                    ke = k // 2; gi = k // 4; m = k % 4
                    if hh == 0:
                        pe_, bpe = pE.get()
                        P([br_, b_identb], [bpe], lambda: nc.tensor.transpose(pe_[:, 0:128], BR["e1b"][:, cs_], identb[:]))
                        P([br_, b_identb], [bpe], lambda: nc.tensor.transpose(pe_[:, 128:256], BR["e2b"][:, cs_], identb[:]))
                        A([bpe], [cbE[ke]], lambda: nc.scalar.copy(cE[ke][:], pe_[:]))
                    yield
                    q12c = q12b[d][j][hp, c, :, :].rearrange("p a t -> p (a t)")
                    q1c = q12b[d][j][hp, c, 0, :]
                    A1m, A2m = cA1[k], cA2[k]; bA1, bA2 = cbA[k]
                    a1f, ba1 = pW.get(); a1 = a1f[:, 0:256]
                    P([br_], [ba1], lambda: nc.tensor.matmul(a1, BR["p1b"][hp, cs_], q12c, start=True, stop=True))
                    V([ba1, b_msk], [bA1], lambda: nc.vector.tensor_tensor(A1m[:].rearrange("p (a t) -> p a t", a=2), a1.rearrange("p (a t) -> p a t", a=2), mT, ALU.mult))
                    PR, bPR = gcur[gi][0]; PT, bPT = gcur[gi][1]
                    V([ba1, b_msk], [bPR], lambda: nc.vector.scalar_tensor_tensor(PR[:, m, 0:128], a1[:, 0:128], -1.0, mT[:, 0, :], ALU.mult, ALU.mult))
                    A([b_identb], [bPR], lambda: nc.scalar.copy(PR[:, m, 128:256], identb[:]))
                    yield
                    a2f, ba2 = pW.get(); a2 = a2f[:, 0:256]
                    P([br_], [ba2], lambda: nc.tensor.matmul(a2, BR["p2b"][hp, cs_], q12c, start=True, stop=True))
                    V([ba2, b_msk], [bA2], lambda: nc.vector.tensor_tensor(A2m[:].rearrange("p (a t) -> p a t", a=2), a2.rearrange("p (a t) -> p a t", a=2), mT, ALU.mult))
                    yield
                    anf, ban = pN.get(); an = anf[:, 0:128]
                    P([br_], [ban], lambda: nc.tensor.matmul(an, q1c, BR["p1b"][hp, cs_], start=True, stop=True))
                    V([ban, b_msk], [bPT], lambda: nc.vector.scalar_tensor_tensor(PT[:, m, :], an, -1.0, mN, ALU.mult, ALU.mult))
                    yield

                def neumann_group(gi):
                    (PR, bPR), (PT, bPT) = gcur[gi]
                    for lev in range(7):
                        if lev < 6:
                            w1, bw1 = pW.get()
                            w1v = w1[:].rearrange("p (m c) -> p m c", c=256)
                            for m in range(4):
                                P([bPT, bPR], [bw1], lambda: nc.tensor.matmul(w1[:, m * 256:(m + 1) * 256], PT[:, m, :], PR[:, m, :], start=True, stop=True))
                            PRn, bPRn = gPR[gi].get()
                            A([bw1], [bPRn], lambda: nc.scalar.copy(PRn[:, :, 0:128], w1v[:, :, 0:128]))
                            V([bw1, bPR], [bPRn], lambda: nc.vector.tensor_tensor(PRn[:, :, 128:256], w1v[:, :, 128:256], PR[:, :, 128:256], ALU.add))
                            w2, bw2 = pN.get()
                            for m in range(4):
                                P([bPT, bPR], [bw2], lambda: nc.tensor.matmul(w2[:, m * 128:(m + 1) * 128], PR[:, m, 0:128], PT[:, m, :], start=True, stop=True))
                            PTn, bPTn = gPT[gi].get()
                            w2v = w2[:].rearrange("p (m c) -> p m c", c=128)
                            if (gi + lev) % 2 == 0:
                                A([bw2], [bPTn], lambda: nc.scalar.copy(PTn[:], w2v))
                            else:
                                V([bw2], [bPTn], lambda: nc.vector.tensor_copy(PTn[:], w2v))
                            PR, bPR, PT, bPT = PRn, bPRn, PTn, bPTn
                        else:
                            w2, bw2 = pN.get()
                            for m in range(4):
                                P([bPT, bPR], [bw2], lambda: nc.tensor.matmul(w2[:, m * 128:(m + 1) * 128], PT[:, m, :], PR[:, m, 128:256], start=True, stop=True))
                            PRn, bPRn = gPR[gi].get()
                            w2v = w2[:].rearrange("p (m c) -> p m c", c=128)
                            V([bw2, bPR], [bPRn], lambda: nc.vector.tensor_tensor(PRn[:, :, 128:256], w2v, PR[:, :, 128:256], ALU.add))
                            PR, bPR = PRn, bPRn
                        yield
                    gR[gi] = (PR, bPR)

                def seq_group(gi, d, bi, c):
                    c0, n = blocks[bi]
                    ti = c0 // 128 + c
                    PRf, bR = gR[gi]
                    hps = [slice(0, 64), slice(64, 128)]
                    ppg, bpp = pQ.get()
                    for m in range(4):
                        j, hh = m // 2, m % 2; k = 4 * gi + m; hp = hps[hh]
                        hcol = slice((2 * j + hh) * 64, (2 * j + hh) * 64 + 64)
                        P([b_row[d][j], b_M[d][j]], [bpp], lambda: nc.tensor.matmul(ppg[:, m * 64:(m + 1) * 64], q12b[d][j][hp, c, 0, :], Mb[d][j][hp, :], start=True, stop=False))
                        P([cbA[k][1], b_Vt[ti]], [bpp], lambda: nc.tensor.matmul(ppg[:, m * 64:(m + 1) * 64], cA2[k][:, 0:128], Vtb[:, ti, hcol], start=False, stop=True))
                    Psb, bPsb = gSq[gi].get()
                    A([bpp], [bPsb], lambda: nc.scalar.copy(Psb[:], ppg[:].rearrange("p (m c) -> p m c", c=64)))
                    yield
                    upg, bup = pQ.get()
                    for m in range(4):
                        P([bR, bPsb], [bup], lambda: nc.tensor.matmul(upg[:, m * 64:(m + 1) * 64], PRf[:, m, 128:256], Psb[:, m, :], start=True, stop=True))
                    nU, bnU = gSq[gi].get()
                    A([bup], [bnU], lambda: nc.scalar.mul(nU[:], upg[:].rearrange("p (m c) -> p m c", c=64), -1.0))
                    yield
                    ypg, byp = pQ.get()
                    for m in range(4):
                        j, hh = m // 2, m % 2; k = 4 * gi + m; hp = hps[hh]
                        hcol = slice((2 * j + hh) * 64, (2 * j + hh) * 64 + 64)
                        P([b_row[d][j], b_M[d][j]], [byp], lambda: nc.tensor.matmul(ypg[:, m * 64:(m + 1) * 64], q12b[d][j][hp, c, 1, :], Mb[d][j][hp, :], start=True, stop=False))
                        P([cbA[k][1], b_Vt[ti]], [byp], lambda: nc.tensor.matmul(ypg[:, m * 64:(m + 1) * 64], cA2[k][:, 128:256], Vtb[:, ti, hcol], start=False, stop=False))
                        P([cbA[k][0], bnU], [byp], lambda: nc.tensor.matmul(ypg[:, m * 64:(m + 1) * 64], cA1[k][:, 128:256], nU[:, m, :], start=False, stop=True))
                    V([byp, b_yacc[ti]], [b_yacc[ti]], lambda: nc.vector.tensor_tensor(yacc[:, ti, :], yacc[:, ti, :], ypg[:], ALU.add))
                    yield
                    for m in range(4):
                        j, hh = m // 2, m % 2; k = 4 * gi + m; hp = hps[hh]
                        hcol = slice((2 * j + hh) * 64, (2 * j + hh) * 64 + 64)
                        E_, bE = cE[k // 2], cbE[k // 2]
                        pm = pM[d][j]
                        P([bE, b_Vt[ti]], [b_pM[d][j]], lambda: nc.tensor.matmul(pm[hp, :], E_[:, 64 * hh:64 * hh + 64], Vtb[:, ti, hcol], start=True, stop=False))
                        P([bE, bnU], [b_pM[d][j]], lambda: nc.tensor.matmul(pm[hp, :], E_[:, 128 + 64 * hh:128 + 64 * hh + 64], nU[:, m, :], start=False, stop=True))
                    for j in range(2):
                        V([b_pM[d][j], b_M[d][j], b_row[d][j]], [b_M[d][j]], lambda: nc.vector.scalar_tensor_tensor(
                            Mst[d][j][:], Mst[d][j][:], gC[d][j][:, c:c + 1], pM[d][j][:], ALU.mult, ALU.add))
                        G([b_M[d][j]], [b_M[d][j]], lambda: nc.gpsimd.tensor_copy(Mb[d][j][:], Mst[d][j][:]))
                    yield

                def run_rr(gens):
                    gens = list(gens)
                    while gens:
                        nxt = []
                        for g in gens:
                            try:
                                next(g); nxt.append(g)
                            except StopIteration:
                                pass
                        gens = nxt

                gR = {}; gcur = {}
                for step in range(9):
                    if RW_STOP in (2, 3) and step > 0:
                        break
                    for d in range(2):
                        prep_shared(d, order[d][step])
                    run_rr([prep_combo(d, j, order[d][step]) for d in range(2) for j in range(2)])
                    if RW_STOP == 2:
                        break
                    chains = []; groups = []
                    for cc in range(2):
                        for d in range(2):
                            bi = order[d][step]
                            c = cc if d == 0 else 1 - cc
                            groups.append((len(groups), d, bi, c))
                            for j in range(2):
                                for hh in range(2):
                                    chains.append((len(chains), d, j, hh, bi, c))
                    for gi in range(4):
                        gcur[gi] = (gPR[gi].get(), gPT[gi].get())
                    run_rr([score_chain(*ch) for ch in chains])
                    run_rr([neumann_group(gi) for gi in range(4)])
                    for cc in range(2):
                        run_rr([seq_group(*g) for g in groups[cc * 2:(cc + 1) * 2]])
                f.barrier()
            if RW_STOP in (2, 3):
                return

            with ExitStack() as es2:
                gs0 = sbt(es2, "gs0", [128, T]); gs1 = sbt(es2, "gs1", [32, T]); b_gs = Buf()
                rw = sbt(es2, "rw", [128, T + 2]); b_rw = Buf()
                for (dst, frow, nrows, tapc) in ((gs0, OFF_GD, 128, 8), (gs1, OFF_GD + 128, 32, 9)):
                    G([], [b_rw], lambda: nc.gpsimd.memset(rw[:], 0.0))
                    D(rw[0:nrows, 1:T + 1], zT[frow:frow + nrows, :], [], [b_rw])
                    w = lambda tap, tapc=tapc, nrows=nrows: cp[0:nrows, tapc * 3 + tap:tapc * 3 + tap + 1]
                    V([b_rw, b_cp], [b_gs], lambda: nc.vector.tensor_scalar(dst[0:nrows, :], rw[0:nrows, 1:T + 1], w(1), None, ALU.mult))
                    for (a, b) in ((0, L), (L, T)):
                        V([b_rw, b_cp, b_gs], [b_gs], lambda a=a, b=b: nc.vector.scalar_tensor_tensor(
                            dst[0:nrows, a + 1:b], rw[0:nrows, a + 1:b], w(0), dst[0:nrows, a + 1:b], ALU.mult, ALU.add))
                        V([b_rw, b_cp, b_gs], [b_gs], lambda a=a, b=b: nc.vector.scalar_tensor_tensor(
                            dst[0:nrows, a:b - 1], rw[0:nrows, a + 2:b + 1], w(2), dst[0:nrows, a:b - 1], ALU.mult, ALU.add))
                    A([b_gs], [b_gs], lambda: nc.scalar.activation(dst[0:nrows, :], dst[0:nrows, :], AF.Sigmoid))
                gb = sbt(es2, "gb", [128, 2, 256]); b_gb = Buf()
                D(gb[:, 0, :], a_lnx_g[l, :].partition_broadcast(128), [], [b_gb])
                D(gb[:, 1, :], a_lnx_b[l, :].partition_broadcast(128), [], [b_gb])
                pg = carve(es2, "pg", 1, 256)
                st4 = Rot(es2, "st4", 2, [128, 4, 6]); mv4 = Rot(es2, "mv4", 2, [128, 4, 2]); rs4 = Rot(es2, "rs4", 2, [128, 4])
                yn = Rot(es2, "yn", 2, [128, 256]); oo = Rot(es2, "oo", 2, [128, 256])
                for i in range(NT):
                    g_p, bg = pg.get()
                    P([b_gs, b_gup], [bg], lambda: nc.tensor.matmul(g_p[:], gs0[:, i * 128:(i + 1) * 128], gup0[:], start=True, stop=False))
                    P([b_gs, b_gup], [bg], lambda: nc.tensor.matmul(g_p[:], gs1[0:32, i * 128:(i + 1) * 128], gup1[:], start=False, stop=True))
                    s4, bs4 = st4.get(); m4, bm4 = mv4.get(); r4, br4 = rs4.get(); y_n, byn = yn.get(); o_t, bo = oo.get()
                    for h in range(4):
                        V([b_yacc[i]], [bs4], lambda h=h: nc.vector.bn_stats(s4[:, h, :], yacc[:, i, h * 64:(h + 1) * 64]))
                    for h in range(4):
                        V([bs4], [bm4], lambda h=h: nc.vector.bn_aggr(m4[:, h, :], s4[:, h, :]))
                    A([bm4], [br4], lambda: nc.scalar.activation(r4[:], m4[:, :, 1], AF.Sqrt, bias=eps_t[:, 1:2], scale=1.0))
                    V([br4], [br4], lambda: nc.vector.reciprocal(r4[:], r4[:]))
                    for h in range(4):
                        V([b_yacc[i], bm4, br4], [byn], lambda h=h: nc.vector.tensor_scalar(
                            y_n[:, h * 64:(h + 1) * 64], yacc[:, i, h * 64:(h + 1) * 64], m4[:, h, 0:1], r4[:, h:h + 1], ALU.subtract, ALU.mult))
                    G([byn, b_gb], [byn], lambda: nc.gpsimd.tensor_tensor(y_n[:], y_n[:], gb[:, 0, :], ALU.mult))
                    G([byn, b_gb], [byn], lambda: nc.gpsimd.tensor_tensor(y_n[:], y_n[:], gb[:, 1, :], ALU.add))
                    for h in range(4):
                        V([byn, b_bacc[i], b_Vt[i]], [byn], lambda h=h: nc.vector.scalar_tensor_tensor(
                            y_n[:, h * 64:(h + 1) * 64], Vt[:, i, h * 64:(h + 1) * 64], bacc[:, i, h:h + 1], y_n[:, h * 64:(h + 1) * 64], ALU.mult, ALU.add))
                    V([byn, bg], [bo], lambda: nc.vector.tensor_tensor(o_t[:], y_n[:], g_p[:], ALU.mult))
                    D(omix[i * 128:(i + 1) * 128, 0:256], o_t[:], [bo], [])
                f.barrier()

    def phase_attn_b(l):
        with_ctx = l < DEPTH - 1
        with ExitStack() as es:
            cosT = sbt(es, "cosT", [64, S]); sinT = sbt(es, "sinT", [64, S]); rPT = sbt(es, "rPT", [64, 64]); b_c = Buf()
            D(cosT[:], ccos[:, :], [], [b_c]); D(sinT[:], csin[:, :], [], [b_c]); D(rPT[:], cropeP[:, :], [], [b_c])
            mkf = sbt(es, "mkf", [128, 4, 128]); mkb = sbt(es, "mkb", [128, 4, 128], BF16); b_mk = Buf()
            D(mkf[:], cmasks[:, :, :], [], [b_mk])
            V([b_mk], [b_mk], lambda: nc.vector.tensor_copy(mkb[:], mkf[:]))
            esk = sbt(es, "esk", [128, 6]); b_esk = Buf()
            D(esk[:], b_sink[l, :].partition_broadcast(128), [], [b_esk])
            A([b_esk], [b_esk], lambda: nc.scalar.activation(esk[:], esk[:], AF.Exp))
            kTb = sbt(es, "kTb", [64, 2, T], BF16); b_k = [Buf() for _ in range(2)]
            qTb = sbt(es, "qTb", [64, 2, NT, 3, 128], BF16); b_q = [Buf() for _ in range(6)]
            Va = sbt(es, "Va", [128, 2, NT, 65], BF16); b_Va = Buf()
            G([], [b_Va], lambda: nc.gpsimd.memset(Va[:], 1.0))
            with ExitStack() as es2:
                vf = sbt(es2, "vf", [128, NT, 128]); b_vf = Buf()
                D(vf[:], vtok[:, 256:384].rearrange("(n p) f -> p n f", p=128), [], [b_vf])
                for g in range(2):
                    V([b_vf, b_Va], [b_Va], lambda g=g: nc.vector.tensor_copy(Va[:, g, :, 0:64], vf[:, :, g * 64:(g + 1) * 64]))
                raw = Rot(es2, "rawb", 2, [64, T])
                pr = Rot(es2, "pr", 2, [128, 512], psum=True)
                t1 = Rot(es2, "t1b", 2, [64, 512]); t2 = Rot(es2, "t2b", 2, [64, 512])
                for hd in range(8):
                    rt, br = raw.get()
                    frow = OFF_QB + 64 * hd if hd < 6 else OFF_KB + 64 * (hd - 6)
                    D(rt[:], zT[frow:frow + 64, :], [], [br])
                    if hd < 6:
                        g, i3 = hd // 3, hd % 3
                        dst_ctx = qTb[:, g, 0:2, i3, :]
                        wb_ = b_q[hd]
                    else:
                        g = hd - 6
                        dst_ctx = kTb[:, g, 0:L].rearrange("p (n t) -> p n t", t=128)
                        wb_ = b_k[g]
                    A([br], [wb_], lambda rt=rt, dst_ctx=dst_ctx: nc.scalar.copy(dst_ctx, rt[:, 0:L].rearrange("p (n t) -> p n t", t=128)))
                    for q4 in range(4):
                        cs_ = slice(L + q4 * 512, L + (q4 + 1) * 512); ps_ = slice(q4 * 512, (q4 + 1) * 512)
                        p_t, bp = pr.get(); a_t, ba = t1.get(); c_t, bc = t2.get()
                        P([br, b_c], [bp], lambda: nc.tensor.matmul(p_t[0:64, :], rPT[:], rt[:, cs_], start=True, stop=True))
                        V([br, b_c], [ba], lambda: nc.vector.tensor_tensor(a_t[:], rt[:, cs_], cosT[:, ps_], ALU.mult))
                        V([bp, b_c], [bc], lambda: nc.vector.tensor_tensor(c_t[:], p_t[0:64, :], sinT[:, ps_], ALU.mult))
                        if hd < 6:
                            dst = qTb[:, g, 2 + 4 * q4:2 + 4 * q4 + 4, i3, :]
                        else:
                            dst = kTb[:, g, cs_].rearrange("p (n t) -> p n t", t=128)
                        G([ba, bc], [wb_], lambda: nc.gpsimd.tensor_tensor(dst, a_t[:].rearrange("p (n t) -> p n t", t=128), c_t[:].rearrange("p (n t) -> p n t", t=128), ALU.add))
                f.barrier()
            psc = Rot(es, "psc", 5, [128, 512], psum=True)
            pov = Rot(es, "pov", 2, [128, 512], psum=True)
            pt = Rot(es, "ptb", 16, [128, 384], BF16)
            ob = Rot(es, "obb", 3, [128, 384])
            dn = Rot(es, "dnb", 4, [128, 3])
            items = [(n, g) for n in range(NT) if not (n < 2 and not with_ctx) for g in range(2)]
            obufs = {}

            def stage1(n, g):
                kbs = [(0, None), (1, None)]
                if n >= 2:
                    if n - 1 >= 2: kbs.append((n - 1, 3))
                    kbs.append((n, None))
                    if n + 1 < NT: kbs.append((n + 1, 1))
                pts = []
                for (m, mk) in kbs:
                    s_p, bs = psc.get(); p_t, bpt = pt.get()
                    P([b_k[g]] + b_q[3 * g:3 * g + 3], [bs], lambda: nc.tensor.matmul(
                        s_p[:, 0:384], kTb[:, g, m * 128:(m + 1) * 128], qTb[:, g, n, :, :].rearrange("p a t -> p (a t)"), start=True, stop=True))
                    A([bs], [bpt], lambda: nc.scalar.activation(p_t[:], s_p[:, 0:384], AF.Exp, scale=SCALE))
                    if mk is not None:
                        G([bpt, b_mk], [bpt], lambda: nc.gpsimd.tensor_tensor(
                            p_t[:].rearrange("p (a t) -> p a t", a=3), p_t[:].rearrange("p (a t) -> p a t", a=3),
                            mkb[:, mk, :].unsqueeze(1).broadcast_to([128, 3, 128]), ALU.mult))
                    pts.append((p_t, bpt, m))
                return pts

            def stage2(n, g, pts):
                if g == 0:
                    obufs[n] = ob.get()
                o_t, bo = obufs[n]
                o_pf, bop = pov.get()
                o_p = o_pf[:, 0:195].rearrange("p (a e) -> p a e", e=65)
                for i3 in range(3):
                    for q, (p_t, bpt, m) in enumerate(pts):
                        P([bpt, b_Va], [bop], lambda: nc.tensor.matmul(
                            o_p[:, i3, :], p_t[:, i3 * 128:(i3 + 1) * 128], Va[:, g, m, :], start=(q == 0), stop=(q == len(pts) - 1)))
                d_t, bd = dn.get()
                V([bop, b_esk], [bd], lambda: nc.vector.tensor_tensor(d_t[:], o_p[:, :, 64], esk[:, 3 * g:3 * g + 3], ALU.add))
                V([bd], [bd], lambda: nc.vector.reciprocal(d_t[:], d_t[:]))
                for i3 in range(3):
                    hc = (3 * g + i3) * 64
                    V([bop, bd], [bo], lambda: nc.vector.tensor_scalar(o_t[:, hc:hc + 64], o_p[:, i3, 0:64], d_t[:, i3:i3 + 1], None, ALU.mult))
                if g == 1:
                    D(omix[n * 128:(n + 1) * 128, 256:640], o_t[:], [bo], [])

            prev = None
            for it in items:
                cur = (it, stage1(*it))
                if prev is not None:
                    stage2(prev[0][0], prev[0][1], prev[1])
                prev = cur
            stage2(prev[0][0], prev[0][1], prev[1])
            f.barrier()

    def phase_attn_c(l):
        with_ctx = l < DEPTH - 1
        with ExitStack() as es:
            qT = sbt(es, "qTn", [64, 6, T], BF16); kTn = sbt(es, "kTn", [64, 6, T], BF16); b_qk = Buf()
            for h in range(6):
                D(qT[:, h, :], zT[OFF_QN + 64 * h:OFF_QN + 64 * h + 64, :], [], [b_qk], eng=POOL)
                D(kTn[:, h, :], zT[OFF_KN + 64 * h:OFF_KN + 64 * h + 64, :], [], [b_qk], eng=POOL)
            Va = sbt(es, "Van", [128, NT, 6, 65], BF16); b_Va = Buf()
            G([], [b_Va], lambda: nc.gpsimd.memset(Va[:], 1.0))
            with ExitStack() as es2:
                vf = sbt(es2, "vfn", [128, NT, 384]); b_vf = Buf()
                D(vf[:], vtok[:, 384:768].rearrange("(n p) f -> p n f", p=128), [], [b_vf])
                V([b_vf, b_Va], [b_Va], lambda: nc.vector.tensor_copy(Va[:, :, :, 0:64], vf[:].rearrange("p n (h e) -> p n h e", e=64)))
                f.barrier()
            cv = sbt(es, "cv", [128, 64]); b_cv = Buf()
            D(cv[:], ccolvalid[:, :], [], [b_cv])
            ebf = Rot(es, "ebf", 2, [128, 15, 64]); ebb = sbt(es, "ebb", [128, 6, 15, 64], BF16); b_eb = [Buf() for _ in range(6)]
            oC = sbt(es, "oC", [128, NT, 384]); b_oC = [Buf() for _ in range(NT)]
            psc = Rot(es, "pscn", 3, [128, 1024], psum=True)
            pov = carve(es, "povn", 2, 128)
            pt = Rot(es, "ptn", 4, [128, 896], BF16)
            dn = Rot(es, "dnn", 4, [128, 1])
            for h in range(6):
                e_f, bef = ebf.get()
                D(e_f[0:64, :, :], rpbtab[l, h, :, :, :], [], [bef])
                D(e_f[64:128, :, :], rpbtab[l, h, :, :, :], [], [bef])
                A([bef], [bef], lambda: nc.scalar.activation(e_f[:], e_f[:], AF.Exp))
                V([bef, b_cv], [b_eb[h]], lambda: nc.vector.tensor_tensor(ebb[:, h, :, :], e_f[:], cv[:].unsqueeze(1).broadcast_to([128, 15, 64]), ALU.mult))
            items = [(h, n) for h in range(6) for n in range(NT) if not (n < 2 and not with_ctx)]
            TB = sbt(es, "TB", [128, 6, 640], BF16); b_TB = [Buf() for _ in range(6)]

            def keyblocks(n):
                kbs = [(0, None), (1, None)]
                if n >= 2:
                    nb = n - 2
                    for kb in range(16):
                        subs = []
                        anyv = False
                        for i in range(2):
                            for jq in range(2):
                                kr = 2 * kb + i; qr = 2 * nb + jq
                                rs_ = min(max(qr - 4, 0), 24)
                                ok = rs_ <= kr < rs_ + 8
                                subs.append((i, jq, kr - qr + 7 if ok else None))
                                anyv = anyv or ok
                        if anyv:
                            kbs.append((kb + 2, subs))
                return kbs

            kb_int = keyblocks(2 + 5)
            assert len(kb_int) == 7
            for h in range(6):
                G([], [b_TB[h]], lambda: nc.gpsimd.memset(TB[:, h, :], 0.0))
                for q, (m, subs) in enumerate(kb_int[2:]):
                    for (i, jq, ro) in subs:
                        if ro is not None:
                            V([b_eb[h], b_TB[h]], [b_TB[h]], lambda: nc.vector.tensor_copy(
                                TB[64 * i:64 * i + 64, h, q * 128 + 64 * jq:q * 128 + 64 * jq + 64], ebb[64 * i:64 * i + 64, h, ro, :]))

            def stage1(h, n):
                kbs = keyblocks(n)
                nk = len(kbs)
                s_p, bs = psc.get(); p_t, bpt = pt.get()
                for q, (m, subs) in enumerate(kbs):
                    P([b_qk], [bs], lambda: nc.tensor.matmul(s_p[:, q * 128:(q + 1) * 128], kTn[:, h, m * 128:(m + 1) * 128], qT[:, h, n * 128:(n + 1) * 128], start=True, stop=True))
                A([bs], [bpt], lambda: nc.scalar.activation(p_t[:, 0:nk * 128], s_p[:, 0:nk * 128], AF.Exp, scale=SCALE))
                if n >= 2 and 2 <= n - 2 <= 13:
                    assert nk == 7 and [sb for (_, sb) in kbs[2:]] == [sb for (_, sb) in kb_int[2:]]
                    V([bpt, b_TB[h]], [bpt], lambda: nc.vector.tensor_tensor(p_t[:, 256:896], p_t[:, 256:896], TB[:, h, :], ALU.mult))
                    return (p_t, bpt, kbs)
                cnt = 0
                for q, (m, subs) in enumerate(kbs):
                    if subs is None:
                        continue
                    for (i, jq, ro) in subs:
                        eng, ee = (V, nc.vector)
                        cnt += 1
                        sl = p_t[64 * i:64 * i + 64, q * 128 + 64 * jq:q * 128 + 64 * jq + 64]
                        if ro is None:
                            eng([bpt], [bpt], lambda: ee.memset(sl, 0.0))
                        else:
                            eng([bpt, b_eb[h]], [bpt], lambda: ee.tensor_tensor(sl, sl, ebb[64 * i:64 * i + 64, h, ro, :], ALU.mult))
                return (p_t, bpt, kbs)

            def stage2(h, n, st):
                p_t, bpt, kbs = st
                o_p, bop = pov.get()
                for q, (m, subs) in enumerate(kbs):
                    P([bpt, b_Va], [bop], lambda: nc.tensor.matmul(o_p[:, 0:65], p_t[:, q * 128:(q + 1) * 128], Va[:, m, h, :], start=(q == 0), stop=(q == len(kbs) - 1)))
                d_t, bd = dn.get()
                V([bop], [bd], lambda: nc.vector.reciprocal(d_t[:], o_p[:, 64:65]))
                V([bop, bd], [b_oC[n]], lambda: nc.vector.tensor_scalar(oC[:, n, h * 64:(h + 1) * 64], o_p[:, 0:64], d_t[:, 0:1], None, ALU.mult))

            prev = None
            for it in items:
                cur = (it, stage1(*it))
                if prev is not None:
                    stage2(prev[0][0], prev[0][1], prev[1])
                prev = cur
            stage2(prev[0][0], prev[0][1], prev[1])
            for n in range(NT):
                if n < 2 and not with_ctx:
                    continue
                D(omix[n * 128:(n + 1) * 128, 640:1024], oC[:, n, :], [b_oC[n]], [])
            f.barrier()

    def phase_out_moe(l):
        with_ctx = l < DEPTH - 1
        tiles = list(range(NT)) if with_ctx else list(range(2, NT))
        with ExitStack() as es:
            h2T = sbt(es, "h2T", [128, 8, T], BF16); b_h2T = [Buf() for _ in range(NT)]
            Wr = sbt(es, "Wr", [128, NT, 32]); b_Wr = [Buf() for _ in range(NT)]
            with ExitStack() as es2:
                mods = sbt(es2, "mods2", [128, 2, 3 * DM]); b_mods = Buf()
                D(mods[:, 0, :], modrow[1, 2 * DM:5 * DM].partition_broadcast(128), [], [b_mods])
                D(mods[:, 1, :], modrow[0, 2 * DM:5 * DM].partition_broadcast(128), [], [b_mods])
                G([b_mods], [b_mods], lambda: nc.gpsimd.tensor_scalar(mods[:, :, 2 * DM:3 * DM], mods[:, :, 2 * DM:3 * DM], 1.0, None, ALU.add))
                lgb = sbt(es2, "lgb", [128, 2, DM]); b_lgb = Buf()
                D(lgb[:, 0, :], ln1_g[l, :].partition_broadcast(128), [], [b_lgb])
                D(lgb[:, 1, :], ln1_b[l, :].partition_broadcast(128), [], [b_lgb])
                wo = sbt(es2, "wo", [128, 8, DM], BF16); b_wo = Buf()
                D(wo[:], w_out[l, :, :].rearrange("(k p) n -> p k n", p=128), [], [b_wo], eng=POOL)
                wr = sbt(es2, "wr", [128, 8, 36]); b_wr = Buf()
                D(wr[:], w_r[l, :, :].rearrange("(k p) n -> p k n", p=128), [], [b_wr])
                brb = sbt(es2, "brb", [128, 36]); D(brb[:], b_r[l, :].partition_broadcast(128), [], [b_wr])
                om = Rot(es2, "om", 2, [128, DM]); omb = Rot(es2, "omb", 2, [128, DM], BF16)
                oT = Rot(es2, "oT", 2, [128, 8, 128], BF16)
                xt = Rot(es2, "xt2", 2, [128, DM]); tt = Rot(es2, "tt", 2, [128, DM]); x1 = Rot(es2, "x1", 2, [128, DM])
                hf = Rot(es2, "hf", 2, [128, DM]); hb = Rot(es2, "hb2", 2, [128, DM], BF16)
                hT32 = Rot(es2, "hT32", 2, [128, 8, 128])
                st = Rot(es2, "st2", 4, [128, 2, 6]); mv = Rot(es2, "mv2", 4, [128, 2]); rs = Rot(es2, "rs2", 4, [128, 1])
                pTb = Rot(es2, "pTb", 2, [128, 8, 128], BF16, psum=True)
                pO = Rot(es2, "pO", 1, [128, 1024], psum=True)
                pT32 = Rot(es2, "pT32", 1, [128, 8, 128], psum=True)
                pL = carve(es2, "pL", 1, 64)
                sm = Rot(es2, "sm", 2, [128, 160])
                for i in tiles:
                    w = 0 if i < 2 else 1
                    o_m, bom = om.get(); o_b, bob = omb.get(); o_T, boT = oT.get()
                    D(o_m[:], omix[i * 128:(i + 1) * 128, :], [], [bom])
                    A([bom], [bob], lambda: nc.scalar.copy(o_b[:], o_m[:]))
                    p_t, bp = pTb.get()
                    for k in range(8):
                        P([bob, b_identb], [bp], lambda k=k: nc.tensor.transpose(p_t[:, k, :], o_b[:, k * 128:(k + 1) * 128], identb[:]))
                    V([bp], [boT], lambda: nc.vector.tensor_copy(o_T[:], p_t[:]))
                    p_o, bpo = pO.get()
                    for hf_ in range(2):
                        for k in range(8):
                            P([boT, b_wo], [bpo], lambda k=k, hf_=hf_: nc.tensor.matmul(
                                p_o[:, hf_ * 512:(hf_ + 1) * 512], o_T[:, k, :], wo[:, k, hf_ * 512:(hf_ + 1) * 512], start=(k == 0), stop=(k == 7)))
                    x_t, bx = xt.get(); t_t, bt = tt.get(); x_1, bx1 = x1.get()
                    D(x_t[:], xsrc(l, i), [], [bx])
                    V([bpo, b_mods], [bt], lambda: nc.vector.tensor_tensor(t_t[:], p_o[:], mods[:, w, 0:DM], ALU.mult))
                    V([bx, bt], [bt], lambda: nc.vector.scalar_tensor_tensor(t_t[:], x_t[:], ALPHA, t_t[:], ALU.mult, ALU.add))
                    s_t, bs = st.get(); m_t, bmv = mv.get(); r_t, br = rs.get()
                    layer_norm_stats(None, t_t, bt, (s_t, bs, m_t, bmv, r_t, br))
                    V([bt, bmv, br], [bt], lambda: nc.vector.tensor_scalar(t_t[:], t_t[:], m_t[:, 0:1], r_t[:, 0:1], ALU.subtract, ALU.mult))
                    G([bt, b_lgb], [bt], lambda: nc.gpsimd.tensor_tensor(t_t[:], t_t[:], lgb[:, 0, :], ALU.mult))
                    G([bt, b_lgb], [bx1], lambda: nc.gpsimd.tensor_tensor(x_1[:], t_t[:], lgb[:, 1, :], ALU.add))
                    D(xs[i * 128:(i + 1) * 128, :], x_1[:], [bx1], [])
                    s_t, bs = st.get(); m_t, bmv = mv.get(); r_t, br = rs.get()
                    layer_norm_stats(None, x_1, bx1, (s_t, bs, m_t, bmv, r_t, br))
                    h_f, bhf = hf.get(); h_b, bhb = hb.get()
                    V([bx1, bmv, br], [bhf], lambda: nc.vector.tensor_scalar(h_f[:], x_1[:], m_t[:, 0:1], r_t[:, 0:1], ALU.subtract, ALU.mult))
                    G([bhf, b_mods], [bhf], lambda: nc.gpsimd.tensor_tensor(h_f[:], h_f[:], mods[:, w, 2 * DM:3 * DM], ALU.mult))
                    G([bhf, b_mods], [bhf], lambda: nc.gpsimd.tensor_tensor(h_f[:], h_f[:], mods[:, w, DM:2 * DM], ALU.add))
                    A([bhf], [bhb], lambda: nc.scalar.copy(h_b[:], h_f[:]))
                    p_t, bp = pTb.get()
                    for k in range(8):
                        P([bhb, b_identb], [bp], lambda k=k: nc.tensor.transpose(p_t[:, k, :], h_b[:, k * 128:(k + 1) * 128], identb[:]))
                    A([bp], [b_h2T[i]], lambda: nc.scalar.copy(h2T[:, :, i * 128:(i + 1) * 128], p_t[:]))
                    p32, bp32 = pT32.get()
                    for k in range(8):
                        P([bhf, b_ident], [bp32], lambda k=k: nc.tensor.transpose(p32[:, k, :], h_f[:, k * 128:(k + 1) * 128], ident[:]))
                    h32, bh32 = hT32.get()
                    V([bp32], [bh32], lambda: nc.vector.tensor_copy(h32[:], p32[:]))
                    p_l, bpl = pL.get()
                    for k in range(8):
                        P([bh32, b_wr], [bpl], lambda k=k: nc.tensor.matmul(p_l[:, 0:36], h32[:, k, :], wr[:, k, :], start=(k == 0), stop=(k == 7)))
                    s_, bsm = sm.get()
                    lg = s_[:, 0:36]; gmx = s_[:, 36:37]; ngm = s_[:, 37:38]; g1h = s_[:, 40:44]; e4 = s_[:, 44:48]
                    esum = s_[:, 48:49]; pgr = s_[:, 49:50]; pen = s_[:, 52:56]; m8 = s_[:, 56:64]; elm = s_[:, 64:96]
                    dd = s_[:, 96:97]; ee_ = s_[:, 97:98]; c1 = s_[:, 98:99]; c2 = s_[:, 99:100]
                    V([bpl, b_wr], [bsm], lambda: nc.vector.tensor_tensor(lg, p_l[:, 0:36], brb[:], ALU.add))
                    V([bsm], [bsm], lambda: nc.vector.tensor_reduce(gmx, s_[:, 0:4], AX.X, ALU.max))
                    V([bsm], [bsm], lambda: nc.vector.tensor_scalar(ngm, gmx, -1.0, None, ALU.mult))
                    V([bsm], [bsm], lambda: nc.vector.tensor_scalar(g1h, s_[:, 0:4], gmx, None, ALU.is_equal))
                    A([bsm], [bsm], lambda: nc.scalar.activation(e4, s_[:, 0:4], AF.Exp, bias=ngm, scale=1.0))
                    V([bsm], [bsm], lambda: nc.vector.tensor_reduce(esum, e4, AX.X, ALU.add))
                    V([bsm], [bsm], lambda: nc.vector.reciprocal(pgr, esum))
                    V([bsm], [bsm], lambda: nc.vector.tensor_scalar(pen, g1h, -1.0, 1e30, ALU.add, ALU.mult))
                    V([bsm], [bsm], lambda: nc.vector.tensor_tensor(elm.rearrange("p (g e) -> p g e", e=8), s_[:, 4:36].rearrange("p (g e) -> p g e", e=8),
                                                                    pen.unsqueeze(2).broadcast_to([128, 4, 8]), ALU.add))
                    V([bsm], [bsm], lambda: nc.vector.max(m8, elm))
                    V([bsm], [bsm], lambda: nc.vector.tensor_tensor(dd, m8[:, 1:2], m8[:, 0:1], ALU.subtract))
                    A([bsm], [bsm], lambda: nc.scalar.activation(ee_, dd, AF.Exp))
                    V([bsm], [bsm], lambda: nc.vector.tensor_scalar(c1, ee_, 1.0, None, ALU.add))
                    V([bsm], [bsm], lambda: nc.vector.reciprocal(c1, c1))
                    V([bsm], [bsm], lambda: nc.vector.tensor_tensor(c2, ee_, c1, ALU.mult))
                    V([bsm], [bsm], lambda: nc.vector.tensor_tensor(c1, c1, pgr, ALU.mult))
                    V([bsm], [bsm], lambda: nc.vector.tensor_tensor(c2, c2, pgr, ALU.mult))
                    V([bsm], [b_Wr[i]], lambda: nc.vector.tensor_scalar(Wr[:, i, :], elm, m8[:, 0:1], c1, ALU.is_equal, ALU.mult))
                    V([bsm], [bsm], lambda: nc.vector.tensor_scalar(s_[:, 100:132], elm, m8[:, 1:2], c2, ALU.is_equal, ALU.mult))
                    V([bsm, b_Wr[i]], [b_Wr[i]], lambda: nc.vector.tensor_tensor(Wr[:, i, :], Wr[:, i, :], s_[:, 100:132], ALU.add))
                f.barrier()
            if state.get("stop_before_moe"):
                return
            facc = sbt(es, "facc", [128, NT, DM]); b_f = [Buf() for _ in range(NT)]
            with ExitStack() as es2:
                wg = Rot(es2, "wg", 2, [128, 8, DE], BF16); wu = Rot(es2, "wu", 2, [128, 8, DE], BF16)
                wd = Rot(es2, "wd", 2, [128, 4, DM], BF16)
                pG = Rot(es2, "pG", 2, [128, 512], psum=True); pU = Rot(es2, "pU", 2, [128, 512], psum=True)
                pD = Rot(es2, "pD", 3, [128, 512], psum=True)
                sg = Rot(es2, "sg", 2, [128, 512]); aT = Rot(es2, "aT", 8, [128, 512], BF16)
                t0_ = tiles[0] * 128
                ntok = len(tiles) * 128
                blocks_ = [(t0_ + b * 512, min(512, ntok - b * 512)) for b in range((ntok + 511) // 512)]
                for e in range(NE):
                    g_w, bg = wg.get(); u_w, bu = wu.get(); d_w, bdw = wd.get()
                    D(g_w[:], w_gate[l, e, :, :].rearrange("(k p) n -> p k n", p=128), [], [bg], eng=POOL)
                    D(u_w[:], w_up[l, e, :, :].rearrange("(k p) n -> p k n", p=128), [], [bu], eng=POOL)
                    D(d_w[:], w_down[l, e, :, :].rearrange("(k p) n -> p k n", p=128), [], [bdw], eng=POOL)
                    for (tb0, tsz) in blocks_:
                        rb = [b_h2T[i] for i in range(tb0 // 128, (tb0 + tsz) // 128)]
                        acts = []
                        for fc in range(4):
                            g_p, bgp = pG.get(); u_p, bup = pU.get()
                            for k in range(8):
                                P(rb + [bg], [bgp], lambda k=k: nc.tensor.matmul(g_p[:, 0:tsz], g_w[:, k, fc * 128:(fc + 1) * 128], h2T[:, k, tb0:tb0 + tsz], start=(k == 0), stop=(k == 7)))
                            for k in range(8):
                                P(rb + [bu], [bup], lambda k=k: nc.tensor.matmul(u_p[:, 0:tsz], u_w[:, k, fc * 128:(fc + 1) * 128], h2T[:, k, tb0:tb0 + tsz], start=(k == 0), stop=(k == 7)))
                            s_g, bsg = sg.get(); a_T, baT = aT.get()
                            A([bgp], [bsg], lambda: nc.scalar.activation(s_g[:, 0:tsz], g_p[:, 0:tsz], AF.Silu))
                            V([bsg, bup], [baT], lambda: nc.vector.tensor_tensor(a_T[:, 0:tsz], s_g[:, 0:tsz], u_p[:, 0:tsz], ALU.mult))
                            acts.append((a_T, baT))
                        for q in range(tsz // 128):
                            ti = tb0 // 128 + q
                            for hf_ in range(2):
                                d_p, bdp = pD.get()
                                for fc in range(4):
                                    a_T, baT = acts[fc]
                                    P([baT, bdw], [bdp], lambda fc=fc, a_T=a_T: nc.tensor.matmul(
                                        d_p[:], a_T[:, q * 128:(q + 1) * 128], d_w[:, fc, hf_ * 512:(hf_ + 1) * 512], start=(fc == 0), stop=(fc == 3)))
                                fs = facc[:, ti, hf_ * 512:(hf_ + 1) * 512]
                                if e == 0:
                                    V([bdp, b_Wr[ti]], [b_f[ti]], lambda: nc.vector.tensor_scalar(fs, d_p[:], Wr[:, ti, e:e + 1], None, ALU.mult))
                                else:
                                    V([bdp, b_Wr[ti], b_f[ti]], [b_f[ti]], lambda: nc.vector.scalar_tensor_tensor(fs, d_p[:], Wr[:, ti, e:e + 1], fs, ALU.mult, ALU.add))
                f.barrier()
            with ExitStack() as es2:
                g2 = sbt(es2, "g2", [128, 2, DM]); b_g2 = Buf()
                D(g2[:, 0, :], modrow[1, 5 * DM:6 * DM].partition_broadcast(128), [], [b_g2])
                D(g2[:, 1, :], modrow[0, 5 * DM:6 * DM].partition_broadcast(128), [], [b_g2])
                lgb = sbt(es2, "lgb2", [128, 2, DM]); b_lgb = Buf()
                D(lgb[:, 0, :], ln2_g[l, :].partition_broadcast(128), [], [b_lgb])
                D(lgb[:, 1, :], ln2_b[l, :].partition_broadcast(128), [], [b_lgb])
                xt = Rot(es2, "xt3", 2, [128, DM]); tt = Rot(es2, "tt3", 2, [128, DM])
                st = Rot(es2, "st3", 2, [128, 2, 6]); mv = Rot(es2, "mv3", 2, [128, 2]); rs = Rot(es2, "rs3", 2, [128, 1])
                for i in tiles:
                    w = 0 if i < 2 else 1
                    x_t, bx = xt.get(); t_t, bt = tt.get()
                    D(x_t[:], xs[i * 128:(i + 1) * 128, :], [], [bx])
                    G([b_f[i], b_g2], [bt], lambda: nc.gpsimd.tensor_tensor(t_t[:], facc[:, i, :], g2[:, w, :], ALU.mult))
                    V([bx, bt], [bt], lambda: nc.vector.scalar_tensor_tensor(t_t[:], x_t[:], ALPHA, t_t[:], ALU.mult, ALU.add))
                    s_t, bs = st.get(); m_t, bmv = mv.get(); r_t, br = rs.get()
                    layer_norm_stats(None, t_t, bt, (s_t, bs, m_t, bmv, r_t, br))
                    V([bt, bmv, br], [bt], lambda: nc.vector.tensor_scalar(t_t[:], t_t[:], m_t[:, 0:1], r_t[:, 0:1], ALU.subtract, ALU.mult))
                    G([bt, b_lgb], [bt], lambda: nc.gpsimd.tensor_tensor(t_t[:], t_t[:], lgb[:, 0, :], ALU.mult))
                    G([bt, b_lgb], [bt], lambda: nc.gpsimd.tensor_tensor(t_t[:], t_t[:], lgb[:, 1, :], ALU.add))
                    if l == DEPTH - 1:
                        D(y_out[(i - 2) * 128:(i - 1) * 128, :], t_t[:], [bt], [])
                    else:
                        D(xs[i * 128:(i + 1) * 128, :], t_t[:], [bt], [])
                f.barrier()

    done = False
    for l in range(DEPTH):
        for tag, fn in (("mod", phase_mod), ("proj", phase_ln_proj), ("rwkv", phase_rwkv),
                        ("attb", phase_attn_b), ("attc", phase_attn_c), ("moe", phase_out_moe)):
            if STOP is not None and (l, tag) == tuple(STOP[:2]) and len(STOP) > 2:
                state["stop_before_moe"] = True
            fn(l)
            if STOP is not None and (l, tag) == tuple(STOP[:2]):
                done = True
                break
        if done:
            break
    f.barrier()
    print("n_inst", f.n_inst, flush=True)
    return nc


def _consts():
    p = np.arange(128)[:, None]; q = np.arange(128)[None, :]
    masks = np.stack([(p < q), (p <= q), (p > q), (p >= q)], 1).astype(np.float32)
    ident = np.eye(128, dtype=np.float32)
    block = (p // 64 == q // 64).astype(np.float32)
    hsel = (np.arange(128)[:, None] // 64 == np.arange(2)[None, :]).astype(np.float32)
    t = np.arange(S)
    pos = np.stack([t // 64, t % 64], -1).astype(np.float32)
    inv = (10000.0 ** (-np.arange(16, dtype=np.float32) / 16)).astype(np.float32)
    ang = pos[:, :, None] * inv
    cos = np.cos(ang).astype(np.float32); sin = np.sin(ang).astype(np.float32)
    cosT = np.zeros((64, S), np.float32); sinT = np.zeros((64, S), np.float32)
    for ax in range(2):
        for half in range(2):
            r0 = ax * 32 + half * 16
            cosT[r0:r0 + 16] = cos[:, ax, :].T
            sinT[r0:r0 + 16] = sin[:, ax, :].T
    Pm = np.zeros((64, 64), np.float32)
    for ax in range(2):
        for i in range(16):
            Pm[ax * 32 + i, ax * 32 + 16 + i] = -1.0
            Pm[ax * 32 + 16 + i, ax * 32 + i] = 1.0
    ropeP = np.ascontiguousarray(Pm.T)
    col = np.arange(64)
    cstart = np.clip(col - 8, 0, 48)
    cvalid = ((col[None, :] >= cstart[:, None]) & (col[None, :] < cstart[:, None] + 16)).astype(np.float32)
    colvalid = np.concatenate([cvalid.T, cvalid.T], 0)
    scan = np.ones((128, 512), np.float32); scan[:, ::128] = 0.0
    return dict(cmasks=masks, cident=ident, cblock=block, chsel=hsel, ccos=cosT, csin=sinT, cropeP=ropeP,
                ccolvalid=np.ascontiguousarray(colvalid), cscanmask=scan)


def _host_layout(inp):
    f32 = lambda a: np.ascontiguousarray(np.asarray(a, dtype=np.float32))
    sh = {}
    sh["w_mod"] = f32(inp["w_mod"]); sh["b_mod"] = f32(inp["b_mod"]); sh["w_in"] = f32(inp["w_in"]); sh["w_out"] = f32(inp["w_out"])
    sh["a_shift"] = f32(inp["a_shift"])
    sh["a_w_up"] = f32(np.asarray(inp["a_w_up"]).reshape(DEPTH, 128, 256))
    sh["a_a_up"] = f32(np.asarray(inp["a_a_up"]).reshape(DEPTH, 128, 256))
    sh["a_g_up"] = f32(inp["a_g_up"])
    cp = np.zeros((DEPTH, 128, 48), np.float32)
    ash = np.zeros((DEPTH, 3, 1280), np.float32); ash[:, :, :A_IN] = inp["a_shift"]
    for l in range(DEPTH):
        for ch in range(10):
            for tap in range(3):
                cp[l, :, ch * 3 + tap] = ash[l, tap, ch * 128:(ch + 1) * 128]
        for d in range(2):
            for j in range(2):
                cp[l, :, 30 + d * 2 + j] = inp["a_w0"][l, d, j * 128:(j + 1) * 128]
                cp[l, :, 34 + d * 2 + j] = inp["a_a0"][l, d, j * 128:(j + 1) * 128]
        for j in range(2):
            cp[l, :, 38 + j] = inp["a_k_k"][l, j * 128:(j + 1) * 128]
            cp[l, :, 40 + j] = inp["a_k_a"][l, j * 128:(j + 1) * 128]
            cp[l, :, 42 + j] = inp["a_r_k"][l, j * 128:(j + 1) * 128]
    sh["colpar"] = cp
    for k in ("a_r_k", "a_lnx_g", "a_lnx_b", "b_sink", "ln1_g", "ln1_b", "ln2_g", "ln2_b"):
        sh[k] = f32(inp[k])
    for k in ("w_gate", "w_up", "w_down"):
        sh[k] = f32(np.asarray(inp[k])[:, :NE_DECL])
    kc = np.arange(64)[:, None]; qc = np.arange(64)[None, :]
    idx = np.clip(kc - qc, -15, 15) + 15
    rpb = np.asarray(inp["c_rpb"], np.float32)
    tab = rpb[:, :, :, idx]
    sh["rpbtab"] = f32(tab.transpose(0, 1, 3, 2, 4))
    sh["w_r"] = f32(np.concatenate([inp["w_rg"], inp["w_re"]], -1))
    sh["b_r"] = f32(np.concatenate([inp["b_rg"], inp["b_re"]], -1))
    sh.update(_consts())
    return sh


_PROG = {}


def kernel(**inputs):
    inp = {k: np.asarray(v) for k, v in inputs.items()}
    shared = _host_layout(inp)
    if "nc" not in _PROG:
        _PROG["nc"] = build_program()
    nc = _PROG["nc"]
    in_maps = []
    for b in range(NCORES):
        m = dict(shared)
        m["x"] = np.ascontiguousarray(inp["x"][b], dtype=np.float32)
        m["ctx"] = np.ascontiguousarray(inp["ctx"][b], dtype=np.float32)
        cc = np.zeros((128, 8, 2), np.float32)
        cc[:, :, 0] = np.asarray(inp["c"][b], np.float32).reshape(8, 128).T
        cc[:, :, 1] = np.asarray(inp["c_ctx"], np.float32).reshape(8, 128).T
        m["ccol"] = cc
        in_maps.append(m)
    res = run_bass_kernel_spmd(nc, in_maps, core_ids=list(range(NCORES)))
    _PROG["res"] = res
    out = np.stack([np.asarray(r["y"], dtype=np.float32) for r in res.results], 0)
    return out
```

```python
from contextlib import ExitStack
import numpy as np
import concourse.bass as bass
import concourse.mybir as mybir
from concourse.bass_utils import run_bass_kernel_spmd

F32 = mybir.dt.float32
BF16 = mybir.dt.bfloat16
ALU = mybir.AluOpType
AF = mybir.ActivationFunctionType
AX = mybir.AxisListType

DM = 1024; S = 2048; L = 256; T = S + L; NT = T // 128; DEPTH = 2
HD = 64; A_IN = 1184; N_IN = 2976; NE = 32; DE = 512
ALPHA = (2.0 * DEPTH) ** 0.25
SCALE = HD ** -0.5
C0 = float(np.exp(-0.5))
LN_EPS = 1e-6; GN_EPS = 64e-5
OFF_R, OFF_K, OFF_V, OFF_WD, OFF_AD, OFF_GD = 0, 256, 512, 768, 896, 1024
OFF_QB, OFF_KB, OFF_VB = 1184, 1568, 1696
OFF_QN, OFF_KN, OFF_VN = 1824, 2208, 2592
EPOCH = 30000
DEBUG = False
RW_STOP = 0
NCORES = 8
NE_DECL = 32
STOP = None


class Buf:
    __slots__ = ("w", "r")

    def __init__(self):
        self.w = {}
        self.r = {}


class Eng:
    def __init__(self, fw, name, eng, dma_only=False):
        self.fw = fw; self.name = name; self.eng = eng
        self.sem = None; self.cnt = 0; self.epoch = 0; self.seen = {}
        self.dma_only = dma_only
        if not dma_only:
            self.new_sem()

    def new_sem(self):
        self.sem = self.fw.nc.alloc_semaphore(f"p_{self.name}_{self.epoch}")
        self.cnt = 0
        self.epoch += 1
        self.fw.all_sems.append(self)


class FW:
    def __init__(self, nc, n_dma_sems=24):
        self.nc = nc
        self.all_sems = []
        self.pe = Eng(self, "pe", nc.tensor)
        self.act = Eng(self, "act", nc.scalar)
        self.dve = Eng(self, "dve", nc.vector)
        self.pool = Eng(self, "pool", nc.gpsimd)
        self.sp = Eng(self, "sp", nc.sync, dma_only=True)
        self.dma_sems = [[nc.alloc_semaphore(f"d{i}"), 0] for i in range(n_dma_sems)]
        self.dma_rr = 0
        self.sw_sems = [[nc.alloc_semaphore(f"w{i}"), 0] for i in range(8)]
        self.sw_rr = 0
        self.n_inst = 0

    def _wait(self, E, sem, val):
        k = sem.name
        if E.seen.get(k, 0) >= val:
            return
        E.eng.wait_ge(sem, val)
        E.seen[k] = val

    def _deps(self, E, reads, writes, skip_self=True):
        need = {}
        for b in reads:
            for k, sv in b.w.items():
                if k not in need or need[k][1] < sv[1]:
                    need[k] = sv
        for b in writes:
            for d in (b.w, b.r):
                for k, sv in d.items():
                    if k not in need or need[k][1] < sv[1]:
                        need[k] = sv
        me = E.sem.name if (skip_self and E.sem is not None and E.name == 'pe') else None
        for k, (s, v) in need.items():
            if k == me:
                continue
            self._wait(E, s, v)

    def _mark(self, sem, val, reads, writes):
        k = sem.name
        for b in reads:
            o = b.r.get(k)
            if o is None or o[1] < val:
                b.r[k] = (sem, val)
        for b in writes:
            o = b.w.get(k)
            if o is None or o[1] < val:
                b.w[k] = (sem, val)

    def op(self, E, reads, writes, fn):
        if E.cnt >= EPOCH:
            E.new_sem()
        self._deps(E, reads, writes)
        ins = fn()
        E.cnt += 1
        ins.then_inc(E.sem, 1)
        self._mark(E.sem, E.cnt, reads, writes)
        self.n_inst += 1
        return ins

    def dma(self, E, out, in_, reads, writes, **kw):
        if E.name == "pool":
            slot = self.sw_sems[self.sw_rr]
            self.sw_rr = (self.sw_rr + 1) % len(self.sw_sems)
        else:
            slot = self.dma_sems[self.dma_rr]
            self.dma_rr = (self.dma_rr + 1) % len(self.dma_sems)
        sem, cnt = slot
        if cnt > 0:
            self._wait(E, sem, cnt)
        self._deps(E, reads, writes, skip_self=False)
        ins = E.eng.dma_start(out=out, in_=in_, **kw)
        slot[1] = cnt + 16
        ins.then_inc(sem, 16)
        self._mark(sem, slot[1], reads, writes)
        self.n_inst += 1
        return ins

    def barrier(self):
        cur = []
        for X in (self.pe, self.act, self.dve, self.pool):
            if X.cnt > 0:
                cur.append((X.sem, X.cnt))
        for sem, cnt in self.dma_sems + self.sw_sems:
            if cnt > 0:
                cur.append((sem, cnt))
        for E in (self.pe, self.act, self.dve, self.pool, self.sp):
            for sem, val in cur:
                if E.sem is not None and sem.name == E.sem.name:
                    continue
                self._wait(E, sem, val)


def build_program():
    nc = bass.Bass("TRN2", target_bir_lowering=False)
    f = FW(nc)
    PE, ACT, DVE, POOL, SP = f.pe, f.act, f.dve, f.pool, f.sp

    def din(name, shape):
        return nc.dram_tensor(name, list(shape), F32, kind="ExternalInput").ap()

    def scr(name, shape):
        return nc.dram_tensor(name, list(shape), F32, kind=("ExternalOutput" if DEBUG else "Internal")).ap()

    x_in = din("x", [S, DM]); ctx_in = din("ctx", [L, DM]); ccol = din("ccol", [128, 8, 2])
    w_mod = din("w_mod", [DEPTH, DM, 6 * DM]); b_mod = din("b_mod", [DEPTH, 6 * DM])
    w_in = din("w_in", [DEPTH, DM, N_IN]); w_out = din("w_out", [DEPTH, DM, DM])
    a_shift = din("a_shift", [DEPTH, 3, A_IN])
    a_w_up = din("a_w_up", [DEPTH, 128, 256]); a_a_up = din("a_a_up", [DEPTH, 128, 256])
    a_g_up = din("a_g_up", [DEPTH, 160, 256])
    colpar = din("colpar", [DEPTH, 128, 48])
    a_r_k = din("a_r_k", [DEPTH, 256]); a_lnx_g = din("a_lnx_g", [DEPTH, 256]); a_lnx_b = din("a_lnx_b", [DEPTH, 256])
    b_sink = din("b_sink", [DEPTH, 6])
    rpbtab = din("rpbtab", [DEPTH, 6, 64, 15, 64])
    ln1_g = din("ln1_g", [DEPTH, DM]); ln1_b = din("ln1_b", [DEPTH, DM])
    ln2_g = din("ln2_g", [DEPTH, DM]); ln2_b = din("ln2_b", [DEPTH, DM])
    w_r = din("w_r", [DEPTH, DM, 36]); b_r = din("b_r", [DEPTH, 36])
    w_gate = din("w_gate", [DEPTH, NE_DECL, DM, DE]); w_up = din("w_up", [DEPTH, NE_DECL, DM, DE])
    w_down = din("w_down", [DEPTH, NE_DECL, DE, DM])
    cmasks = din("cmasks", [128, 4, 128]); cident = din("cident", [128, 128])
    cblock = din("cblock", [128, 128]); chsel = din("chsel", [128, 2])
    ccos = din("ccos", [64, S]); csin = din("csin", [64, S]); cropeP = din("cropeP", [64, 64])
    ccolvalid = din("ccolvalid", [128, 64]); cscanmask = din("cscanmask", [128, 512])
    y_out = nc.dram_tensor("y", [S, DM], F32, kind="ExternalOutput").ap()

    xs = scr("xs", [T, DM]); modrow = scr("modrow", [2, 6 * DM])
    zT = scr("zT", [N_IN, T]); vtok = scr("vtok", [T, 768]); omix = scr("omix", [T, DM])
    dbgh = scr("dbgh", [T, DM]) if DEBUG else None
    dbgs = scr("dbgs", [T, 4]) if DEBUG else None

    state = {"stop": False}

    def xsrc(l, i):
        if l == 0:
            return ctx_in[i * 128:(i + 1) * 128, :] if i < 2 else x_in[(i - 2) * 128:(i - 1) * 128, :]
        return xs[i * 128:(i + 1) * 128, :]

    top = ExitStack()
    uid = [0]

    def sbt(es, name, shape, dt=F32):
        uid[0] += 1
        return es.enter_context(nc.sbuf_tensor(f"{name}_{uid[0]}", list(shape), dt))

    def pst(es, name, shape, dt=F32):
        uid[0] += 1
        return es.enter_context(nc.psum_tensor(f"{name}_{uid[0]}", list(shape), dt))
    ident = sbt(top, "ident", [128, 128]); b_ident = Buf()
    identb = sbt(top, "identb", [128, 128], BF16); b_identb = Buf()
    f.dma(SP, ident[:], cident[:, :], [], [b_ident])
    f.op(DVE, [b_ident], [b_identb], lambda: nc.vector.tensor_copy(identb[:], ident[:]))

    def V(r, w, fn): return f.op(DVE, r, w, fn)
    def A(r, w, fn): return f.op(ACT, r, w, fn)
    def G(r, w, fn): return f.op(POOL, r, w, fn)
    def P(r, w, fn): return f.op(PE, r, w, fn)
    def D(out, in_, r, w, eng=None, **kw): return f.dma(eng or SP, out, in_, r, w, **kw)

    class Rot:
        def __init__(self, es, name, n, shape, dt=F32, psum=False):
            mk = pst if psum else sbt
            self.t = [mk(es, f"{name}{i}", shape, dt) for i in range(n)]
            self.b = [Buf() for _ in range(n)]
            self.i = 0

        def get(self):
            k = self.i; self.i = (self.i + 1) % len(self.t)
            return self.t[k], self.b[k]

    class Slots:
        def __init__(self, aps):
            self.t = list(aps); self.b = [Buf() for _ in self.t]; self.i = 0

        def get(self):
            k = self.i; self.i = (self.i + 1) % len(self.t)
            return self.t[k], self.b[k]

    def carve(es, name, nbanks, width):
        per = 512 // width
        views = [[None] * nbanks for _ in range(per)]
        bufs = [Buf() for _ in range(nbanks)]
        for i in range(nbanks):
            bk = pst(es, f"{name}{i}", [128, 512])
            for q in range(per):
                views[q][i] = bk[:, q * width:(q + 1) * width]
        sl = Slots([views[q][i] for q in range(per) for i in range(nbanks)])
        sl.b = [bufs[i] for q in range(per) for i in range(nbanks)]
        return sl

    def layer_norm_stats(es_tiles, src_ap, b_src, tagbufs):
        st, b_st, mv, b_mv, rs, b_rs = tagbufs
        V([b_src], [b_st], lambda: nc.vector.bn_stats(st[:, 0, :], src_ap[:, 0:512]))
        V([b_src], [b_st], lambda: nc.vector.bn_stats(st[:, 1, :], src_ap[:, 512:1024]))
        V([b_st], [b_mv], lambda: nc.vector.bn_aggr(mv[:], st[:].rearrange("p a b -> p (a b)")))
        A([b_mv], [b_rs], lambda: nc.scalar.activation(rs[:], mv[:, 1:2], AF.Sqrt, bias=eps_t[:, 0:1], scale=1.0))
        V([b_rs], [b_rs], lambda: nc.vector.reciprocal(rs[:], rs[:]))

    eps_t = sbt(top, "eps_t", [128, 4]); b_eps = Buf()
    G([], [b_eps], lambda: nc.gpsimd.memset(eps_t[:, 0:1], LN_EPS))
    G([], [b_eps], lambda: nc.gpsimd.memset(eps_t[:, 1:2], GN_EPS))
    G([], [b_eps], lambda: nc.gpsimd.memset(eps_t[:, 2:3], 1e-12))
    G([], [b_eps], lambda: nc.gpsimd.memset(eps_t[:, 3:4], 1.0))
    f.barrier()

    def phase_mod(l):
        with ExitStack() as es:
            cr = sbt(es, "cr", [128, 8, 2]); b_cr = Buf()
            cs = sbt(es, "cs", [128, 8, 2]); b_cs = Buf()
            D(cr[:], ccol[:, :, :], [], [b_cr])
            A([b_cr], [b_cs], lambda: nc.scalar.activation(cs[:], cr[:], AF.Silu))
            bm2 = sbt(es, "bm2", [2, 6 * DM]); b_bm2 = Buf()
            D(bm2[:], b_mod[l, :].partition_broadcast(2), [], [b_bm2])
            msb = sbt(es, "msb", [2, 6 * DM]); b_msb = Buf()
            wm = Rot(es, "wm", 3, [128, 3072])
            acc = [pst(es, f"macc{j}", [128, 512]) for j in range(6)]
            b_acc = [Buf() for _ in range(6)]
            for hf in range(2):
                for k in range(8):
                    wt, bw = wm.get()
                    D(wt[:], w_mod[l, k * 128:(k + 1) * 128, hf * 3072:(hf + 1) * 3072], [], [bw])
                    for j in range(6):
                        P([bw, b_cs], [b_acc[j]], lambda j=j, wt=wt, k=k: nc.tensor.matmul(
                            acc[j][0:2, :], cs[:, k, :], wt[:, j * 512:(j + 1) * 512], start=(k == 0), stop=(k == 7)))
                for j in range(6):
                    c0 = hf * 3072 + j * 512
                    V([b_acc[j], b_bm2], [b_msb], lambda j=j, c0=c0: nc.vector.tensor_tensor(
                        msb[:, c0:c0 + 512], acc[j][0:2, :], bm2[:, c0:c0 + 512], ALU.add))
            D(modrow[:, :], msb[:], [b_msb], [])
            f.barrier()

    def phase_ln_proj(l):
        with ExitStack() as es:
            hT = sbt(es, "hT", [128, 8, T], BF16); b_hT = [Buf() for _ in range(NT)]
            with ExitStack() as es2:
                mods = sbt(es2, "mods", [128, 2, 2 * DM]); b_mods = Buf()
                D(mods[:, 0, :], modrow[1, 0:2 * DM].partition_broadcast(128), [], [b_mods])
                D(mods[:, 1, :], modrow[0, 0:2 * DM].partition_broadcast(128), [], [b_mods])
                G([b_mods], [b_mods], lambda: nc.gpsimd.tensor_scalar(mods[:, :, DM:2 * DM], mods[:, :, DM:2 * DM], 1.0, None, ALU.add))
                xt = Rot(es2, "xt", 2, [128, DM]); xn = Rot(es2, "xn", 2, [128, DM]); hb = Rot(es2, "hb", 2, [128, DM], BF16)
                st = Rot(es2, "st", 2, [128, 2, 6]); mv = Rot(es2, "mv", 2, [128, 2]); rs = Rot(es2, "rs", 2, [128, 1])
                pT = Rot(es2, "pT", 2, [128, 8, 128], BF16, psum=True)
                for i in range(NT):
                    x_t, bx = xt.get(); n_t, bn = xn.get(); h_t, bh = hb.get()
                    s_t, bs = st.get(); m_t, bmv = mv.get(); r_t, br = rs.get(); p_t, bp = pT.get()
                    D(x_t[:], xsrc(l, i), [], [bx])
                    layer_norm_stats(None, x_t, bx, (s_t, bs, m_t, bmv, r_t, br))
                    V([bx, bmv, br], [bn], lambda x_t=x_t, n_t=n_t, m_t=m_t, r_t=r_t: nc.vector.tensor_scalar(
                        n_t[:], x_t[:], m_t[:, 0:1], r_t[:, 0:1], ALU.subtract, ALU.mult))
                    w = 0 if i < 2 else 1
                    G([bn, b_mods], [bn], lambda n_t=n_t, w=w: nc.gpsimd.tensor_tensor(n_t[:], n_t[:], mods[:, w, DM:2 * DM], ALU.mult))
                    G([bn, b_mods], [bn], lambda n_t=n_t, w=w: nc.gpsimd.tensor_tensor(n_t[:], n_t[:], mods[:, w, 0:DM], ALU.add))
                    A([bn], [bh], lambda n_t=n_t, h_t=h_t: nc.scalar.copy(h_t[:], n_t[:]))
                    if DEBUG:
                        D(dbgh[i * 128:(i + 1) * 128, :], n_t[:], [bn], [])
                        D(dbgs[i * 128:(i + 1) * 128, 0:2], m_t[:], [bmv], [], allow_slow_non_contiguous=True)
                        D(dbgs[i * 128:(i + 1) * 128, 2:3], r_t[:], [br], [], allow_slow_non_contiguous=True)
                    for k in range(8):
                        P([bh, b_identb], [bp], lambda k=k, h_t=h_t, p_t=p_t: nc.tensor.transpose(p_t[:, k, :], h_t[:, k * 128:(k + 1) * 128], identb[:]))
                    A([bp], [b_hT[i]], lambda i=i, p_t=p_t: nc.scalar.copy(hT[:, :, i * 128:(i + 1) * 128], p_t[:]))
                f.barrier()
            wb = sbt(es, "wb", [128, 8, N_IN], BF16); b_wb = [Buf() for _ in range(8)]
            for k in range(8):
                D(wb[:, k, :], w_in[l, k * 128:(k + 1) * 128, :], [], [b_wb[k]], eng=POOL)
            pp = Rot(es, "pp", 4, [128, 512], psum=True)
            stg = Rot(es, "stg", 4, [128, 512])
            cnt = 0
            for fc in range(24):
                fsz = 128 if fc < 23 else 32
                for tb in range(5):
                    t0 = tb * 512; tsz = min(512, T - t0)
                    p_t, bp = pp.get(); s_t, bs = stg.get()
                    rb = [b_hT[i] for i in range(t0 // 128, (t0 + tsz) // 128)]
                    for k in range(8):
                        P(rb + [b_wb[k]], [bp], lambda k=k, p_t=p_t, fc=fc, fsz=fsz, t0=t0, tsz=tsz: nc.tensor.matmul(
                            p_t[0:fsz, 0:tsz], wb[:, k, fc * 128:fc * 128 + fsz], hT[:, k, t0:t0 + tsz], start=(k == 0), stop=(k == 7)))
                    if cnt % 2 == 0:
                        A([bp], [bs], lambda p_t=p_t, s_t=s_t, fsz=fsz, tsz=tsz: nc.scalar.copy(s_t[0:fsz, 0:tsz], p_t[0:fsz, 0:tsz]))
                    else:
                        V([bp], [bs], lambda p_t=p_t, s_t=s_t, fsz=fsz, tsz=tsz: nc.vector.tensor_copy(s_t[0:fsz, 0:tsz], p_t[0:fsz, 0:tsz]))
                    cnt += 1
                    D(zT[fc * 128:fc * 128 + fsz, t0:t0 + tsz], s_t[0:fsz, 0:tsz], [bs], [])
            vst = Rot(es, "vst", 2, [128, 768])
            for i in range(NT):
                p1, bp1 = pp.get(); p2, bp2 = pp.get(); s_t, bs = vst.get()
                for (pt_, bpt_, c0_, c1_, w0_, wn_) in ((p1, bp1, 0, 256, OFF_V, 256), (p1, bp1, 256, 384, OFF_VB, 128), (p2, bp2, 0, 384, OFF_VN, 384)):
                    for k in range(8):
                        P([b_hT[i], b_wb[k]], [bpt_], lambda k=k: nc.tensor.matmul(
                            pt_[:, c0_:c1_], hT[:, k, i * 128:(i + 1) * 128], wb[:, k, w0_:w0_ + wn_], start=(k == 0), stop=(k == 7)))
                A([bp1], [bs], lambda p1=p1, s_t=s_t: nc.scalar.copy(s_t[:, 0:384], p1[:, 0:384]))
                V([bp2], [bs], lambda p2=p2, s_t=s_t: nc.vector.tensor_copy(s_t[:, 384:768], p2[:, 0:384]))
                D(vtok[i * 128:(i + 1) * 128, :], s_t[:], [bs], [])
            f.barrier()

    def conv_rows(dst, b_dst, src, b_src, wcol, b_w, c0, n):
        V([b_src, b_w], [b_dst], lambda: nc.vector.tensor_scalar(dst[:, 0:n], src[:, 0:n], wcol[:, 1:2], None, ALU.mult))
        segs = []
        for (a, b) in ((0, L), (L, T)):
            lo = max(a, c0); hi = min(b, c0 + n)
            if lo < hi:
                segs.append((lo - c0, hi - c0, lo == a, hi == b))
        return segs

    def phase_rwkv(l):
        with ExitStack() as es:
            cp = sbt(es, "cp", [128, 48]); b_cp = Buf()
            D(cp[:], colpar[l, :, :], [], [b_cp])
            wup = sbt(es, "wup", [128, 256]); aup = sbt(es, "aup", [128, 256]); b_up = Buf()
            D(wup[:], a_w_up[l, :, :], [], [b_up]); D(aup[:], a_a_up[l, :, :], [], [b_up])
            gup0 = sbt(es, "gup0", [128, 256]); gup1 = sbt(es, "gup1", [32, 256]); b_gup = Buf()
            D(gup0[:], a_g_up[l, 0:128, :], [], [b_gup]); D(gup1[:], a_g_up[l, 128:160, :], [], [b_gup])
            msk = sbt(es, "msk", [128, 4, 128]); b_msk = Buf()
            D(msk[:], cmasks[:, :, :], [], [b_msk])
            blk = sbt(es, "blk", [128, 128]); hsel = sbt(es, "hsel", [128, 2]); b_cst = Buf()
            D(blk[:], cblock[:, :], [], [b_cst]); D(hsel[:], chsel[:, :], [], [b_cst])
            scm = sbt(es, "scm", [128, 512]); D(scm[:], cscanmask[:, :], [], [b_cst])
            vsh = sbt(es, "vsh", [128, 3, 256]); b_vsh = Buf()
            D(vsh[:], a_shift[l, :, OFF_V:OFF_V + 256].partition_broadcast(128), [], [b_vsh])
            yacc = sbt(es, "yacc", [128, NT, 256]); b_yacc = [Buf() for _ in range(NT)]
            bacc = sbt(es, "bacc", [128, NT, 4]); b_bacc = [Buf() for _ in range(NT)]
            Vt = sbt(es, "Vt", [128, NT, 256]); b_Vt = [Buf() for _ in range(NT)]
            Vtb = sbt(es, "Vtb", [128, NT, 256], BF16)
            G([], b_yacc, lambda: nc.gpsimd.memset(yacc[:], 0.0))
            G([], b_bacc, lambda: nc.gpsimd.memset(bacc[:], 0.0))
            Mst = [[sbt(es, f"M{d}{j}", [128, 64]) for j in range(2)] for d in range(2)]
            b_M = [[Buf() for j in range(2)] for d in range(2)]
            for d in range(2):
                for j in range(2):
                    G([], [b_M[d][j]], lambda d=d, j=j: nc.gpsimd.memset(Mst[d][j][:], 0.0))
            with ExitStack() as es2:
                vr = Rot(es2, "vr", 6, [128, 256])
                for i in range(NT):
                    tp, bp_ = vr.get(); tc, bc_ = vr.get(); tn, bn_ = vr.get()
                    first = i in (0, 2); last = i in (1, NT - 1)
                    r0 = i * 128
                    D(tc[:], vtok[r0:r0 + 128, 0:256], [], [bc_])
                    if first:
                        G([], [bp_], lambda tp=tp: nc.gpsimd.memset(tp[:], 0.0))
                        D(tp[1:128, :], vtok[r0:r0 + 127, 0:256], [], [bp_])
                    else:
                        D(tp[:], vtok[r0 - 1:r0 + 127, 0:256], [], [bp_])
                    if last:
                        G([], [bn_], lambda tn=tn: nc.gpsimd.memset(tn[:], 0.0))
                        D(tn[0:127, :], vtok[r0 + 1:r0 + 128, 0:256], [], [bn_])
                    else:
                        D(tn[:], vtok[r0 + 1:r0 + 129, 0:256], [], [bn_])
                    V([bc_, b_vsh], [b_Vt[i]], lambda i=i, tc=tc: nc.vector.tensor_tensor(Vt[:, i, :], tc[:], vsh[:, 1, :], ALU.mult))
                    G([bp_, b_vsh], [bp_], lambda tp=tp: nc.gpsimd.tensor_tensor(tp[:], tp[:], vsh[:, 0, :], ALU.mult))
                    G([bn_, b_vsh], [bn_], lambda tn=tn: nc.gpsimd.tensor_tensor(tn[:], tn[:], vsh[:, 2, :], ALU.mult))
                    V([bp_, b_Vt[i]], [b_Vt[i]], lambda i=i, tp=tp: nc.vector.tensor_tensor(Vt[:, i, :], Vt[:, i, :], tp[:], ALU.add))
                    V([bn_, b_Vt[i]], [b_Vt[i]], lambda i=i, tn=tn: nc.vector.tensor_tensor(Vt[:, i, :], Vt[:, i, :], tn[:], ALU.add))
                    A([b_Vt[i]], [b_Vt[i]], lambda i=i: nc.scalar.copy(Vtb[:, i, :], Vt[:, i, :]))
                f.barrier()
            if RW_STOP == 1:
                return

            blocks = [(0, 256)] + [(256 + 256 * q, 256) for q in range(8)]
            order = {0: list(range(9)), 1: [0] + list(range(8, 0, -1))}
            with ExitStack() as es2:
                BW = 256
                rawS = [[(sbt(es2, f"rawS{d}{q}", [128, BW + 2]), Buf()) for q in range(2)] for d in range(2)]
                rawC = [[[(sbt(es2, f"rawC{d}{j}{q}", [128, BW + 2]), Buf()) for q in range(2)] for j in range(2)] for d in range(2)]
                tmpC = [[[(sbt(es2, f"tmpC{d}{j}{q}", [128, BW]), Buf()) for q in range(2)] for j in range(2)] for d in range(2)]
                rT = [[sbt(es2, f"rT{d}{j}", [128, BW]) for j in range(2)] for d in range(2)]
                kT = [[sbt(es2, f"kT{d}{j}", [128, BW]) for j in range(2)] for d in range(2)]
                kkT = [[sbt(es2, f"kkT{d}{j}", [128, BW]) for j in range(2)] for d in range(2)]
                twd = [sbt(es2, f"twd{d}", [128, BW]) for d in range(2)]
                tad = [sbt(es2, f"tad{d}", [128, BW]) for d in range(2)]
                names = ["t0", "t1", "t2", "bb", "kd", "p1", "e1"]
                rows = {nm: [[sbt(es2, f"{nm}{d}{j}", [128, BW]) for j in range(2)] for d in range(2)] for nm in names}
                q12 = [[sbt(es2, f"q12{d}{j}", [128, 2, 2, 128]) for j in range(2)] for d in range(2)]
                tot = [[sbt(es2, f"tot{d}{j}", [128, 4]) for j in range(2)] for d in range(2)]
                gC = [[sbt(es2, f"gC{d}{j}", [128, 4]) for j in range(2)] for d in range(2)]
                b_row = [[Buf() for j in range(2)] for d in range(2)]
                b_sh = [Buf() for d in range(2)]
                bnames = ["p1b", "p2b", "e1b", "e2b"]
                brows = {nm: [[sbt(es2, f"{nm}{d}{j}", [128, BW], BF16) for j in range(2)] for d in range(2)] for nm in bnames}
                q12b = [[sbt(es2, f"q12b{d}{j}", [128, 2, 2, 128], BF16) for j in range(2)] for d in range(2)]
                Mb = [[sbt(es2, f"Mb{d}{j}", [128, 64], BF16) for j in range(2)] for d in range(2)]
                for d in range(2):
                    for j in range(2):
                        G([], [b_M[d][j]], lambda d=d, j=j: nc.gpsimd.memset(Mb[d][j][:], 0.0))
                bankX = pst(es2, "bankX", [128, 512]); b_bankX = Buf()
                pM = [[bankX[:, (2 * d + j) * 64:(2 * d + j) * 64 + 64] for j in range(2)] for d in range(2)]
                b_pM = [[b_bankX for j in range(2)] for d in range(2)]
                ps5 = Slots([bankX[:, 256:512]]); ps5.b = [b_bankX]
                pN = Rot(es2, "pN", 1, [128, 512], psum=True)
                pW = Rot(es2, "pW", 2, [128, 1024], psum=True)
                pQ = carve(es2, "pQ", 1, 256)
                pEbank = pst(es2, "pEb", [128, 1024], BF16); b_pE = Buf()
                pE = Slots([pEbank[:, q * 256:(q + 1) * 256] for q in range(4)]); pE.b = [b_pE] * 4
                NCH = 16
                cA1 = [sbt(es2, f"cA1_{k}", [128, 256], BF16) for k in range(NCH)]
                cA2 = [sbt(es2, f"cA2_{k}", [128, 256], BF16) for k in range(NCH)]
                cbA = [(Buf(), Buf()) for k in range(NCH)]
                gPR = [Rot(es2, f"gPR{g}_", 3, [128, 4, 256], BF16) for g in range(4)]
                gPT = [Rot(es2, f"gPT{g}_", 3, [128, 4, 128], BF16) for g in range(4)]
                gSq = [Rot(es2, f"gSq{g}_", 2, [128, 4, 64], BF16) for g in range(4)]
                cE = [sbt(es2, f"cE_{k}", [128, 256], BF16) for k in range(NCH // 2)]
                cbE = [Buf() for k in range(NCH // 2)]

                def load_conv(dst, b_dst, frow, nrows, rawt, tap_chunk, c0, n):
                    rt, br = rawt
                    seq_lo, seq_hi = (0, L) if c0 < L else (L, T)
                    lo = max(seq_lo, c0 - 1); hi = min(seq_hi, c0 + n + 1)
                    if lo > c0 - 1 or hi < c0 + n + 1:
                        G([], [br], lambda rt=rt: nc.gpsimd.memset(rt[:], 0.0))
                    D(rt[0:nrows, lo - (c0 - 1):hi - (c0 - 1)], zT[frow:frow + nrows, lo:hi], [], [br])
                    w = lambda tap: cp[0:nrows, tap_chunk * 3 + tap:tap_chunk * 3 + tap + 1]
                    V([br, b_cp], [b_dst], lambda: nc.vector.tensor_scalar(dst[0:nrows, 0:n], rt[0:nrows, 1:n + 1], w(1), None, ALU.mult))
                    V([br, b_cp, b_dst], [b_dst], lambda: nc.vector.scalar_tensor_tensor(dst[0:nrows, 0:n], rt[0:nrows, 0:n], w(0), dst[0:nrows, 0:n], ALU.mult, ALU.add))
                    V([br, b_cp, b_dst], [b_dst], lambda: nc.vector.scalar_tensor_tensor(dst[0:nrows, 0:n], rt[0:nrows, 2:n + 2], w(2), dst[0:nrows, 0:n], ALU.mult, ALU.add))

                def prep_shared(d, bi):
                    c0, n = blocks[bi]
                    load_conv(twd[d], b_sh[d], OFF_WD, 128, rawS[d][0], 6, c0, n)
                    A([b_sh[d]], [b_sh[d]], lambda: nc.scalar.activation(twd[d][:, 0:n], twd[d][:, 0:n], AF.Tanh))
                    load_conv(tad[d], b_sh[d], OFF_AD, 128, rawS[d][1], 7, c0, n)

                def prep_combo(d, j, bi):
                    c0, n = blocks[bi]
                    nch = n // 128
                    hp = slice(64 * d, 64 * d + 64)
                    tmpi = [0]

                    def tmp_get():
                        t = tmpC[d][j][tmpi[0] % 2]; tmpi[0] += 1
                        return t
                    if True:
                        R = {nm: rows[nm][d][j] for nm in names}
                        br_ = b_row[d][j]
                        load_conv(rT[d][j], br_, OFF_R + 128 * j, 128, rawC[d][j][0], j, c0, n)
                        yield
                        load_conv(kT[d][j], br_, OFF_K + 128 * j, 128, rawC[d][j][1], 2 + j, c0, n)
                        yield
                        V([br_, b_cp], [br_], lambda: nc.vector.tensor_scalar(kkT[d][j][:, 0:n], kT[d][j][:, 0:n], cp[:, 38 + j:39 + j], None, ALU.mult))
                        tq, btq = tmp_get()
                        G([br_], [btq], lambda: nc.gpsimd.tensor_tensor(tq[:, 0:n], kkT[d][j][:, 0:n], kkT[d][j][:, 0:n], ALU.mult))
                        p5, bp5 = ps5.get()
                        P([btq, b_cst], [bp5], lambda: nc.tensor.matmul(p5[:, 0:n], blk[:], tq[:, 0:n], start=True, stop=True))
                        A([bp5], [btq], lambda: nc.scalar.activation(tq[:, 0:n], p5[:, 0:n], AF.Sqrt, bias=eps_t[:, 2:3], scale=1.0))
                        yield
                        V([btq], [btq], lambda: nc.vector.reciprocal(tq[:, 0:n], tq[:, 0:n]))
                        V([btq, br_], [br_], lambda: nc.vector.tensor_tensor(kkT[d][j][:, 0:n], kkT[d][j][:, 0:n], tq[:, 0:n], ALU.mult))
                        yield
                        p5, bp5 = ps5.get()
                        P([b_sh[d], b_up], [bp5], lambda: nc.tensor.matmul(p5[:, 0:n], wup[hp, j * 128:(j + 1) * 128], twd[d][hp, 0:n], start=True, stop=True))
                        A([bp5, b_cp], [br_], lambda: nc.scalar.activation(R["t0"][:, 0:n], p5[:, 0:n], AF.Sigmoid, bias=cp[:, 30 + d * 2 + j:31 + d * 2 + j], scale=1.0))
                        yield
                        p5, bp5 = ps5.get()
                        P([b_sh[d], b_up], [bp5], lambda: nc.tensor.matmul(p5[:, 0:n], aup[hp, j * 128:(j + 1) * 128], tad[d][hp, 0:n], start=True, stop=True))
                        A([bp5, b_cp], [br_], lambda: nc.scalar.activation(R["bb"][:, 0:n], p5[:, 0:n], AF.Sigmoid, bias=cp[:, 34 + d * 2 + j:35 + d * 2 + j], scale=1.0))
                        yield
                        V([br_, b_cp], [br_], lambda: nc.vector.tensor_scalar(R["kd"][:, 0:n], R["bb"][:, 0:n], -1.0, cp[:, 40 + j:41 + j], ALU.add, ALU.mult))
                        V([br_], [br_], lambda: nc.vector.scalar_tensor_tensor(R["kd"][:, 0:n], R["kd"][:, 0:n], 1.0, kT[d][j][:, 0:n], ALU.add, ALU.mult))
                        V([br_], [br_], lambda: nc.vector.tensor_tensor(R["bb"][:, 0:n], R["bb"][:, 0:n], kkT[d][j][:, 0:n], ALU.mult))
                        yield
                        tq, btq = tmp_get()
                        V([br_, b_cp], [btq], lambda: nc.vector.scalar_tensor_tensor(tq[:, 0:n], rT[d][j][:, 0:n], cp[:, 42 + j:43 + j], R["kd"][:, 0:n], ALU.mult, ALU.mult))
                        for c in range(nch):
                            ti = c0 // 128 + c
                            pq, bpq = pQ.get()
                            P([btq, b_cst], [bpq], lambda c=c, pq=pq: nc.tensor.matmul(pq[:, 0:2], tq[:, c * 128:(c + 1) * 128], hsel[:], start=True, stop=True))
                            V([bpq, b_bacc[ti]], [b_bacc[ti]], lambda ti=ti, pq=pq: nc.vector.tensor_tensor(bacc[:, ti, 2 * j:2 * j + 2], bacc[:, ti, 2 * j:2 * j + 2], pq[:, 0:2], ALU.add))
                        yield
                        V([br_, b_cst], [br_], lambda: nc.vector.tensor_tensor_scan(R["t1"][:, 0:n], scm[:, 0:n], R["t0"][:, 0:n], 0.0, ALU.mult, ALU.add))
                        V([br_], [br_], lambda: nc.vector.tensor_tensor(R["t0"][:, 0:n], R["t1"][:, 0:n], R["t0"][:, 0:n], ALU.subtract))
                        V([br_], [br_], lambda: nc.vector.tensor_copy(tot[d][j][:, 0:nch], R["t1"][:, 127:n:128]))
                        A([br_], [br_], lambda: nc.scalar.activation(gC[d][j][:, 0:nch], tot[d][j][:, 0:nch], AF.Exp, scale=-C0))
                        yield
                        totb = tot[d][j][:, 0:nch].unsqueeze(2).broadcast_to([128, nch, 128])
                        v3 = lambda ap: ap[:, 0:n].rearrange("p (c t) -> p c t", t=128)
                        q1v = q12[d][j][:, 0:nch, 0, :]; q2v = q12[d][j][:, 0:nch, 1, :]
                        if d == 0:
                            V([br_], [br_], lambda: nc.vector.tensor_tensor(v3(R["t2"]), totb, v3(R["t1"]), ALU.subtract))
                            A([br_], [br_], lambda: nc.scalar.activation(q1v, v3(R["t0"]), AF.Exp, scale=-C0))
                            A([br_], [br_], lambda: nc.scalar.activation(q2v, v3(R["t1"]), AF.Exp, scale=-C0))
                            A([br_], [br_], lambda: nc.scalar.activation(R["p1"][:, 0:n], R["t1"][:, 0:n], AF.Exp, scale=C0))
                            A([br_], [br_], lambda: nc.scalar.activation(R["e1"][:, 0:n], R["t2"][:, 0:n], AF.Exp, scale=-C0))
                        else:
                            V([br_], [br_], lambda: nc.vector.tensor_tensor(v3(R["t2"]), totb, v3(R["t1"]), ALU.subtract))
                            V([br_], [br_], lambda: nc.vector.tensor_tensor(v3(R["t1"]), totb, v3(R["t0"]), ALU.subtract))
                            A([br_], [br_], lambda: nc.scalar.activation(q1v, v3(R["t2"]), AF.Exp, scale=-C0))
                            A([br_], [br_], lambda: nc.scalar.activation(q2v, v3(R["t1"]), AF.Exp, scale=-C0))
                            A([br_], [br_], lambda: nc.scalar.activation(R["p1"][:, 0:n], R["t1"][:, 0:n], AF.Exp, scale=C0))
                            A([br_], [br_], lambda: nc.scalar.activation(R["e1"][:, 0:n], R["t0"][:, 0:n], AF.Exp, scale=-C0))
                        yield
                        BR = {nm: brows[nm][d][j] for nm in bnames}
                        q1b = q12b[d][j][:, 0:nch, 0, :]; q2b = q12b[d][j][:, 0:nch, 1, :]
                        V([br_], [br_], lambda: nc.vector.tensor_tensor(q1b, q1v, v3(kkT[d][j]), ALU.mult))
                        G([br_], [br_], lambda: nc.gpsimd.tensor_tensor(q2b, q2v, v3(rT[d][j]), ALU.mult))
                        yield
                        V([br_], [br_], lambda: nc.vector.tensor_tensor(BR["p2b"][:, 0:n], R["p1"][:, 0:n], R["kd"][:, 0:n], ALU.mult))
                        G([br_], [br_], lambda: nc.gpsimd.tensor_tensor(BR["p1b"][:, 0:n], R["p1"][:, 0:n], R["bb"][:, 0:n], ALU.mult))
                        V([br_], [br_], lambda: nc.vector.tensor_tensor(BR["e2b"][:, 0:n], R["e1"][:, 0:n], R["bb"][:, 0:n], ALU.mult))
                        G([br_], [br_], lambda: nc.gpsimd.tensor_tensor(BR["e1b"][:, 0:n], R["e1"][:, 0:n], R["kd"][:, 0:n], ALU.mult))

                def score_chain(k, d, j, hh, bi, c):
                    cs_ = slice(c * 128, (c + 1) * 128)
                    br_ = b_row[d][j]
                    BR = {nm: brows[nm][d][j] for nm in bnames}
                    mT = msk[:, 0:2, :] if d == 0 else msk[:, 2:4, :]
                    mN = msk[:, 2, :] if d == 0 else msk[:, 0, :]
                    hp = slice(64 * hh, 64 * hh + 64)
                    ke = k // 2; gi = k // 4; m = k % 4
                    if hh == 0:
                        pe_, bpe = pE.get()
                        P([br_, b_identb], [bpe], lambda: nc.tensor.transpose(pe_[:, 0:128], BR["e1b"][:, cs_], identb[:]))
                        P([br_, b_identb], [bpe], lambda: nc.tensor.transpose(pe_[:, 128:256], BR["e2b"][:, cs_], identb[:]))
                        A([bpe], [cbE[ke]], lambda: nc.scalar.copy(cE[ke][:], pe_[:]))
                    yield
                    q12c = q12b[d][j][hp, c, :, :].rearrange("p a t -> p (a t)")
                    q1c = q12b[d][j][hp, c, 0, :]
                    A1m, A2m = cA1[k], cA2[k]; bA1, bA2 = cbA[k]
                    a1f, ba1 = pW.get(); a1 = a1f[:, 0:256]
                    P([br_], [ba1], lambda: nc.tensor.matmul(a1, BR["p1b"][hp, cs_], q12c, start=True, stop=True))
                    V([ba1, b_msk], [bA1], lambda: nc.vector.tensor_tensor(A1m[:].rearrange("p (a t) -> p a t", a=2), a1.rearrange("p (a t) -> p a t", a=2), mT, ALU.mult))
                    PR, bPR = gcur[gi][0]; PT, bPT = gcur[gi][1]
                    V([ba1, b_msk], [bPR], lambda: nc.vector.scalar_tensor_tensor(PR[:, m, 0:128], a1[:, 0:128], -1.0, mT[:, 0, :], ALU.mult, ALU.mult))
                    A([b_identb], [bPR], lambda: nc.scalar.copy(PR[:, m, 128:256], identb[:]))
                    yield
                    a2f, ba2 = pW.get(); a2 = a2f[:, 0:256]
                    P([br_], [ba2], lambda: nc.tensor.matmul(a2, BR["p2b"][hp, cs_], q12c, start=True, stop=True))
                    V([ba2, b_msk], [bA2], lambda: nc.vector.tensor_tensor(A2m[:].rearrange("p (a t) -> p a t", a=2), a2.rearrange("p (a t) -> p a t", a=2), mT, ALU.mult))
                    yield
                    anf, ban = pN.get(); an = anf[:, 0:128]
                    P([br_], [ban], lambda: nc.tensor.matmul(an, q1c, BR["p1b"][hp, cs_], start=True, stop=True))
                    V([ban, b_msk], [bPT], lambda: nc.vector.scalar_tensor_tensor(PT[:, m, :], an, -1.0, mN, ALU.mult, ALU.mult))
                    yield

                def neumann_group(gi):
                    (PR, bPR), (PT, bPT) = gcur[gi]
                    for lev in range(7):
                        if lev < 6:
                            w1, bw1 = pW.get()
                            w1v = w1[:].rearrange("p (m c) -> p m c", c=256)
                            for m in range(4):
                                P([bPT, bPR], [bw1], lambda: nc.tensor.matmul(w1[:, m * 256:(m + 1) * 256], PT[:, m, :], PR[:, m, :], start=True, stop=True))
                            PRn, bPRn = gPR[gi].get()
                            A([bw1], [bPRn], lambda: nc.scalar.copy(PRn[:, :, 0:128], w1v[:, :, 0:128]))
                            V([bw1, bPR], [bPRn], lambda: nc.vector.tensor_tensor(PRn[:, :, 128:256], w1v[:, :, 128:256], PR[:, :, 128:256], ALU.add))
                            w2, bw2 = pN.get()
                            for m in range(4):
                                P([bPT, bPR], [bw2], lambda: nc.tensor.matmul(w2[:, m * 128:(m + 1) * 128], PR[:, m, 0:128], PT[:, m, :], start=True, stop=True))
                            PTn, bPTn = gPT[gi].get()
                            w2v = w2[:].rearrange("p (m c) -> p m c", c=128)
                            if (gi + lev) % 2 == 0:
                                A([bw2], [bPTn], lambda: nc.scalar.copy(PTn[:], w2v))
                            else:
                                V([bw2], [bPTn], lambda: nc.vector.tensor_copy(PTn[:], w2v))
                            PR, bPR, PT, bPT = PRn, bPRn, PTn, bPTn
                        else:
                            w2, bw2 = pN.get()
                            for m in range(4):
                                P([bPT, bPR], [bw2], lambda: nc.tensor.matmul(w2[:, m * 128:(m + 1) * 128], PT[:, m, :], PR[:, m, 128:256], start=True, stop=True))
                            PRn, bPRn = gPR[gi].get()
                            w2v = w2[:].rearrange("p (m c) -> p m c", c=128)
                            V([bw2, bPR], [bPRn], lambda: nc.vector.tensor_tensor(PRn[:, :, 128:256], w2v, PR[:, :, 128:256], ALU.add))
                            PR, bPR = PRn, bPRn
                        yield
                    gR[gi] = (PR, bPR)

                def seq_group(gi, d, bi, c):
                    c0, n = blocks[bi]
                    ti = c0 // 128 + c
                    PRf, bR = gR[gi]
                    hps = [slice(0, 64), slice(64, 128)]
                    ppg, bpp = pQ.get()
                    for m in range(4):
                        j, hh = m // 2, m % 2; k = 4 * gi + m; hp = hps[hh]
                        hcol = slice((2 * j + hh) * 64, (2 * j + hh) * 64 + 64)
                        P([b_row[d][j], b_M[d][j]], [bpp], lambda: nc.tensor.matmul(ppg[:, m * 64:(m + 1) * 64], q12b[d][j][hp, c, 0, :], Mb[d][j][hp, :], start=True, stop=False))
                        P([cbA[k][1], b_Vt[ti]], [bpp], lambda: nc.tensor.matmul(ppg[:, m * 64:(m + 1) * 64], cA2[k][:, 0:128], Vtb[:, ti, hcol], start=False, stop=True))
                    Psb, bPsb = gSq[gi].get()
                    A([bpp], [bPsb], lambda: nc.scalar.copy(Psb[:], ppg[:].rearrange("p (m c) -> p m c", c=64)))
                    yield
                    upg, bup = pQ.get()
                    for m in range(4):
                        P([bR, bPsb], [bup], lambda: nc.tensor.matmul(upg[:, m * 64:(m + 1) * 64], PRf[:, m, 128:256], Psb[:, m, :], start=True, stop=True))
                    nU, bnU = gSq[gi].get()
                    A([bup], [bnU], lambda: nc.scalar.mul(nU[:], upg[:].rearrange("p (m c) -> p m c", c=64), -1.0))
                    yield
                    ypg, byp = pQ.get()
                    for m in range(4):
                        j, hh = m // 2, m % 2; k = 4 * gi + m; hp = hps[hh]
                        hcol = slice((2 * j + hh) * 64, (2 * j + hh) * 64 + 64)
                        P([b_row[d][j], b_M[d][j]], [byp], lambda: nc.tensor.matmul(ypg[:, m * 64:(m + 1) * 64], q12b[d][j][hp, c, 1, :], Mb[d][j][hp, :], start=True, stop=False))
                        P([cbA[k][1], b_Vt[ti]], [byp], lambda: nc.tensor.matmul(ypg[:, m * 64:(m + 1) * 64], cA2[k][:, 128:256], Vtb[:, ti, hcol], start=False, stop=False))
                        P([cbA[k][0], bnU], [byp], lambda: nc.tensor.matmul(ypg[:, m * 64:(m + 1) * 64], cA1[k][:, 128:256], nU[:, m, :], start=False, stop=True))
                    V([byp, b_yacc[ti]], [b_yacc[ti]], lambda: nc.vector.tensor_tensor(yacc[:, ti, :], yacc[:, ti, :], ypg[:], ALU.add))
                    yield
                    for m in range(4):
                        j, hh = m // 2, m % 2; k = 4 * gi + m; hp = hps[hh]
                        hcol = slice((2 * j + hh) * 64, (2 * j + hh) * 64 + 64)
                        E_, bE = cE[k // 2], cbE[k // 2]
                        pm = pM[d][j]
                        P([bE, b_Vt[ti]], [b_pM[d][j]], lambda: nc.tensor.matmul(pm[hp, :], E_[:, 64 * hh:64 * hh + 64], Vtb[:, ti, hcol], start=True, stop=False))
                        P([bE, bnU], [b_pM[d][j]], lambda: nc.tensor.matmul(pm[hp, :], E_[:, 128 + 64 * hh:128 + 64 * hh + 64], nU[:, m, :], start=False, stop=True))
                    for j in range(2):
                        V([b_pM[d][j], b_M[d][j], b_row[d][j]], [b_M[d][j]], lambda: nc.vector.scalar_tensor_tensor(
                            Mst[d][j][:], Mst[d][j][:], gC[d][j][:, c:c + 1], pM[d][j][:], ALU.mult, ALU.add))
                        G([b_M[d][j]], [b_M[d][j]], lambda: nc.gpsimd.tensor_copy(Mb[d][j][:], Mst[d][j][:]))
                    yield

                def run_rr(gens):
                    gens = list(gens)
                    while gens:
                        nxt = []
                        for g in gens:
                            try:
                                next(g); nxt.append(g)
                            except StopIteration:
                                pass
                        gens = nxt

                gR = {}; gcur = {}
                for step in range(9):
                    if RW_STOP in (2, 3) and step > 0:
                        break
                    for d in range(2):
                        prep_shared(d, order[d][step])
                    run_rr([prep_combo(d, j, order[d][step]) for d in range(2) for j in range(2)])
                    if RW_STOP == 2:
                        break
                    chains = []; groups = []
                    for cc in range(2):
                        for d in range(2):
                            bi = order[d][step]
                            c = cc if d == 0 else 1 - cc
                            groups.append((len(groups), d, bi, c))
                            for j in range(2):
                                for hh in range(2):
                                    chains.append((len(chains), d, j, hh, bi, c))
                    for gi in range(4):
                        gcur[gi] = (gPR[gi].get(), gPT[gi].get())
                    run_rr([score_chain(*ch) for ch in chains])
                    run_rr([neumann_group(gi) for gi in range(4)])
                    for cc in range(2):
                        run_rr([seq_group(*g) for g in groups[cc * 2:(cc + 1) * 2]])
                f.barrier()
            if RW_STOP in (2, 3):
                return

            with ExitStack() as es2:
                gs0 = sbt(es2, "gs0", [128, T]); gs1 = sbt(es2, "gs1", [32, T]); b_gs = Buf()
                rw = sbt(es2, "rw", [128, T + 2]); b_rw = Buf()
                for (dst, frow, nrows, tapc) in ((gs0, OFF_GD, 128, 8), (gs1, OFF_GD + 128, 32, 9)):
                    G([], [b_rw], lambda: nc.gpsimd.memset(rw[:], 0.0))
                    D(rw[0:nrows, 1:T + 1], zT[frow:frow + nrows, :], [], [b_rw])
                    w = lambda tap, tapc=tapc, nrows=nrows: cp[0:nrows, tapc * 3 + tap:tapc * 3 + tap + 1]
                    V([b_rw, b_cp], [b_gs], lambda: nc.vector.tensor_scalar(dst[0:nrows, :], rw[0:nrows, 1:T + 1], w(1), None, ALU.mult))
                    for (a, b) in ((0, L), (L, T)):
                        V([b_rw, b_cp, b_gs], [b_gs], lambda a=a, b=b: nc.vector.scalar_tensor_tensor(
                            dst[0:nrows, a + 1:b], rw[0:nrows, a + 1:b], w(0), dst[0:nrows, a + 1:b], ALU.mult, ALU.add))
                        V([b_rw, b_cp, b_gs], [b_gs], lambda a=a, b=b: nc.vector.scalar_tensor_tensor(
                            dst[0:nrows, a:b - 1], rw[0:nrows, a + 2:b + 1], w(2), dst[0:nrows, a:b - 1], ALU.mult, ALU.add))
                    A([b_gs], [b_gs], lambda: nc.scalar.activation(dst[0:nrows, :], dst[0:nrows, :], AF.Sigmoid))
                gb = sbt(es2, "gb", [128, 2, 256]); b_gb = Buf()
                D(gb[:, 0, :], a_lnx_g[l, :].partition_broadcast(128), [], [b_gb])
                D(gb[:, 1, :], a_lnx_b[l, :].partition_broadcast(128), [], [b_gb])
                pg = carve(es2, "pg", 1, 256)
                st4 = Rot(es2, "st4", 2, [128, 4, 6]); mv4 = Rot(es2, "mv4", 2, [128, 4, 2]); rs4 = Rot(es2, "rs4", 2, [128, 4])
                yn = Rot(es2, "yn", 2, [128, 256]); oo = Rot(es2, "oo", 2, [128, 256])
                for i in range(NT):
                    g_p, bg = pg.get()
                    P([b_gs, b_gup], [bg], lambda: nc.tensor.matmul(g_p[:], gs0[:, i * 128:(i + 1) * 128], gup0[:], start=True, stop=False))
                    P([b_gs, b_gup], [bg], lambda: nc.tensor.matmul(g_p[:], gs1[0:32, i * 128:(i + 1) * 128], gup1[:], start=False, stop=True))
                    s4, bs4 = st4.get(); m4, bm4 = mv4.get(); r4, br4 = rs4.get(); y_n, byn = yn.get(); o_t, bo = oo.get()
                    for h in range(4):
                        V([b_yacc[i]], [bs4], lambda h=h: nc.vector.bn_stats(s4[:, h, :], yacc[:, i, h * 64:(h + 1) * 64]))
                    for h in range(4):
                        V([bs4], [bm4], lambda h=h: nc.vector.bn_aggr(m4[:, h, :], s4[:, h, :]))
                    A([bm4], [br4], lambda: nc.scalar.activation(r4[:], m4[:, :, 1], AF.Sqrt, bias=eps_t[:, 1:2], scale=1.0))
                    V([br4], [br4], lambda: nc.vector.reciprocal(r4[:], r4[:]))
                    for h in range(4):
                        V([b_yacc[i], bm4, br4], [byn], lambda h=h: nc.vector.tensor_scalar(
                            y_n[:, h * 64:(h + 1) * 64], yacc[:, i, h * 64:(h + 1) * 64], m4[:, h, 0:1], r4[:, h:h + 1], ALU.subtract, ALU.mult))
                    G([byn, b_gb], [byn], lambda: nc.gpsimd.tensor_tensor(y_n[:], y_n[:], gb[:, 0, :], ALU.mult))
                    G([byn, b_gb], [byn], lambda: nc.gpsimd.tensor_tensor(y_n[:], y_n[:], gb[:, 1, :], ALU.add))
                    for h in range(4):
                        V([byn, b_bacc[i], b_Vt[i]], [byn], lambda h=h: nc.vector.scalar_tensor_tensor(
                            y_n[:, h * 64:(h + 1) * 64], Vt[:, i, h * 64:(h + 1) * 64], bacc[:, i, h:h + 1], y_n[:, h * 64:(h + 1) * 64], ALU.mult, ALU.add))
                    V([byn, bg], [bo], lambda: nc.vector.tensor_tensor(o_t[:], y_n[:], g_p[:], ALU.mult))
                    D(omix[i * 128:(i + 1) * 128, 0:256], o_t[:], [bo], [])
                f.barrier()

    def phase_attn_b(l):
        with_ctx = l < DEPTH - 1
        with ExitStack() as es:
            cosT = sbt(es, "cosT", [64, S]); sinT = sbt(es, "sinT", [64, S]); rPT = sbt(es, "rPT", [64, 64]); b_c = Buf()
            D(cosT[:], ccos[:, :], [], [b_c]); D(sinT[:], csin[:, :], [], [b_c]); D(rPT[:], cropeP[:, :], [], [b_c])
            mkf = sbt(es, "mkf", [128, 4, 128]); mkb = sbt(es, "mkb", [128, 4, 128], BF16); b_mk = Buf()
            D(mkf[:], cmasks[:, :, :], [], [b_mk])
            V([b_mk], [b_mk], lambda: nc.vector.tensor_copy(mkb[:], mkf[:]))
            esk = sbt(es, "esk", [128, 6]); b_esk = Buf()
            D(esk[:], b_sink[l, :].partition_broadcast(128), [], [b_esk])
            A([b_esk], [b_esk], lambda: nc.scalar.activation(esk[:], esk[:], AF.Exp))
            kTb = sbt(es, "kTb", [64, 2, T], BF16); b_k = [Buf() for _ in range(2)]
            qTb = sbt(es, "qTb", [64, 2, NT, 3, 128], BF16); b_q = [Buf() for _ in range(6)]
            Va = sbt(es, "Va", [128, 2, NT, 65], BF16); b_Va = Buf()
            G([], [b_Va], lambda: nc.gpsimd.memset(Va[:], 1.0))
            with ExitStack() as es2:
                vf = sbt(es2, "vf", [128, NT, 128]); b_vf = Buf()
                D(vf[:], vtok[:, 256:384].rearrange("(n p) f -> p n f", p=128), [], [b_vf])
                for g in range(2):
                    V([b_vf, b_Va], [b_Va], lambda g=g: nc.vector.tensor_copy(Va[:, g, :, 0:64], vf[:, :, g * 64:(g + 1) * 64]))
                raw = Rot(es2, "rawb", 2, [64, T])
                pr = Rot(es2, "pr", 2, [128, 512], psum=True)
                t1 = Rot(es2, "t1b", 2, [64, 512]); t2 = Rot(es2, "t2b", 2, [64, 512])
                for hd in range(8):
                    rt, br = raw.get()
                    frow = OFF_QB + 64 * hd if hd < 6 else OFF_KB + 64 * (hd - 6)
                    D(rt[:], zT[frow:frow + 64, :], [], [br])
                    if hd < 6:
                        g, i3 = hd // 3, hd % 3
                        dst_ctx = qTb[:, g, 0:2, i3, :]
                        wb_ = b_q[hd]
                    else:
                        g = hd - 6
                        dst_ctx = kTb[:, g, 0:L].rearrange("p (n t) -> p n t", t=128)
                        wb_ = b_k[g]
                    A([br], [wb_], lambda rt=rt, dst_ctx=dst_ctx: nc.scalar.copy(dst_ctx, rt[:, 0:L].rearrange("p (n t) -> p n t", t=128)))
                    for q4 in range(4):
                        cs_ = slice(L + q4 * 512, L + (q4 + 1) * 512); ps_ = slice(q4 * 512, (q4 + 1) * 512)
                        p_t, bp = pr.get(); a_t, ba = t1.get(); c_t, bc = t2.get()
                        P([br, b_c], [bp], lambda: nc.tensor.matmul(p_t[0:64, :], rPT[:], rt[:, cs_], start=True, stop=True))
                        V([br, b_c], [ba], lambda: nc.vector.tensor_tensor(a_t[:], rt[:, cs_], cosT[:, ps_], ALU.mult))
                        V([bp, b_c], [bc], lambda: nc.vector.tensor_tensor(c_t[:], p_t[0:64, :], sinT[:, ps_], ALU.mult))
                        if hd < 6:
                            dst = qTb[:, g, 2 + 4 * q4:2 + 4 * q4 + 4, i3, :]
                        else:
                            dst = kTb[:, g, cs_].rearrange("p (n t) -> p n t", t=128)
                        G([ba, bc], [wb_], lambda: nc.gpsimd.tensor_tensor(dst, a_t[:].rearrange("p (n t) -> p n t", t=128), c_t[:].rearrange("p (n t) -> p n t", t=128), ALU.add))
                f.barrier()
            psc = Rot(es, "psc", 5, [128, 512], psum=True)
            pov = Rot(es, "pov", 2, [128, 512], psum=True)
            pt = Rot(es, "ptb", 16, [128, 384], BF16)
            ob = Rot(es, "obb", 3, [128, 384])
            dn = Rot(es, "dnb", 4, [128, 3])
            items = [(n, g) for n in range(NT) if not (n < 2 and not with_ctx) for g in range(2)]
            obufs = {}

            def stage1(n, g):
                kbs = [(0, None), (1, None)]
                if n >= 2:
                    if n - 1 >= 2: kbs.append((n - 1, 3))
                    kbs.append((n, None))
                    if n + 1 < NT: kbs.append((n + 1, 1))
                pts = []
                for (m, mk) in kbs:
                    s_p, bs = psc.get(); p_t, bpt = pt.get()
                    P([b_k[g]] + b_q[3 * g:3 * g + 3], [bs], lambda: nc.tensor.matmul(
                        s_p[:, 0:384], kTb[:, g, m * 128:(m + 1) * 128], qTb[:, g, n, :, :].rearrange("p a t -> p (a t)"), start=True, stop=True))
                    A([bs], [bpt], lambda: nc.scalar.activation(p_t[:], s_p[:, 0:384], AF.Exp, scale=SCALE))
                    if mk is not None:
                        V([bpt, b_mk], [bpt], lambda: nc.vector.tensor_tensor(
                            p_t[:].rearrange("p (a t) -> p a t", a=3), p_t[:].rearrange("p (a t) -> p a t", a=3),
                            mkb[:, mk, :].unsqueeze(1).broadcast_to([128, 3, 128]), ALU.mult))
                    pts.append((p_t, bpt, m))
                return pts

            def stage2(n, g, pts):
                if g == 0:
                    obufs[n] = ob.get()
                o_t, bo = obufs[n]
                o_pf, bop = pov.get()
                o_p = o_pf[:, 0:195].rearrange("p (a e) -> p a e", e=65)
                for i3 in range(3):
                    for q, (p_t, bpt, m) in enumerate(pts):
                        P([bpt, b_Va], [bop], lambda: nc.tensor.matmul(
                            o_p[:, i3, :], p_t[:, i3 * 128:(i3 + 1) * 128], Va[:, g, m, :], start=(q == 0), stop=(q == len(pts) - 1)))
                d_t, bd = dn.get()
                V([bop, b_esk], [bd], lambda: nc.vector.tensor_tensor(d_t[:], o_p[:, :, 64], esk[:, 3 * g:3 * g + 3], ALU.add))
                V([bd], [bd], lambda: nc.vector.reciprocal(d_t[:], d_t[:]))
                for i3 in range(3):
                    hc = (3 * g + i3) * 64
                    V([bop, bd], [bo], lambda: nc.vector.tensor_scalar(o_t[:, hc:hc + 64], o_p[:, i3, 0:64], d_t[:, i3:i3 + 1], None, ALU.mult))
                if g == 1:
                    D(omix[n * 128:(n + 1) * 128, 256:640], o_t[:], [bo], [])

            prev = None
            for it in items:
                cur = (it, stage1(*it))
                if prev is not None:
                    stage2(prev[0][0], prev[0][1], prev[1])
                prev = cur
            stage2(prev[0][0], prev[0][1], prev[1])
            f.barrier()

    def phase_attn_c(l):
        with_ctx = l < DEPTH - 1
        with ExitStack() as es:
            qT = sbt(es, "qTn", [64, 6, T], BF16); kTn = sbt(es, "kTn", [64, 6, T], BF16); b_qk = Buf()
            for h in range(6):
                D(qT[:, h, :], zT[OFF_QN + 64 * h:OFF_QN + 64 * h + 64, :], [], [b_qk], eng=POOL)
                D(kTn[:, h, :], zT[OFF_KN + 64 * h:OFF_KN + 64 * h + 64, :], [], [b_qk], eng=POOL)
            Va = sbt(es, "Van", [128, NT, 6, 65], BF16); b_Va = Buf()
            G([], [b_Va], lambda: nc.gpsimd.memset(Va[:], 1.0))
            with ExitStack() as es2:
                vf = sbt(es2, "vfn", [128, NT, 384]); b_vf = Buf()
                D(vf[:], vtok[:, 384:768].rearrange("(n p) f -> p n f", p=128), [], [b_vf])
                V([b_vf, b_Va], [b_Va], lambda: nc.vector.tensor_copy(Va[:, :, :, 0:64], vf[:].rearrange("p n (h e) -> p n h e", e=64)))
                f.barrier()
            cv = sbt(es, "cv", [128, 64]); b_cv = Buf()
            D(cv[:], ccolvalid[:, :], [], [b_cv])
            ebf = Rot(es, "ebf", 2, [128, 15, 64]); ebb = sbt(es, "ebb", [128, 6, 15, 64], BF16); b_eb = [Buf() for _ in range(6)]
            oC = sbt(es, "oC", [128, NT, 384]); b_oC = [Buf() for _ in range(NT)]
            psc = Rot(es, "pscn", 3, [128, 1024], psum=True)
            pov = carve(es, "povn", 2, 128)
            pt = Rot(es, "ptn", 4, [128, 896], BF16)
            dn = Rot(es, "dnn", 4, [128, 1])
            for h in range(6):
                e_f, bef = ebf.get()
                D(e_f[0:64, :, :], rpbtab[l, h, :, :, :], [], [bef])
                D(e_f[64:128, :, :], rpbtab[l, h, :, :, :], [], [bef])
                A([bef], [bef], lambda: nc.scalar.activation(e_f[:], e_f[:], AF.Exp))
                V([bef, b_cv], [b_eb[h]], lambda: nc.vector.tensor_tensor(ebb[:, h, :, :], e_f[:], cv[:].unsqueeze(1).broadcast_to([128, 15, 64]), ALU.mult))
            items = [(h, n) for h in range(6) for n in range(NT) if not (n < 2 and not with_ctx)]
            TB = sbt(es, "TB", [128, 6, 640], BF16); b_TB = [Buf() for _ in range(6)]

            def keyblocks(n):
                kbs = [(0, None), (1, None)]
                if n >= 2:
                    nb = n - 2
                    for kb in range(16):
                        subs = []
                        anyv = False
                        for i in range(2):
                            for jq in range(2):
                                kr = 2 * kb + i; qr = 2 * nb + jq
                                rs_ = min(max(qr - 4, 0), 24)
                                ok = rs_ <= kr < rs_ + 8
                                subs.append((i, jq, kr - qr + 7 if ok else None))
                                anyv = anyv or ok
                        if anyv:
                            kbs.append((kb + 2, subs))
                return kbs

            kb_int = keyblocks(2 + 5)
            assert len(kb_int) == 7
            for h in range(6):
                G([], [b_TB[h]], lambda: nc.gpsimd.memset(TB[:, h, :], 0.0))
                for q, (m, subs) in enumerate(kb_int[2:]):
                    for (i, jq, ro) in subs:
                        if ro is not None:
                            V([b_eb[h], b_TB[h]], [b_TB[h]], lambda: nc.vector.tensor_copy(
                                TB[64 * i:64 * i + 64, h, q * 128 + 64 * jq:q * 128 + 64 * jq + 64], ebb[64 * i:64 * i + 64, h, ro, :]))

            def stage1(h, n):
                kbs = keyblocks(n)
                nk = len(kbs)
                s_p, bs = psc.get(); p_t, bpt = pt.get()
                for q, (m, subs) in enumerate(kbs):
                    P([b_qk], [bs], lambda: nc.tensor.matmul(s_p[:, q * 128:(q + 1) * 128], kTn[:, h, m * 128:(m + 1) * 128], qT[:, h, n * 128:(n + 1) * 128], start=True, stop=True))
                A([bs], [bpt], lambda: nc.scalar.activation(p_t[:, 0:nk * 128], s_p[:, 0:nk * 128], AF.Exp, scale=SCALE))
                if n >= 2 and 2 <= n - 2 <= 13:
                    assert nk == 7 and [sb for (_, sb) in kbs[2:]] == [sb for (_, sb) in kb_int[2:]]
                    V([bpt, b_TB[h]], [bpt], lambda: nc.vector.tensor_tensor(p_t[:, 256:896], p_t[:, 256:896], TB[:, h, :], ALU.mult))
                    return (p_t, bpt, kbs)
                cnt = 0
                for q, (m, subs) in enumerate(kbs):
                    if subs is None:
                        continue
                    for (i, jq, ro) in subs:
                        eng, ee = (V, nc.vector)
                        cnt += 1
                        sl = p_t[64 * i:64 * i + 64, q * 128 + 64 * jq:q * 128 + 64 * jq + 64]
                        if ro is None:
                            eng([bpt], [bpt], lambda: ee.memset(sl, 0.0))
                        else:
                            eng([bpt, b_eb[h]], [bpt], lambda: ee.tensor_tensor(sl, sl, ebb[64 * i:64 * i + 64, h, ro, :], ALU.mult))
                return (p_t, bpt, kbs)

            def stage2(h, n, st):
                p_t, bpt, kbs = st
                o_p, bop = pov.get()
                for q, (m, subs) in enumerate(kbs):
                    P([bpt, b_Va], [bop], lambda: nc.tensor.matmul(o_p[:, 0:65], p_t[:, q * 128:(q + 1) * 128], Va[:, m, h, :], start=(q == 0), stop=(q == len(kbs) - 1)))
                d_t, bd = dn.get()
                V([bop], [bd], lambda: nc.vector.reciprocal(d_t[:], o_p[:, 64:65]))
                V([bop, bd], [b_oC[n]], lambda: nc.vector.tensor_scalar(oC[:, n, h * 64:(h + 1) * 64], o_p[:, 0:64], d_t[:, 0:1], None, ALU.mult))

            prev = None
            for it in items:
                cur = (it, stage1(*it))
                if prev is not None:
                    stage2(prev[0][0], prev[0][1], prev[1])
                prev = cur
            stage2(prev[0][0], prev[0][1], prev[1])
            for n in range(NT):
                if n < 2 and not with_ctx:
                    continue
                D(omix[n * 128:(n + 1) * 128, 640:1024], oC[:, n, :], [b_oC[n]], [])
            f.barrier()

    def phase_out_moe(l):
        with_ctx = l < DEPTH - 1
        tiles = list(range(NT)) if with_ctx else list(range(2, NT))
        with ExitStack() as es:
            h2T = sbt(es, "h2T", [128, 8, T], BF16); b_h2T = [Buf() for _ in range(NT)]
            Wr = sbt(es, "Wr", [128, NT, 32]); b_Wr = [Buf() for _ in range(NT)]
            with ExitStack() as es2:
                mods = sbt(es2, "mods2", [128, 2, 3 * DM]); b_mods = Buf()
                D(mods[:, 0, :], modrow[1, 2 * DM:5 * DM].partition_broadcast(128), [], [b_mods])
                D(mods[:, 1, :], modrow[0, 2 * DM:5 * DM].partition_broadcast(128), [], [b_mods])
                G([b_mods], [b_mods], lambda: nc.gpsimd.tensor_scalar(mods[:, :, 2 * DM:3 * DM], mods[:, :, 2 * DM:3 * DM], 1.0, None, ALU.add))
                lgb = sbt(es2, "lgb", [128, 2, DM]); b_lgb = Buf()
                D(lgb[:, 0, :], ln1_g[l, :].partition_broadcast(128), [], [b_lgb])
                D(lgb[:, 1, :], ln1_b[l, :].partition_broadcast(128), [], [b_lgb])
                wo = sbt(es2, "wo", [128, 8, DM], BF16); b_wo = Buf()
                D(wo[:], w_out[l, :, :].rearrange("(k p) n -> p k n", p=128), [], [b_wo], eng=POOL)
                wr = sbt(es2, "wr", [128, 8, 36]); b_wr = Buf()
                D(wr[:], w_r[l, :, :].rearrange("(k p) n -> p k n", p=128), [], [b_wr])
                brb = sbt(es2, "brb", [128, 36]); D(brb[:], b_r[l, :].partition_broadcast(128), [], [b_wr])
                om = Rot(es2, "om", 2, [128, DM]); omb = Rot(es2, "omb", 2, [128, DM], BF16)
                oT = Rot(es2, "oT", 2, [128, 8, 128], BF16)
                xt = Rot(es2, "xt2", 2, [128, DM]); tt = Rot(es2, "tt", 2, [128, DM]); x1 = Rot(es2, "x1", 2, [128, DM])
                hf = Rot(es2, "hf", 2, [128, DM]); hb = Rot(es2, "hb2", 2, [128, DM], BF16)
                hT32 = Rot(es2, "hT32", 2, [128, 8, 128])
                st = Rot(es2, "st2", 4, [128, 2, 6]); mv = Rot(es2, "mv2", 4, [128, 2]); rs = Rot(es2, "rs2", 4, [128, 1])
                pTb = Rot(es2, "pTb", 2, [128, 8, 128], BF16, psum=True)
                pO = Rot(es2, "pO", 1, [128, 1024], psum=True)
                pT32 = Rot(es2, "pT32", 1, [128, 8, 128], psum=True)
                pL = carve(es2, "pL", 1, 64)
                sm = Rot(es2, "sm", 2, [128, 160])
                for i in tiles:
                    w = 0 if i < 2 else 1
                    o_m, bom = om.get(); o_b, bob = omb.get(); o_T, boT = oT.get()
                    D(o_m[:], omix[i * 128:(i + 1) * 128, :], [], [bom])
                    A([bom], [bob], lambda: nc.scalar.copy(o_b[:], o_m[:]))
                    p_t, bp = pTb.get()
                    for k in range(8):
                        P([bob, b_identb], [bp], lambda k=k: nc.tensor.transpose(p_t[:, k, :], o_b[:, k * 128:(k + 1) * 128], identb[:]))
                    V([bp], [boT], lambda: nc.vector.tensor_copy(o_T[:], p_t[:]))
                    p_o, bpo = pO.get()
                    for hf_ in range(2):
                        for k in range(8):
                            P([boT, b_wo], [bpo], lambda k=k, hf_=hf_: nc.tensor.matmul(
                                p_o[:, hf_ * 512:(hf_ + 1) * 512], o_T[:, k, :], wo[:, k, hf_ * 512:(hf_ + 1) * 512], start=(k == 0), stop=(k == 7)))
                    x_t, bx = xt.get(); t_t, bt = tt.get(); x_1, bx1 = x1.get()
                    D(x_t[:], xsrc(l, i), [], [bx])
                    V([bpo, b_mods], [bt], lambda: nc.vector.tensor_tensor(t_t[:], p_o[:], mods[:, w, 0:DM], ALU.mult))
                    V([bx, bt], [bt], lambda: nc.vector.scalar_tensor_tensor(t_t[:], x_t[:], ALPHA, t_t[:], ALU.mult, ALU.add))
                    s_t, bs = st.get(); m_t, bmv = mv.get(); r_t, br = rs.get()
                    layer_norm_stats(None, t_t, bt, (s_t, bs, m_t, bmv, r_t, br))
                    V([bt, bmv, br], [bt], lambda: nc.vector.tensor_scalar(t_t[:], t_t[:], m_t[:, 0:1], r_t[:, 0:1], ALU.subtract, ALU.mult))
                    G([bt, b_lgb], [bt], lambda: nc.gpsimd.tensor_tensor(t_t[:], t_t[:], lgb[:, 0, :], ALU.mult))
                    G([bt, b_lgb], [bx1], lambda: nc.gpsimd.tensor_tensor(x_1[:], t_t[:], lgb[:, 1, :], ALU.add))
                    D(xs[i * 128:(i + 1) * 128, :], x_1[:], [bx1], [])
                    s_t, bs = st.get(); m_t, bmv = mv.get(); r_t, br = rs.get()
                    layer_norm_stats(None, x_1, bx1, (s_t, bs, m_t, bmv, r_t, br))
                    h_f, bhf = hf.get(); h_b, bhb = hb.get()
                    V([bx1, bmv, br], [bhf], lambda: nc.vector.tensor_scalar(h_f[:], x_1[:], m_t[:, 0:1], r_t[:, 0:1], ALU.subtract, ALU.mult))
                    G([bhf, b_mods], [bhf], lambda: nc.gpsimd.tensor_tensor(h_f[:], h_f[:], mods[:, w, 2 * DM:3 * DM], ALU.mult))
                    G([bhf, b_mods], [bhf], lambda: nc.gpsimd.tensor_tensor(h_f[:], h_f[:], mods[:, w, DM:2 * DM], ALU.add))
                    A([bhf], [bhb], lambda: nc.scalar.copy(h_b[:], h_f[:]))
                    p_t, bp = pTb.get()
                    for k in range(8):
                        P([bhb, b_identb], [bp], lambda k=k: nc.tensor.transpose(p_t[:, k, :], h_b[:, k * 128:(k + 1) * 128], identb[:]))
                    A([bp], [b_h2T[i]], lambda: nc.scalar.copy(h2T[:, :, i * 128:(i + 1) * 128], p_t[:]))
                    p32, bp32 = pT32.get()
                    for k in range(8):
                        P([bhf, b_ident], [bp32], lambda k=k: nc.tensor.transpose(p32[:, k, :], h_f[:, k * 128:(k + 1) * 128], ident[:]))
                    h32, bh32 = hT32.get()
                    V([bp32], [bh32], lambda: nc.vector.tensor_copy(h32[:], p32[:]))
                    p_l, bpl = pL.get()
                    for k in range(8):
                        P([bh32, b_wr], [bpl], lambda k=k: nc.tensor.matmul(p_l[:, 0:36], h32[:, k, :], wr[:, k, :], start=(k == 0), stop=(k == 7)))
                    s_, bsm = sm.get()
                    lg = s_[:, 0:36]; gmx = s_[:, 36:37]; ngm = s_[:, 37:38]; g1h = s_[:, 40:44]; e4 = s_[:, 44:48]
                    esum = s_[:, 48:49]; pgr = s_[:, 49:50]; pen = s_[:, 52:56]; m8 = s_[:, 56:64]; elm = s_[:, 64:96]
                    dd = s_[:, 96:97]; ee_ = s_[:, 97:98]; c1 = s_[:, 98:99]; c2 = s_[:, 99:100]
                    V([bpl, b_wr], [bsm], lambda: nc.vector.tensor_tensor(lg, p_l[:, 0:36], brb[:], ALU.add))
                    V([bsm], [bsm], lambda: nc.vector.tensor_reduce(gmx, s_[:, 0:4], AX.X, ALU.max))
                    V([bsm], [bsm], lambda: nc.vector.tensor_scalar(ngm, gmx, -1.0, None, ALU.mult))
                    V([bsm], [bsm], lambda: nc.vector.tensor_scalar(g1h, s_[:, 0:4], gmx, None, ALU.is_equal))
                    A([bsm], [bsm], lambda: nc.scalar.activation(e4, s_[:, 0:4], AF.Exp, bias=ngm, scale=1.0))
                    V([bsm], [bsm], lambda: nc.vector.tensor_reduce(esum, e4, AX.X, ALU.add))
                    V([bsm], [bsm], lambda: nc.vector.reciprocal(pgr, esum))
                    V([bsm], [bsm], lambda: nc.vector.tensor_scalar(pen, g1h, -1.0, 1e30, ALU.add, ALU.mult))
                    V([bsm], [bsm], lambda: nc.vector.tensor_tensor(elm.rearrange("p (g e) -> p g e", e=8), s_[:, 4:36].rearrange("p (g e) -> p g e", e=8),
                                                                    pen.unsqueeze(2).broadcast_to([128, 4, 8]), ALU.add))
                    V([bsm], [bsm], lambda: nc.vector.max(m8, elm))
                    V([bsm], [bsm], lambda: nc.vector.tensor_tensor(dd, m8[:, 1:2], m8[:, 0:1], ALU.subtract))
                    A([bsm], [bsm], lambda: nc.scalar.activation(ee_, dd, AF.Exp))
                    V([bsm], [bsm], lambda: nc.vector.tensor_scalar(c1, ee_, 1.0, None, ALU.add))
                    V([bsm], [bsm], lambda: nc.vector.reciprocal(c1, c1))
                    V([bsm], [bsm], lambda: nc.vector.tensor_tensor(c2, ee_, c1, ALU.mult))
                    V([bsm], [bsm], lambda: nc.vector.tensor_tensor(c1, c1, pgr, ALU.mult))
                    V([bsm], [bsm], lambda: nc.vector.tensor_tensor(c2, c2, pgr, ALU.mult))
                    V([bsm], [b_Wr[i]], lambda: nc.vector.tensor_scalar(Wr[:, i, :], elm, m8[:, 0:1], c1, ALU.is_equal, ALU.mult))
                    V([bsm], [bsm], lambda: nc.vector.tensor_scalar(s_[:, 100:132], elm, m8[:, 1:2], c2, ALU.is_equal, ALU.mult))
                    V([bsm, b_Wr[i]], [b_Wr[i]], lambda: nc.vector.tensor_tensor(Wr[:, i, :], Wr[:, i, :], s_[:, 100:132], ALU.add))
                f.barrier()
            if state.get("stop_before_moe"):
                return
            facc = sbt(es, "facc", [128, NT, DM]); b_f = [Buf() for _ in range(NT)]
            with ExitStack() as es2:
                wg = Rot(es2, "wg", 2, [128, 8, DE], BF16); wu = Rot(es2, "wu", 2, [128, 8, DE], BF16)
                wd = Rot(es2, "wd", 2, [128, 4, DM], BF16)
                pG = Rot(es2, "pG", 2, [128, 512], psum=True); pU = Rot(es2, "pU", 2, [128, 512], psum=True)
                pD = Rot(es2, "pD", 3, [128, 512], psum=True)
                sg = Rot(es2, "sg", 2, [128, 512]); aT = Rot(es2, "aT", 8, [128, 512], BF16)
                t0_ = tiles[0] * 128
                ntok = len(tiles) * 128
                blocks_ = [(t0_ + b * 512, min(512, ntok - b * 512)) for b in range((ntok + 511) // 512)]
                for e in range(NE):
                    g_w, bg = wg.get(); u_w, bu = wu.get(); d_w, bdw = wd.get()
                    D(g_w[:], w_gate[l, e, :, :].rearrange("(k p) n -> p k n", p=128), [], [bg], eng=POOL)
                    D(u_w[:], w_up[l, e, :, :].rearrange("(k p) n -> p k n", p=128), [], [bu], eng=POOL)
                    D(d_w[:], w_down[l, e, :, :].rearrange("(k p) n -> p k n", p=128), [], [bdw], eng=POOL)
                    for (tb0, tsz) in blocks_:
                        rb = [b_h2T[i] for i in range(tb0 // 128, (tb0 + tsz) // 128)]
                        acts = []
                        for fc in range(4):
                            g_p, bgp = pG.get(); u_p, bup = pU.get()
                            for k in range(8):
                                P(rb + [bg], [bgp], lambda k=k: nc.tensor.matmul(g_p[:, 0:tsz], g_w[:, k, fc * 128:(fc + 1) * 128], h2T[:, k, tb0:tb0 + tsz], start=(k == 0), stop=(k == 7)))
                            for k in range(8):
                                P(rb + [bu], [bup], lambda k=k: nc.tensor.matmul(u_p[:, 0:tsz], u_w[:, k, fc * 128:(fc + 1) * 128], h2T[:, k, tb0:tb0 + tsz], start=(k == 0), stop=(k == 7)))
                            s_g, bsg = sg.get(); a_T, baT = aT.get()
                            A([bgp], [bsg], lambda: nc.scalar.activation(s_g[:, 0:tsz], g_p[:, 0:tsz], AF.Silu))
                            V([bsg, bup], [baT], lambda: nc.vector.tensor_tensor(a_T[:, 0:tsz], s_g[:, 0:tsz], u_p[:, 0:tsz], ALU.mult))
                            acts.append((a_T, baT))
                        for q in range(tsz // 128):
                            ti = tb0 // 128 + q
                            for hf_ in range(2):
                                d_p, bdp = pD.get()
                                for fc in range(4):
                                    a_T, baT = acts[fc]
                                    P([baT, bdw], [bdp], lambda fc=fc, a_T=a_T: nc.tensor.matmul(
                                        d_p[:], a_T[:, q * 128:(q + 1) * 128], d_w[:, fc, hf_ * 512:(hf_ + 1) * 512], start=(fc == 0), stop=(fc == 3)))
                                fs = facc[:, ti, hf_ * 512:(hf_ + 1) * 512]
                                if e == 0:
                                    V([bdp, b_Wr[ti]], [b_f[ti]], lambda: nc.vector.tensor_scalar(fs, d_p[:], Wr[:, ti, e:e + 1], None, ALU.mult))
                                else:
                                    V([bdp, b_Wr[ti], b_f[ti]], [b_f[ti]], lambda: nc.vector.scalar_tensor_tensor(fs, d_p[:], Wr[:, ti, e:e + 1], fs, ALU.mult, ALU.add))
                f.barrier()
            with ExitStack() as es2:
                g2 = sbt(es2, "g2", [128, 2, DM]); b_g2 = Buf()
                D(g2[:, 0, :], modrow[1, 5 * DM:6 * DM].partition_broadcast(128), [], [b_g2])
                D(g2[:, 1, :], modrow[0, 5 * DM:6 * DM].partition_broadcast(128), [], [b_g2])
                lgb = sbt(es2, "lgb2", [128, 2, DM]); b_lgb = Buf()
                D(lgb[:, 0, :], ln2_g[l, :].partition_broadcast(128), [], [b_lgb])
                D(lgb[:, 1, :], ln2_b[l, :].partition_broadcast(128), [], [b_lgb])
                xt = Rot(es2, "xt3", 2, [128, DM]); tt = Rot(es2, "tt3", 2, [128, DM])
                st = Rot(es2, "st3", 2, [128, 2, 6]); mv = Rot(es2, "mv3", 2, [128, 2]); rs = Rot(es2, "rs3", 2, [128, 1])
                for i in tiles:
                    w = 0 if i < 2 else 1
                    x_t, bx = xt.get(); t_t, bt = tt.get()
                    D(x_t[:], xs[i * 128:(i + 1) * 128, :], [], [bx])
                    G([b_f[i], b_g2], [bt], lambda: nc.gpsimd.tensor_tensor(t_t[:], facc[:, i, :], g2[:, w, :], ALU.mult))
                    V([bx, bt], [bt], lambda: nc.vector.scalar_tensor_tensor(t_t[:], x_t[:], ALPHA, t_t[:], ALU.mult, ALU.add))
                    s_t, bs = st.get(); m_t, bmv = mv.get(); r_t, br = rs.get()
                    layer_norm_stats(None, t_t, bt, (s_t, bs, m_t, bmv, r_t, br))
                    V([bt, bmv, br], [bt], lambda: nc.vector.tensor_scalar(t_t[:], t_t[:], m_t[:, 0:1], r_t[:, 0:1], ALU.subtract, ALU.mult))
                    G([bt, b_lgb], [bt], lambda: nc.gpsimd.tensor_tensor(t_t[:], t_t[:], lgb[:, 0, :], ALU.mult))
                    G([bt, b_lgb], [bt], lambda: nc.gpsimd.tensor_tensor(t_t[:], t_t[:], lgb[:, 1, :], ALU.add))
                    if l == DEPTH - 1:
                        D(y_out[(i - 2) * 128:(i - 1) * 128, :], t_t[:], [bt], [])
                    else:
                        D(xs[i * 128:(i + 1) * 128, :], t_t[:], [bt], [])
                f.barrier()

    done = False
    for l in range(DEPTH):
        for tag, fn in (("mod", phase_mod), ("proj", phase_ln_proj), ("rwkv", phase_rwkv),
                        ("attb", phase_attn_b), ("attc", phase_attn_c), ("moe", phase_out_moe)):
            if STOP is not None and (l, tag) == tuple(STOP[:2]) and len(STOP) > 2:
                state["stop_before_moe"] = True
            fn(l)
            if STOP is not None and (l, tag) == tuple(STOP[:2]):
                done = True
                break
        if done:
            break
    f.barrier()
    print("n_inst", f.n_inst, flush=True)
    return nc


def _consts():
    p = np.arange(128)[:, None]; q = np.arange(128)[None, :]
    masks = np.stack([(p < q), (p <= q), (p > q), (p >= q)], 1).astype(np.float32)
    ident = np.eye(128, dtype=np.float32)
    block = (p // 64 == q // 64).astype(np.float32)
    hsel = (np.arange(128)[:, None] // 64 == np.arange(2)[None, :]).astype(np.float32)
    t = np.arange(S)
    pos = np.stack([t // 64, t % 64], -1).astype(np.float32)
    inv = (10000.0 ** (-np.arange(16, dtype=np.float32) / 16)).astype(np.float32)
    ang = pos[:, :, None] * inv
    cos = np.cos(ang).astype(np.float32); sin = np.sin(ang).astype(np.float32)
    cosT = np.zeros((64, S), np.float32); sinT = np.zeros((64, S), np.float32)
    for ax in range(2):
        for half in range(2):
            r0 = ax * 32 + half * 16
            cosT[r0:r0 + 16] = cos[:, ax, :].T
            sinT[r0:r0 + 16] = sin[:, ax, :].T
    Pm = np.zeros((64, 64), np.float32)
    for ax in range(2):
        for i in range(16):
            Pm[ax * 32 + i, ax * 32 + 16 + i] = -1.0
            Pm[ax * 32 + 16 + i, ax * 32 + i] = 1.0
    ropeP = np.ascontiguousarray(Pm.T)
    col = np.arange(64)
    cstart = np.clip(col - 8, 0, 48)
    cvalid = ((col[None, :] >= cstart[:, None]) & (col[None, :] < cstart[:, None] + 16)).astype(np.float32)
    colvalid = np.concatenate([cvalid.T, cvalid.T], 0)
    scan = np.ones((128, 512), np.float32); scan[:, ::128] = 0.0
    return dict(cmasks=masks, cident=ident, cblock=block, chsel=hsel, ccos=cosT, csin=sinT, cropeP=ropeP,
                ccolvalid=np.ascontiguousarray(colvalid), cscanmask=scan)


def _host_layout(inp):
    f32 = lambda a: np.ascontiguousarray(np.asarray(a, dtype=np.float32))
    sh = {}
    sh["w_mod"] = f32(inp["w_mod"]); sh["b_mod"] = f32(inp["b_mod"]); sh["w_in"] = f32(inp["w_in"]); sh["w_out"] = f32(inp["w_out"])
    sh["a_shift"] = f32(inp["a_shift"])
    sh["a_w_up"] = f32(np.asarray(inp["a_w_up"]).reshape(DEPTH, 128, 256))
    sh["a_a_up"] = f32(np.asarray(inp["a_a_up"]).reshape(DEPTH, 128, 256))
    sh["a_g_up"] = f32(inp["a_g_up"])
    cp = np.zeros((DEPTH, 128, 48), np.float32)
    ash = np.zeros((DEPTH, 3, 1280), np.float32); ash[:, :, :A_IN] = inp["a_shift"]
    for l in range(DEPTH):
        for ch in range(10):
            for tap in range(3):
                cp[l, :, ch * 3 + tap] = ash[l, tap, ch * 128:(ch + 1) * 128]
        for d in range(2):
            for j in range(2):
                cp[l, :, 30 + d * 2 + j] = inp["a_w0"][l, d, j * 128:(j + 1) * 128]
                cp[l, :, 34 + d * 2 + j] = inp["a_a0"][l, d, j * 128:(j + 1) * 128]
        for j in range(2):
            cp[l, :, 38 + j] = inp["a_k_k"][l, j * 128:(j + 1) * 128]
            cp[l, :, 40 + j] = inp["a_k_a"][l, j * 128:(j + 1) * 128]
            cp[l, :, 42 + j] = inp["a_r_k"][l, j * 128:(j + 1) * 128]
    sh["colpar"] = cp
    for k in ("a_r_k", "a_lnx_g", "a_lnx_b", "b_sink", "ln1_g", "ln1_b", "ln2_g", "ln2_b"):
        sh[k] = f32(inp[k])
    for k in ("w_gate", "w_up", "w_down"):
        sh[k] = f32(np.asarray(inp[k])[:, :NE_DECL])
    kc = np.arange(64)[:, None]; qc = np.arange(64)[None, :]
    idx = np.clip(kc - qc, -15, 15) + 15
    rpb = np.asarray(inp["c_rpb"], np.float32)
    tab = rpb[:, :, :, idx]
    sh["rpbtab"] = f32(tab.transpose(0, 1, 3, 2, 4))
    sh["w_r"] = f32(np.concatenate([inp["w_rg"], inp["w_re"]], -1))
    sh["b_r"] = f32(np.concatenate([inp["b_rg"], inp["b_re"]], -1))
    sh.update(_consts())
    return sh


_PROG = {}


def kernel(**inputs):
    inp = {k: np.asarray(v) for k, v in inputs.items()}
    shared = _host_layout(inp)
    if "nc" not in _PROG:
        _PROG["nc"] = build_program()
    nc = _PROG["nc"]
    in_maps = []
    for b in range(NCORES):
        m = dict(shared)
        m["x"] = np.ascontiguousarray(inp["x"][b], dtype=np.float32)
        m["ctx"] = np.ascontiguousarray(inp["ctx"][b], dtype=np.float32)
        cc = np.zeros((128, 8, 2), np.float32)
        cc[:, :, 0] = np.asarray(inp["c"][b], np.float32).reshape(8, 128).T
        cc[:, :, 1] = np.asarray(inp["c_ctx"], np.float32).reshape(8, 128).T
        m["ccol"] = cc
        in_maps.append(m)
    res = run_bass_kernel_spmd(nc, in_maps, core_ids=list(range(NCORES)))
    _PROG["res"] = res
    out = np.stack([np.asarray(r["y"], dtype=np.float32) for r in res.results], 0)
    return out
```
